# Optimizing a Trainium2 kernel written in Bass

```python
import math
import jax, jax.numpy as jnp
from jax import lax
import numpy as np

D_MODEL = 1024
BATCH = 1
SEQ = 16384
DEPTH = 2

HEAD_DIM = 64
A_HEADS = 4
A_KEY_DIM = 64
A_VAL_DIM = 64
A_CHUNK = 16
B_CHANNELS = 256
B_KERNEL = 31
C_HEADS = 4
C_PATTERNS = ((128, 1), (512, 4), (2048, 16))
C_BLOCK = 64
D_HEADS = 4
D_KV_HEADS = 2
D_HALF_WINDOW = 128
ROPE_THETA = 500000.0
ROPE_DIM = HEAD_DIM // 4
FFN_DIM = 2816
N_EXPERTS = 8
TOP_K = 2
EXPERT_DIM = 3584
MOE_BLOCK = 128
A_WIDTH = A_HEADS * A_VAL_DIM
B_WIDTH = B_CHANNELS
C_WIDTH = C_HEADS * HEAD_DIM
D_WIDTH = D_HEADS * HEAD_DIM
MIX_WIDTH = A_WIDTH + B_WIDTH + C_WIDTH + D_WIDTH
IN_SIZES = (A_HEADS * A_KEY_DIM, A_HEADS * A_KEY_DIM, A_HEADS * A_KEY_DIM, A_WIDTH, A_WIDTH,
            B_CHANNELS, B_CHANNELS,
            C_WIDTH, C_WIDTH, C_WIDTH,
            D_WIDTH, D_KV_HEADS * HEAD_DIM, D_KV_HEADS * HEAD_DIM)
IN_WIDTH = sum(IN_SIZES)
DEEPNORM_ALPHA = (2 * DEPTH) ** 0.25
DEEPNORM_BETA = (8 * DEPTH) ** -0.25
LN_EPS = 1e-5
RMS_EPS = 1e-6
NEG_INF = -1e30

kernel_name = "hybrid_parallel_groups_deepnorm_encoder"


def layer_norm(x, w, b):
    xf = x.astype(jnp.float32)
    mu = jnp.mean(xf, -1, keepdims=True)
    var = jnp.mean(jnp.square(xf - mu), -1, keepdims=True)
    return ((xf - mu) * lax.rsqrt(var + LN_EPS) * w.astype(jnp.float32) + b.astype(jnp.float32)).astype(x.dtype)


def rms_norm(x, w):
    xf = x.astype(jnp.float32)
    return xf * lax.rsqrt(jnp.mean(xf * xf, -1, keepdims=True) + RMS_EPS) * w.astype(jnp.float32)


def split_heads(t, n_heads):
    b, s, w = t.shape
    return t.reshape(b, s, n_heads, w // n_heads).transpose(0, 2, 1, 3)


def merge_heads(t):
    b, h, s, d = t.shape
    return t.transpose(0, 2, 1, 3).reshape(b, s, h * d)


def partial_rotary(x, positions):
    half = ROPE_DIM // 2
    inv_freq = ROPE_THETA ** (-jnp.arange(half, dtype=jnp.float32) * (2.0 / ROPE_DIM))
    ang = positions.astype(jnp.float32)[:, None, :, None] * inv_freq
    cos, sin = jnp.cos(ang), jnp.sin(ang)
    xr = x[..., :ROPE_DIM].astype(jnp.float32)
    x1, x2 = xr[..., :half], xr[..., half:]
    rot = jnp.concatenate([x1 * cos - x2 * sin, x2 * cos + x1 * sin], -1).astype(x.dtype)
    return jnp.concatenate([rot, x[..., ROPE_DIM:]], -1)


def banded_attention(q, k, v, half_window, block):
    n, g, length, hd = q.shape
    nb = -(-length // block)
    lp = nb * block
    pad = lp - length
    qb = jnp.pad(q, ((0, 0), (0, 0), (0, pad), (0, 0))).reshape(n, g, nb, block, hd)
    kp = jnp.pad(k, ((0, 0), (block, pad + block), (0, 0)))
    vp = jnp.pad(v, ((0, 0), (block, pad + block), (0, 0)))

    def windows(t):
        return jnp.concatenate([t[:, j * block: j * block + lp].reshape(n, nb, block, hd) for j in range(3)], axis=2)

    kw, vw = windows(kp), windows(vp)
    s = jnp.einsum('ngiqd,nikd->ngiqk', qb, kw, preferred_element_type=jnp.float32) * (hd ** -0.5)
    qpos = jnp.arange(lp).reshape(nb, block)
    kpos = (jnp.arange(nb)[:, None] - 1) * block + jnp.arange(3 * block)[None, :]
    rel = kpos[:, None, :] - qpos[:, :, None]
    mask = (jnp.abs(rel) <= half_window) & (kpos[:, None, :] >= 0) & (kpos[:, None, :] < length)
    s = jnp.where(mask, s, NEG_INF)
    m = jnp.max(s, -1)
    p = jnp.exp(s - m[..., None])
    l = jnp.sum(p, -1)
    o = jnp.einsum('ngiqk,nikd->ngiqd', p, vw.astype(jnp.float32))
    return (o.reshape(n, g, lp, hd)[:, :, :length],
            m.reshape(n, g, lp)[:, :, :length],
            l.reshape(n, g, lp)[:, :, :length])


def hgrn2_direction(q, k, v, logf):
    b_, h_, s_, dk = q.shape
    dv = v.shape[-1]
    n = s_ // A_CHUNK
    q, k, v, logf = [t.reshape(b_, h_, n, A_CHUNK, t.shape[-1]) for t in (q, k, v, logf)]
    cum = jnp.cumsum(logf, axis=3)
    lower = jnp.tril(jnp.ones((A_CHUNK, A_CHUNK), bool))
    diff = cum[:, :, :, :, None, :] - cum[:, :, :, None, :, :]
    decay = jnp.where(lower[:, :, None], jnp.exp(jnp.minimum(diff, 0.0)), 0.0)
    scores = jnp.einsum('bhntk,bhnsk,bhntsk->bhnts', q, k, decay)
    o_intra = jnp.einsum('bhnts,bhnsv->bhntv', scores, v)
    last = cum[:, :, :, -1:, :]
    chunk_kv = jnp.einsum('bhnsk,bhnsv->bhnkv', k * jnp.exp(last - cum), v)
    chunk_decay = jnp.exp(last[:, :, :, 0, :])

    def step(state, inp):
        dec, kv = inp
        return dec[..., None] * state + kv, state

    _, states = lax.scan(step, jnp.zeros((b_, h_, dk, dv), jnp.float32),
                         (jnp.moveaxis(chunk_decay, 2, 0), jnp.moveaxis(chunk_kv, 2, 0)))
    o_inter = jnp.einsum('bhntk,nbhkv->bhntv', q * jnp.exp(cum), states)
    return (o_intra + o_inter).reshape(b_, h_, s_, dv)


def hgrn2_mixer(q_raw, ffwd_raw, fbwd_raw, i_raw, g_raw, lb, norm_w):
    f32 = jnp.float32
    q = jax.nn.silu(split_heads(q_raw, A_HEADS).astype(f32))
    v = split_heads(i_raw, A_HEADS).astype(f32)
    lb = lb.astype(f32).reshape(A_HEADS, 1, A_KEY_DIM)

    def gates(z_raw):
        z = split_heads(z_raw, A_HEADS).astype(f32)
        logf = jnp.logaddexp(jnp.log(lb), jnp.log1p(-lb) + jax.nn.log_sigmoid(z))
        return logf, (1.0 - lb) * jax.nn.sigmoid(-z)

    logf_f, k_f = gates(ffwd_raw)
    logf_b, k_b = gates(fbwd_raw)
    flip = lambda t: jnp.flip(t, axis=2)
    o = hgrn2_direction(q, k_f, v, logf_f) + flip(hgrn2_direction(flip(q), flip(k_b), flip(v), flip(logf_b)))
    o = rms_norm(o, norm_w) * jax.nn.silu(split_heads(g_raw, A_HEADS).astype(f32))
    return merge_heads(o)


def conv_module(val, gate, conv_w, conv_b, norm_w, norm_b):
    u = val * jax.nn.sigmoid(gate)
    pad = B_KERNEL // 2
    y = lax.conv_general_dilated(u, conv_w[:, None, :].astype(u.dtype), window_strides=(1,),
                                 padding=((pad, pad),), dimension_numbers=('NWC', 'WIO', 'NWC'),
                                 feature_group_count=B_CHANNELS) + conv_b
    return jax.nn.silu(layer_norm(y, norm_w, norm_b))


def stride_split(t, dil):
    b, h, s, d = t.shape
    return t.reshape(b, h, s // dil, dil, d).transpose(0, 1, 3, 2, 4).reshape(b * h * dil, s // dil, d)


def stride_merge(t, b, h, dil):
    rest = t.shape[2:]
    t = t.reshape((b, h, dil) + t.shape[1:])
    t = jnp.swapaxes(t, 2, 3)
    return t.reshape((b, h, t.shape[2] * dil) + rest)


def dilated_attention(q, k, v):
    b, h, s, hd = q.shape
    nums, maxes, dens = [], [], []
    for window, dil in C_PATTERNS:
        o, m, l = banded_attention(stride_split(q, dil)[:, None], stride_split(k, dil), stride_split(v, dil),
                                   window // (2 * dil), C_BLOCK)
        nums.append(stride_merge(o[:, 0], b, h, dil))
        maxes.append(stride_merge(m[:, 0], b, h, dil))
        dens.append(stride_merge(l[:, 0], b, h, dil))
    m_all = jnp.stack(maxes)
    w = jnp.exp(m_all - jnp.max(m_all, 0))
    den = jnp.sum(w * jnp.stack(dens), 0)
    num = jnp.sum(w[..., None] * jnp.stack(nums), 0)
    return num / den[..., None]


def window_gqa_with_sink(q, k, v, sink):
    b, hq, s, hd = q.shape
    hkv = k.shape[1]
    g = hq // hkv
    o, m, l = banded_attention(q.reshape(b * hkv, g, s, hd), k.reshape(b * hkv, s, hd),
                               v.reshape(b * hkv, s, hd), D_HALF_WINDOW, D_HALF_WINDOW)
    o = o.reshape(b, hq, s, hd)
    m = m.reshape(b, hq, s)
    l = l.reshape(b, hq, s)
    sk = sink.astype(jnp.float32)[None, :, None]
    m_tot = jnp.maximum(m, sk)
    w = jnp.exp(m - m_tot)
    den = l * w + jnp.exp(sk - m_tot)
    return o * (w / den)[..., None]


def hybrid_mixer(h, positions, w_in, w_out, lb, a_norm_w, conv_w, conv_b, cn_w, cn_b, sink):
    proj = h @ w_in
    cuts = np.cumsum(IN_SIZES)[:-1].tolist()
    aq, aff, afb, ai, ag, bv, bg, cq, ck, cv, dq, dk, dv = jnp.split(proj, cuts, axis=-1)
    ya = hgrn2_mixer(aq, aff, afb, ai, ag, lb, a_norm_w)
    yb = conv_module(bv, bg, conv_w, conv_b, cn_w, cn_b)
    yc = merge_heads(dilated_attention(partial_rotary(split_heads(cq, C_HEADS), positions),
                                       partial_rotary(split_heads(ck, C_HEADS), positions),
                                       split_heads(cv, C_HEADS)))
    yd = merge_heads(window_gqa_with_sink(partial_rotary(split_heads(dq, D_HEADS), positions),
                                          partial_rotary(split_heads(dk, D_KV_HEADS), positions),
                                          split_heads(dv, D_KV_HEADS), sink))
    y = jnp.concatenate([ya.astype(h.dtype), yb.astype(h.dtype), yc.astype(h.dtype), yd.astype(h.dtype)], -1)
    return y @ w_out


def swiglu(h, w_up, w_down):
    gate, up = jnp.split(h @ w_up, 2, axis=-1)
    return (jax.nn.silu(gate) * up) @ w_down


def moe_swiglu(h, w_router, w_up, w_down):
    b, s, d = h.shape
    t = b * s
    hf = h.reshape(t, d)
    logits = jnp.einsum('td,de->te', hf, w_router, preferred_element_type=jnp.float32)
    top_logit, top_e = lax.top_k(logits, TOP_K)
    top_gate = jax.nn.softmax(top_logit, axis=-1)
    flat_e = top_e.reshape(-1).astype(jnp.int32)
    flat_tok = jnp.repeat(jnp.arange(t, dtype=jnp.int32), TOP_K)
    flat_gate = top_gate.reshape(-1)
    n_assign = t * TOP_K
    n_blocks = -(-n_assign // MOE_BLOCK) + N_EXPERTS
    counts = jnp.zeros((N_EXPERTS,), jnp.int32).at[flat_e].add(1)
    padded = (counts + MOE_BLOCK - 1) // MOE_BLOCK * MOE_BLOCK
    padded_end = jnp.cumsum(padded)
    padded_start = padded_end - padded
    start = jnp.cumsum(counts) - counts
    order = jnp.argsort(flat_e)
    sorted_e = flat_e[order]
    slot = padded_start[sorted_e] + jnp.arange(n_assign, dtype=jnp.int32) - start[sorted_e]
    slot_tok = jnp.full((n_blocks * MOE_BLOCK,), t, jnp.int32).at[slot].set(flat_tok[order])
    slot_gate = jnp.zeros((n_blocks * MOE_BLOCK,), jnp.float32).at[slot].set(flat_gate[order])
    block_e = jnp.minimum(jnp.searchsorted(padded_end, jnp.arange(n_blocks, dtype=jnp.int32) * MOE_BLOCK,
                                           side='right'), N_EXPERTS - 1)
    h_pad = jnp.concatenate([hf, jnp.zeros((1, d), hf.dtype)], 0)

    def expert_block(args):
        tok, e = args
        gate, up = jnp.split(h_pad[tok] @ w_up[e], 2, axis=-1)
        return (jax.nn.silu(gate) * up) @ w_down[e]

    y = lax.map(expert_block, (slot_tok.reshape(n_blocks, MOE_BLOCK), block_e))
    out = jnp.zeros((t + 1, d), jnp.float32).at[slot_tok].add(
        slot_gate[:, None] * y.reshape(-1, d).astype(jnp.float32))
    return out[:t].reshape(b, s, d).astype(h.dtype)


def setup_inputs(seed: int = 0) -> dict:
    key = jax.random.key(seed)
    ks = jax.random.split(key, 24)
    nrm = lambda k, shape, scale: jax.random.normal(k, shape, jnp.float32) * scale
    n_dense = (DEPTH + 1) // 2
    n_moe = DEPTH // 2
    return {
        "x": nrm(ks[0], (BATCH, SEQ, D_MODEL), 1.0),
        "c": nrm(ks[1], (BATCH, D_MODEL), 1.0),
        "positions": jnp.broadcast_to(jnp.arange(SEQ, dtype=jnp.int32), (BATCH, SEQ)),
        "w_ada": nrm(ks[2], (DEPTH, D_MODEL, 6 * D_MODEL), 0.1 * D_MODEL ** -0.5),
        "b_ada": nrm(ks[3], (DEPTH, 6 * D_MODEL), 0.01),
        "w_in": nrm(ks[4], (DEPTH, D_MODEL, IN_WIDTH), D_MODEL ** -0.5),
        "w_out": nrm(ks[5], (DEPTH, MIX_WIDTH, D_MODEL), DEEPNORM_BETA * MIX_WIDTH ** -0.5),
        "a_lower_bound": nrm(ks[6], (DEPTH, A_HEADS * A_KEY_DIM), 0.5),
        "a_norm_w": 1.0 + nrm(ks[7], (DEPTH, A_VAL_DIM), 0.02),
        "b_conv_w": nrm(ks[8], (DEPTH, B_KERNEL, B_CHANNELS), B_KERNEL ** -0.5),
        "b_conv_b": nrm(ks[9], (DEPTH, B_CHANNELS), 0.02),
        "b_norm_w": 1.0 + nrm(ks[10], (DEPTH, B_CHANNELS), 0.02),
        "b_norm_b": nrm(ks[11], (DEPTH, B_CHANNELS), 0.02),
        "d_sink": nrm(ks[12], (DEPTH, D_HEADS), 1.0),
        "ln_w": 1.0 + nrm(ks[13], (DEPTH, 2, D_MODEL), 0.02),
        "ln_b": nrm(ks[14], (DEPTH, 2, D_MODEL), 0.02),
        "ffn_w_up": nrm(ks[15], (n_dense, D_MODEL, 2 * FFN_DIM), D_MODEL ** -0.5),
        "ffn_w_down": nrm(ks[16], (n_dense, FFN_DIM, D_MODEL), DEEPNORM_BETA * FFN_DIM ** -0.5),
        "moe_router": nrm(ks[17], (n_moe, D_MODEL, N_EXPERTS), D_MODEL ** -0.5),
        "moe_w_up": nrm(ks[18], (n_moe, N_EXPERTS, D_MODEL, 2 * EXPERT_DIM), D_MODEL ** -0.5),
        "moe_w_down": nrm(ks[19], (n_moe, N_EXPERTS, EXPERT_DIM, D_MODEL), DEEPNORM_BETA * EXPERT_DIM ** -0.5),
    }


def reference(x, c, positions, w_ada, b_ada, w_in, w_out, a_lower_bound, a_norm_w, b_conv_w, b_conv_b,
              b_norm_w, b_norm_b, d_sink, ln_w, ln_b, ffn_w_up, ffn_w_down, moe_router, moe_w_up, moe_w_down):
    mod = jnp.einsum('bd,lde->lbe', jax.nn.silu(c), w_ada) + b_ada[:, None, :]
    lb_cum = jnp.cumsum(jax.nn.softmax(a_lower_bound.astype(jnp.float32), axis=0), axis=0)
    lb_all = lb_cum - lb_cum[0]
    for layer in range(DEPTH):
        shift1, scale1, gate1, shift2, scale2, gate2 = jnp.split(mod[layer][:, None, :], 6, axis=-1)
        h = x * (1.0 + scale1) + shift1
        y = hybrid_mixer(h, positions, w_in[layer], w_out[layer], lb_all[layer], a_norm_w[layer],
                         b_conv_w[layer], b_conv_b[layer], b_norm_w[layer], b_norm_b[layer], d_sink[layer])
        x = layer_norm(DEEPNORM_ALPHA * x + (1.0 + gate1) * y, ln_w[layer, 0], ln_b[layer, 0])
        h = x * (1.0 + scale2) + shift2
        if layer % 2 == 0:
            f = swiglu(h, ffn_w_up[layer // 2], ffn_w_down[layer // 2])
        else:
            f = moe_swiglu(h, moe_router[layer // 2], moe_w_up[layer // 2], moe_w_down[layer // 2])
        x = layer_norm(DEEPNORM_ALPHA * x + (1.0 + gate2) * f, ln_w[layer, 1], ln_b[layer, 1])
    return x
```

```python
import math
from contextlib import ExitStack

import numpy as np
import ml_dtypes

import concourse.bass as bass
import concourse.mybir as mybir
from concourse.bass_utils import run_bass_kernel_spmd

F32 = mybir.dt.float32
BF16 = mybir.dt.bfloat16
I32 = mybir.dt.int32
AF = mybir.ActivationFunctionType
ALU = mybir.AluOpType
AX = mybir.AxisListType

NCORES = 8
D_MODEL = 1024
SEQ = 16384
TOK = SEQ // NCORES
DEPTH = 2
HD = 64
FFN_DIM = 2816
N_EXPERTS = 8
EXPERT_DIM = 3584
B_KERNEL = 31
ROPE_THETA = 500000.0
ROPE_DIM = 16
DEEPNORM_ALPHA = (2 * DEPTH) ** 0.25
LN_EPS = 1e-5
RMS_EPS = 1e-6
NEG = -30000.0
TWO_PI = 2.0 * math.pi


class Prog:
    NDS = 24

    def __init__(self):
        self.nc = bass.Bass("TRN2", target_bir_lowering=False)
        nc = self.nc
        self.es = ExitStack()
        self.q = {"pe": nc.tensor, "dve": nc.vector, "act": nc.scalar, "pool": nc.gpsimd, "sp": nc.sync}
        self.esem = {e: self.es.enter_context(nc.semaphore("es_" + e)) for e in ("pe", "dve", "act", "pool")}
        self.ecnt = {e: 0 for e in self.esem}
        self.dsem = [self.es.enter_context(nc.semaphore(f"ds{i}")) for i in range(self.NDS)]
        self.dval = [0] * self.NDS
        self.dnext = 0
        self.seen = {e: {} for e in self.q}
        self.lastw = {}
        self.readers = {}
        self.out_tokens = []
        self.n_inst = 0
        self._ps_id = 0

    def dram_in(self, name, shape, dt):
        return self.nc.dram_tensor(name, list(shape), dt, kind="ExternalInput").ap()

    def dram_out(self, name, shape, dt):
        return self.nc.dram_tensor(name, list(shape), dt, kind="ExternalOutput").ap()

    def sb(self, name, shape, dt):
        return self.es.enter_context(self.nc.sbuf_tensor("sb_" + name, list(shape), dt))

    def ps(self, name, shape, dt=F32):
        return self.es.enter_context(self.nc.psum_tensor("pm_" + name, list(shape), dt))

    def _wait(self, e, tok):
        sem, v, owner = tok
        if owner == e and e == "pe":
            return
        k = id(sem)
        if self.seen[e].get(k, 0) >= v:
            return
        self.q[e].wait_ge(sem, v)
        self.seen[e][k] = v

    def _deps(self, e, reads, writes):
        for k in reads:
            t = self.lastw.get(k)
            if t is not None:
                self._wait(e, t)
        for k in writes:
            t = self.lastw.get(k)
            if t is not None:
                self._wait(e, t)
            for t in self.readers.get(k, {}).values():
                self._wait(e, t)

    def _record(self, tok, reads, writes):
        for k in writes:
            self.lastw[k] = tok
            self.readers[k] = {}
        for k in reads:
            self.readers.setdefault(k, {})[id(tok[0])] = tok

    def op(self, e, fn, reads=(), writes=()):
        self._deps(e, reads, writes)
        inst = fn(self.q[e])
        self.ecnt[e] += 1
        inst.then_inc(self.esem[e], 1)
        tok = (self.esem[e], self.ecnt[e], e)
        self._record(tok, reads, writes)
        self.n_inst += 1
        return tok

    def dma(self, e, out, in_, reads=(), writes=(), is_output=False, **kw):
        self._deps(e, reads, writes)
        j = self.dnext
        self.dnext = (self.dnext + 1) % self.NDS
        if self.dval[j] > 0:
            self._wait(e, (self.dsem[j], self.dval[j], "dma"))
        self.q[e].dma_start(out=out, in_=in_, **kw).then_inc(self.dsem[j], 16)
        self.dval[j] += 16
        tok = (self.dsem[j], self.dval[j], "dma")
        self._record(tok, reads, writes)
        if is_output:
            self.out_tokens.append(tok)
        self.n_inst += 1
        return tok

    def finish(self):
        for j in range(self.NDS):
            if self.dval[j] > 0:
                self._wait("sp", (self.dsem[j], self.dval[j], "dma"))
        return self.nc


def chunks(n, c):
    return [(i, min(c, n - i)) for i in range(0, n, c)]


class PsumPool:
    def __init__(self, P, n=8, prefix="pb"):
        self.t = [P.ps(f"{prefix}{i}", [128, 512], F32) for i in range(n)]
        self.k = [f"{prefix}{i}" for i in range(n)]
        self.i = 0
        self.n = n

    def get(self):
        i = self.i
        self.i = (self.i + 1) % self.n
        return self.t[i], self.k[i]


def build_consts(P):
    c = {}
    c["ones_f"] = P.sb("ones_f", [128, 512], F32)
    P.op("pool", lambda q: q.memset(c["ones_f"][:], 1.0), writes=["ones_f"])
    c["ones_b"] = P.sb("ones_b", [128, 512], BF16)
    P.op("pool", lambda q: q.memset(c["ones_b"][:], 1.0), writes=["ones_b"])
    return c


CH = {"aq": (0, 2), "aff": (2, 2), "afb": (4, 2), "ai": (6, 2), "ag": (8, 2), "bv": (10, 2), "bg": (12, 2),
      "cq": (14, 2), "ck": (16, 2), "cv": (18, 2), "dq": (20, 2), "dk": (22, 1), "dv": (23, 1)}
ROT_CHUNKS = [14, 15, 16, 17, 20, 21, 22]
O32 = {"qa": 0, "kf": 256, "lf": 512, "kb": 768, "lb": 1024, "va": 1280, "ga": 1536, "u": 1792}
OBF = {"cq": 0, "ck": 256, "cv": 512, "dq": 768, "dk": 1024, "dv": 1152}
NBF = 1280


def build_k1(layer):
    P = Prog()
    nc = P.nc
    T = TOK
    xT = P.dram_in("xT", [D_MODEL, T], F32)
    ccol = P.dram_in("ccol", [128, 8], F32)
    w_ada = P.dram_in("w_ada", [D_MODEL, 6 * D_MODEL], F32)
    bada = P.dram_in("bada", [128, 48], F32)
    w_in = P.dram_in("w_in", [D_MODEL, 3072], F32)
    w_sw = P.dram_in("w_sw", [D_MODEL, 7 * 128], F32)
    pos = P.dram_in("pos", [T], I32)
    rcol = P.dram_in("rcol", [128, 2], F32)
    alb = P.dram_in("alb", [128, 4], F32)
    o32 = P.dram_out("o32", [2048, T], F32)
    obf = P.dram_out("obf", [NBF, T], BF16)
    omod = P.dram_out("omod", [128, 48], F32)

    C = build_consts(P)
    pp = PsumPool(P)

    sc = P.sb("sc", [128, 8], F32)
    P.dma("sp", sc[:], ccol, writes=["sc"])
    P.op("act", lambda q: q.activation(out=sc[:], in_=sc[:], func=AF.Silu), reads=["sc"], writes=["sc"])
    mod = P.sb("mod", [128, 48], F32)
    badas = P.sb("badas", [128, 48], F32)
    P.dma("sp", badas[:], bada, writes=["badas"])
    wst = [P.sb(f"wada_st{i}", [128, 8, 256], F32) for i in range(2)]
    w_ada_v = w_ada.rearrange("(k p) e -> p k e", p=128)
    mps, mpk = pp.get()
    for g in range(24):
        st = wst[g % 2]
        sk = f"wada_st{g % 2}"
        P.dma("sp", st[:], w_ada_v[:, :, g * 256:(g + 1) * 256], writes=[sk])
        for j in range(2):
            e = g * 2 + j
            for k in range(8):
                P.op("pe", lambda q, k=k, j=j, e=e: q.matmul(mps[:, e:e + 1], lhsT=st[:, k, j * 128:(j + 1) * 128],
                                                           rhs=sc[:, k:k + 1], start=(k == 0), stop=(k == 7)),
                     reads=[sk, "sc"], writes=[mpk])
    P.op("dve", lambda q: q.tensor_tensor(out=mod[:], in0=mps[:, 0:48], in1=badas[:], op=ALU.add),
         reads=[mpk, "badas"], writes=["mod"])
    P.dma("pool", omod, mod[:], reads=["mod"], is_output=True)
    sc1 = P.sb("sc1", [128, 8], F32)
    P.op("dve", lambda q: q.tensor_scalar(out=sc1[:], in0=mod[:, 8:16], scalar1=1.0, scalar2=None, op0=ALU.add),
         reads=["mod"], writes=["sc1"])

    hT = P.sb("hT", [128, 8, T], BF16)
    xst = [P.sb(f"xst{i}", [128, T], F32) for i in range(2)]
    xT_v = xT.rearrange("(k p) t -> p k t", p=128)
    for k in range(8):
        st = xst[k % 2]
        sk = f"xst{k % 2}"
        P.dma("sp", st[:], xT_v[:, k, :], writes=[sk])
        P.op("dve", lambda q, k=k, st=st: q.tensor_scalar(out=hT[:, k, :], in0=st[:], scalar1=sc1[:, k:k + 1],
                                                        scalar2=mod[:, k:k + 1], op0=ALU.mult, op1=ALU.add),
             reads=[sk, "sc1", "mod"], writes=[f"hT{k}"])
    hkeys = [f"hT{k}" for k in range(8)]

    wb = P.sb("wb", [128, 8, 3072], BF16)
    wsw = P.sb("wsw", [128, 8, 896], BF16)
    w_in_v = w_in.rearrange("(k p) e -> p k e", p=128)
    w_sw_v = w_sw.rearrange("(k p) e -> p k e", p=128)
    for k in range(8):
        P.dma("pool", wb[:, k, :], w_in_v[:, k, :], writes=[f"wb{k}"])
        P.dma("pool", wsw[:, k, :], w_sw_v[:, k, :], writes=[f"wsw{k}"])
    wkeys = [f"wb{k}" for k in range(8)]
    wswkeys = [f"wsw{k}" for k in range(8)]

    rc = P.sb("rc", [128, 2], F32)
    P.dma("sp", rc[:], rcol, writes=["rc"])
    tmpi = P.sb("tmpi", [128, T], I32)
    P.dma("sp", tmpi[:], pos.partition_broadcast(128), writes=["tmpi"])
    ang = P.sb("ang", [128, T], F32)
    cosT = P.sb("cosT", [128, T], F32)
    sinT = P.sb("sinT", [128, T], F32)
    tmpf = P.sb("tmpf", [128, T], F32)
    P.op("dve", lambda q: q.tensor_copy(out=ang[:], in_=tmpi[:]), reads=["tmpi"], writes=["ang"])
    P.op("dve", lambda q: q.tensor_scalar(out=ang[:], in0=ang[:], scalar1=rc[:, 0:1], scalar2=None, op0=ALU.mult),
         reads=["ang", "rc"], writes=["ang"])
    C1 = 6.28125
    C2 = TWO_PI - C1

    def sin_table(dst, dkey, phase):
        P.op("dve", lambda q: q.tensor_scalar(out=tmpf[:], in0=ang[:], scalar1=phase, scalar2=1.0 / TWO_PI,
                                              op0=ALU.add, op1=ALU.mult), reads=["ang"], writes=["tmpf"])
        P.op("dve", lambda q: q.tensor_copy(out=tmpi[:], in_=tmpf[:]), reads=["tmpf"], writes=["tmpi"])
        P.op("dve", lambda q: q.tensor_copy(out=tmpf[:], in_=tmpi[:]), reads=["tmpi"], writes=["tmpf"])
        P.op("dve", lambda q: q.scalar_tensor_tensor(out=dst[:], in0=tmpf[:], scalar=-C1, in1=ang[:],
                                                     op0=ALU.mult, op1=ALU.add), reads=["tmpf", "ang"], writes=[dkey])
        P.op("dve", lambda q: q.scalar_tensor_tensor(out=dst[:], in0=tmpf[:], scalar=-C2, in1=dst[:],
                                                     op0=ALU.mult, op1=ALU.add), reads=["tmpf", dkey], writes=[dkey])
        P.op("dve", lambda q: q.tensor_scalar(out=dst[:], in0=dst[:], scalar1=phase, scalar2=-math.pi,
                                              op0=ALU.add, op1=ALU.max), reads=[dkey], writes=[dkey])
        P.op("dve", lambda q: q.tensor_scalar(out=dst[:], in0=dst[:], scalar1=math.pi, scalar2=None,
                                              op0=ALU.min), reads=[dkey], writes=[dkey])
        P.op("act", lambda q: q.activation(out=dst[:], in_=dst[:], func=AF.Sin), reads=[dkey], writes=[dkey])

    sin_table(cosT, "cosT", math.pi / 2)
    sin_table(sinT, "sinT", 0.0)
    P.op("dve", lambda q: q.tensor_scalar(out=sinT[:], in0=sinT[:], scalar1=rc[:, 1:2], scalar2=None, op0=ALU.mult),
         reads=["sinT", "rc"], writes=["sinT"])

    albs = P.sb("albs", [128, 4], F32)
    P.dma("sp", albs[:], alb, writes=["albs"])
    lbc = P.sb("lbc", [128, 2], F32)
    oml = P.sb("oml", [128, 2], F32)
    if layer == 0:
        P.op("pool", lambda q: q.memset(lbc[:], 0.0), writes=["lbc"])
        P.op("pool", lambda q: q.memset(oml[:], 1.0), writes=["oml"])
    else:
        ex = P.sb("alb_ex", [128, 4], F32)
        P.op("act", lambda q: q.activation(out=ex[:], in_=albs[:], func=AF.Exp), reads=["albs"], writes=["alb_ex"])
        exv = ex[:].rearrange("p (t l) -> p t l", l=2)
        sm = P.sb("alb_sm", [128, 2], F32)
        P.op("dve", lambda q: q.tensor_tensor(out=sm[:], in0=exv[:, :, 0], in1=exv[:, :, 1], op=ALU.add),
             reads=["alb_ex"], writes=["alb_sm"])
        P.op("dve", lambda q: q.reciprocal(out=sm[:], in_=sm[:]), reads=["alb_sm"], writes=["alb_sm"])
        P.op("dve", lambda q: q.tensor_tensor(out=lbc[:], in0=exv[:, :, 1], in1=sm[:], op=ALU.mult),
             reads=["alb_ex", "alb_sm"], writes=["lbc"])
        P.op("dve", lambda q: q.tensor_scalar(out=oml[:], in0=lbc[:], scalar1=-1.0, scalar2=1.0, op0=ALU.mult,
                                              op1=ALU.add), reads=["lbc"], writes=["oml"])

    ob_f = [P.sb(f"obf{i}", [128, 512], F32) for i in range(4)]
    ob_b = [P.sb(f"obb{i}", [128, 512], BF16) for i in range(4)]
    sg = [P.sb(f"sg{i}", [128, T], F32) for i in range(2)]
    cnt = {"f": 0, "b": 0}

    def getf():
        i = cnt["f"] % 4
        cnt["f"] += 1
        return ob_f[i], f"obf{i}"

    def getb():
        i = cnt["b"] % 4
        cnt["b"] += 1
        return ob_b[i], f"obb{i}"

    def proj(ps, psk, wt, wkeys_, col0, tg):
        for k in range(8):
            P.op("pe", lambda q, k=k: q.matmul(ps[:, :], lhsT=wt[:, k, col0:col0 + 128],
                                               rhs=hT[:, k, tg * 512:(tg + 1) * 512], start=(k == 0), stop=(k == 7)),
                 reads=[wkeys_[k], hkeys[k]], writes=[psk])

    def store32(name, tile_i, tg, buf, bkey):
        r0 = O32[name] + tile_i * 128
        P.dma("sp", o32[r0:r0 + 128, tg * 512:(tg + 1) * 512], buf[:], reads=[bkey], is_output=True)

    def storebf(name, tile_i, tg, buf, bkey):
        r0 = OBF[name] + tile_i * 128
        P.dma("sp", obf[r0:r0 + 128, tg * 512:(tg + 1) * 512], buf[:], reads=[bkey], is_output=True)

    order = ["bg", "bv", "aq", "aff", "afb", "ai", "ag", "cq", "ck", "cv", "dq", "dk", "dv"]
    for name in order:
        c0, ncn = CH[name]
        for ti in range(ncn):
            ch = c0 + ti
            for tg in range(4):
                tsl = slice(tg * 512, (tg + 1) * 512)
                ps, psk = pp.get()
                proj(ps, psk, wb, wkeys, ch * 128, tg)
                if name == "bg":
                    P.op("act", lambda q, ps=ps, ti=ti, tsl=tsl: q.activation(out=sg[ti][:, tsl], in_=ps[:], func=AF.Sigmoid),
                         reads=[psk], writes=[f"sg{ti}_{tg}"])
                elif name == "bv":
                    b, bk = getf()
                    P.op("dve", lambda q, ps=ps, b=b, ti=ti, tsl=tsl: q.tensor_tensor(out=b[:], in0=ps[:], in1=sg[ti][:, tsl], op=ALU.mult),
                         reads=[psk, f"sg{ti}_{tg}"], writes=[bk])
                    store32("u", ti, tg, b, bk)
                elif name in ("aq", "ag"):
                    b, bk = getf()
                    P.op("act", lambda q, ps=ps, b=b: q.activation(out=b[:], in_=ps[:], func=AF.Silu), reads=[psk], writes=[bk])
                    store32("qa" if name == "aq" else "ga", ti, tg, b, bk)
                elif name == "ai":
                    b, bk = getf()
                    P.op("act", lambda q, ps=ps, b=b: q.activation(out=b[:], in_=ps[:], func=AF.Copy), reads=[psk], writes=[bk])
                    store32("va", ti, tg, b, bk)
                elif name in ("aff", "afb"):
                    fb_, fk = getf()
                    P.op("act", lambda q, ps=ps, fb_=fb_: q.activation(out=fb_[:], in_=ps[:], func=AF.Sigmoid), reads=[psk], writes=[fk])
                    P.op("dve", lambda q, fb_=fb_, ti=ti: q.tensor_scalar(out=fb_[:], in0=fb_[:], scalar1=oml[:, ti:ti + 1],
                                                                        scalar2=lbc[:, ti:ti + 1], op0=ALU.mult, op1=ALU.add),
                         reads=[fk, "oml", "lbc"], writes=[fk])
                    kb_, kk = getf()
                    P.op("dve", lambda q, fb_=fb_, kb_=kb_: q.tensor_scalar(out=kb_[:], in0=fb_[:], scalar1=-1.0, scalar2=1.0,
                                                                          op0=ALU.mult, op1=ALU.add), reads=[fk], writes=[kk])
                    store32("kf" if name == "aff" else "kb", ti, tg, kb_, kk)
                    P.op("act", lambda q, fb_=fb_: q.activation(out=fb_[:], in_=fb_[:], func=AF.Ln), reads=[fk], writes=[fk])
                    store32("lf" if name == "aff" else "lb", ti, tg, fb_, fk)
                elif name in ("cv", "dv"):
                    b, bk = getb()
                    P.op("act", lambda q, ps=ps, b=b: q.activation(out=b[:], in_=ps[:], func=AF.Copy), reads=[psk], writes=[bk])
                    storebf(name, ti, tg, b, bk)
                else:
                    ri = ROT_CHUNKS.index(ch)
                    ps2, psk2 = pp.get()
                    proj(ps2, psk2, wsw, wswkeys, ri * 128, tg)
                    t1, t1k = getf()
                    t2, t2k = getf()
                    P.op("dve", lambda q, ps=ps, t1=t1, tsl=tsl: q.tensor_tensor(out=t1[:], in0=ps[:], in1=cosT[:, tsl], op=ALU.mult),
                         reads=[psk, "cosT"], writes=[t1k])
                    P.op("dve", lambda q, ps2=ps2, t2=t2, tsl=tsl: q.tensor_tensor(out=t2[:], in0=ps2[:], in1=sinT[:, tsl], op=ALU.mult),
                         reads=[psk2, "sinT"], writes=[t2k])
                    b, bk = getb()
                    P.op("pool", lambda q, t1=t1, t2=t2, b=b: q.tensor_tensor(out=b[:], in0=t1[:], in1=t2[:], op=ALU.add),
                         reads=[t1k, t2k], writes=[bk])
                    storebf(name, ti, tg, b, bk)
    return P.finish()


def col128(v):
    v = np.asarray(v)
    return np.ascontiguousarray(v.reshape(-1, 128).T)


def rot_cols():
    idx = []
    for ch in ROT_CHUNKS:
        for h in range(2):
            base = ch * 128 + h * 64
            loc = np.arange(64)
            loc[:8] = np.arange(8, 16)
            loc[8:16] = np.arange(0, 8)
            idx.append(base + loc)
    return np.concatenate(idx)


def rot_consts():
    half = ROPE_DIM // 2
    inv = (np.float32(ROPE_THETA) ** (-np.arange(half, dtype=np.float32) * np.float32(2.0 / ROPE_DIM))).astype(np.float32)
    rc = np.zeros((128, 2), np.float32)
    for p in range(128):
        d = p % 64
        if d < 16:
            rc[p, 0] = inv[d % 8]
            rc[p, 1] = -1.0 if d < 8 else 1.0
    return rc


_cache = {}


def run(nc_key, builder, in_maps):
    if nc_key not in _cache:
        _cache[nc_key] = builder()
    nc = _cache[nc_key]
    res = run_bass_kernel_spmd(nc, in_maps, core_ids=list(range(NCORES)))
    return res.results


def run_k1(layer, xT_shards, inp):
    rc = rot_consts()
    sw = rot_cols()
    w_in_l = np.ascontiguousarray(inp["w_in"][layer])
    w_sw = np.ascontiguousarray(w_in_l[:, sw])
    alb = np.zeros((128, 4), np.float32)
    a = np.asarray(inp["a_lower_bound"])
    for t in range(2):
        for l in range(2):
            alb[:, t * 2 + l] = a[l, t * 128:(t + 1) * 128]
    ccol = col128(np.asarray(inp["c"])[0])
    bada = col128(np.asarray(inp["b_ada"])[layer])
    w_ada_l = np.ascontiguousarray(inp["w_ada"][layer])
    pos = np.asarray(inp["positions"])[0].astype(np.int32)
    in_maps = []
    for c in range(NCORES):
        in_maps.append({"xT": xT_shards[c], "ccol": ccol, "w_ada": w_ada_l, "bada": bada, "w_in": w_in_l, "w_sw": w_sw,
                        "pos": np.ascontiguousarray(pos[c * TOK:(c + 1) * TOK]), "rcol": rc, "alb": alb})
    return run(("k1", layer), lambda: build_k1(layer), in_maps)


SEG = 1024
NSEG = SEQ // SEG
NCH = SEG // 16
NTL = SEG // 128


def hgrn_consts():
    ident = np.eye(128, dtype=np.float32).astype(ml_dtypes.bfloat16)
    s = np.arange(128)
    tri = ((s[:, None] // 16 == s[None, :] // 16) & (s[:, None] <= s[None, :])).astype(np.float32).astype(ml_dtypes.bfloat16)
    ind = (s[:, None] // 16 == np.arange(8)[None, :]).astype(np.float32).astype(ml_dtypes.bfloat16)
    return ident, tri, ind


def build_k2a():
    P = Prog()
    qT = P.dram_in("qT", [64, SEQ], F32)
    kT = P.dram_in("kT", [64, SEQ], F32)
    lT = P.dram_in("lT", [64, SEQ], F32)
    vtok = P.dram_in("vtok", [SEQ, 64], F32)
    ident_d = P.dram_in("ident_d", [128, 128], BF16)
    tri_d = P.dram_in("tri_d", [128, 128], BF16)
    ind_d = P.dram_in("ind_d", [128, 8], BF16)
    oT = P.dram_out("oT", [64, SEQ], F32)

    ident = P.sb("ident", [128, 128], BF16)
    tri = P.sb("tri", [128, 128], BF16)
    ind = P.sb("ind", [128, 8], BF16)
    P.dma("sp", ident[:], ident_d, writes=["ident"])
    P.dma("sp", tri[:], tri_d, writes=["tri"])
    P.dma("sp", ind[:], ind_d, writes=["ind"])

    reset = P.sb("reset", [64, SEG], F32)
    P.op("pool", lambda q: q.memset(reset[:], 1.0), writes=["reset"])
    P.op("pool", lambda q: q.memset(reset[:].rearrange("p (n c) -> p n c", c=16)[:, :, 0:1], 0.0), reads=["reset"], writes=["reset"])

    def two(name, shape, dt):
        return [P.sb(f"{name}{i}", shape, dt) for i in range(2)]

    qs_, ks_, ls_ = two("qs", [64, SEG], F32), two("ks", [64, SEG], F32), two("ls", [64, SEG], F32)
    vb_ = two("vb", [128, NTL, 64], BF16)
    cum_, ex_ = two("cum", [64, SEG], F32), two("ex", [64, SEG], F32)
    qt_, kt_, kh_ = two("qt", [64, SEG], BF16), two("kt", [64, SEG], BF16), two("kh", [64, SEG], BF16)
    dec_ = two("dec", [64, NCH], F32)
    kvs_ = two("kvs", [64, 64 * NCH], F32)
    sprev_ = two("sprev", [64, NCH, 64], BF16)
    obuf_ = two("obuf", [64, SEG], F32)
    decrep = P.sb("decrep", [64, 64 * NCH], F32)
    decz = P.sb("decz", [64, NCH], F32)
    P.op("pool", lambda q: q.memset(decz[:], 0.0), writes=["decz"])
    sall = P.sb("sall", [64, 64 * NCH], F32)
    s_in = P.sb("s_in", [64, 64], F32)
    khtok = [P.sb(f"khtok{i}", [128, 64], BF16) for i in range(2)]
    vblk = [P.sb(f"vblk{i}", [128, 8, 64], BF16) for i in range(2)]
    am = [P.sb(f"am{i}", [128, 128], BF16) for i in range(2)]
    P.op("pool", lambda q: q.memset(s_in[:], 0.0), writes=["s_in"])

    ps_kv = [P.ps(f"ps_kv{i}", [128, 512], F32) for i in range(2)]
    ps_a = [P.ps(f"ps_a{i}", [128, 512], F32) for i in range(2)]
    ps_o = [P.ps(f"ps_o{i}", [128, 512], F32) for i in range(2)]
    ps_t = [P.ps(f"ps_t{i}", [128, 1024], BF16) for i in range(2)]

    dec3 = decrep[:].rearrange("p (v n) -> p v n", n=NCH)
    sall3 = sall[:].rearrange("p (v n) -> p v n", n=NCH)
    vt_v = vtok.rearrange("(s i p) v -> s p i v", p=128, i=NTL)
    cnt = [0]

    def stage(sgi, part):
        pb = sgi % 2
        K_ = lambda n: f"{n}{pb}"
        qs, ks, ls, vb, cum, ex = qs_[pb], ks_[pb], ls_[pb], vb_[pb], cum_[pb], ex_[pb]
        qt, kt, kh, dec, kvs, sprev, obuf = qt_[pb], kt_[pb], kh_[pb], dec_[pb], kvs_[pb], sprev_[pb], obuf_[pb]
        kvs3 = kvs[:].rearrange("p (v n) -> p v n", n=NCH)
        cum3 = cum[:].rearrange("p (n c) -> p n c", c=16)
        ex3 = ex[:].rearrange("p (n c) -> p n c", c=16)
        tsl = slice(sgi * SEG, (sgi + 1) * SEG)
        if part == "B":
            return stageB(sgi, pb, K_, qt, kt, vb, dec, kvs, kvs3, sprev, obuf, tsl)
        P.dma("sp", qs[:], qT[:, tsl], writes=[K_("qs")])
        P.dma("sp", ks[:], kT[:, tsl], writes=[K_("ks")])
        P.dma("sp", ls[:], lT[:, tsl], writes=[K_("ls")])
        P.dma("pool", vb[:], vt_v[sgi], writes=[K_("vb")])
        P.op("dve", lambda q: q.tensor_tensor_scan(out=cum[:], data0=reset[:], data1=ls[:], initial=0.0,
                                                   op0=ALU.mult, op1=ALU.add), reads=["reset", K_("ls")], writes=[K_("cum")])
        P.op("act", lambda q: q.activation(out=ex[:], in_=cum[:], func=AF.Exp), reads=[K_("cum")], writes=[K_("ex")])
        P.op("dve", lambda q: q.tensor_tensor(out=qt[:], in0=qs[:], in1=ex[:], op=ALU.mult), reads=[K_("qs"), K_("ex")], writes=[K_("qt")])
        P.op("act", lambda q: q.activation(out=ex[:], in_=cum[:], func=AF.Exp, scale=-1.0), reads=[K_("cum")], writes=[K_("ex")])
        P.op("pool", lambda q: q.tensor_tensor(out=kt[:], in0=ks[:], in1=ex[:], op=ALU.mult), reads=[K_("ks"), K_("ex")], writes=[K_("kt")])
        P.op("dve", lambda q: q.tensor_tensor(out=ex3, in0=cum3[:, :, 15:16].broadcast_to([64, NCH, 16]), in1=cum3,
                                              op=ALU.subtract), reads=[K_("cum")], writes=[K_("ex")])
        P.op("act", lambda q: q.activation(out=ex[:], in_=ex[:], func=AF.Exp), reads=[K_("ex")], writes=[K_("ex")])
        P.op("pool", lambda q: q.tensor_tensor(out=kh[:], in0=ks[:], in1=ex[:], op=ALU.mult), reads=[K_("ks"), K_("ex")], writes=[K_("kh")])
        P.op("act", lambda q: q.activation(out=dec[:], in_=cum3[:, :, 15], func=AF.Exp), reads=[K_("cum")], writes=[K_("dec")])
        bs = []
        for i in range(NTL):
            bs.append(cnt[0] % 2)
            cnt[0] += 1

        def emit_T(i):
            b = bs[i]
            P.op("pe", lambda q: q.transpose(ps_t[b][:, 0:64], kh[:, i * 128:(i + 1) * 128], ident[0:64, 0:64]),
                 reads=[K_("kh"), "ident"], writes=[f"ps_t{b}"])
            P.op("act", lambda q: q.activation(out=khtok[b][:], in_=ps_t[b][:, 0:64], func=AF.Copy),
                 reads=[f"ps_t{b}"], writes=[f"khtok{b}"])
            P.op("pool", lambda q: q.tensor_tensor(out=vblk[b][:], in0=vb[:, i:i + 1, :].broadcast_to([128, 8, 64]),
                                                   in1=ind[:].unsqueeze(2).broadcast_to([128, 8, 64]), op=ALU.mult),
                 reads=[K_("vb"), "ind"], writes=[f"vblk{b}"])

        def emit_KV(i):
            b = bs[i]
            P.op("pe", lambda q: q.matmul(ps_kv[b][0:64, :], lhsT=khtok[b][:], rhs=vblk[b][:].rearrange("p c v -> p (c v)"),
                                          start=True, stop=True), reads=[f"khtok{b}", f"vblk{b}"], writes=[f"ps_kv{b}"])
            P.op("dve", lambda q: q.tensor_copy(out=kvs3[:, :, 8 * i:8 * i + 8],
                                                in_=ps_kv[b][0:64, :].rearrange("p (c v) -> p v c", v=64)),
                 reads=[f"ps_kv{b}"], writes=[K_("kvs")])

        emit_T(0)
        for i in range(NTL):
            if i + 1 < NTL:
                emit_T(i + 1)
            emit_KV(i)

    def stageB(sgi, pb, K_, qt, kt, vb, dec, kvs, kvs3, sprev, obuf, tsl):
        P.op("act", lambda q: q.activation(out=decz[:, 1:NCH], in_=dec[:, 1:NCH], func=AF.Copy), reads=[K_("dec")], writes=["decz"])
        P.op("act", lambda q: q.activation(out=dec3, in_=decz[:].unsqueeze(1).broadcast_to([64, 64, NCH]), func=AF.Copy),
             reads=["decz"], writes=["decrep"])
        P.op("dve", lambda q: q.scalar_tensor_tensor(out=kvs3[:, :, 0], in0=s_in[:], scalar=dec[:, 0:1], in1=kvs3[:, :, 0],
                                                     op0=ALU.mult, op1=ALU.add), reads=["s_in", K_("dec"), K_("kvs")], writes=[K_("kvs")])
        P.op("dve", lambda q: q.tensor_tensor_scan(out=sall[:], data0=decrep[:], data1=kvs[:], initial=0.0,
                                                   op0=ALU.mult, op1=ALU.add), reads=["decrep", K_("kvs")], writes=["sall"])
        P.op("act", lambda q: q.activation(out=sprev[:, 0, :], in_=s_in[:], func=AF.Copy), reads=["s_in"], writes=[K_("sprev")])
        P.op("act", lambda q: q.activation(out=sprev[:, 1:NCH, :], in_=sall3[:, :, 0:NCH - 1].rearrange("p v n -> p n v"), func=AF.Copy),
             reads=["sall"], writes=[K_("sprev")])
        P.op("dve", lambda q: q.tensor_copy(out=s_in[:], in_=sall3[:, :, NCH - 1]), reads=["sall", K_("sprev")], writes=["s_in"])
        bs = []
        for i in range(NTL):
            bs.append(cnt[0] % 2)
            cnt[0] += 1

        def emit_A(i):
            b = bs[i]
            csl = slice(i * 128, (i + 1) * 128)
            P.op("pe", lambda q: q.matmul(ps_a[b][:, 0:128], lhsT=kt[:, csl], rhs=qt[:, csl], start=True, stop=True),
                 reads=[K_("kt"), K_("qt")], writes=[f"ps_a{b}"])
            P.op("dve", lambda q: q.tensor_tensor(out=am[b][:], in0=ps_a[b][:, 0:128], in1=tri[:], op=ALU.mult),
                 reads=[f"ps_a{b}", "tri"], writes=[f"am{b}"])

        def emit_O(i):
            b = bs[i]
            csl = slice(i * 128, (i + 1) * 128)
            P.op("pe", lambda q: q.matmul(ps_o[b][0:64, 0:128], lhsT=vb[:, i, :], rhs=am[b][:], start=True, stop=False),
                 reads=[K_("vb"), f"am{b}"], writes=[f"ps_o{b}"])
            for c in range(8):
                n = 8 * i + c
                P.op("pe", lambda q, c=c, n=n: q.matmul(ps_o[b][0:64, 16 * c:16 * c + 16], lhsT=sprev[:, n, :],
                                                       rhs=qt[:, i * 128 + 16 * c:i * 128 + 16 * c + 16],
                                                       start=False, stop=(c == 7), skip_group_check=True),
                     reads=[K_("sprev"), K_("qt")], writes=[f"ps_o{b}"])
            P.op("act", lambda q: q.activation(out=obuf[:, csl], in_=ps_o[b][0:64, 0:128], func=AF.Copy),
                 reads=[f"ps_o{b}"], writes=[K_("obuf")])

        emit_A(0)
        for i in range(NTL):
            if i + 1 < NTL:
                emit_A(i + 1)
            emit_O(i)
        P.dma("sp", oT[:, tsl], obuf[:], reads=[K_("obuf")], is_output=True)

    stage(0, "A")
    for sgi in range(NSEG):
        if sgi + 1 < NSEG:
            stage(sgi + 1, "A")
        stage(sgi, "B")
    return P.finish()


def run_k2a(o32_full):
    ident, tri, ind = hgrn_consts()
    in_maps = []
    for c in range(NCORES):
        h, d = c % 4, c // 4
        rows = slice(h * 64, (h + 1) * 64)
        q = o32_full["qa"][rows]
        k = o32_full["kf" if d == 0 else "kb"][rows]
        l = o32_full["lf" if d == 0 else "lb"][rows]
        v = o32_full["va"][rows]
        if d == 1:
            q, k, l, v = q[:, ::-1], k[:, ::-1], l[:, ::-1], v[:, ::-1]
        in_maps.append({"qT": np.ascontiguousarray(q), "kT": np.ascontiguousarray(k), "lT": np.ascontiguousarray(l),
                        "vtok": np.ascontiguousarray(v.T), "ident_d": ident, "tri_d": tri, "ind_d": ind})
    res = run(("k2a",), build_k2a, in_maps)
    of = np.concatenate([res[h]["oT"] for h in range(4)], 0)
    ob = np.concatenate([res[4 + h]["oT"][:, ::-1] for h in range(4)], 0)
    return of, np.ascontiguousarray(ob)


GU = 16
C_PAT = ((128, 1), (512, 4), (2048, 16))
NUC = 3 * 4 * 16
NUD = 4 * 16


def attn_masks():
    k = np.arange(256)[:, None]
    q = np.arange(128)[None, :]
    mc = np.where(np.abs(k - 64 - q) <= 64, 1.0, 0.0).astype(np.float32)
    k = np.arange(384)[:, None]
    md = np.where(np.abs(k - 128 - q) <= 128, 1.0, 0.0).astype(np.float32)
    mc = mc.reshape(2, 128, 128).transpose(1, 0, 2)
    md = md.reshape(3, 128, 128).transpose(1, 0, 2)
    return (np.ascontiguousarray(mc).astype(ml_dtypes.bfloat16), np.ascontiguousarray(md).astype(ml_dtypes.bfloat16))


def build_k2b():
    P = Prog()
    specs = []
    for nm, nu, nb in (("c", NUC, 2), ("d", NUD, 3)):
        ng = nu // GU
        specs.append(dict(nm=nm, nb=nb, ng=ng,
                          Q=P.dram_in(nm + "Q", [ng, 64, GU * 128], BF16),
                          K=P.dram_in(nm + "K", [ng, 64, GU * nb * 128], BF16),
                          V=P.dram_in(nm + "V", [ng, 128, GU * nb * 65], BF16),
                          M=P.dram_in(nm + "M", [128, nb, 128], BF16),
                          O=P.dram_out(nm + "O", [ng, 65, GU * 128], F32)))
    ident_d = P.dram_in("ident_d", [128, 128], BF16)
    ident = P.sb("ident", [128, 128], BF16)
    P.dma("sp", ident[:], ident_d, writes=["ident"])
    Qg = [P.sb(f"Qg{i}", [64, GU * 128], BF16) for i in range(2)]
    Kg = [P.sb(f"Kg{i}", [64, GU * 3 * 128], BF16) for i in range(2)]
    Vg = [P.sb(f"Vg{i}", [128, GU * 3 * 65], BF16) for i in range(2)]
    Og = [P.sb(f"Og{i}", [65, GU * 128], F32) for i in range(2)]
    Pt = [P.sb(f"Pt{i}", [128, 384], BF16) for i in range(3)]
    psS = [P.ps(f"psS{i}", [128, 512], F32) for i in range(4)]
    psO = [P.ps(f"psO{i}", [128, 512], F32) for i in range(3)]
    gi = 0
    ui = 0
    for sp_ in specs:
        nb = sp_["nb"]
        mk = P.sb("mask_" + sp_["nm"], [128, nb, 128], BF16)
        mkk = "mask_" + sp_["nm"]
        P.dma("sp", mk[:], sp_["M"], writes=[mkk])
        for g in range(sp_["ng"]):
            b2 = gi % 2
            gi += 1
            P.dma("sp", Qg[b2][:], sp_["Q"][g], writes=[f"Qg{b2}"])
            P.dma("sp", Kg[b2][:, 0:GU * nb * 128], sp_["K"][g], writes=[f"Kg{b2}"])
            P.dma("sp", Vg[b2][:, 0:GU * nb * 65], sp_["V"][g], writes=[f"Vg{b2}"])
            def emit_S(u, s3, p2):
                for b in range(nb):
                    ksl = slice((u * nb + b) * 128, (u * nb + b + 1) * 128)
                    P.op("pe", lambda q, b=b, ksl=ksl: q.matmul(
                        psS[s3][:, b * 128:(b + 1) * 128], lhsT=Kg[b2][:, ksl], rhs=Qg[b2][:, u * 128:(u + 1) * 128],
                        start=True, stop=True), reads=[f"Kg{b2}", f"Qg{b2}"], writes=[f"psS{s3}"])
                P.op("act", lambda q: q.activation(out=Pt[p2][:, 0:nb * 128], in_=psS[s3][:, 0:nb * 128], func=AF.Exp, scale=0.125),
                     reads=[f"psS{s3}"], writes=[f"Pt{p2}"])
                P.op("dve", lambda q: q.tensor_tensor(out=Pt[p2][:, 0:nb * 128], in0=Pt[p2][:, 0:nb * 128],
                                                      in1=mk[:].rearrange("p b q -> p (b q)"), op=ALU.mult),
                     reads=[f"Pt{p2}", mkk], writes=[f"Pt{p2}"])

            def emit_PV(u, s3, p2, so):
                for b in range(nb):
                    vsl = slice((u * nb + b) * 65, (u * nb + b + 1) * 65)
                    P.op("pe", lambda q, b=b, vsl=vsl: q.matmul(
                        psO[so][0:65, 0:128], lhsT=Vg[b2][:, vsl], rhs=Pt[p2][:, b * 128:(b + 1) * 128],
                        start=(b == 0), stop=(b == nb - 1)), reads=[f"Vg{b2}", f"Pt{p2}"], writes=[f"psO{so}"])
                P.op("act", lambda q: q.activation(out=Og[b2][:, u * 128:(u + 1) * 128], in_=psO[so][0:65, 0:128], func=AF.Copy),
                     reads=[f"psO{so}"], writes=[f"Og{b2}"])

            ids = []
            for u in range(GU):
                ids.append((u, ui % 4, ui % 3, ui % 3))
                ui += 1
            emit_S(*ids[0][:3])
            emit_S(*ids[1][:3])
            for j in range(GU):
                if j + 2 < GU:
                    emit_S(*ids[j + 2][:3])
                emit_PV(*ids[j])
            P.dma("sp", sp_["O"][g], Og[b2][:], reads=[f"Og{b2}"], is_output=True)
    return P.finish()


def _windows(Kseq, Vseq, nb, halo, ntile):
    L = Kseq.shape[1]
    W = nb * 128
    Kp = np.zeros((64, L + 2 * halo + 128), Kseq.dtype)
    Kp[:, halo:halo + L] = Kseq
    Vp = np.zeros((L + 2 * halo + 128, 65), Vseq.dtype)
    Vp[halo:halo + L, :64] = Vseq.T
    Vp[halo:halo + L, 64] = 1.0
    Kw = np.stack([Kp[:, 128 * j:128 * j + W] for j in range(ntile)], 0)
    Vw = np.stack([Vp[128 * j:128 * j + W] for j in range(ntile)], 0)
    return Kw, Vw


def _pack_units(Qu, Ku, Vu, nb):
    U = Qu.shape[0]
    ng = U // GU
    Q = Qu.reshape(ng, GU, 64, 128).transpose(0, 2, 1, 3).reshape(ng, 64, GU * 128)
    K = Ku.reshape(ng, GU, 64, nb * 128).transpose(0, 2, 1, 3).reshape(ng, 64, GU * nb * 128)
    V = Vu.reshape(ng, GU, nb, 128, 65).transpose(0, 3, 1, 2, 4).reshape(ng, 128, GU * nb * 65)
    return np.ascontiguousarray(Q), np.ascontiguousarray(K), np.ascontiguousarray(V)


def run_k2b(obf_full):
    mc, md = attn_masks()
    ident = np.eye(128, dtype=np.float32).astype(ml_dtypes.bfloat16)
    cQ = np.zeros((3, 4, 128, 64, 128), ml_dtypes.bfloat16)
    cK = np.zeros((3, 4, 128, 64, 256), ml_dtypes.bfloat16)
    cV = np.zeros((3, 4, 128, 256, 65), ml_dtypes.bfloat16)
    for p, (w, d) in enumerate(C_PAT):
        L = SEQ // d
        nt = L // 128
        for h in range(4):
            rows = slice(h * 64, (h + 1) * 64)
            Qs = obf_full["cq"][rows].reshape(64, L, d)
            Ks = obf_full["ck"][rows].reshape(64, L, d)
            Vs = obf_full["cv"][rows].reshape(64, L, d)
            for r in range(d):
                Kw, Vw = _windows(Ks[:, :, r], Vs[:, :, r], 2, 64, nt)
                cK[p, h, r * nt:(r + 1) * nt] = Kw
                cV[p, h, r * nt:(r + 1) * nt] = Vw
                cQ[p, h, r * nt:(r + 1) * nt] = Qs[:, :, r].reshape(64, nt, 128).transpose(1, 0, 2)
    dQ = np.zeros((4, 128, 64, 128), ml_dtypes.bfloat16)
    dK = np.zeros((4, 128, 64, 384), ml_dtypes.bfloat16)
    dV = np.zeros((4, 128, 384, 65), ml_dtypes.bfloat16)
    for h in range(4):
        kvh = h // 2
        Kw, Vw = _windows(obf_full["dk"][kvh * 64:(kvh + 1) * 64], obf_full["dv"][kvh * 64:(kvh + 1) * 64], 3, 128, 128)
        dK[h] = Kw
        dV[h] = Vw
        dQ[h] = obf_full["dq"][h * 64:(h + 1) * 64].reshape(64, 128, 128).transpose(1, 0, 2)
    in_maps = []
    for c in range(NCORES):
        ts = slice(16 * c, 16 * c + 16)
        q, k, v = _pack_units(cQ[:, :, ts].reshape(NUC, 64, 128), cK[:, :, ts].reshape(NUC, 64, 256), cV[:, :, ts].reshape(NUC, 256, 65), 2)
        q2, k2, v2 = _pack_units(dQ[:, ts].reshape(NUD, 64, 128), dK[:, ts].reshape(NUD, 64, 384), dV[:, ts].reshape(NUD, 384, 65), 3)
        in_maps.append({"cQ": q, "cK": k, "cV": v, "cM": mc, "dQ": q2, "dK": k2, "dV": v2, "dM": md, "ident_d": ident})
    res = run(("k2b",), build_k2b, in_maps)
    numC = np.zeros((3, 256, SEQ), np.float32)
    denC = np.zeros((3, 4, SEQ), np.float32)
    numD = np.zeros((256, SEQ), np.float32)
    denD = np.zeros((4, SEQ), np.float32)
    for c in range(NCORES):
        co = res[c]["cO"].reshape(NUC // GU, 65, GU, 128).transpose(0, 2, 1, 3).reshape(3, 4, 16, 65, 128)
        do = res[c]["dO"].reshape(NUD // GU, 65, GU, 128).transpose(0, 2, 1, 3).reshape(4, 16, 65, 128)
        for p, (w, d) in enumerate(C_PAT):
            L = SEQ // d
            nt = L // 128
            for h in range(4):
                nv = numC[p, h * 64:(h + 1) * 64].reshape(64, L, d)
                dv_ = denC[p, h].reshape(L, d)
                for tl in range(16):
                    tau = 16 * c + tl
                    r, j = tau // nt, tau % nt
                    nv[:, 128 * j:128 * j + 128, r] = co[p, h, tl, 0:64]
                    dv_[128 * j:128 * j + 128, r] = co[p, h, tl, 64]
        for h in range(4):
            for tl in range(16):
                tau = 16 * c + tl
                numD[h * 64:(h + 1) * 64, 128 * tau:128 * tau + 128] = do[h, tl, 0:64]
                denD[h, 128 * tau:128 * tau + 128] = do[h, tl, 64]
    return numC, denC, numD, denD


FB = 4


def build_k3(moe):
    P = Prog()
    T = TOK
    xT = P.dram_in("xT", [D_MODEL, T], F32)
    modc_d = P.dram_in("modc", [128, 48], F32)
    ofT = P.dram_in("ofT", [256, T], F32)
    obT = P.dram_in("obT", [256, T], F32)
    gaT = P.dram_in("gaT", [256, T], F32)
    uext = P.dram_in("uext", [256, T + 32], F32)
    numC = P.dram_in("numC", [3, 256, T], F32)
    denC = P.dram_in("denC", [3, 256, T], F32)
    numD = P.dram_in("numD", [256, T], F32)
    denD = P.dram_in("denD", [256, T], F32)
    smalls_d = P.dram_in("smalls", [128, 128], F32)
    blk64_d = P.dram_in("blk64", [128, 128], F32)
    identb_d = P.dram_in("identb", [128, 128], BF16)
    w_out = P.dram_in("w_out", [1024, 1024], F32)
    if moe:
        wr_d = P.dram_in("wr", [1024, 8], F32)
        w_up = P.dram_in("w_up", [N_EXPERTS, 1024, 2 * EXPERT_DIM], F32)
        w_down = P.dram_in("w_down", [N_EXPERTS, EXPERT_DIM, 1024], F32)
        sel_d = P.dram_in("sel", [8, 8 * 128], F32)
        identf_d = P.dram_in("identf", [128, 128], F32)
    else:
        w_up = P.dram_in("w_up", [1024, 2 * FFN_DIM], F32)
        w_down = P.dram_in("w_down", [FFN_DIM, 1024], F32)
    xo = P.dram_out("xo", [D_MODEL, T], F32)

    pp = PsumPool(P)
    sm = P.sb("sm", [128, 128], F32)
    P.dma("sp", sm[:], smalls_d, writes=["sm"])
    O_ANW, O_CW, O_CB, O_BNW, O_BNB, O_SINK, O_LNW, O_LNB = 0, 1, 63, 65, 67, 69, 71, 87
    modc = P.sb("modc", [128, 48], F32)
    P.dma("sp", modc[:], modc_d, writes=["modc"])
    blk64 = P.sb("blk64", [128, 128], F32)
    P.dma("sp", blk64[:], blk64_d, writes=["blk64"])
    ones_f = P.sb("ones_f", [128, 128], F32)
    P.op("pool", lambda q: q.memset(ones_f[:], 1.0), writes=["ones_f"])
    eps = P.sb("eps", [128, 2], F32)
    P.op("pool", lambda q: q.memset(eps[:, 0:1], LN_EPS), writes=["eps"])
    P.op("pool", lambda q: q.memset(eps[:, 1:2], RMS_EPS), reads=["eps"], writes=["eps"])
    dcol = P.sb("dcol", [128, 32], F32)
    P.op("dve", lambda q: q.tensor_scalar(out=dcol[:, 0:8], in0=modc[:, 16:24], scalar1=1.0, scalar2=None, op0=ALU.add),
         reads=["modc"], writes=["dcol"])
    P.op("dve", lambda q: q.tensor_scalar(out=dcol[:, 8:16], in0=modc[:, 32:40], scalar1=1.0, scalar2=None, op0=ALU.add),
         reads=["modc", "dcol"], writes=["dcol"])
    P.op("dve", lambda q: q.tensor_scalar(out=dcol[:, 16:24], in0=modc[:, 40:48], scalar1=1.0, scalar2=None, op0=ALU.add),
         reads=["modc", "dcol"], writes=["dcol"])
    P.op("act", lambda q: q.activation(out=dcol[:, 24:26], in_=sm[:, O_SINK:O_SINK + 2], func=AF.Exp),
         reads=["sm", "dcol"], writes=["dcol"])
    G1, SC2, G2, ESINK = 0, 8, 16, 24

    x = P.sb("x", [128, 8, T], F32)
    y = P.sb("y", [128, 8, T], BF16)
    wu = [P.sb(f"wu{i}", [128, 8192], BF16) for i in range(2)]
    wd = [P.sb(f"wd{i}", [128, FB, 1024], BF16) for i in range(2)]
    abuf = P.sb("abuf", [128, FB, T], BF16)
    S = [P.sb(f"S{i}", [128, T + 32], F32) for i in range(3)]
    NT = 5
    tmp = [P.sb(f"tmp{i}", [128, 512], F32) for i in range(NT)]
    lnm = P.sb("lnm", [128, 512], F32)
    lnv = P.sb("lnv", [128, 512], F32)
    tcnt = [0]

    def gett():
        i = tcnt[0] % NT
        tcnt[0] += 1
        return tmp[i], f"tmp{i}"

    def tsl(tg):
        return slice(tg * 512, (tg + 1) * 512)

    xk = lambda i, tg: f"x{i}_{tg}"
    yk = lambda i, tg: f"y{i}_{tg}"

    xT_v = xT.rearrange("(k p) t -> p k t", p=128)
    for k in range(8):
        P.dma("sp", x[:, k, :], xT_v[:, k, :], writes=[xk(k, tg) for tg in range(4)])
    wo_v = w_out.rearrange("(k p) e -> p k e", p=128)
    wo_s = wu[0][:].rearrange("p (k e) -> p k e", k=8)
    for k in range(8):
        P.dma("pool", wo_s[:, k, :], wo_v[:, k, :], writes=["wu0"])

    def ln_fm(srcs, nfeat, wcol, bcol, dst, silu=False):
        n = len(srcs)
        for tg in range(4):
            ps_s, ks_ = pp.get()
            ps_q, kq_ = pp.get()
            for i, (af, kf) in enumerate(srcs):
                P.op("pe", lambda q, af=af, i=i: q.matmul(ps_s[:, :], lhsT=ones_f[:], rhs=af(tg), start=(i == 0), stop=(i == n - 1)),
                     reads=["ones_f", kf(tg)], writes=[ks_])
            for i, (af, kf) in enumerate(srcs):
                sq, sqk = gett()
                P.op("act", lambda q, af=af, sq=sq: q.activation(out=sq[:], in_=af(tg), func=AF.Square), reads=[kf(tg)], writes=[sqk])
                P.op("pe", lambda q, sq=sq, i=i: q.matmul(ps_q[:, :], lhsT=ones_f[:], rhs=sq[:], start=(i == 0), stop=(i == n - 1)),
                     reads=["ones_f", sqk], writes=[kq_])
            mean, mk = lnm, "lnm"
            P.op("act", lambda q: q.activation(out=mean[:], in_=ps_s[:], func=AF.Copy, scale=1.0 / nfeat), reads=[ks_], writes=[mk])
            var, vk = lnv, "lnv"
            P.op("dve", lambda q: q.tensor_tensor(out=var[:], in0=mean[:], in1=mean[:], op=ALU.mult), reads=[mk], writes=[vk])
            P.op("dve", lambda q: q.scalar_tensor_tensor(out=var[:], in0=ps_q[:], scalar=1.0 / nfeat, in1=var[:], op0=ALU.mult,
                                                         op1=ALU.subtract), reads=[kq_, vk], writes=[vk])
            P.op("act", lambda q: q.activation(out=var[:], in_=var[:], func=AF.Sqrt, bias=eps[:, 0:1], scale=1.0), reads=[vk, "eps"], writes=[vk])
            P.op("dve", lambda q: q.reciprocal(out=var[:], in_=var[:]), reads=[vk], writes=[vk])
            for i, ((af, kf), (df, dkf)) in enumerate(zip(srcs, dst)):
                t, tk = gett()
                P.op("dve", lambda q, af=af, t=t: q.tensor_tensor(out=t[:], in0=af(tg), in1=mean[:], op=ALU.subtract),
                     reads=[kf(tg), mk], writes=[tk])
                P.op("pool", lambda q, t=t: q.tensor_tensor(out=t[:], in0=t[:], in1=var[:], op=ALU.mult), reads=[tk, vk], writes=[tk])
                if silu:
                    P.op("act", lambda q, t=t, df=df, i=i: q.activation(out=df(tg), in_=t[:], func=AF.Silu, scale=wcol(i), bias=bcol(i)),
                         reads=[tk, "sm"], writes=[dkf(tg)])
                else:
                    P.op("dve", lambda q, t=t, df=df, i=i: q.tensor_scalar(out=df(tg), in0=t[:], scalar1=wcol(i), scalar2=bcol(i),
                                                                         op0=ALU.mult, op1=ALU.add), reads=[tk, "sm"], writes=[dkf(tg)])

    for a in range(2):
        rows = slice(a * 128, (a + 1) * 128)
        for tg in range(4):
            t0, k0 = gett()
            t1, k1 = gett()
            t2, k2 = gett()
            P.dma("sp", t0[:], ofT[rows, tsl(tg)], writes=[k0])
            P.dma("sp", t1[:], obT[rows, tsl(tg)], writes=[k1])
            P.dma("sp", t2[:], gaT[rows, tsl(tg)], writes=[k2])
            P.op("pool", lambda q, t0=t0, t1=t1: q.tensor_tensor(out=t0[:], in0=t0[:], in1=t1[:], op=ALU.add), reads=[k0, k1], writes=[k0])
            P.op("act", lambda q, t0=t0, t1=t1: q.activation(out=t1[:], in_=t0[:], func=AF.Square), reads=[k0], writes=[k1])
            ps, pk = pp.get()
            P.op("pe", lambda q, ps=ps, t1=t1: q.matmul(ps[:, :], lhsT=blk64[:], rhs=t1[:], start=True, stop=True), reads=["blk64", k1], writes=[pk])
            P.op("act", lambda q, ps=ps, t1=t1: q.activation(out=t1[:], in_=ps[:], func=AF.Sqrt, bias=eps[:, 1:2], scale=1.0 / 64),
                 reads=[pk, "eps"], writes=[k1])
            P.op("dve", lambda q, t1=t1: q.reciprocal(out=t1[:], in_=t1[:]), reads=[k1], writes=[k1])
            P.op("dve", lambda q, t0=t0, t1=t1: q.scalar_tensor_tensor(out=t0[:], in0=t0[:], scalar=sm[:, O_ANW:O_ANW + 1], in1=t1[:],
                                                                     op0=ALU.mult, op1=ALU.mult), reads=[k0, k1, "sm"], writes=[k0])
            P.op("dve", lambda q, t0=t0, t2=t2, a=a, tg=tg: q.tensor_tensor(out=y[:, a, tsl(tg)], in0=t0[:], in1=t2[:], op=ALU.mult),
                 reads=[k0, k2], writes=[yk(a, tg)])

    identb = wd[1][:, 1, 0:128]
    P.dma("sp", identb, identb_d, writes=["identb"])
    dg = [wd[1][:, 0, i * 128:(i + 1) * 128] for i in range(4)]
    ubf = abuf[:].rearrange("p f t -> p (f t)")[:, 0:T + 32]
    for b in range(2):
        ub, ubk = S[0], "S0"
        acc, acck = S[1 + b], f"S{1 + b}"
        P.dma("sp", ub[:], uext[b * 128:(b + 1) * 128, :], writes=[ubk])
        P.op("act", lambda q: q.activation(out=ubf, in_=ub[:], func=AF.Copy), reads=[ubk], writes=["ubf"])
        pss = [pp.get() for _ in range(4)]
        for j in range(B_KERNEL):
            d_, dk_ = dg[j % 4], f"dg{j % 4}"
            P.op("dve", lambda q, d_=d_, j=j, b=b: q.tensor_scalar(out=d_, in0=identb, scalar1=sm[:, O_CW + b * 31 + j:O_CW + b * 31 + j + 1],
                                                                 scalar2=None, op0=ALU.mult), reads=["identb", "sm"], writes=[dk_])
            for tg in range(4):
                ps, pk = pss[tg]
                P.op("pe", lambda q, ps=ps, d_=d_, j=j, tg=tg: q.matmul(ps[:, :], lhsT=d_, rhs=ubf[:, tg * 512 + j:tg * 512 + j + 512],
                                                                        start=(j == 0), stop=(j == B_KERNEL - 1)),
                     reads=[dk_, "ubf"], writes=[pk])
        for tg in range(4):
            ps, pk = pss[tg]
            P.op("act", lambda q, ps=ps, acc=acc, tg=tg, b=b: q.activation(out=acc[:, tsl(tg)], in_=ps[:], func=AF.Identity,
                                                                         bias=sm[:, O_CB + b:O_CB + b + 1], scale=1.0),
                 reads=[pk, "sm"], writes=[acck])
    ln_fm([(lambda tg, b=b: S[1 + b][:, tsl(tg)], lambda tg, b=b: f"S{1 + b}") for b in range(2)], 256,
          lambda i: sm[:, O_BNW + i:O_BNW + i + 1], lambda i: sm[:, O_BNB + i:O_BNB + i + 1],
          [(lambda tg, b=b: y[:, 2 + b, tsl(tg)], lambda tg, b=b: yk(2 + b, tg)) for b in range(2)], silu=True)

    for a in range(2):
        rows = slice(a * 128, (a + 1) * 128)
        for tg in range(4):
            n0, kn0 = gett()
            d0, kd0 = gett()
            P.dma("sp", n0[:], numC[0, rows, tsl(tg)], writes=[kn0])
            P.dma("sp", d0[:], denC[0, rows, tsl(tg)], writes=[kd0])
            n1, kn1 = gett()
            d1, kd1 = gett()
            for p in (1, 2):
                P.dma("sp", n1[:], numC[p, rows, tsl(tg)], writes=[kn1])
                P.dma("sp", d1[:], denC[p, rows, tsl(tg)], writes=[kd1])
                P.op("pool", lambda q, n0=n0, n1=n1: q.tensor_tensor(out=n0[:], in0=n0[:], in1=n1[:], op=ALU.add), reads=[kn0, kn1], writes=[kn0])
                P.op("dve", lambda q, d0=d0, d1=d1: q.tensor_tensor(out=d0[:], in0=d0[:], in1=d1[:], op=ALU.add), reads=[kd0, kd1], writes=[kd0])
            P.op("dve", lambda q, d0=d0: q.reciprocal(out=d0[:], in_=d0[:]), reads=[kd0], writes=[kd0])
            P.op("dve", lambda q, n0=n0, d0=d0, a=a, tg=tg: q.tensor_tensor(out=y[:, 4 + a, tsl(tg)], in0=n0[:], in1=d0[:], op=ALU.mult),
                 reads=[kn0, kd0], writes=[yk(4 + a, tg)])
            n0, kn0 = gett()
            d0, kd0 = gett()
            P.dma("sp", n0[:], numD[rows, tsl(tg)], writes=[kn0])
            P.dma("sp", d0[:], denD[rows, tsl(tg)], writes=[kd0])
            P.op("dve", lambda q, d0=d0, a=a: q.tensor_scalar(out=d0[:], in0=d0[:], scalar1=dcol[:, ESINK + a:ESINK + a + 1], scalar2=None,
                                                            op0=ALU.add), reads=[kd0, "dcol"], writes=[kd0])
            P.op("dve", lambda q, d0=d0: q.reciprocal(out=d0[:], in_=d0[:]), reads=[kd0], writes=[kd0])
            P.op("dve", lambda q, n0=n0, d0=d0, a=a, tg=tg: q.tensor_tensor(out=y[:, 6 + a, tsl(tg)], in0=n0[:], in1=d0[:], op=ALU.mult),
                 reads=[kn0, kd0], writes=[yk(6 + a, tg)])

    for k in range(8):
        for tg in range(4):
            P.op("act", lambda q, k=k, tg=tg: q.activation(out=x[:, k, tsl(tg)], in_=x[:, k, tsl(tg)], func=AF.Copy, scale=DEEPNORM_ALPHA),
                 reads=[xk(k, tg)], writes=[xk(k, tg)])
    for dc in range(8):
        for tg in range(4):
            ps, pk = pp.get()
            for k in range(8):
                P.op("pe", lambda q, ps=ps, k=k, dc=dc, tg=tg: q.matmul(ps[:, :], lhsT=wo_s[:, k, dc * 128:(dc + 1) * 128], rhs=y[:, k, tsl(tg)],
                                                                        start=(k == 0), stop=(k == 7)), reads=["wu0", yk(k, tg)], writes=[pk])
            P.op("dve", lambda q, ps=ps, dc=dc, tg=tg: q.scalar_tensor_tensor(out=x[:, dc, tsl(tg)], in0=ps[:], scalar=dcol[:, G1 + dc:G1 + dc + 1],
                                                                             in1=x[:, dc, tsl(tg)], op0=ALU.mult, op1=ALU.add),
                 reads=[pk, "dcol", xk(dc, tg)], writes=[xk(dc, tg)])
    xs = [(lambda tg, k=k: x[:, k, tsl(tg)], lambda tg, k=k: xk(k, tg)) for k in range(8)]
    ln_fm(xs, 1024, lambda i: sm[:, O_LNW + i:O_LNW + i + 1], lambda i: sm[:, O_LNB + i:O_LNB + i + 1], xs)

    for k in range(8):
        for tg in range(4):
            P.op("dve", lambda q, k=k, tg=tg: q.tensor_scalar(out=y[:, k, tsl(tg)], in0=x[:, k, tsl(tg)], scalar1=dcol[:, SC2 + k:SC2 + k + 1],
                                                            scalar2=modc[:, 24 + k:25 + k], op0=ALU.mult, op1=ALU.add),
                 reads=[xk(k, tg), "dcol", "modc"], writes=[yk(k, tg)])

    gT = None
    if moe:
        wr = P.sb("wr", [128, 8, 8], F32)
        P.dma("sp", wr[:], wr_d.rearrange("(k p) e -> p k e", p=128), writes=["wr"])
        sel = P.sb("sel", [8, 8 * 128], F32)
        P.dma("sp", sel[:], sel_d, writes=["sel"])
        identf = P.sb("identf", [128, 128], F32)
        P.dma("sp", identf[:], identf_d, writes=["identf"])
        psl, pslk = pp.get()
        for k in range(8):
            h2f, hk = S[0], "S0"
            P.op("dve", lambda q, k=k: q.tensor_scalar(out=h2f[:, 0:T], in0=x[:, k, :], scalar1=dcol[:, SC2 + k:SC2 + k + 1],
                                                       scalar2=modc[:, 24 + k:25 + k], op0=ALU.mult, op1=ALU.add),
                 reads=[xk(k, tg) for tg in range(4)] + ["dcol", "modc"], writes=[hk])
            for ti in range(16):
                P.op("pe", lambda q, k=k, ti=ti: q.matmul(psl[:, ti * 8:(ti + 1) * 8], lhsT=h2f[:, ti * 128:(ti + 1) * 128], rhs=wr[:, k, :],
                                                          start=(k == 0 and ti == 0), stop=(k == 7 and ti == 15), skip_group_check=True),
                     reads=[hk, "wr"], writes=[pslk])
        lg = P.sb("lg", [128, 16, 8], F32)
        lg2 = P.sb("lg2", [128, 16, 8], F32)
        eq1 = S[1][:, 0:128].rearrange("p (t e) -> p t e", e=8)
        eq2 = S[1][:, 128:256].rearrange("p (t e) -> p t e", e=8)
        m1 = P.sb("m1", [128, 16], F32)
        m2 = P.sb("m2", [128, 16], F32)
        g1 = P.sb("g1", [128, 16], F32)
        bc3 = lambda t: t[:].unsqueeze(2).broadcast_to([128, 16, 8])
        P.op("dve", lambda q: q.tensor_copy(out=lg[:], in_=psl[:, 0:128].rearrange("p (t e) -> p t e", e=8)), reads=[pslk], writes=["lg"])
        P.op("dve", lambda q: q.tensor_reduce(out=m1[:], in_=lg[:], axis=AX.X, op=ALU.max), reads=["lg"], writes=["m1"])
        P.op("dve", lambda q: q.tensor_tensor(out=eq1, in0=lg[:], in1=bc3(m1), op=ALU.is_equal), reads=["lg", "m1"], writes=["eq1"])
        P.op("dve", lambda q: q.scalar_tensor_tensor(out=lg2[:], in0=eq1, scalar=-1e30, in1=lg[:], op0=ALU.mult, op1=ALU.add),
             reads=["eq1", "lg"], writes=["lg2"])
        P.op("dve", lambda q: q.tensor_reduce(out=m2[:], in_=lg2[:], axis=AX.X, op=ALU.max), reads=["lg2"], writes=["m2"])
        P.op("dve", lambda q: q.tensor_tensor(out=eq2, in0=lg2[:], in1=bc3(m2), op=ALU.is_equal), reads=["lg2", "m2"], writes=["eq2"])
        P.op("dve", lambda q: q.tensor_tensor(out=m2[:], in0=m2[:], in1=m1[:], op=ALU.subtract), reads=["m2", "m1"], writes=["m2"])
        P.op("act", lambda q: q.activation(out=m2[:], in_=m2[:], func=AF.Exp), reads=["m2"], writes=["m2"])
        P.op("dve", lambda q: q.tensor_scalar(out=g1[:], in0=m2[:], scalar1=1.0, scalar2=None, op0=ALU.add), reads=["m2"], writes=["g1"])
        P.op("dve", lambda q: q.reciprocal(out=g1[:], in_=g1[:]), reads=["g1"], writes=["g1"])
        P.op("dve", lambda q: q.tensor_tensor(out=m2[:], in0=m2[:], in1=g1[:], op=ALU.mult), reads=["m2", "g1"], writes=["m2"])
        P.op("dve", lambda q: q.tensor_tensor(out=eq1, in0=eq1, in1=bc3(g1), op=ALU.mult), reads=["eq1", "g1"], writes=["eq1"])
        P.op("dve", lambda q: q.tensor_tensor(out=eq2, in0=eq2, in1=bc3(m2), op=ALU.mult), reads=["eq2", "m2"], writes=["eq2"])
        P.op("dve", lambda q: q.tensor_tensor(out=eq1, in0=eq1, in1=eq2, op=ALU.add), reads=["eq1", "eq2"], writes=["eq1"])
        gT = S[0][0:8, 0:T]
        for tg in range(4):
            ps, pk = pp.get()
            for j in range(4):
                ti = tg * 4 + j
                P.op("pe", lambda q, ps=ps, j=j, ti=ti: q.transpose(ps[0:8, j * 128:(j + 1) * 128], eq1[:, ti, :], identf[:]),
                     reads=["eq1", "identf"], writes=[pk])
            P.op("act", lambda q, ps=ps, tg=tg: q.activation(out=gT[:, tsl(tg)], in_=ps[0:8, :], func=AF.Copy), reads=[pk], writes=["S0"])

    for k in range(8):
        for tg in range(4):
            P.op("act", lambda q, k=k, tg=tg: q.activation(out=x[:, k, tsl(tg)], in_=x[:, k, tsl(tg)], func=AF.Copy, scale=DEEPNORM_ALPHA),
                 reads=[xk(k, tg)], writes=[xk(k, tg)])

    blocks = []
    if moe:
        for e in range(N_EXPERTS):
            for (f0, nf) in chunks(EXPERT_DIM // 128, FB):
                blocks.append((w_up[e], w_down[e], EXPERT_DIM, f0, nf, e))
    else:
        for (f0, nf) in chunks(FFN_DIM // 128, FB):
            blocks.append((w_up, w_down, FFN_DIM, f0, nf, None))

    stg = [S[2][:, i * 512:(i + 1) * 512] for i in range(4)]
    stg_n = [0]

    def block_pieces(bi):
        wup, wdn, F, f0, nf, e = blocks[bi]
        b2 = bi % 2
        wuv = wu[b2][:].rearrange("p (k s c) -> p k s c", k=8, s=2)
        upv = wup.rearrange("(k p) c -> p k c", p=128)
        pcs = []
        for k in range(8):
            for s_ in range(2):
                pcs.append((wuv[:, k, s_, 0:nf * 128], upv[:, k, s_ * F + f0 * 128:s_ * F + (f0 + nf) * 128], nf * 128, f"wu{b2}"))
        for fc in range(nf):
            for hf in range(2):
                pcs.append((wd[b2][:, fc, hf * 512:(hf + 1) * 512],
                            wdn[(f0 + fc) * 128:(f0 + fc + 1) * 128, hf * 512:(hf + 1) * 512], 512, f"wd{b2}"))
        return pcs

    def emit_piece(pc):
        dst, src, n, dkey = pc
        i = stg_n[0] % 4
        first = stg_n[0] < 4
        stg_n[0] += 1
        P.dma("sp", stg[i][:, 0:n], src, writes=(["S2", f"stg{i}"] if first else [f"stg{i}"]))
        P.op("act", lambda q: q.activation(out=dst, in_=stg[i][:, 0:n], func=AF.Copy), reads=[f"stg{i}"], writes=[dkey])

    for pc in block_pieces(0):
        emit_piece(pc)
    for bi, (wup, wdn, F, f0, nf, e) in enumerate(blocks):
        b2 = bi % 2
        nxt = block_pieces(bi + 1) if bi + 1 < len(blocks) else []
        per_it = -(-len(nxt) // (nf * 4)) if nxt else 0
        gmul = None
        gk = None
        if moe:
            gmul, gk = S[1], "S1"
            if f0 == 0:
                for tg in range(4):
                    ps, pk = pp.get()
                    P.op("pe", lambda q, ps=ps, e=e, tg=tg: q.matmul(ps[:, :], lhsT=sel[:, e * 128:(e + 1) * 128], rhs=gT[:, tsl(tg)],
                                                                    start=True, stop=True), reads=["sel", "S0"], writes=[pk])
                    P.op("act", lambda q, ps=ps, gmul=gmul, tg=tg: q.activation(out=gmul[:, tsl(tg)], in_=ps[:], func=AF.Copy),
                         reads=[pk], writes=[gk + f"_{tg}"])
        wuv = wu[b2][:].rearrange("p (k s c) -> p k s c", k=8, s=2)
        for fc in range(nf):
            for tg in range(4):
                psg, kg = pp.get()
                psu, ku = pp.get()
                for s_, ps_, pk_ in ((0, psg, kg), (1, psu, ku)):
                    for k in range(8):
                        P.op("pe", lambda q, ps_=ps_, k=k, s_=s_, fc=fc, tg=tg: q.matmul(ps_[:, :], lhsT=wuv[:, k, s_, fc * 128:(fc + 1) * 128],
                                                                                      rhs=y[:, k, tsl(tg)], start=(k == 0), stop=(k == 7)),
                             reads=[f"wu{b2}", yk(k, tg)], writes=[pk_])
                sg, sgk = gett()
                P.op("act", lambda q, sg=sg, psg=psg: q.activation(out=sg[:], in_=psg[:], func=AF.Silu), reads=[kg], writes=[sgk])
                if gmul is not None:
                    P.op("pool", lambda q, sg=sg, gmul=gmul, tg=tg: q.tensor_tensor(out=sg[:], in0=sg[:], in1=gmul[:, tsl(tg)], op=ALU.mult),
                         reads=[sgk, gk + f"_{tg}"], writes=[sgk])
                P.op("dve", lambda q, sg=sg, psu=psu, fc=fc, tg=tg: q.tensor_tensor(out=abuf[:, fc, tsl(tg)], in0=sg[:], in1=psu[:], op=ALU.mult),
                     reads=[sgk, ku], writes=[f"a{fc}_{tg}"])
                for _ in range(per_it):
                    if nxt:
                        emit_piece(nxt.pop(0))
        while nxt:
            emit_piece(nxt.pop(0))
        for dc in range(8):
            for tg in range(4):
                ps, pk = pp.get()
                for fc in range(nf):
                    P.op("pe", lambda q, ps=ps, fc=fc, dc=dc, tg=tg: q.matmul(ps[:, :], lhsT=wd[b2][:, fc, dc * 128:(dc + 1) * 128],
                                                                              rhs=abuf[:, fc, tsl(tg)], start=(fc == 0), stop=(fc == nf - 1)),
                         reads=[f"wd{b2}", f"a{fc}_{tg}"], writes=[pk])
                P.op("dve", lambda q, ps=ps, dc=dc, tg=tg: q.scalar_tensor_tensor(out=x[:, dc, tsl(tg)], in0=ps[:], scalar=dcol[:, G2 + dc:G2 + dc + 1],
                                                                                 in1=x[:, dc, tsl(tg)], op0=ALU.mult, op1=ALU.add),
                     reads=[pk, "dcol", xk(dc, tg)], writes=[xk(dc, tg)])

    ln_fm(xs, 1024, lambda i: sm[:, O_LNW + 8 + i:O_LNW + 9 + i], lambda i: sm[:, O_LNB + 8 + i:O_LNB + 9 + i], xs)
    xo_v = xo.rearrange("(k p) t -> p k t", p=128)
    for k in range(8):
        P.dma("sp", xo_v[:, k, :], x[:, k, :], reads=[xk(k, tg) for tg in range(4)], is_output=True)
    return P.finish()


def run_k3(layer, xT_shards, mods, of, ob, o32_full, numC, denC, numD, denD, inp):
    moe = (layer % 2 == 1)
    T = TOK
    sm = np.zeros((128, 128), np.float32)
    anw = np.asarray(inp["a_norm_w"])[layer]
    sm[:, 0] = np.concatenate([anw, anw])
    cw = np.asarray(inp["b_conv_w"])[layer]
    for b in range(2):
        sm[:, 1 + b * 31:1 + (b + 1) * 31] = cw[:, b * 128:(b + 1) * 128].T
    sm[:, 63:65] = col128(np.asarray(inp["b_conv_b"])[layer])
    sm[:, 65:67] = col128(np.asarray(inp["b_norm_w"])[layer])
    sm[:, 67:69] = col128(np.asarray(inp["b_norm_b"])[layer])
    sink = np.asarray(inp["d_sink"])[layer]
    sm[:, 69:71] = col128(np.repeat(sink, 64))
    sm[:, 71:79] = col128(np.asarray(inp["ln_w"])[layer, 0])
    sm[:, 79:87] = col128(np.asarray(inp["ln_w"])[layer, 1])
    sm[:, 87:95] = col128(np.asarray(inp["ln_b"])[layer, 0])
    sm[:, 95:103] = col128(np.asarray(inp["ln_b"])[layer, 1])
    blk64 = np.kron(np.eye(2, dtype=np.float32), np.ones((64, 64), np.float32))
    u = o32_full["u"]
    upad = np.zeros((256, SEQ + 32), np.float32)
    upad[:, 15:15 + SEQ] = u
    denC_rep = np.repeat(denC, 64, axis=1)
    denD_rep = np.repeat(denD, 64, axis=0)
    w_out = np.ascontiguousarray(inp["w_out"][layer])
    common = {"smalls": sm, "blk64": blk64, "w_out": w_out, "identb": np.eye(128, dtype=np.float32).astype(ml_dtypes.bfloat16)}
    if moe:
        li = layer // 2
        sel = np.zeros((8, 8 * 128), np.float32)
        for e in range(8):
            sel[e, e * 128:(e + 1) * 128] = 1.0
        common.update({"wr": np.ascontiguousarray(inp["moe_router"][li]), "w_up": np.ascontiguousarray(inp["moe_w_up"][li]),
                       "w_down": np.ascontiguousarray(inp["moe_w_down"][li]), "sel": sel, "identf": np.eye(128, dtype=np.float32)})
    else:
        li = layer // 2
        common.update({"w_up": np.ascontiguousarray(inp["ffn_w_up"][li]), "w_down": np.ascontiguousarray(inp["ffn_w_down"][li])})
    in_maps = []
    for c in range(NCORES):
        ts = slice(c * T, (c + 1) * T)
        m = dict(common)
        m.update({"xT": xT_shards[c], "modc": mods[c], "ofT": np.ascontiguousarray(of[:, ts]), "obT": np.ascontiguousarray(ob[:, ts]),
                  "gaT": np.ascontiguousarray(o32_full["ga"][:, ts]), "uext": np.ascontiguousarray(upad[:, c * T:c * T + T + 32]),
                  "numC": np.ascontiguousarray(numC[:, :, ts]), "denC": np.ascontiguousarray(denC_rep[:, :, ts]),
                  "numD": np.ascontiguousarray(numD[:, ts]), "denD": np.ascontiguousarray(denD_rep[:, ts])})
        in_maps.append(m)
    res = run(("k3", moe), lambda: build_k3(moe), in_maps)
    return [r["xo"] for r in res]


def run_layer(layer, xT_shards, inp):
    r1 = run_k1(layer, xT_shards, inp)
    o32_full = {nm: np.concatenate([r["o32"][off:off + 256] for r in r1], axis=1) for nm, off in O32.items()}
    obf_full = {nm: np.concatenate([r["obf"][off:off + (128 if nm in ("dk", "dv") else 256)] for r in r1], axis=1)
                for nm, off in OBF.items()}
    mods = [r["omod"] for r in r1]
    of, ob = run_k2a(o32_full)
    numC, denC, numD, denD = run_k2b(obf_full)
    return run_k3(layer, xT_shards, mods, of, ob, o32_full, numC, denC, numD, denD, inp)


def kernel(**inp):
    inp = {k: np.asarray(v) for k, v in inp.items()}
    x = inp["x"][0]
    xT_shards = [np.ascontiguousarray(x[c * TOK:(c + 1) * TOK].T) for c in range(NCORES)]
    for layer in range(DEPTH):
        xT_shards = run_layer(layer, xT_shards, inp)
    out = np.concatenate([s.T for s in xT_shards], axis=0)[None]
    return np.ascontiguousarray(out.astype(np.float32))
```

```python
import math
from contextlib import ExitStack

import numpy as np
import ml_dtypes

import concourse.bass as bass
import concourse.mybir as mybir
from concourse.bass_utils import run_bass_kernel_spmd

F32 = mybir.dt.float32
BF16 = mybir.dt.bfloat16
I32 = mybir.dt.int32
AF = mybir.ActivationFunctionType
ALU = mybir.AluOpType
AX = mybir.AxisListType

NCORES = 8
D_MODEL = 1024
SEQ = 16384
TOK = SEQ // NCORES
DEPTH = 2
HD = 64
FFN_DIM = 2816
N_EXPERTS = 8
EXPERT_DIM = 3584
B_KERNEL = 31
ROPE_THETA = 500000.0
ROPE_DIM = 16
DEEPNORM_ALPHA = (2 * DEPTH) ** 0.25
LN_EPS = 1e-5
RMS_EPS = 1e-6
NEG = -30000.0
TWO_PI = 2.0 * math.pi


class Prog:
    NDS = 24

    def __init__(self):
        self.nc = bass.Bass("TRN2", target_bir_lowering=False)
        nc = self.nc
        self.es = ExitStack()
        self.q = {"pe": nc.tensor, "dve": nc.vector, "act": nc.scalar, "pool": nc.gpsimd, "sp": nc.sync}
        self.esem = {e: self.es.enter_context(nc.semaphore("es_" + e)) for e in ("pe", "dve", "act", "pool")}
        self.ecnt = {e: 0 for e in self.esem}
        self.dsem = [self.es.enter_context(nc.semaphore(f"ds{i}")) for i in range(self.NDS)]
        self.dval = [0] * self.NDS
        self.dnext = 0
        self.seen = {e: {} for e in self.q}
        self.lastw = {}
        self.readers = {}
        self.out_tokens = []
        self.n_inst = 0
        self._ps_id = 0

    def dram_in(self, name, shape, dt):
        return self.nc.dram_tensor(name, list(shape), dt, kind="ExternalInput").ap()

    def dram_out(self, name, shape, dt):
        return self.nc.dram_tensor(name, list(shape), dt, kind="ExternalOutput").ap()

    def sb(self, name, shape, dt):
        return self.es.enter_context(self.nc.sbuf_tensor("sb_" + name, list(shape), dt))

    def ps(self, name, shape, dt=F32):
        return self.es.enter_context(self.nc.psum_tensor("pm_" + name, list(shape), dt))

    def _wait(self, e, tok):
        sem, v, owner = tok
        if owner == e and e == "pe":
            return
        k = id(sem)
        if self.seen[e].get(k, 0) >= v:
            return
        self.q[e].wait_ge(sem, v)
        self.seen[e][k] = v

    def _deps(self, e, reads, writes):
        for k in reads:
            t = self.lastw.get(k)
            if t is not None:
                self._wait(e, t)
        for k in writes:
            t = self.lastw.get(k)
            if t is not None:
                self._wait(e, t)
            for t in self.readers.get(k, {}).values():
                self._wait(e, t)

    def _record(self, tok, reads, writes):
        for k in writes:
            self.lastw[k] = tok
            self.readers[k] = {}
        for k in reads:
            self.readers.setdefault(k, {})[id(tok[0])] = tok

    def op(self, e, fn, reads=(), writes=()):
        self._deps(e, reads, writes)
        inst = fn(self.q[e])
        self.ecnt[e] += 1
        inst.then_inc(self.esem[e], 1)
        tok = (self.esem[e], self.ecnt[e], e)
        self._record(tok, reads, writes)
        self.n_inst += 1
        return tok

    def dma(self, e, out, in_, reads=(), writes=(), is_output=False, **kw):
        self._deps(e, reads, writes)
        j = self.dnext
        self.dnext = (self.dnext + 1) % self.NDS
        if self.dval[j] > 0:
            self._wait(e, (self.dsem[j], self.dval[j], "dma"))
        self.q[e].dma_start(out=out, in_=in_, **kw).then_inc(self.dsem[j], 16)
        self.dval[j] += 16
        tok = (self.dsem[j], self.dval[j], "dma")
        self._record(tok, reads, writes)
        if is_output:
            self.out_tokens.append(tok)
        self.n_inst += 1
        return tok

    def finish(self):
        for j in range(self.NDS):
            if self.dval[j] > 0:
                self._wait("sp", (self.dsem[j], self.dval[j], "dma"))
        return self.nc


def chunks(n, c):
    return [(i, min(c, n - i)) for i in range(0, n, c)]


class PsumPool:
    def __init__(self, P, n=8, prefix="pb"):
        self.t = [P.ps(f"{prefix}{i}", [128, 512], F32) for i in range(n)]
        self.k = [f"{prefix}{i}" for i in range(n)]
        self.i = 0
        self.n = n

    def get(self):
        i = self.i
        self.i = (self.i + 1) % self.n
        return self.t[i], self.k[i]


def build_consts(P):
    c = {}
    c["ones_f"] = P.sb("ones_f", [128, 512], F32)
    P.op("pool", lambda q: q.memset(c["ones_f"][:], 1.0), writes=["ones_f"])
    c["ones_b"] = P.sb("ones_b", [128, 512], BF16)
    P.op("pool", lambda q: q.memset(c["ones_b"][:], 1.0), writes=["ones_b"])
    return c


CH = {"aq": (0, 2), "aff": (2, 2), "afb": (4, 2), "ai": (6, 2), "ag": (8, 2), "bv": (10, 2), "bg": (12, 2),
      "cq": (14, 2), "ck": (16, 2), "cv": (18, 2), "dq": (20, 2), "dk": (22, 1), "dv": (23, 1)}
ROT_CHUNKS = [14, 15, 16, 17, 20, 21, 22]
O32 = {"qa": 0, "kf": 256, "lf": 512, "kb": 768, "lb": 1024, "va": 1280, "ga": 1536, "u": 1792}
OBF = {"cq": 0, "ck": 256, "cv": 512, "dq": 768, "dk": 1024, "dv": 1152}
NBF = 1280


def build_k1(layer):
    P = Prog()
    nc = P.nc
    T = TOK
    xT = P.dram_in("xT", [D_MODEL, T], F32)
    ccol = P.dram_in("ccol", [128, 8], F32)
    w_ada = P.dram_in("w_ada", [D_MODEL, 6 * D_MODEL], F32)
    bada = P.dram_in("bada", [128, 48], F32)
    w_in = P.dram_in("w_in", [D_MODEL, 3072], F32)
    w_sw = P.dram_in("w_sw", [D_MODEL, 7 * 128], F32)
    pos = P.dram_in("pos", [T], I32)
    rcol = P.dram_in("rcol", [128, 2], F32)
    alb = P.dram_in("alb", [128, 4], F32)
    o32 = P.dram_out("o32", [2048, T], F32)
    obf = P.dram_out("obf", [NBF, T], BF16)
    omod = P.dram_out("omod", [128, 48], F32)

    C = build_consts(P)
    pp = PsumPool(P)

    sc = P.sb("sc", [128, 8], F32)
    P.dma("sp", sc[:], ccol, writes=["sc"])
    P.op("act", lambda q: q.activation(out=sc[:], in_=sc[:], func=AF.Silu), reads=["sc"], writes=["sc"])
    mod = P.sb("mod", [128, 48], F32)
    badas = P.sb("badas", [128, 48], F32)
    P.dma("sp", badas[:], bada, writes=["badas"])
    wst = [P.sb(f"wada_st{i}", [128, 8, 256], F32) for i in range(2)]
    w_ada_v = w_ada.rearrange("(k p) e -> p k e", p=128)
    mps, mpk = pp.get()
    for g in range(24):
        st = wst[g % 2]
        sk = f"wada_st{g % 2}"
        P.dma("sp", st[:], w_ada_v[:, :, g * 256:(g + 1) * 256], writes=[sk])
        for j in range(2):
            e = g * 2 + j
            for k in range(8):
                P.op("pe", lambda q, k=k, j=j, e=e: q.matmul(mps[:, e:e + 1], lhsT=st[:, k, j * 128:(j + 1) * 128],
                                                           rhs=sc[:, k:k + 1], start=(k == 0), stop=(k == 7)),
                     reads=[sk, "sc"], writes=[mpk])
    P.op("dve", lambda q: q.tensor_tensor(out=mod[:], in0=mps[:, 0:48], in1=badas[:], op=ALU.add),
         reads=[mpk, "badas"], writes=["mod"])
    P.dma("pool", omod, mod[:], reads=["mod"], is_output=True)
    sc1 = P.sb("sc1", [128, 8], F32)
    P.op("dve", lambda q: q.tensor_scalar(out=sc1[:], in0=mod[:, 8:16], scalar1=1.0, scalar2=None, op0=ALU.add),
         reads=["mod"], writes=["sc1"])

    hT = P.sb("hT", [128, 8, T], BF16)
    xst = [P.sb(f"xst{i}", [128, T], F32) for i in range(2)]
    xT_v = xT.rearrange("(k p) t -> p k t", p=128)
    for k in range(8):
        st = xst[k % 2]
        sk = f"xst{k % 2}"
        P.dma("sp", st[:], xT_v[:, k, :], writes=[sk])
        P.op("dve", lambda q, k=k, st=st: q.tensor_scalar(out=hT[:, k, :], in0=st[:], scalar1=sc1[:, k:k + 1],
                                                        scalar2=mod[:, k:k + 1], op0=ALU.mult, op1=ALU.add),
             reads=[sk, "sc1", "mod"], writes=[f"hT{k}"])
    hkeys = [f"hT{k}" for k in range(8)]

    wb = P.sb("wb", [128, 8, 3072], BF16)
    wsw = P.sb("wsw", [128, 8, 896], BF16)
    w_in_v = w_in.rearrange("(k p) e -> p k e", p=128)
    w_sw_v = w_sw.rearrange("(k p) e -> p k e", p=128)
    for k in range(8):
        P.dma("pool", wb[:, k, :], w_in_v[:, k, :], writes=[f"wb{k}"])
        P.dma("pool", wsw[:, k, :], w_sw_v[:, k, :], writes=[f"wsw{k}"])
    wkeys = [f"wb{k}" for k in range(8)]
    wswkeys = [f"wsw{k}" for k in range(8)]

    rc = P.sb("rc", [128, 2], F32)
    P.dma("sp", rc[:], rcol, writes=["rc"])
    tmpi = P.sb("tmpi", [128, T], I32)
    P.dma("sp", tmpi[:], pos.partition_broadcast(128), writes=["tmpi"])
    ang = P.sb("ang", [128, T], F32)
    cosT = P.sb("cosT", [128, T], F32)
    sinT = P.sb("sinT", [128, T], F32)
    tmpf = P.sb("tmpf", [128, T], F32)
    P.op("dve", lambda q: q.tensor_copy(out=ang[:], in_=tmpi[:]), reads=["tmpi"], writes=["ang"])
    P.op("dve", lambda q: q.tensor_scalar(out=ang[:], in0=ang[:], scalar1=rc[:, 0:1], scalar2=None, op0=ALU.mult),
         reads=["ang", "rc"], writes=["ang"])
    C1 = 6.28125
    C2 = TWO_PI - C1

    def sin_table(dst, dkey, phase):
        P.op("dve", lambda q: q.tensor_scalar(out=tmpf[:], in0=ang[:], scalar1=phase, scalar2=1.0 / TWO_PI,
                                              op0=ALU.add, op1=ALU.mult), reads=["ang"], writes=["tmpf"])
        P.op("dve", lambda q: q.tensor_copy(out=tmpi[:], in_=tmpf[:]), reads=["tmpf"], writes=["tmpi"])
        P.op("dve", lambda q: q.tensor_copy(out=tmpf[:], in_=tmpi[:]), reads=["tmpi"], writes=["tmpf"])
        P.op("dve", lambda q: q.scalar_tensor_tensor(out=dst[:], in0=tmpf[:], scalar=-C1, in1=ang[:],
                                                     op0=ALU.mult, op1=ALU.add), reads=["tmpf", "ang"], writes=[dkey])
        P.op("dve", lambda q: q.scalar_tensor_tensor(out=dst[:], in0=tmpf[:], scalar=-C2, in1=dst[:],
                                                     op0=ALU.mult, op1=ALU.add), reads=["tmpf", dkey], writes=[dkey])
        P.op("dve", lambda q: q.tensor_scalar(out=dst[:], in0=dst[:], scalar1=phase, scalar2=-math.pi,
                                              op0=ALU.add, op1=ALU.max), reads=[dkey], writes=[dkey])
        P.op("dve", lambda q: q.tensor_scalar(out=dst[:], in0=dst[:], scalar1=math.pi, scalar2=None,
                                              op0=ALU.min), reads=[dkey], writes=[dkey])
        P.op("act", lambda q: q.activation(out=dst[:], in_=dst[:], func=AF.Sin), reads=[dkey], writes=[dkey])

    sin_table(cosT, "cosT", math.pi / 2)
    sin_table(sinT, "sinT", 0.0)
    P.op("dve", lambda q: q.tensor_scalar(out=sinT[:], in0=sinT[:], scalar1=rc[:, 1:2], scalar2=None, op0=ALU.mult),
         reads=["sinT", "rc"], writes=["sinT"])

    albs = P.sb("albs", [128, 4], F32)
    P.dma("sp", albs[:], alb, writes=["albs"])
    lbc = P.sb("lbc", [128, 2], F32)
    oml = P.sb("oml", [128, 2], F32)
    if layer == 0:
        P.op("pool", lambda q: q.memset(lbc[:], 0.0), writes=["lbc"])
        P.op("pool", lambda q: q.memset(oml[:], 1.0), writes=["oml"])
    else:
        ex = P.sb("alb_ex", [128, 4], F32)
        P.op("act", lambda q: q.activation(out=ex[:], in_=albs[:], func=AF.Exp), reads=["albs"], writes=["alb_ex"])
        exv = ex[:].rearrange("p (t l) -> p t l", l=2)
        sm = P.sb("alb_sm", [128, 2], F32)
        P.op("dve", lambda q: q.tensor_tensor(out=sm[:], in0=exv[:, :, 0], in1=exv[:, :, 1], op=ALU.add),
             reads=["alb_ex"], writes=["alb_sm"])
        P.op("dve", lambda q: q.reciprocal(out=sm[:], in_=sm[:]), reads=["alb_sm"], writes=["alb_sm"])
        P.op("dve", lambda q: q.tensor_tensor(out=lbc[:], in0=exv[:, :, 1], in1=sm[:], op=ALU.mult),
             reads=["alb_ex", "alb_sm"], writes=["lbc"])
        P.op("dve", lambda q: q.tensor_scalar(out=oml[:], in0=lbc[:], scalar1=-1.0, scalar2=1.0, op0=ALU.mult,
                                              op1=ALU.add), reads=["lbc"], writes=["oml"])

    ob_f = [P.sb(f"obf{i}", [128, 512], F32) for i in range(4)]
    ob_b = [P.sb(f"obb{i}", [128, 512], BF16) for i in range(4)]
    sg = [P.sb(f"sg{i}", [128, T], F32) for i in range(2)]
    cnt = {"f": 0, "b": 0}

    def getf():
        i = cnt["f"] % 4
        cnt["f"] += 1
        return ob_f[i], f"obf{i}"

    def getb():
        i = cnt["b"] % 4
        cnt["b"] += 1
        return ob_b[i], f"obb{i}"

    def proj(ps, psk, wt, wkeys_, col0, tg):
        for k in range(8):
            P.op("pe", lambda q, k=k: q.matmul(ps[:, :], lhsT=wt[:, k, col0:col0 + 128],
                                               rhs=hT[:, k, tg * 512:(tg + 1) * 512], start=(k == 0), stop=(k == 7)),
                 reads=[wkeys_[k], hkeys[k]], writes=[psk])

    def store32(name, tile_i, tg, buf, bkey):
        r0 = O32[name] + tile_i * 128
        P.dma("sp", o32[r0:r0 + 128, tg * 512:(tg + 1) * 512], buf[:], reads=[bkey], is_output=True)

    def storebf(name, tile_i, tg, buf, bkey):
        r0 = OBF[name] + tile_i * 128
        P.dma("sp", obf[r0:r0 + 128, tg * 512:(tg + 1) * 512], buf[:], reads=[bkey], is_output=True)

    order = ["bg", "bv", "aq", "aff", "afb", "ai", "ag", "cq", "ck", "cv", "dq", "dk", "dv"]
    for name in order:
        c0, ncn = CH[name]
        for ti in range(ncn):
            ch = c0 + ti
            for tg in range(4):
                tsl = slice(tg * 512, (tg + 1) * 512)
                ps, psk = pp.get()
                proj(ps, psk, wb, wkeys, ch * 128, tg)
                if name == "bg":
                    P.op("act", lambda q, ps=ps, ti=ti, tsl=tsl: q.activation(out=sg[ti][:, tsl], in_=ps[:], func=AF.Sigmoid),
                         reads=[psk], writes=[f"sg{ti}_{tg}"])
                elif name == "bv":
                    b, bk = getf()
                    P.op("dve", lambda q, ps=ps, b=b, ti=ti, tsl=tsl: q.tensor_tensor(out=b[:], in0=ps[:], in1=sg[ti][:, tsl], op=ALU.mult),
                         reads=[psk, f"sg{ti}_{tg}"], writes=[bk])
                    store32("u", ti, tg, b, bk)
                elif name in ("aq", "ag"):
                    b, bk = getf()
                    P.op("act", lambda q, ps=ps, b=b: q.activation(out=b[:], in_=ps[:], func=AF.Silu), reads=[psk], writes=[bk])
                    store32("qa" if name == "aq" else "ga", ti, tg, b, bk)
                elif name == "ai":
                    b, bk = getf()
                    P.op("act", lambda q, ps=ps, b=b: q.activation(out=b[:], in_=ps[:], func=AF.Copy), reads=[psk], writes=[bk])
                    store32("va", ti, tg, b, bk)
                elif name in ("aff", "afb"):
                    fb_, fk = getf()
                    P.op("act", lambda q, ps=ps, fb_=fb_: q.activation(out=fb_[:], in_=ps[:], func=AF.Sigmoid), reads=[psk], writes=[fk])
                    P.op("dve", lambda q, fb_=fb_, ti=ti: q.tensor_scalar(out=fb_[:], in0=fb_[:], scalar1=oml[:, ti:ti + 1],
                                                                        scalar2=lbc[:, ti:ti + 1], op0=ALU.mult, op1=ALU.add),
                         reads=[fk, "oml", "lbc"], writes=[fk])
                    kb_, kk = getf()
                    P.op("dve", lambda q, fb_=fb_, kb_=kb_: q.tensor_scalar(out=kb_[:], in0=fb_[:], scalar1=-1.0, scalar2=1.0,
                                                                          op0=ALU.mult, op1=ALU.add), reads=[fk], writes=[kk])
                    store32("kf" if name == "aff" else "kb", ti, tg, kb_, kk)
                    P.op("act", lambda q, fb_=fb_: q.activation(out=fb_[:], in_=fb_[:], func=AF.Ln), reads=[fk], writes=[fk])
                    store32("lf" if name == "aff" else "lb", ti, tg, fb_, fk)
                elif name in ("cv", "dv"):
                    b, bk = getb()
                    P.op("act", lambda q, ps=ps, b=b: q.activation(out=b[:], in_=ps[:], func=AF.Copy), reads=[psk], writes=[bk])
                    storebf(name, ti, tg, b, bk)
                else:
                    ri = ROT_CHUNKS.index(ch)
                    ps2, psk2 = pp.get()
                    proj(ps2, psk2, wsw, wswkeys, ri * 128, tg)
                    t1, t1k = getf()
                    t2, t2k = getf()
                    P.op("dve", lambda q, ps=ps, t1=t1, tsl=tsl: q.tensor_tensor(out=t1[:], in0=ps[:], in1=cosT[:, tsl], op=ALU.mult),
                         reads=[psk, "cosT"], writes=[t1k])
                    P.op("dve", lambda q, ps2=ps2, t2=t2, tsl=tsl: q.tensor_tensor(out=t2[:], in0=ps2[:], in1=sinT[:, tsl], op=ALU.mult),
                         reads=[psk2, "sinT"], writes=[t2k])
                    b, bk = getb()
                    P.op("pool", lambda q, t1=t1, t2=t2, b=b: q.tensor_tensor(out=b[:], in0=t1[:], in1=t2[:], op=ALU.add),
                         reads=[t1k, t2k], writes=[bk])
                    storebf(name, ti, tg, b, bk)
    return P.finish()


def col128(v):
    v = np.asarray(v)
    return np.ascontiguousarray(v.reshape(-1, 128).T)


def rot_cols():
    idx = []
    for ch in ROT_CHUNKS:
        for h in range(2):
            base = ch * 128 + h * 64
            loc = np.arange(64)
            loc[:8] = np.arange(8, 16)
            loc[8:16] = np.arange(0, 8)
            idx.append(base + loc)
    return np.concatenate(idx)


def rot_consts():
    half = ROPE_DIM // 2
    inv = (np.float32(ROPE_THETA) ** (-np.arange(half, dtype=np.float32) * np.float32(2.0 / ROPE_DIM))).astype(np.float32)
    rc = np.zeros((128, 2), np.float32)
    for p in range(128):
        d = p % 64
        if d < 16:
            rc[p, 0] = inv[d % 8]
            rc[p, 1] = -1.0 if d < 8 else 1.0
    return rc


_cache = {}


def run(nc_key, builder, in_maps):
    if nc_key not in _cache:
        _cache[nc_key] = builder()
    nc = _cache[nc_key]
    res = run_bass_kernel_spmd(nc, in_maps, core_ids=list(range(NCORES)))
    return res.results


def run_k1(layer, xT_shards, inp):
    rc = rot_consts()
    sw = rot_cols()
    w_in_l = np.ascontiguousarray(inp["w_in"][layer])
    w_sw = np.ascontiguousarray(w_in_l[:, sw])
    alb = np.zeros((128, 4), np.float32)
    a = np.asarray(inp["a_lower_bound"])
    for t in range(2):
        for l in range(2):
            alb[:, t * 2 + l] = a[l, t * 128:(t + 1) * 128]
    ccol = col128(np.asarray(inp["c"])[0])
    bada = col128(np.asarray(inp["b_ada"])[layer])
    w_ada_l = np.ascontiguousarray(inp["w_ada"][layer])
    pos = np.asarray(inp["positions"])[0].astype(np.int32)
    in_maps = []
    for c in range(NCORES):
        in_maps.append({"xT": xT_shards[c], "ccol": ccol, "w_ada": w_ada_l, "bada": bada, "w_in": w_in_l, "w_sw": w_sw,
                        "pos": np.ascontiguousarray(pos[c * TOK:(c + 1) * TOK]), "rcol": rc, "alb": alb})
    return run(("k1", layer), lambda: build_k1(layer), in_maps)


SEG = 1024
NSEG = SEQ // SEG
NCH = SEG // 16
NTL = SEG // 128


def hgrn_consts():
    ident = np.eye(128, dtype=np.float32).astype(ml_dtypes.bfloat16)
    s = np.arange(128)
    tri = ((s[:, None] // 16 == s[None, :] // 16) & (s[:, None] <= s[None, :])).astype(np.float32).astype(ml_dtypes.bfloat16)
    ind = (s[:, None] // 16 == np.arange(8)[None, :]).astype(np.float32).astype(ml_dtypes.bfloat16)
    return ident, tri, ind


def build_k2a():
    P = Prog()
    qT = P.dram_in("qT", [64, SEQ], F32)
    kT = P.dram_in("kT", [64, SEQ], F32)
    lT = P.dram_in("lT", [64, SEQ], F32)
    vtok = P.dram_in("vtok", [SEQ, 64], F32)
    ident_d = P.dram_in("ident_d", [128, 128], BF16)
    tri_d = P.dram_in("tri_d", [128, 128], BF16)
    ind_d = P.dram_in("ind_d", [128, 8], BF16)
    oT = P.dram_out("oT", [64, SEQ], F32)

    ident = P.sb("ident", [128, 128], BF16)
    tri = P.sb("tri", [128, 128], BF16)
    ind = P.sb("ind", [128, 8], BF16)
    P.dma("sp", ident[:], ident_d, writes=["ident"])
    P.dma("sp", tri[:], tri_d, writes=["tri"])
    P.dma("sp", ind[:], ind_d, writes=["ind"])

    reset = P.sb("reset", [64, SEG], F32)
    P.op("pool", lambda q: q.memset(reset[:], 1.0), writes=["reset"])
    P.op("pool", lambda q: q.memset(reset[:].rearrange("p (n c) -> p n c", c=16)[:, :, 0:1], 0.0), reads=["reset"], writes=["reset"])

    def two(name, shape, dt):
        return [P.sb(f"{name}{i}", shape, dt) for i in range(2)]

    qs_, ks_, ls_ = two("qs", [64, SEG], F32), two("ks", [64, SEG], F32), two("ls", [64, SEG], F32)
    vb_ = two("vb", [128, NTL, 64], BF16)
    cum_, ex_ = two("cum", [64, SEG], F32), two("ex", [64, SEG], F32)
    qt_, kt_, kh_ = two("qt", [64, SEG], BF16), two("kt", [64, SEG], BF16), two("kh", [64, SEG], BF16)
    dec_ = two("dec", [64, NCH], F32)
    kvs_ = two("kvs", [64, 64 * NCH], F32)
    sprev_ = two("sprev", [64, NCH, 64], BF16)
    obuf_ = two("obuf", [64, SEG], F32)
    decrep = P.sb("decrep", [64, 64 * NCH], F32)
    decz = P.sb("decz", [64, NCH], F32)
    P.op("pool", lambda q: q.memset(decz[:], 0.0), writes=["decz"])
    sall = P.sb("sall", [64, 64 * NCH], F32)
    s_in = P.sb("s_in", [64, 64], F32)
    khtok = [P.sb(f"khtok{i}", [128, 64], BF16) for i in range(2)]
    vblk = [P.sb(f"vblk{i}", [128, 8, 64], BF16) for i in range(2)]
    am = [P.sb(f"am{i}", [128, 128], BF16) for i in range(2)]
    P.op("pool", lambda q: q.memset(s_in[:], 0.0), writes=["s_in"])

    ps_kv = [P.ps(f"ps_kv{i}", [128, 512], F32) for i in range(2)]
    ps_a = [P.ps(f"ps_a{i}", [128, 512], F32) for i in range(2)]
    ps_o = [P.ps(f"ps_o{i}", [128, 512], F32) for i in range(2)]
    ps_t = [P.ps(f"ps_t{i}", [128, 1024], BF16) for i in range(2)]

    dec3 = decrep[:].rearrange("p (v n) -> p v n", n=NCH)
    sall3 = sall[:].rearrange("p (v n) -> p v n", n=NCH)
    vt_v = vtok.rearrange("(s i p) v -> s p i v", p=128, i=NTL)
    cnt = [0]

    def stage(sgi, part):
        pb = sgi % 2
        K_ = lambda n: f"{n}{pb}"
        qs, ks, ls, vb, cum, ex = qs_[pb], ks_[pb], ls_[pb], vb_[pb], cum_[pb], ex_[pb]
        qt, kt, kh, dec, kvs, sprev, obuf = qt_[pb], kt_[pb], kh_[pb], dec_[pb], kvs_[pb], sprev_[pb], obuf_[pb]
        kvs3 = kvs[:].rearrange("p (v n) -> p v n", n=NCH)
        cum3 = cum[:].rearrange("p (n c) -> p n c", c=16)
        ex3 = ex[:].rearrange("p (n c) -> p n c", c=16)
        tsl = slice(sgi * SEG, (sgi + 1) * SEG)
        if part == "B":
            yield from stageB(sgi, pb, K_, qt, kt, vb, dec, kvs, kvs3, sprev, obuf, tsl)
            return
        yield P.dma("sp", qs[:], qT[:, tsl], writes=[K_("qs")])
        yield P.dma("sp", ks[:], kT[:, tsl], writes=[K_("ks")])
        yield P.dma("sp", ls[:], lT[:, tsl], writes=[K_("ls")])
        yield P.dma("pool", vb[:], vt_v[sgi], writes=[K_("vb")])
        yield P.op("dve", lambda q: q.tensor_tensor_scan(out=cum[:], data0=reset[:], data1=ls[:], initial=0.0,
                                                   op0=ALU.mult, op1=ALU.add), reads=["reset", K_("ls")], writes=[K_("cum")])
        yield P.op("act", lambda q: q.activation(out=ex[:], in_=cum[:], func=AF.Exp), reads=[K_("cum")], writes=[K_("ex")])
        yield P.op("dve", lambda q: q.tensor_tensor(out=qt[:], in0=qs[:], in1=ex[:], op=ALU.mult), reads=[K_("qs"), K_("ex")], writes=[K_("qt")])
        yield P.op("act", lambda q: q.activation(out=ex[:], in_=cum[:], func=AF.Exp, scale=-1.0), reads=[K_("cum")], writes=[K_("ex")])
        yield P.op("dve", lambda q: q.tensor_tensor(out=kt[:], in0=ks[:], in1=ex[:], op=ALU.mult), reads=[K_("ks"), K_("ex")], writes=[K_("kt")])
        yield P.op("dve", lambda q: q.tensor_tensor(out=ex3, in0=cum3[:, :, 15:16].broadcast_to([64, NCH, 16]), in1=cum3,
                                              op=ALU.subtract), reads=[K_("cum")], writes=[K_("ex")])
        yield P.op("act", lambda q: q.activation(out=ex[:], in_=ex[:], func=AF.Exp), reads=[K_("ex")], writes=[K_("ex")])
        yield P.op("dve", lambda q: q.tensor_tensor(out=kh[:], in0=ks[:], in1=ex[:], op=ALU.mult), reads=[K_("ks"), K_("ex")], writes=[K_("kh")])
        yield P.op("act", lambda q: q.activation(out=dec[:], in_=cum3[:, :, 15], func=AF.Exp), reads=[K_("cum")], writes=[K_("dec")])
        bs = []
        for i in range(NTL):
            bs.append(cnt[0] % 2)
            cnt[0] += 1

        def emit_T(i):
            b = bs[i]
            yield P.op("pe", lambda q: q.transpose(ps_t[b][:, 0:64], kh[:, i * 128:(i + 1) * 128], ident[0:64, 0:64]),
                 reads=[K_("kh"), "ident"], writes=[f"ps_t{b}"])
            yield P.op("act", lambda q: q.activation(out=khtok[b][:], in_=ps_t[b][:, 0:64], func=AF.Copy),
                 reads=[f"ps_t{b}"], writes=[f"khtok{b}"])
            yield P.op("dve", lambda q: q.tensor_tensor(out=vblk[b][:], in0=vb[:, i:i + 1, :].broadcast_to([128, 8, 64]),
                                                   in1=ind[:].unsqueeze(2).broadcast_to([128, 8, 64]), op=ALU.mult),
                 reads=[K_("vb"), "ind"], writes=[f"vblk{b}"])

        def emit_KV(i):
            b = bs[i]
            yield P.op("pe", lambda q: q.matmul(ps_kv[b][0:64, :], lhsT=khtok[b][:], rhs=vblk[b][:].rearrange("p c v -> p (c v)"),
                                          start=True, stop=True), reads=[f"khtok{b}", f"vblk{b}"], writes=[f"ps_kv{b}"])
            yield P.op("act", lambda q: q.activation(out=kvs3[:, :, 8 * i:8 * i + 8],
                                               in_=ps_kv[b][0:64, :].rearrange("p (c v) -> p v c", v=64), func=AF.Copy),
                 reads=[f"ps_kv{b}"], writes=[K_("kvs")])

        yield from emit_T(0)
        for i in range(NTL):
            if i + 1 < NTL:
                yield from emit_T(i + 1)
            yield from emit_KV(i)

    def stageB(sgi, pb, K_, qt, kt, vb, dec, kvs, kvs3, sprev, obuf, tsl):
        yield P.op("act", lambda q: q.activation(out=decz[:, 1:NCH], in_=dec[:, 1:NCH], func=AF.Copy), reads=[K_("dec")], writes=["decz"])
        yield P.op("act", lambda q: q.activation(out=dec3, in_=decz[:].unsqueeze(1).broadcast_to([64, 64, NCH]), func=AF.Copy),
             reads=["decz"], writes=["decrep"])
        yield P.op("dve", lambda q: q.scalar_tensor_tensor(out=kvs3[:, :, 0], in0=s_in[:], scalar=dec[:, 0:1], in1=kvs3[:, :, 0],
                                                     op0=ALU.mult, op1=ALU.add), reads=["s_in", K_("dec"), K_("kvs")], writes=[K_("kvs")])
        yield P.op("dve", lambda q: q.tensor_tensor_scan(out=sall[:], data0=decrep[:], data1=kvs[:], initial=0.0,
                                                   op0=ALU.mult, op1=ALU.add), reads=["decrep", K_("kvs")], writes=["sall"])
        yield P.op("act", lambda q: q.activation(out=sprev[:, 0, :], in_=s_in[:], func=AF.Copy), reads=["s_in"], writes=[K_("sprev")])
        yield P.op("act", lambda q: q.activation(out=sprev[:, 1:NCH, :], in_=sall3[:, :, 0:NCH - 1].rearrange("p v n -> p n v"), func=AF.Copy),
             reads=["sall"], writes=[K_("sprev")])
        yield P.op("dve", lambda q: q.tensor_copy(out=s_in[:], in_=sall3[:, :, NCH - 1]), reads=["sall", K_("sprev")], writes=["s_in"])
        bs = []
        for i in range(NTL):
            bs.append(cnt[0] % 2)
            cnt[0] += 1

        def emit_A(i):
            b = bs[i]
            csl = slice(i * 128, (i + 1) * 128)
            yield P.op("pe", lambda q: q.matmul(ps_a[b][:, 0:128], lhsT=kt[:, csl], rhs=qt[:, csl], start=True, stop=True),
                 reads=[K_("kt"), K_("qt")], writes=[f"ps_a{b}"])
            yield P.op("dve", lambda q: q.tensor_tensor(out=am[b][:], in0=ps_a[b][:, 0:128], in1=tri[:], op=ALU.mult),
                 reads=[f"ps_a{b}", "tri"], writes=[f"am{b}"])

        def emit_O(i):
            b = bs[i]
            csl = slice(i * 128, (i + 1) * 128)
            yield P.op("pe", lambda q: q.matmul(ps_o[b][0:64, 0:128], lhsT=vb[:, i, :], rhs=am[b][:], start=True, stop=False),
                 reads=[K_("vb"), f"am{b}"], writes=[f"ps_o{b}"])
            for c in range(8):
                n = 8 * i + c
                yield P.op("pe", lambda q, c=c, n=n: q.matmul(ps_o[b][0:64, 16 * c:16 * c + 16], lhsT=sprev[:, n, :],
                                                       rhs=qt[:, i * 128 + 16 * c:i * 128 + 16 * c + 16],
                                                       start=False, stop=(c == 7), skip_group_check=True),
                     reads=[K_("sprev"), K_("qt")], writes=[f"ps_o{b}"])
            yield P.op("act", lambda q: q.activation(out=obuf[:, csl], in_=ps_o[b][0:64, 0:128], func=AF.Copy),
                 reads=[f"ps_o{b}"], writes=[K_("obuf")])

        yield from emit_A(0)
        for i in range(NTL):
            if i + 1 < NTL:
                yield from emit_A(i + 1)
            yield from emit_O(i)
        yield P.dma("sp", oT[:, tsl], obuf[:], reads=[K_("obuf")], is_output=True)

    def drain(*gens):
        gens = [g for g in gens if g is not None]
        while gens:
            for g in list(gens):
                try:
                    next(g)
                except StopIteration:
                    gens.remove(g)

    drain(stage(0, "A"))
    for sgi in range(NSEG):
        drain(stage(sgi, "B"), stage(sgi + 1, "A") if sgi + 1 < NSEG else None)
    return P.finish()


def run_k2a(o32_full):
    ident, tri, ind = hgrn_consts()
    in_maps = []
    for c in range(NCORES):
        h, d = c % 4, c // 4
        rows = slice(h * 64, (h + 1) * 64)
        q = o32_full["qa"][rows]
        k = o32_full["kf" if d == 0 else "kb"][rows]
        l = o32_full["lf" if d == 0 else "lb"][rows]
        v = o32_full["va"][rows]
        if d == 1:
            q, k, l, v = q[:, ::-1], k[:, ::-1], l[:, ::-1], v[:, ::-1]
        in_maps.append({"qT": np.ascontiguousarray(q), "kT": np.ascontiguousarray(k), "lT": np.ascontiguousarray(l),
                        "vtok": np.ascontiguousarray(v.T), "ident_d": ident, "tri_d": tri, "ind_d": ind})
    res = run(("k2a",), build_k2a, in_maps)
    of = np.concatenate([res[h]["oT"] for h in range(4)], 0)
    ob = np.concatenate([res[4 + h]["oT"][:, ::-1] for h in range(4)], 0)
    return of, np.ascontiguousarray(ob)


GU = 16
C_PAT = ((128, 1), (512, 4), (2048, 16))
NUC = 3 * 4 * 16
NUD = 4 * 16


def attn_masks():
    k = np.arange(256)[:, None]
    q = np.arange(128)[None, :]
    mc = np.where(np.abs(k - 64 - q) <= 64, 1.0, 0.0).astype(np.float32)
    k = np.arange(384)[:, None]
    md = np.where(np.abs(k - 128 - q) <= 128, 1.0, 0.0).astype(np.float32)
    mc = mc.reshape(2, 128, 128).transpose(1, 0, 2)
    md = md.reshape(3, 128, 128).transpose(1, 0, 2)
    return (np.ascontiguousarray(mc).astype(ml_dtypes.bfloat16), np.ascontiguousarray(md).astype(ml_dtypes.bfloat16))


def build_k2b():
    P = Prog()
    specs = []
    for nm, nu, nb in (("c", NUC, 2), ("d", NUD, 3)):
        ng = nu // GU
        specs.append(dict(nm=nm, nb=nb, ng=ng,
                          Q=P.dram_in(nm + "Q", [ng, 64, GU * 128], BF16),
                          K=P.dram_in(nm + "K", [ng, 64, GU * nb * 128], BF16),
                          V=P.dram_in(nm + "V", [ng, 128, GU * nb * 65], BF16),
                          M=P.dram_in(nm + "M", [128, nb, 128], BF16),
                          O=P.dram_out(nm + "O", [ng, 65, GU * 128], F32)))
    ident_d = P.dram_in("ident_d", [128, 128], BF16)
    ident = P.sb("ident", [128, 128], BF16)
    P.dma("sp", ident[:], ident_d, writes=["ident"])
    Qg = [P.sb(f"Qg{i}", [64, GU * 128], BF16) for i in range(2)]
    Kg = [P.sb(f"Kg{i}", [64, GU * 3 * 128], BF16) for i in range(2)]
    Vg = [P.sb(f"Vg{i}", [128, GU * 3 * 65], BF16) for i in range(2)]
    Og = [P.sb(f"Og{i}", [65, GU * 128], F32) for i in range(2)]
    Pt = [P.sb(f"Pt{i}", [128, 384], BF16) for i in range(3)]
    psS = [P.ps(f"psS{i}", [128, 512], F32) for i in range(4)]
    psO = [P.ps(f"psO{i}", [128, 512], F32) for i in range(3)]
    gi = 0
    ui = 0
    for sp_ in specs:
        nb = sp_["nb"]
        mk = P.sb("mask_" + sp_["nm"], [128, nb, 128], BF16)
        mkk = "mask_" + sp_["nm"]
        P.dma("sp", mk[:], sp_["M"], writes=[mkk])
        for g in range(sp_["ng"]):
            b2 = gi % 2
            gi += 1
            P.dma("sp", Qg[b2][:], sp_["Q"][g], writes=[f"Qg{b2}"])
            P.dma("sp", Kg[b2][:, 0:GU * nb * 128], sp_["K"][g], writes=[f"Kg{b2}"])
            P.dma("sp", Vg[b2][:, 0:GU * nb * 65], sp_["V"][g], writes=[f"Vg{b2}"])
            def emit_S(u, s3, p2):
                for b in range(nb):
                    ksl = slice((u * nb + b) * 128, (u * nb + b + 1) * 128)
                    P.op("pe", lambda q, b=b, ksl=ksl: q.matmul(
                        psS[s3][:, b * 128:(b + 1) * 128], lhsT=Kg[b2][:, ksl], rhs=Qg[b2][:, u * 128:(u + 1) * 128],
                        start=True, stop=True), reads=[f"Kg{b2}", f"Qg{b2}"], writes=[f"psS{s3}"])
                P.op("act", lambda q: q.activation(out=Pt[p2][:, 0:nb * 128], in_=psS[s3][:, 0:nb * 128], func=AF.Exp, scale=0.125),
                     reads=[f"psS{s3}"], writes=[f"Pt{p2}"])
                P.op("dve", lambda q: q.tensor_tensor(out=Pt[p2][:, 0:nb * 128], in0=Pt[p2][:, 0:nb * 128],
                                                      in1=mk[:].rearrange("p b q -> p (b q)"), op=ALU.mult),
                     reads=[f"Pt{p2}", mkk], writes=[f"Pt{p2}"])

            def emit_PV(u, s3, p2, so):
                for b in range(nb):
                    vsl = slice((u * nb + b) * 65, (u * nb + b + 1) * 65)
                    P.op("pe", lambda q, b=b, vsl=vsl: q.matmul(
                        psO[so][0:65, 0:128], lhsT=Vg[b2][:, vsl], rhs=Pt[p2][:, b * 128:(b + 1) * 128],
                        start=(b == 0), stop=(b == nb - 1)), reads=[f"Vg{b2}", f"Pt{p2}"], writes=[f"psO{so}"])
                P.op("act", lambda q: q.activation(out=Og[b2][:, u * 128:(u + 1) * 128], in_=psO[so][0:65, 0:128], func=AF.Copy),
                     reads=[f"psO{so}"], writes=[f"Og{b2}"])

            ids = []
            for u in range(GU):
                ids.append((u, ui % 4, ui % 3, ui % 3))
                ui += 1
            emit_S(*ids[0][:3])
            emit_S(*ids[1][:3])
            for j in range(GU):
                if j + 2 < GU:
                    emit_S(*ids[j + 2][:3])
                emit_PV(*ids[j])
            P.dma("sp", sp_["O"][g], Og[b2][:], reads=[f"Og{b2}"], is_output=True)
    return P.finish()


def _windows(Kseq, Vseq, nb, halo, ntile):
    L = Kseq.shape[1]
    W = nb * 128
    Kp = np.zeros((64, L + 2 * halo + 128), Kseq.dtype)
    Kp[:, halo:halo + L] = Kseq
    Vp = np.zeros((L + 2 * halo + 128, 65), Vseq.dtype)
    Vp[halo:halo + L, :64] = Vseq.T
    Vp[halo:halo + L, 64] = 1.0
    Kw = np.stack([Kp[:, 128 * j:128 * j + W] for j in range(ntile)], 0)
    Vw = np.stack([Vp[128 * j:128 * j + W] for j in range(ntile)], 0)
    return Kw, Vw


def _pack_units(Qu, Ku, Vu, nb):
    U = Qu.shape[0]
    ng = U // GU
    Q = Qu.reshape(ng, GU, 64, 128).transpose(0, 2, 1, 3).reshape(ng, 64, GU * 128)
    K = Ku.reshape(ng, GU, 64, nb * 128).transpose(0, 2, 1, 3).reshape(ng, 64, GU * nb * 128)
    V = Vu.reshape(ng, GU, nb, 128, 65).transpose(0, 3, 1, 2, 4).reshape(ng, 128, GU * nb * 65)
    return np.ascontiguousarray(Q), np.ascontiguousarray(K), np.ascontiguousarray(V)


def run_k2b(obf_full):
    mc, md = attn_masks()
    ident = np.eye(128, dtype=np.float32).astype(ml_dtypes.bfloat16)
    cQ = np.zeros((3, 4, 128, 64, 128), ml_dtypes.bfloat16)
    cK = np.zeros((3, 4, 128, 64, 256), ml_dtypes.bfloat16)
    cV = np.zeros((3, 4, 128, 256, 65), ml_dtypes.bfloat16)
    for p, (w, d) in enumerate(C_PAT):
        L = SEQ // d
        nt = L // 128
        for h in range(4):
            rows = slice(h * 64, (h + 1) * 64)
            Qs = obf_full["cq"][rows].reshape(64, L, d)
            Ks = obf_full["ck"][rows].reshape(64, L, d)
            Vs = obf_full["cv"][rows].reshape(64, L, d)
            for r in range(d):
                Kw, Vw = _windows(Ks[:, :, r], Vs[:, :, r], 2, 64, nt)
                cK[p, h, r * nt:(r + 1) * nt] = Kw
                cV[p, h, r * nt:(r + 1) * nt] = Vw
                cQ[p, h, r * nt:(r + 1) * nt] = Qs[:, :, r].reshape(64, nt, 128).transpose(1, 0, 2)
    dQ = np.zeros((4, 128, 64, 128), ml_dtypes.bfloat16)
    dK = np.zeros((4, 128, 64, 384), ml_dtypes.bfloat16)
    dV = np.zeros((4, 128, 384, 65), ml_dtypes.bfloat16)
    for h in range(4):
        kvh = h // 2
        Kw, Vw = _windows(obf_full["dk"][kvh * 64:(kvh + 1) * 64], obf_full["dv"][kvh * 64:(kvh + 1) * 64], 3, 128, 128)
        dK[h] = Kw
        dV[h] = Vw
        dQ[h] = obf_full["dq"][h * 64:(h + 1) * 64].reshape(64, 128, 128).transpose(1, 0, 2)
    in_maps = []
    for c in range(NCORES):
        ts = slice(16 * c, 16 * c + 16)
        q, k, v = _pack_units(cQ[:, :, ts].reshape(NUC, 64, 128), cK[:, :, ts].reshape(NUC, 64, 256), cV[:, :, ts].reshape(NUC, 256, 65), 2)
        q2, k2, v2 = _pack_units(dQ[:, ts].reshape(NUD, 64, 128), dK[:, ts].reshape(NUD, 64, 384), dV[:, ts].reshape(NUD, 384, 65), 3)
        in_maps.append({"cQ": q, "cK": k, "cV": v, "cM": mc, "dQ": q2, "dK": k2, "dV": v2, "dM": md, "ident_d": ident})
    res = run(("k2b",), build_k2b, in_maps)
    numC = np.zeros((3, 256, SEQ), np.float32)
    denC = np.zeros((3, 4, SEQ), np.float32)
    numD = np.zeros((256, SEQ), np.float32)
    denD = np.zeros((4, SEQ), np.float32)
    for c in range(NCORES):
        co = res[c]["cO"].reshape(NUC // GU, 65, GU, 128).transpose(0, 2, 1, 3).reshape(3, 4, 16, 65, 128)
        do = res[c]["dO"].reshape(NUD // GU, 65, GU, 128).transpose(0, 2, 1, 3).reshape(4, 16, 65, 128)
        for p, (w, d) in enumerate(C_PAT):
            L = SEQ // d
            nt = L // 128
            for h in range(4):
                nv = numC[p, h * 64:(h + 1) * 64].reshape(64, L, d)
                dv_ = denC[p, h].reshape(L, d)
                for tl in range(16):
                    tau = 16 * c + tl
                    r, j = tau // nt, tau % nt
                    nv[:, 128 * j:128 * j + 128, r] = co[p, h, tl, 0:64]
                    dv_[128 * j:128 * j + 128, r] = co[p, h, tl, 64]
        for h in range(4):
            for tl in range(16):
                tau = 16 * c + tl
                numD[h * 64:(h + 1) * 64, 128 * tau:128 * tau + 128] = do[h, tl, 0:64]
                denD[h, 128 * tau:128 * tau + 128] = do[h, tl, 64]
    return numC, denC, numD, denD


FB = 4


def build_k3(moe):
    P = Prog()
    T = TOK
    xT = P.dram_in("xT", [D_MODEL, T], F32)
    modc_d = P.dram_in("modc", [128, 48], F32)
    ofT = P.dram_in("ofT", [256, T], F32)
    obT = P.dram_in("obT", [256, T], F32)
    gaT = P.dram_in("gaT", [256, T], F32)
    uext = P.dram_in("uext", [256, T + 32], F32)
    numC = P.dram_in("numC", [3, 256, T], F32)
    denC = P.dram_in("denC", [3, 256, T], F32)
    numD = P.dram_in("numD", [256, T], F32)
    denD = P.dram_in("denD", [256, T], F32)
    smalls_d = P.dram_in("smalls", [128, 128], F32)
    blk64_d = P.dram_in("blk64", [128, 128], F32)
    identb_d = P.dram_in("identb", [128, 128], BF16)
    w_out = P.dram_in("w_out", [1024, 1024], F32)
    if moe:
        wr_d = P.dram_in("wr", [1024, 8], F32)
        w_up = P.dram_in("w_up", [N_EXPERTS, 1024, 2 * EXPERT_DIM], F32)
        w_down = P.dram_in("w_down", [N_EXPERTS, EXPERT_DIM, 1024], F32)
        sel_d = P.dram_in("sel", [8, 8 * 128], F32)
        identf_d = P.dram_in("identf", [128, 128], F32)
    else:
        w_up = P.dram_in("w_up", [1024, 2 * FFN_DIM], F32)
        w_down = P.dram_in("w_down", [FFN_DIM, 1024], F32)
    xo = P.dram_out("xo", [D_MODEL, T], F32)

    pp = PsumPool(P)
    sm = P.sb("sm", [128, 128], F32)
    P.dma("sp", sm[:], smalls_d, writes=["sm"])
    O_ANW, O_CW, O_CB, O_BNW, O_BNB, O_SINK, O_LNW, O_LNB = 0, 1, 63, 65, 67, 69, 71, 87
    modc = P.sb("modc", [128, 48], F32)
    P.dma("sp", modc[:], modc_d, writes=["modc"])
    blk64 = P.sb("blk64", [128, 128], F32)
    P.dma("sp", blk64[:], blk64_d, writes=["blk64"])
    ones_f = P.sb("ones_f", [128, 128], F32)
    P.op("pool", lambda q: q.memset(ones_f[:], 1.0), writes=["ones_f"])
    eps = P.sb("eps", [128, 2], F32)
    P.op("pool", lambda q: q.memset(eps[:, 0:1], LN_EPS), writes=["eps"])
    P.op("pool", lambda q: q.memset(eps[:, 1:2], RMS_EPS), reads=["eps"], writes=["eps"])
    dcol = P.sb("dcol", [128, 32], F32)
    P.op("dve", lambda q: q.tensor_scalar(out=dcol[:, 0:8], in0=modc[:, 16:24], scalar1=1.0, scalar2=None, op0=ALU.add),
         reads=["modc"], writes=["dcol"])
    P.op("dve", lambda q: q.tensor_scalar(out=dcol[:, 8:16], in0=modc[:, 32:40], scalar1=1.0, scalar2=None, op0=ALU.add),
         reads=["modc", "dcol"], writes=["dcol"])
    P.op("dve", lambda q: q.tensor_scalar(out=dcol[:, 16:24], in0=modc[:, 40:48], scalar1=1.0, scalar2=None, op0=ALU.add),
         reads=["modc", "dcol"], writes=["dcol"])
    P.op("act", lambda q: q.activation(out=dcol[:, 24:26], in_=sm[:, O_SINK:O_SINK + 2], func=AF.Exp),
         reads=["sm", "dcol"], writes=["dcol"])
    G1, SC2, G2, ESINK = 0, 8, 16, 24

    x = P.sb("x", [128, 8, T], F32)
    y = P.sb("y", [128, 8, T], BF16)
    wu = [P.sb(f"wu{i}", [128, 8192], BF16) for i in range(2)]
    wd = [P.sb(f"wd{i}", [128, FB, 1024], BF16) for i in range(2)]
    abuf = P.sb("abuf", [128, FB, T], BF16)
    S = [P.sb(f"S{i}", [128, T + 32], F32) for i in range(3)]
    NT = 5
    tmp = [P.sb(f"tmp{i}", [128, 512], F32) for i in range(NT)]
    lnm = P.sb("lnm", [128, 512], F32)
    lnv = P.sb("lnv", [128, 512], F32)
    tcnt = [0]

    def gett():
        i = tcnt[0] % NT
        tcnt[0] += 1
        return tmp[i], f"tmp{i}"

    def tsl(tg):
        return slice(tg * 512, (tg + 1) * 512)

    xk = lambda i, tg: f"x{i}_{tg}"
    yk = lambda i, tg: f"y{i}_{tg}"

    xT_v = xT.rearrange("(k p) t -> p k t", p=128)
    for k in range(8):
        P.dma("sp", x[:, k, :], xT_v[:, k, :], writes=[xk(k, tg) for tg in range(4)])
    wo_v = w_out.rearrange("(k p) e -> p k e", p=128)
    wo_s = wu[0][:].rearrange("p (k e) -> p k e", k=8)
    for k in range(8):
        P.dma("pool", wo_s[:, k, :], wo_v[:, k, :], writes=["wu0"])

    def ln_fm(srcs, nfeat, wcol, bcol, dst, silu=False):
        n = len(srcs)
        for tg in range(4):
            ps_s, ks_ = pp.get()
            ps_q, kq_ = pp.get()
            for i, (af, kf) in enumerate(srcs):
                P.op("pe", lambda q, af=af, i=i: q.matmul(ps_s[:, :], lhsT=ones_f[:], rhs=af(tg), start=(i == 0), stop=(i == n - 1)),
                     reads=["ones_f", kf(tg)], writes=[ks_])
            for i, (af, kf) in enumerate(srcs):
                sq, sqk = gett()
                P.op("act", lambda q, af=af, sq=sq: q.activation(out=sq[:], in_=af(tg), func=AF.Square), reads=[kf(tg)], writes=[sqk])
                P.op("pe", lambda q, sq=sq, i=i: q.matmul(ps_q[:, :], lhsT=ones_f[:], rhs=sq[:], start=(i == 0), stop=(i == n - 1)),
                     reads=["ones_f", sqk], writes=[kq_])
            mean, mk = lnm, "lnm"
            P.op("act", lambda q: q.activation(out=mean[:], in_=ps_s[:], func=AF.Copy, scale=1.0 / nfeat), reads=[ks_], writes=[mk])
            var, vk = lnv, "lnv"
            P.op("dve", lambda q: q.tensor_tensor(out=var[:], in0=mean[:], in1=mean[:], op=ALU.mult), reads=[mk], writes=[vk])
            P.op("dve", lambda q: q.scalar_tensor_tensor(out=var[:], in0=ps_q[:], scalar=1.0 / nfeat, in1=var[:], op0=ALU.mult,
                                                         op1=ALU.subtract), reads=[kq_, vk], writes=[vk])
            P.op("act", lambda q: q.activation(out=var[:], in_=var[:], func=AF.Sqrt, bias=eps[:, 0:1], scale=1.0), reads=[vk, "eps"], writes=[vk])
            P.op("dve", lambda q: q.reciprocal(out=var[:], in_=var[:]), reads=[vk], writes=[vk])
            for i, ((af, kf), (df, dkf)) in enumerate(zip(srcs, dst)):
                t, tk = gett()
                P.op("dve", lambda q, af=af, t=t: q.tensor_tensor(out=t[:], in0=af(tg), in1=mean[:], op=ALU.subtract),
                     reads=[kf(tg), mk], writes=[tk])
                P.op("dve", lambda q, t=t: q.tensor_tensor(out=t[:], in0=t[:], in1=var[:], op=ALU.mult), reads=[tk, vk], writes=[tk])
                if silu:
                    P.op("act", lambda q, t=t, df=df, i=i: q.activation(out=df(tg), in_=t[:], func=AF.Silu, scale=wcol(i), bias=bcol(i)),
                         reads=[tk, "sm"], writes=[dkf(tg)])
                else:
                    P.op("act", lambda q, t=t, df=df, i=i: q.activation(out=df(tg), in_=t[:], func=AF.Identity, scale=wcol(i), bias=bcol(i)),
                         reads=[tk, "sm"], writes=[dkf(tg)])

    for a in range(2):
        rows = slice(a * 128, (a + 1) * 128)
        for tg in range(4):
            t0, k0 = gett()
            t1, k1 = gett()
            t2, k2 = gett()
            P.dma("sp", t0[:], ofT[rows, tsl(tg)], writes=[k0])
            P.dma("sp", t1[:], obT[rows, tsl(tg)], writes=[k1])
            P.dma("sp", t2[:], gaT[rows, tsl(tg)], writes=[k2])
            P.op("dve", lambda q, t0=t0, t1=t1: q.tensor_tensor(out=t0[:], in0=t0[:], in1=t1[:], op=ALU.add), reads=[k0, k1], writes=[k0])
            P.op("act", lambda q, t0=t0, t1=t1: q.activation(out=t1[:], in_=t0[:], func=AF.Square), reads=[k0], writes=[k1])
            ps, pk = pp.get()
            P.op("pe", lambda q, ps=ps, t1=t1: q.matmul(ps[:, :], lhsT=blk64[:], rhs=t1[:], start=True, stop=True), reads=["blk64", k1], writes=[pk])
            P.op("act", lambda q, ps=ps, t1=t1: q.activation(out=t1[:], in_=ps[:], func=AF.Sqrt, bias=eps[:, 1:2], scale=1.0 / 64),
                 reads=[pk, "eps"], writes=[k1])
            P.op("dve", lambda q, t1=t1: q.reciprocal(out=t1[:], in_=t1[:]), reads=[k1], writes=[k1])
            P.op("dve", lambda q, t0=t0, t1=t1: q.scalar_tensor_tensor(out=t0[:], in0=t0[:], scalar=sm[:, O_ANW:O_ANW + 1], in1=t1[:],
                                                                     op0=ALU.mult, op1=ALU.mult), reads=[k0, k1, "sm"], writes=[k0])
            P.op("dve", lambda q, t0=t0, t2=t2, a=a, tg=tg: q.tensor_tensor(out=y[:, a, tsl(tg)], in0=t0[:], in1=t2[:], op=ALU.mult),
                 reads=[k0, k2], writes=[yk(a, tg)])

    identb = wd[1][:, 1, 0:128]
    P.dma("sp", identb, identb_d, writes=["identb"])
    dg = [wd[1][:, 0, i * 128:(i + 1) * 128] for i in range(4)]
    ubf = abuf[:].rearrange("p f t -> p (f t)")[:, 0:T + 32]
    for b in range(2):
        ub, ubk = S[0], "S0"
        acc, acck = S[1 + b], f"S{1 + b}"
        P.dma("sp", ub[:], uext[b * 128:(b + 1) * 128, :], writes=[ubk])
        P.op("act", lambda q: q.activation(out=ubf, in_=ub[:], func=AF.Copy), reads=[ubk], writes=["ubf"])
        pss = [pp.get() for _ in range(4)]
        for j in range(B_KERNEL):
            d_, dk_ = dg[j % 4], f"dg{j % 4}"
            P.op("dve", lambda q, d_=d_, j=j, b=b: q.tensor_scalar(out=d_, in0=identb, scalar1=sm[:, O_CW + b * 31 + j:O_CW + b * 31 + j + 1],
                                                                 scalar2=None, op0=ALU.mult), reads=["identb", "sm"], writes=[dk_])
            for tg in range(4):
                ps, pk = pss[tg]
                P.op("pe", lambda q, ps=ps, d_=d_, j=j, tg=tg: q.matmul(ps[:, :], lhsT=d_, rhs=ubf[:, tg * 512 + j:tg * 512 + j + 512],
                                                                        start=(j == 0), stop=(j == B_KERNEL - 1)),
                     reads=[dk_, "ubf"], writes=[pk])
        for tg in range(4):
            ps, pk = pss[tg]
            P.op("act", lambda q, ps=ps, acc=acc, tg=tg, b=b: q.activation(out=acc[:, tsl(tg)], in_=ps[:], func=AF.Identity,
                                                                         bias=sm[:, O_CB + b:O_CB + b + 1], scale=1.0),
                 reads=[pk, "sm"], writes=[acck])
    ln_fm([(lambda tg, b=b: S[1 + b][:, tsl(tg)], lambda tg, b=b: f"S{1 + b}") for b in range(2)], 256,
          lambda i: sm[:, O_BNW + i:O_BNW + i + 1], lambda i: sm[:, O_BNB + i:O_BNB + i + 1],
          [(lambda tg, b=b: y[:, 2 + b, tsl(tg)], lambda tg, b=b: yk(2 + b, tg)) for b in range(2)], silu=True)

    for a in range(2):
        rows = slice(a * 128, (a + 1) * 128)
        for tg in range(4):
            n0, kn0 = gett()
            d0, kd0 = gett()
            P.dma("sp", n0[:], numC[0, rows, tsl(tg)], writes=[kn0])
            P.dma("sp", d0[:], denC[0, rows, tsl(tg)], writes=[kd0])
            n1, kn1 = gett()
            d1, kd1 = gett()
            for p in (1, 2):
                P.dma("sp", n1[:], numC[p, rows, tsl(tg)], writes=[kn1])
                P.dma("sp", d1[:], denC[p, rows, tsl(tg)], writes=[kd1])
                P.op("dve", lambda q, n0=n0, n1=n1: q.tensor_tensor(out=n0[:], in0=n0[:], in1=n1[:], op=ALU.add), reads=[kn0, kn1], writes=[kn0])
                P.op("dve", lambda q, d0=d0, d1=d1: q.tensor_tensor(out=d0[:], in0=d0[:], in1=d1[:], op=ALU.add), reads=[kd0, kd1], writes=[kd0])
            P.op("dve", lambda q, d0=d0: q.reciprocal(out=d0[:], in_=d0[:]), reads=[kd0], writes=[kd0])
            P.op("dve", lambda q, n0=n0, d0=d0, a=a, tg=tg: q.tensor_tensor(out=y[:, 4 + a, tsl(tg)], in0=n0[:], in1=d0[:], op=ALU.mult),
                 reads=[kn0, kd0], writes=[yk(4 + a, tg)])
            n0, kn0 = gett()
            d0, kd0 = gett()
            P.dma("sp", n0[:], numD[rows, tsl(tg)], writes=[kn0])
            P.dma("sp", d0[:], denD[rows, tsl(tg)], writes=[kd0])
            P.op("dve", lambda q, d0=d0, a=a: q.tensor_scalar(out=d0[:], in0=d0[:], scalar1=dcol[:, ESINK + a:ESINK + a + 1], scalar2=None,
                                                            op0=ALU.add), reads=[kd0, "dcol"], writes=[kd0])
            P.op("dve", lambda q, d0=d0: q.reciprocal(out=d0[:], in_=d0[:]), reads=[kd0], writes=[kd0])
            P.op("dve", lambda q, n0=n0, d0=d0, a=a, tg=tg: q.tensor_tensor(out=y[:, 6 + a, tsl(tg)], in0=n0[:], in1=d0[:], op=ALU.mult),
                 reads=[kn0, kd0], writes=[yk(6 + a, tg)])

    for k in range(8):
        for tg in range(4):
            P.op("act", lambda q, k=k, tg=tg: q.activation(out=x[:, k, tsl(tg)], in_=x[:, k, tsl(tg)], func=AF.Copy, scale=DEEPNORM_ALPHA),
                 reads=[xk(k, tg)], writes=[xk(k, tg)])
    for dc in range(8):
        for tg in range(4):
            ps, pk = pp.get()
            for k in range(8):
                P.op("pe", lambda q, ps=ps, k=k, dc=dc, tg=tg: q.matmul(ps[:, :], lhsT=wo_s[:, k, dc * 128:(dc + 1) * 128], rhs=y[:, k, tsl(tg)],
                                                                        start=(k == 0), stop=(k == 7)), reads=["wu0", yk(k, tg)], writes=[pk])
            P.op("dve", lambda q, ps=ps, dc=dc, tg=tg: q.scalar_tensor_tensor(out=x[:, dc, tsl(tg)], in0=ps[:], scalar=dcol[:, G1 + dc:G1 + dc + 1],
                                                                             in1=x[:, dc, tsl(tg)], op0=ALU.mult, op1=ALU.add),
                 reads=[pk, "dcol", xk(dc, tg)], writes=[xk(dc, tg)])
    xs = [(lambda tg, k=k: x[:, k, tsl(tg)], lambda tg, k=k: xk(k, tg)) for k in range(8)]
    ln_fm(xs, 1024, lambda i: sm[:, O_LNW + i:O_LNW + i + 1], lambda i: sm[:, O_LNB + i:O_LNB + i + 1], xs)

    for k in range(8):
        for tg in range(4):
            P.op("dve", lambda q, k=k, tg=tg: q.tensor_scalar(out=y[:, k, tsl(tg)], in0=x[:, k, tsl(tg)], scalar1=dcol[:, SC2 + k:SC2 + k + 1],
                                                            scalar2=modc[:, 24 + k:25 + k], op0=ALU.mult, op1=ALU.add),
                 reads=[xk(k, tg), "dcol", "modc"], writes=[yk(k, tg)])

    gT = None
    if moe:
        wr = P.sb("wr", [128, 8, 8], F32)
        P.dma("sp", wr[:], wr_d.rearrange("(k p) e -> p k e", p=128), writes=["wr"])
        sel = P.sb("sel", [8, 8 * 128], F32)
        P.dma("sp", sel[:], sel_d, writes=["sel"])
        identf = P.sb("identf", [128, 128], F32)
        P.dma("sp", identf[:], identf_d, writes=["identf"])
        psl, pslk = pp.get()
        for k in range(8):
            h2f, hk = S[0], "S0"
            P.op("dve", lambda q, k=k: q.tensor_scalar(out=h2f[:, 0:T], in0=x[:, k, :], scalar1=dcol[:, SC2 + k:SC2 + k + 1],
                                                       scalar2=modc[:, 24 + k:25 + k], op0=ALU.mult, op1=ALU.add),
                 reads=[xk(k, tg) for tg in range(4)] + ["dcol", "modc"], writes=[hk])
            for ti in range(16):
                P.op("pe", lambda q, k=k, ti=ti: q.matmul(psl[:, ti * 8:(ti + 1) * 8], lhsT=h2f[:, ti * 128:(ti + 1) * 128], rhs=wr[:, k, :],
                                                          start=(k == 0 and ti == 0), stop=(k == 7 and ti == 15), skip_group_check=True),
                     reads=[hk, "wr"], writes=[pslk])
        lg = P.sb("lg", [128, 16, 8], F32)
        lg2 = P.sb("lg2", [128, 16, 8], F32)
        eq1 = S[1][:, 0:128].rearrange("p (t e) -> p t e", e=8)
        eq2 = S[1][:, 128:256].rearrange("p (t e) -> p t e", e=8)
        m1 = P.sb("m1", [128, 16], F32)
        m2 = P.sb("m2", [128, 16], F32)
        g1 = P.sb("g1", [128, 16], F32)
        bc3 = lambda t: t[:].unsqueeze(2).broadcast_to([128, 16, 8])
        P.op("dve", lambda q: q.tensor_copy(out=lg[:], in_=psl[:, 0:128].rearrange("p (t e) -> p t e", e=8)), reads=[pslk], writes=["lg"])
        P.op("dve", lambda q: q.tensor_reduce(out=m1[:], in_=lg[:], axis=AX.X, op=ALU.max), reads=["lg"], writes=["m1"])
        P.op("dve", lambda q: q.tensor_tensor(out=eq1, in0=lg[:], in1=bc3(m1), op=ALU.is_equal), reads=["lg", "m1"], writes=["eq1"])
        P.op("dve", lambda q: q.scalar_tensor_tensor(out=lg2[:], in0=eq1, scalar=-1e30, in1=lg[:], op0=ALU.mult, op1=ALU.add),
             reads=["eq1", "lg"], writes=["lg2"])
        P.op("dve", lambda q: q.tensor_reduce(out=m2[:], in_=lg2[:], axis=AX.X, op=ALU.max), reads=["lg2"], writes=["m2"])
        P.op("dve", lambda q: q.tensor_tensor(out=eq2, in0=lg2[:], in1=bc3(m2), op=ALU.is_equal), reads=["lg2", "m2"], writes=["eq2"])
        P.op("dve", lambda q: q.tensor_tensor(out=m2[:], in0=m2[:], in1=m1[:], op=ALU.subtract), reads=["m2", "m1"], writes=["m2"])
        P.op("act", lambda q: q.activation(out=m2[:], in_=m2[:], func=AF.Exp), reads=["m2"], writes=["m2"])
        P.op("dve", lambda q: q.tensor_scalar(out=g1[:], in0=m2[:], scalar1=1.0, scalar2=None, op0=ALU.add), reads=["m2"], writes=["g1"])
        P.op("dve", lambda q: q.reciprocal(out=g1[:], in_=g1[:]), reads=["g1"], writes=["g1"])
        P.op("dve", lambda q: q.tensor_tensor(out=m2[:], in0=m2[:], in1=g1[:], op=ALU.mult), reads=["m2", "g1"], writes=["m2"])
        P.op("dve", lambda q: q.tensor_tensor(out=eq1, in0=eq1, in1=bc3(g1), op=ALU.mult), reads=["eq1", "g1"], writes=["eq1"])
        P.op("dve", lambda q: q.tensor_tensor(out=eq2, in0=eq2, in1=bc3(m2), op=ALU.mult), reads=["eq2", "m2"], writes=["eq2"])
        P.op("dve", lambda q: q.tensor_tensor(out=eq1, in0=eq1, in1=eq2, op=ALU.add), reads=["eq1", "eq2"], writes=["eq1"])
        gT = S[0][0:8, 0:T]
        for tg in range(4):
            ps, pk = pp.get()
            for j in range(4):
                ti = tg * 4 + j
                P.op("pe", lambda q, ps=ps, j=j, ti=ti: q.transpose(ps[0:8, j * 128:(j + 1) * 128], eq1[:, ti, :], identf[:]),
                     reads=["eq1", "identf"], writes=[pk])
            P.op("act", lambda q, ps=ps, tg=tg: q.activation(out=gT[:, tsl(tg)], in_=ps[0:8, :], func=AF.Copy), reads=[pk], writes=["S0"])

    for k in range(8):
        for tg in range(4):
            P.op("act", lambda q, k=k, tg=tg: q.activation(out=x[:, k, tsl(tg)], in_=x[:, k, tsl(tg)], func=AF.Copy, scale=DEEPNORM_ALPHA),
                 reads=[xk(k, tg)], writes=[xk(k, tg)])

    blocks = []
    if moe:
        for e in range(N_EXPERTS):
            for (f0, nf) in chunks(EXPERT_DIM // 128, FB):
                blocks.append((w_up[e], w_down[e], EXPERT_DIM, f0, nf, e))
    else:
        for (f0, nf) in chunks(FFN_DIM // 128, FB):
            blocks.append((w_up, w_down, FFN_DIM, f0, nf, None))

    stg = [S[2][:, i * 512:(i + 1) * 512] for i in range(4)]
    stg_n = [0]

    def block_pieces(bi):
        wup, wdn, F, f0, nf, e = blocks[bi]
        b2 = bi % 2
        wuv = wu[b2][:].rearrange("p (k s c) -> p k s c", k=8, s=2)
        upv = wup.rearrange("(k p) c -> p k c", p=128)
        pcs = []
        for k in range(8):
            for s_ in range(2):
                pcs.append((wuv[:, k, s_, 0:nf * 128], upv[:, k, s_ * F + f0 * 128:s_ * F + (f0 + nf) * 128], nf * 128, f"wu{b2}"))
        for fc in range(nf):
            for hf in range(2):
                pcs.append((wd[b2][:, fc, hf * 512:(hf + 1) * 512],
                            wdn[(f0 + fc) * 128:(f0 + fc + 1) * 128, hf * 512:(hf + 1) * 512], 512, f"wd{b2}"))
        return pcs

    def emit_piece(pc):
        dst, src, n, dkey = pc
        i = stg_n[0] % 4
        first = stg_n[0] < 4
        stg_n[0] += 1
        P.dma("sp", stg[i][:, 0:n], src, writes=(["S2", f"stg{i}"] if first else [f"stg{i}"]))
        P.op("act", lambda q: q.activation(out=dst, in_=stg[i][:, 0:n], func=AF.Copy), reads=[f"stg{i}"], writes=[dkey])

    for pc in block_pieces(0):
        emit_piece(pc)
    for bi, (wup, wdn, F, f0, nf, e) in enumerate(blocks):
        b2 = bi % 2
        nxt = block_pieces(bi + 1) if bi + 1 < len(blocks) else []
        per_it = -(-len(nxt) // (nf * 4)) if nxt else 0
        gmul = None
        gk = None
        if moe:
            gmul, gk = S[1], "S1"
            if f0 == 0:
                for tg in range(4):
                    ps, pk = pp.get()
                    P.op("pe", lambda q, ps=ps, e=e, tg=tg: q.matmul(ps[:, :], lhsT=sel[:, e * 128:(e + 1) * 128], rhs=gT[:, tsl(tg)],
                                                                    start=True, stop=True), reads=["sel", "S0"], writes=[pk])
                    P.op("act", lambda q, ps=ps, gmul=gmul, tg=tg: q.activation(out=gmul[:, tsl(tg)], in_=ps[:], func=AF.Copy),
                         reads=[pk], writes=[gk + f"_{tg}"])
        wuv = wu[b2][:].rearrange("p (k s c) -> p k s c", k=8, s=2)
        for fc in range(nf):
            for tg in range(4):
                psg, kg = pp.get()
                psu, ku = pp.get()
                for s_, ps_, pk_ in ((0, psg, kg), (1, psu, ku)):
                    for k in range(8):
                        P.op("pe", lambda q, ps_=ps_, k=k, s_=s_, fc=fc, tg=tg: q.matmul(ps_[:, :], lhsT=wuv[:, k, s_, fc * 128:(fc + 1) * 128],
                                                                                      rhs=y[:, k, tsl(tg)], start=(k == 0), stop=(k == 7)),
                             reads=[f"wu{b2}", yk(k, tg)], writes=[pk_])
                sg, sgk = gett()
                P.op("act", lambda q, sg=sg, psg=psg: q.activation(out=sg[:], in_=psg[:], func=AF.Silu), reads=[kg], writes=[sgk])
                if gmul is not None:
                    P.op("pool", lambda q, sg=sg, gmul=gmul, tg=tg: q.tensor_tensor(out=sg[:], in0=sg[:], in1=gmul[:, tsl(tg)], op=ALU.mult),
                         reads=[sgk, gk + f"_{tg}"], writes=[sgk])
                P.op("dve", lambda q, sg=sg, psu=psu, fc=fc, tg=tg: q.tensor_tensor(out=abuf[:, fc, tsl(tg)], in0=sg[:], in1=psu[:], op=ALU.mult),
                     reads=[sgk, ku], writes=[f"a{fc}_{tg}"])
                for _ in range(per_it):
                    if nxt:
                        emit_piece(nxt.pop(0))
        while nxt:
            emit_piece(nxt.pop(0))
        for dc in range(8):
            for tg in range(4):
                ps, pk = pp.get()
                for fc in range(nf):
                    P.op("pe", lambda q, ps=ps, fc=fc, dc=dc, tg=tg: q.matmul(ps[:, :], lhsT=wd[b2][:, fc, dc * 128:(dc + 1) * 128],
                                                                              rhs=abuf[:, fc, tsl(tg)], start=(fc == 0), stop=(fc == nf - 1)),
                         reads=[f"wd{b2}", f"a{fc}_{tg}"], writes=[pk])
                P.op("dve", lambda q, ps=ps, dc=dc, tg=tg: q.scalar_tensor_tensor(out=x[:, dc, tsl(tg)], in0=ps[:], scalar=dcol[:, G2 + dc:G2 + dc + 1],
                                                                                 in1=x[:, dc, tsl(tg)], op0=ALU.mult, op1=ALU.add),
                     reads=[pk, "dcol", xk(dc, tg)], writes=[xk(dc, tg)])

    ln_fm(xs, 1024, lambda i: sm[:, O_LNW + 8 + i:O_LNW + 9 + i], lambda i: sm[:, O_LNB + 8 + i:O_LNB + 9 + i], xs)
    xo_v = xo.rearrange("(k p) t -> p k t", p=128)
    for k in range(8):
        P.dma("sp", xo_v[:, k, :], x[:, k, :], reads=[xk(k, tg) for tg in range(4)], is_output=True)
    return P.finish()


def run_k3(layer, xT_shards, mods, of, ob, o32_full, numC, denC, numD, denD, inp):
    moe = (layer % 2 == 1)
    T = TOK
    sm = np.zeros((128, 128), np.float32)
    anw = np.asarray(inp["a_norm_w"])[layer]
    sm[:, 0] = np.concatenate([anw, anw])
    cw = np.asarray(inp["b_conv_w"])[layer]
    for b in range(2):
        sm[:, 1 + b * 31:1 + (b + 1) * 31] = cw[:, b * 128:(b + 1) * 128].T
    sm[:, 63:65] = col128(np.asarray(inp["b_conv_b"])[layer])
    sm[:, 65:67] = col128(np.asarray(inp["b_norm_w"])[layer])
    sm[:, 67:69] = col128(np.asarray(inp["b_norm_b"])[layer])
    sink = np.asarray(inp["d_sink"])[layer]
    sm[:, 69:71] = col128(np.repeat(sink, 64))
    sm[:, 71:79] = col128(np.asarray(inp["ln_w"])[layer, 0])
    sm[:, 79:87] = col128(np.asarray(inp["ln_w"])[layer, 1])
    sm[:, 87:95] = col128(np.asarray(inp["ln_b"])[layer, 0])
    sm[:, 95:103] = col128(np.asarray(inp["ln_b"])[layer, 1])
    blk64 = np.kron(np.eye(2, dtype=np.float32), np.ones((64, 64), np.float32))
    u = o32_full["u"]
    upad = np.zeros((256, SEQ + 32), np.float32)
    upad[:, 15:15 + SEQ] = u
    denC_rep = np.repeat(denC, 64, axis=1)
    denD_rep = np.repeat(denD, 64, axis=0)
    w_out = np.ascontiguousarray(inp["w_out"][layer])
    common = {"smalls": sm, "blk64": blk64, "w_out": w_out, "identb": np.eye(128, dtype=np.float32).astype(ml_dtypes.bfloat16)}
    if moe:
        li = layer // 2
        sel = np.zeros((8, 8 * 128), np.float32)
        for e in range(8):
            sel[e, e * 128:(e + 1) * 128] = 1.0
        common.update({"wr": np.ascontiguousarray(inp["moe_router"][li]), "w_up": np.ascontiguousarray(inp["moe_w_up"][li]),
                       "w_down": np.ascontiguousarray(inp["moe_w_down"][li]), "sel": sel, "identf": np.eye(128, dtype=np.float32)})
    else:
        li = layer // 2
        common.update({"w_up": np.ascontiguousarray(inp["ffn_w_up"][li]), "w_down": np.ascontiguousarray(inp["ffn_w_down"][li])})
    in_maps = []
    for c in range(NCORES):
        ts = slice(c * T, (c + 1) * T)
        m = dict(common)
        m.update({"xT": xT_shards[c], "modc": mods[c], "ofT": np.ascontiguousarray(of[:, ts]), "obT": np.ascontiguousarray(ob[:, ts]),
                  "gaT": np.ascontiguousarray(o32_full["ga"][:, ts]), "uext": np.ascontiguousarray(upad[:, c * T:c * T + T + 32]),
                  "numC": np.ascontiguousarray(numC[:, :, ts]), "denC": np.ascontiguousarray(denC_rep[:, :, ts]),
                  "numD": np.ascontiguousarray(numD[:, ts]), "denD": np.ascontiguousarray(denD_rep[:, ts])})
        in_maps.append(m)
    res = run(("k3", moe), lambda: build_k3(moe), in_maps)
    return [r["xo"] for r in res]


def run_layer(layer, xT_shards, inp):
    r1 = run_k1(layer, xT_shards, inp)
    o32_full = {nm: np.concatenate([r["o32"][off:off + 256] for r in r1], axis=1) for nm, off in O32.items()}
    obf_full = {nm: np.concatenate([r["obf"][off:off + (128 if nm in ("dk", "dv") else 256)] for r in r1], axis=1)
                for nm, off in OBF.items()}
    mods = [r["omod"] for r in r1]
    of, ob = run_k2a(o32_full)
    numC, denC, numD, denD = run_k2b(obf_full)
    return run_k3(layer, xT_shards, mods, of, ob, o32_full, numC, denC, numD, denD, inp)


def kernel(**inp):
    inp = {k: np.asarray(v) for k, v in inp.items()}
    x = inp["x"][0]
    xT_shards = [np.ascontiguousarray(x[c * TOK:(c + 1) * TOK].T) for c in range(NCORES)]
    for layer in range(DEPTH):
        xT_shards = run_layer(layer, xT_shards, inp)
    out = np.concatenate([s.T for s in xT_shards], axis=0)[None]
    return np.ascontiguousarray(out.astype(np.float32))
```

```python
import math
from contextlib import ExitStack

import numpy as np
import ml_dtypes

import concourse.bass as bass
import concourse.mybir as mybir
from concourse.bass_utils import run_bass_kernel_spmd

F32 = mybir.dt.float32
BF16 = mybir.dt.bfloat16
I32 = mybir.dt.int32
AF = mybir.ActivationFunctionType
ALU = mybir.AluOpType
AX = mybir.AxisListType

NCORES = 8
D_MODEL = 1024
SEQ = 16384
TOK = SEQ // NCORES
DEPTH = 2
HD = 64
FFN_DIM = 2816
N_EXPERTS = 8
EXPERT_DIM = 3584
B_KERNEL = 31
ROPE_THETA = 500000.0
ROPE_DIM = 16
DEEPNORM_ALPHA = (2 * DEPTH) ** 0.25
LN_EPS = 1e-5
RMS_EPS = 1e-6
NEG = -30000.0
TWO_PI = 2.0 * math.pi


class Prog:
    NDS = 24

    def __init__(self):
        self.nc = bass.Bass("TRN2", target_bir_lowering=False)
        nc = self.nc
        self.es = ExitStack()
        self.q = {"pe": nc.tensor, "dve": nc.vector, "act": nc.scalar, "pool": nc.gpsimd, "sp": nc.sync}
        self.esem = {e: self.es.enter_context(nc.semaphore("es_" + e)) for e in ("pe", "dve", "act", "pool")}
        self.ecnt = {e: 0 for e in self.esem}
        self.dsem = [self.es.enter_context(nc.semaphore(f"ds{i}")) for i in range(self.NDS)]
        self.dval = [0] * self.NDS
        self.dnext = 0
        self.seen = {e: {} for e in self.q}
        self.lastw = {}
        self.readers = {}
        self.out_tokens = []
        self.n_inst = 0
        self._ps_id = 0

    def dram_in(self, name, shape, dt):
        return self.nc.dram_tensor(name, list(shape), dt, kind="ExternalInput").ap()

    def dram_out(self, name, shape, dt):
        return self.nc.dram_tensor(name, list(shape), dt, kind="ExternalOutput").ap()

    def sb(self, name, shape, dt):
        return self.es.enter_context(self.nc.sbuf_tensor("sb_" + name, list(shape), dt))

    def ps(self, name, shape, dt=F32):
        return self.es.enter_context(self.nc.psum_tensor("pm_" + name, list(shape), dt))

    def _wait(self, e, tok):
        sem, v, owner = tok
        if owner == e and e == "pe":
            return
        k = id(sem)
        if self.seen[e].get(k, 0) >= v:
            return
        self.q[e].wait_ge(sem, v)
        self.seen[e][k] = v

    def _deps(self, e, reads, writes):
        for k in reads:
            t = self.lastw.get(k)
            if t is not None:
                self._wait(e, t)
        for k in writes:
            t = self.lastw.get(k)
            if t is not None:
                self._wait(e, t)
            for t in self.readers.get(k, {}).values():
                self._wait(e, t)

    def _record(self, tok, reads, writes):
        for k in writes:
            self.lastw[k] = tok
            self.readers[k] = {}
        for k in reads:
            self.readers.setdefault(k, {})[id(tok[0])] = tok

    def op(self, e, fn, reads=(), writes=()):
        self._deps(e, reads, writes)
        inst = fn(self.q[e])
        self.ecnt[e] += 1
        inst.then_inc(self.esem[e], 1)
        tok = (self.esem[e], self.ecnt[e], e)
        self._record(tok, reads, writes)
        self.n_inst += 1
        return tok

    def dma(self, e, out, in_, reads=(), writes=(), is_output=False, **kw):
        self._deps(e, reads, writes)
        j = self.dnext
        self.dnext = (self.dnext + 1) % self.NDS
        if self.dval[j] > 0:
            self._wait(e, (self.dsem[j], self.dval[j], "dma"))
        self.q[e].dma_start(out=out, in_=in_, **kw).then_inc(self.dsem[j], 16)
        self.dval[j] += 16
        tok = (self.dsem[j], self.dval[j], "dma")
        self._record(tok, reads, writes)
        if is_output:
            self.out_tokens.append(tok)
        self.n_inst += 1
        return tok

    def finish(self):
        for j in range(self.NDS):
            if self.dval[j] > 0:
                self._wait("sp", (self.dsem[j], self.dval[j], "dma"))
        return self.nc


def chunks(n, c):
    return [(i, min(c, n - i)) for i in range(0, n, c)]


class PsumPool:
    def __init__(self, P, n=8, prefix="pb"):
        self.t = [P.ps(f"{prefix}{i}", [128, 512], F32) for i in range(n)]
        self.k = [f"{prefix}{i}" for i in range(n)]
        self.i = 0
        self.n = n

    def get(self):
        i = self.i
        self.i = (self.i + 1) % self.n
        return self.t[i], self.k[i]


def build_consts(P):
    c = {}
    c["ones_f"] = P.sb("ones_f", [128, 512], F32)
    P.op("pool", lambda q: q.memset(c["ones_f"][:], 1.0), writes=["ones_f"])
    c["ones_b"] = P.sb("ones_b", [128, 512], BF16)
    P.op("pool", lambda q: q.memset(c["ones_b"][:], 1.0), writes=["ones_b"])
    return c


CH = {"aq": (0, 2), "aff": (2, 2), "afb": (4, 2), "ai": (6, 2), "ag": (8, 2), "bv": (10, 2), "bg": (12, 2),
      "cq": (14, 2), "ck": (16, 2), "cv": (18, 2), "dq": (20, 2), "dk": (22, 1), "dv": (23, 1)}
ROT_CHUNKS = [14, 15, 16, 17, 20, 21, 22]
O32 = {"qa": 0, "kf": 256, "lf": 512, "kb": 768, "lb": 1024, "va": 1280, "ga": 1536, "u": 1792}
OBF = {"cq": 0, "ck": 256, "cv": 512, "dq": 768, "dk": 1024, "dv": 1152}
NBF = 1280


def build_k1(layer):
    P = Prog()
    nc = P.nc
    T = TOK
    xT = P.dram_in("xT", [D_MODEL, T], F32)
    modc_in = P.dram_in("modc", [128, 48], F32)
    w_in = P.dram_in("w_in", [D_MODEL, 3072], F32)
    w_sw = P.dram_in("w_sw", [D_MODEL, 7 * 128], F32)
    pos = P.dram_in("pos", [T], I32)
    rcol = P.dram_in("rcol", [128, 2], F32)
    alb = P.dram_in("alb", [128, 4], F32)
    o32 = P.dram_out("o32", [2048, T], F32)
    obf = P.dram_out("obf", [NBF, T], BF16)

    C = build_consts(P)
    pp = PsumPool(P)

    mod = P.sb("mod", [128, 48], F32)
    P.dma("sp", mod[:], modc_in, writes=["mod"])
    sc1 = P.sb("sc1", [128, 8], F32)
    P.op("dve", lambda q: q.tensor_scalar(out=sc1[:], in0=mod[:, 8:16], scalar1=1.0, scalar2=None, op0=ALU.add),
         reads=["mod"], writes=["sc1"])

    hT = P.sb("hT", [128, 8, T], BF16)
    xst = [P.sb(f"xst{i}", [128, T], F32) for i in range(2)]
    xT_v = xT.rearrange("(k p) t -> p k t", p=128)
    for k in range(8):
        st = xst[k % 2]
        sk = f"xst{k % 2}"
        P.dma("sp", st[:], xT_v[:, k, :], writes=[sk])
        P.op("dve", lambda q, k=k, st=st: q.tensor_scalar(out=hT[:, k, :], in0=st[:], scalar1=sc1[:, k:k + 1],
                                                        scalar2=mod[:, k:k + 1], op0=ALU.mult, op1=ALU.add),
             reads=[sk, "sc1", "mod"], writes=[f"hT{k}"])
    hkeys = [f"hT{k}" for k in range(8)]

    wb = P.sb("wb", [128, 8, 3072], BF16)
    wsw = P.sb("wsw", [128, 8, 896], BF16)
    w_in_v = w_in.rearrange("(k p) e -> p k e", p=128)
    w_sw_v = w_sw.rearrange("(k p) e -> p k e", p=128)
    for (c0, nch) in ((10, 4), (0, 4), (4, 4), (8, 2)):
        P.dma("pool", wb[:, :, c0 * 128:(c0 + nch) * 128], w_in_v[:, :, c0 * 128:(c0 + nch) * 128], writes=[f"wbc{c}" for c in range(c0, c0 + nch)])
    P.dma("pool", wb[:, :, 14 * 128:18 * 128], w_in_v[:, :, 14 * 128:18 * 128], writes=[f"wbc{c}" for c in range(14, 18)])
    P.dma("pool", wsw[:, :, 0:4 * 128], w_sw_v[:, :, 0:4 * 128], writes=[f"wswc{c}" for c in range(0, 4)])
    P.dma("pool", wb[:, :, 18 * 128:24 * 128], w_in_v[:, :, 18 * 128:24 * 128], writes=[f"wbc{c}" for c in range(18, 24)])
    P.dma("pool", wsw[:, :, 4 * 128:7 * 128], w_sw_v[:, :, 4 * 128:7 * 128], writes=[f"wswc{c}" for c in range(4, 7)])

    rc = P.sb("rc", [128, 2], F32)
    P.dma("sp", rc[:], rcol, writes=["rc"])
    tmpi = P.sb("tmpi", [128, T], I32)
    P.dma("sp", tmpi[:], pos.partition_broadcast(128), writes=["tmpi"])
    ang = P.sb("ang", [128, T], F32)
    cosT = P.sb("cosT", [128, T], F32)
    sinT = P.sb("sinT", [128, T], F32)
    tmpf = P.sb("tmpf", [128, T], F32)
    P.op("dve", lambda q: q.tensor_copy(out=ang[:], in_=tmpi[:]), reads=["tmpi"], writes=["ang"])
    P.op("dve", lambda q: q.tensor_scalar(out=ang[:], in0=ang[:], scalar1=rc[:, 0:1], scalar2=None, op0=ALU.mult),
         reads=["ang", "rc"], writes=["ang"])
    C1 = 6.28125
    C2 = TWO_PI - C1

    def sin_table(dst, dkey, phase):
        P.op("dve", lambda q: q.tensor_scalar(out=tmpf[:], in0=ang[:], scalar1=phase, scalar2=1.0 / TWO_PI,
                                              op0=ALU.add, op1=ALU.mult), reads=["ang"], writes=["tmpf"])
        P.op("dve", lambda q: q.tensor_copy(out=tmpi[:], in_=tmpf[:]), reads=["tmpf"], writes=["tmpi"])
        P.op("dve", lambda q: q.tensor_copy(out=tmpf[:], in_=tmpi[:]), reads=["tmpi"], writes=["tmpf"])
        P.op("dve", lambda q: q.scalar_tensor_tensor(out=dst[:], in0=tmpf[:], scalar=-C1, in1=ang[:],
                                                     op0=ALU.mult, op1=ALU.add), reads=["tmpf", "ang"], writes=[dkey])
        P.op("dve", lambda q: q.scalar_tensor_tensor(out=dst[:], in0=tmpf[:], scalar=-C2, in1=dst[:],
                                                     op0=ALU.mult, op1=ALU.add), reads=["tmpf", dkey], writes=[dkey])
        P.op("dve", lambda q: q.tensor_scalar(out=dst[:], in0=dst[:], scalar1=phase, scalar2=-math.pi,
                                              op0=ALU.add, op1=ALU.max), reads=[dkey], writes=[dkey])
        P.op("dve", lambda q: q.tensor_scalar(out=dst[:], in0=dst[:], scalar1=math.pi, scalar2=None,
                                              op0=ALU.min), reads=[dkey], writes=[dkey])
        P.op("act", lambda q: q.activation(out=dst[:], in_=dst[:], func=AF.Sin), reads=[dkey], writes=[dkey])

    sin_table(cosT, "cosT", math.pi / 2)
    sin_table(sinT, "sinT", 0.0)
    P.op("dve", lambda q: q.tensor_scalar(out=sinT[:], in0=sinT[:], scalar1=rc[:, 1:2], scalar2=None, op0=ALU.mult),
         reads=["sinT", "rc"], writes=["sinT"])

    albs = P.sb("albs", [128, 4], F32)
    P.dma("sp", albs[:], alb, writes=["albs"])
    lbc = P.sb("lbc", [128, 2], F32)
    oml = P.sb("oml", [128, 2], F32)
    if layer == 0:
        P.op("pool", lambda q: q.memset(lbc[:], 0.0), writes=["lbc"])
        P.op("pool", lambda q: q.memset(oml[:], 1.0), writes=["oml"])
    else:
        ex = P.sb("alb_ex", [128, 4], F32)
        P.op("act", lambda q: q.activation(out=ex[:], in_=albs[:], func=AF.Exp), reads=["albs"], writes=["alb_ex"])
        exv = ex[:].rearrange("p (t l) -> p t l", l=2)
        sm = P.sb("alb_sm", [128, 2], F32)
        P.op("dve", lambda q: q.tensor_tensor(out=sm[:], in0=exv[:, :, 0], in1=exv[:, :, 1], op=ALU.add),
             reads=["alb_ex"], writes=["alb_sm"])
        P.op("dve", lambda q: q.reciprocal(out=sm[:], in_=sm[:]), reads=["alb_sm"], writes=["alb_sm"])
        P.op("dve", lambda q: q.tensor_tensor(out=lbc[:], in0=exv[:, :, 1], in1=sm[:], op=ALU.mult),
             reads=["alb_ex", "alb_sm"], writes=["lbc"])
        P.op("dve", lambda q: q.tensor_scalar(out=oml[:], in0=lbc[:], scalar1=-1.0, scalar2=1.0, op0=ALU.mult,
                                              op1=ALU.add), reads=["lbc"], writes=["oml"])

    ob_f = [P.sb(f"obf{i}", [128, 512], F32) for i in range(4)]
    ob_b = [P.sb(f"obb{i}", [128, 512], BF16) for i in range(4)]
    sg = [P.sb(f"sg{i}", [128, T], F32) for i in range(2)]
    cnt = {"f": 0, "b": 0}

    def getf():
        i = cnt["f"] % 4
        cnt["f"] += 1
        return ob_f[i], f"obf{i}"

    def getb():
        i = cnt["b"] % 4
        cnt["b"] += 1
        return ob_b[i], f"obb{i}"

    def proj(ps, psk, wt, wkey, col0, tg):
        for k in range(8):
            P.op("pe", lambda q, k=k: q.matmul(ps[:, :], lhsT=wt[:, k, col0:col0 + 128],
                                               rhs=hT[:, k, tg * 512:(tg + 1) * 512], start=(k == 0), stop=(k == 7)),
                 reads=[wkey, hkeys[k]], writes=[psk])

    def store32(name, tile_i, tg, buf, bkey):
        r0 = O32[name] + tile_i * 128
        P.dma("sp", o32[r0:r0 + 128, tg * 512:(tg + 1) * 512], buf[:], reads=[bkey], is_output=True)

    def storebf(name, tile_i, tg, buf, bkey):
        r0 = OBF[name] + tile_i * 128
        P.dma("sp", obf[r0:r0 + 128, tg * 512:(tg + 1) * 512], buf[:], reads=[bkey], is_output=True)

    order = ["bg", "bv", "aq", "aff", "afb", "ai", "ag", "cq", "ck", "cv", "dq", "dk", "dv"]
    for name in order:
        c0, ncn = CH[name]
        for ti in range(ncn):
            ch = c0 + ti
            for tg in range(4):
                tsl = slice(tg * 512, (tg + 1) * 512)
                ps, psk = pp.get()
                proj(ps, psk, wb, f"wbc{ch}", ch * 128, tg)
                if name == "bg":
                    P.op("act", lambda q, ps=ps, ti=ti, tsl=tsl: q.activation(out=sg[ti][:, tsl], in_=ps[:], func=AF.Sigmoid),
                         reads=[psk], writes=[f"sg{ti}_{tg}"])
                elif name == "bv":
                    b, bk = getf()
                    P.op("dve", lambda q, ps=ps, b=b, ti=ti, tsl=tsl: q.tensor_tensor(out=b[:], in0=ps[:], in1=sg[ti][:, tsl], op=ALU.mult),
                         reads=[psk, f"sg{ti}_{tg}"], writes=[bk])
                    store32("u", ti, tg, b, bk)
                elif name in ("aq", "ag"):
                    b, bk = getf()
                    P.op("act", lambda q, ps=ps, b=b: q.activation(out=b[:], in_=ps[:], func=AF.Silu), reads=[psk], writes=[bk])
                    store32("qa" if name == "aq" else "ga", ti, tg, b, bk)
                elif name == "ai":
                    b, bk = getf()
                    P.op("act", lambda q, ps=ps, b=b: q.activation(out=b[:], in_=ps[:], func=AF.Copy), reads=[psk], writes=[bk])
                    store32("va", ti, tg, b, bk)
                elif name in ("aff", "afb"):
                    fb_, fk = getf()
                    P.op("act", lambda q, ps=ps, fb_=fb_: q.activation(out=fb_[:], in_=ps[:], func=AF.Sigmoid), reads=[psk], writes=[fk])
                    P.op("dve", lambda q, fb_=fb_, ti=ti: q.tensor_scalar(out=fb_[:], in0=fb_[:], scalar1=oml[:, ti:ti + 1],
                                                                        scalar2=lbc[:, ti:ti + 1], op0=ALU.mult, op1=ALU.add),
                         reads=[fk, "oml", "lbc"], writes=[fk])
                    kb_, kk = getf()
                    P.op("dve", lambda q, fb_=fb_, kb_=kb_: q.tensor_scalar(out=kb_[:], in0=fb_[:], scalar1=-1.0, scalar2=1.0,
                                                                          op0=ALU.mult, op1=ALU.add), reads=[fk], writes=[kk])
                    store32("kf" if name == "aff" else "kb", ti, tg, kb_, kk)
                    P.op("act", lambda q, fb_=fb_: q.activation(out=fb_[:], in_=fb_[:], func=AF.Ln), reads=[fk], writes=[fk])
                    store32("lf" if name == "aff" else "lb", ti, tg, fb_, fk)
                elif name in ("cv", "dv"):
                    b, bk = getb()
                    P.op("act", lambda q, ps=ps, b=b: q.activation(out=b[:], in_=ps[:], func=AF.Copy), reads=[psk], writes=[bk])
                    storebf(name, ti, tg, b, bk)
                else:
                    ri = ROT_CHUNKS.index(ch)
                    ps2, psk2 = pp.get()
                    proj(ps2, psk2, wsw, f"wswc{ri}", ri * 128, tg)
                    t1, t1k = getf()
                    t2, t2k = getf()
                    P.op("dve", lambda q, ps=ps, t1=t1, tsl=tsl: q.tensor_tensor(out=t1[:], in0=ps[:], in1=cosT[:, tsl], op=ALU.mult),
                         reads=[psk, "cosT"], writes=[t1k])
                    P.op("dve", lambda q, ps2=ps2, t2=t2, tsl=tsl: q.tensor_tensor(out=t2[:], in0=ps2[:], in1=sinT[:, tsl], op=ALU.mult),
                         reads=[psk2, "sinT"], writes=[t2k])
                    b, bk = getb()
                    P.op("pool", lambda q, t1=t1, t2=t2, b=b: q.tensor_tensor(out=b[:], in0=t1[:], in1=t2[:], op=ALU.add),
                         reads=[t1k, t2k], writes=[bk])
                    storebf(name, ti, tg, b, bk)
    return P.finish()


def col128(v):
    v = np.asarray(v)
    return np.ascontiguousarray(v.reshape(-1, 128).T)


def rot_cols():
    idx = []
    for ch in ROT_CHUNKS:
        for h in range(2):
            base = ch * 128 + h * 64
            loc = np.arange(64)
            loc[:8] = np.arange(8, 16)
            loc[8:16] = np.arange(0, 8)
            idx.append(base + loc)
    return np.concatenate(idx)


def rot_consts():
    half = ROPE_DIM // 2
    inv = (np.float32(ROPE_THETA) ** (-np.arange(half, dtype=np.float32) * np.float32(2.0 / ROPE_DIM))).astype(np.float32)
    rc = np.zeros((128, 2), np.float32)
    for p in range(128):
        d = p % 64
        if d < 16:
            rc[p, 0] = inv[d % 8]
            rc[p, 1] = -1.0 if d < 8 else 1.0
    return rc


_cache = {}


def run(nc_key, builder, in_maps):
    if nc_key not in _cache:
        _cache[nc_key] = builder()
    nc = _cache[nc_key]
    res = run_bass_kernel_spmd(nc, in_maps, core_ids=list(range(NCORES)))
    return res.results


def build_k0():
    P = Prog()
    ccol = P.dram_in("ccol", [128, 8], F32)
    w = P.dram_in("w", [D_MODEL, 1536], F32)
    bias = P.dram_in("bias", [1, 1536], F32)
    o = P.dram_out("o", [1, 1536], F32)
    sc = P.sb("sc", [128, 8], F32)
    P.dma("sp", sc[:], ccol, writes=["sc"])
    P.op("act", lambda q: q.activation(out=sc[:], in_=sc[:], func=AF.Silu), reads=["sc"], writes=["sc"])
    bs = P.sb("bs", [1, 1536], F32)
    P.dma("sp", bs[:], bias, writes=["bs"])
    ws = P.sb("ws", [128, 8, 1536], F32)
    wv = w.rearrange("(k p) e -> p k e", p=128)
    for g in range(3):
        P.dma("sp", ws[:, :, g * 512:(g + 1) * 512], wv[:, :, g * 512:(g + 1) * 512], writes=[f"ws{g}"])
    ob = P.sb("ob", [1, 1536], F32)
    pss = [P.ps(f"p{i}", [128, 512], F32) for i in range(3)]
    for g in range(3):
        for k in range(8):
            P.op("pe", lambda q, g=g, k=k: q.matmul(pss[g][0:1, :], lhsT=sc[:, k:k + 1], rhs=ws[:, k, g * 512:(g + 1) * 512],
                                                   start=(k == 0), stop=(k == 7)), reads=["sc", f"ws{g}"], writes=[f"p{g}"])
        P.op("dve", lambda q, g=g: q.tensor_tensor(out=ob[:, g * 512:(g + 1) * 512], in0=pss[g][0:1, :], in1=bs[:, g * 512:(g + 1) * 512], op=ALU.add),
             reads=[f"p{g}", "bs"], writes=["ob"])
    P.dma("sp", o, ob[:], reads=["ob"], is_output=True)
    return P.finish()


def run_k0(inp):
    wa = np.asarray(inp["w_ada"])
    wcat = np.concatenate([wa[l] for l in range(DEPTH)], axis=1)
    bcat = np.concatenate([np.asarray(inp["b_ada"])[l] for l in range(DEPTH)])[None, :]
    ccol = col128(np.asarray(inp["c"])[0])
    in_maps = [{"ccol": ccol, "w": np.ascontiguousarray(wcat[:, c * 1536:(c + 1) * 1536]),
                "bias": np.ascontiguousarray(bcat[:, c * 1536:(c + 1) * 1536])} for c in range(NCORES)]
    res = run(("k0",), build_k0, in_maps)
    mod = np.concatenate([r["o"][0] for r in res])
    return [col128(mod[l * 6144:(l + 1) * 6144]) for l in range(DEPTH)]


def run_k1(layer, xT_shards, modc, inp):
    rc = rot_consts()
    sw = rot_cols()
    w_in_l = np.ascontiguousarray(inp["w_in"][layer])
    w_sw = np.ascontiguousarray(w_in_l[:, sw])
    alb = np.zeros((128, 4), np.float32)
    a = np.asarray(inp["a_lower_bound"])
    for t in range(2):
        for l in range(2):
            alb[:, t * 2 + l] = a[l, t * 128:(t + 1) * 128]
    pos = np.asarray(inp["positions"])[0].astype(np.int32)
    in_maps = []
    for c in range(NCORES):
        in_maps.append({"xT": xT_shards[c], "modc": modc, "w_in": w_in_l, "w_sw": w_sw,
                        "pos": np.ascontiguousarray(pos[c * TOK:(c + 1) * TOK]), "rcol": rc, "alb": alb})
    return run(("k1", layer), lambda: build_k1(layer), in_maps)


SEG = 1024
NSEG = SEQ // SEG
NCH = SEG // 16
NTL = SEG // 128


def hgrn_consts():
    ident = np.eye(128, dtype=np.float32).astype(ml_dtypes.bfloat16)
    s = np.arange(128)
    tri = ((s[:, None] // 16 == s[None, :] // 16) & (s[:, None] <= s[None, :])).astype(np.float32).astype(ml_dtypes.bfloat16)
    ind = (s[:, None] // 16 == np.arange(8)[None, :]).astype(np.float32).astype(ml_dtypes.bfloat16)
    return ident, tri, ind


def build_k2a():
    P = Prog()
    qT = P.dram_in("qT", [64, SEQ], F32)
    kT = P.dram_in("kT", [64, SEQ], F32)
    lT = P.dram_in("lT", [64, SEQ], F32)
    vtok = P.dram_in("vtok", [SEQ, 64], F32)
    ident_d = P.dram_in("ident_d", [128, 128], BF16)
    tri_d = P.dram_in("tri_d", [128, 128], BF16)
    ind_d = P.dram_in("ind_d", [128, 8], BF16)
    oT = P.dram_out("oT", [64, SEQ], F32)

    ident = P.sb("ident", [128, 128], BF16)
    tri = P.sb("tri", [128, 128], BF16)
    ind = P.sb("ind", [128, 8], BF16)
    P.dma("sp", ident[:], ident_d, writes=["ident"])
    P.dma("sp", tri[:], tri_d, writes=["tri"])
    P.dma("sp", ind[:], ind_d, writes=["ind"])

    reset = P.sb("reset", [64, SEG], F32)
    P.op("pool", lambda q: q.memset(reset[:], 1.0), writes=["reset"])
    P.op("pool", lambda q: q.memset(reset[:].rearrange("p (n c) -> p n c", c=16)[:, :, 0:1], 0.0), reads=["reset"], writes=["reset"])

    def two(name, shape, dt):
        return [P.sb(f"{name}{i}", shape, dt) for i in range(2)]

    qs_, ks_, ls_ = two("qs", [64, SEG], F32), two("ks", [64, SEG], F32), two("ls", [64, SEG], F32)
    vb_ = two("vb", [128, NTL, 64], BF16)
    cum_, ex_ = two("cum", [64, SEG], F32), two("ex", [64, SEG], F32)
    qt_, kt_, kh_ = two("qt", [64, SEG], BF16), two("kt", [64, SEG], BF16), two("kh", [64, SEG], BF16)
    dec_ = two("dec", [64, NCH], F32)
    kvs_ = two("kvs", [64, 64 * NCH], F32)
    sprev_ = two("sprev", [64, NCH, 64], BF16)
    obuf_ = two("obuf", [64, SEG], F32)
    decrep = P.sb("decrep", [64, 64 * NCH], F32)
    decz = P.sb("decz", [64, NCH], F32)
    P.op("pool", lambda q: q.memset(decz[:], 0.0), writes=["decz"])
    sall = P.sb("sall", [64, 64 * NCH], F32)
    s_in = P.sb("s_in", [64, 64], F32)
    khtok = [P.sb(f"khtok{i}", [128, 64], BF16) for i in range(2)]
    vblk = [P.sb(f"vblk{i}", [128, 8, 64], BF16) for i in range(2)]
    am = [P.sb(f"am{i}", [128, 128], BF16) for i in range(2)]
    P.op("pool", lambda q: q.memset(s_in[:], 0.0), writes=["s_in"])

    ps_kv = [P.ps(f"ps_kv{i}", [128, 512], F32) for i in range(2)]
    ps_a = [P.ps(f"ps_a{i}", [128, 512], F32) for i in range(2)]
    ps_o = [P.ps(f"ps_o{i}", [128, 512], F32) for i in range(2)]
    ps_t = [P.ps(f"ps_t{i}", [128, 1024], BF16) for i in range(2)]

    dec3 = decrep[:].rearrange("p (v n) -> p v n", n=NCH)
    sall3 = sall[:].rearrange("p (v n) -> p v n", n=NCH)
    vt_v = vtok.rearrange("(s i p) v -> s p i v", p=128, i=NTL)
    cnt = [0]

    def stage(sgi, part):
        pb = sgi % 2
        K_ = lambda n: f"{n}{pb}"
        qs, ks, ls, vb, cum, ex = qs_[pb], ks_[pb], ls_[pb], vb_[pb], cum_[pb], ex_[pb]
        qt, kt, kh, dec, kvs, sprev, obuf = qt_[pb], kt_[pb], kh_[pb], dec_[pb], kvs_[pb], sprev_[pb], obuf_[pb]
        kvs3 = kvs[:].rearrange("p (v n) -> p v n", n=NCH)
        cum3 = cum[:].rearrange("p (n c) -> p n c", c=16)
        ex3 = ex[:].rearrange("p (n c) -> p n c", c=16)
        tsl = slice(sgi * SEG, (sgi + 1) * SEG)
        if part == "B":
            yield from stageB(sgi, pb, K_, qt, kt, vb, dec, kvs, kvs3, sprev, obuf, tsl)
            return
        yield P.dma("sp", qs[:], qT[:, tsl], writes=[K_("qs")])
        yield P.dma("sp", ks[:], kT[:, tsl], writes=[K_("ks")])
        yield P.dma("sp", ls[:], lT[:, tsl], writes=[K_("ls")])
        yield P.dma("pool", vb[:], vt_v[sgi], writes=[K_("vb")])
        yield P.op("dve", lambda q: q.tensor_tensor_scan(out=cum[:], data0=reset[:], data1=ls[:], initial=0.0,
                                                   op0=ALU.mult, op1=ALU.add), reads=["reset", K_("ls")], writes=[K_("cum")])
        yield P.op("act", lambda q: q.activation(out=ex[:], in_=cum[:], func=AF.Exp), reads=[K_("cum")], writes=[K_("ex")])
        yield P.op("dve", lambda q: q.tensor_tensor(out=qt[:], in0=qs[:], in1=ex[:], op=ALU.mult), reads=[K_("qs"), K_("ex")], writes=[K_("qt")])
        yield P.op("act", lambda q: q.activation(out=ex[:], in_=cum[:], func=AF.Exp, scale=-1.0), reads=[K_("cum")], writes=[K_("ex")])
        yield P.op("dve", lambda q: q.tensor_tensor(out=kt[:], in0=ks[:], in1=ex[:], op=ALU.mult), reads=[K_("ks"), K_("ex")], writes=[K_("kt")])
        yield P.op("dve", lambda q: q.tensor_tensor(out=ex3, in0=cum3[:, :, 15:16].broadcast_to([64, NCH, 16]), in1=cum3,
                                              op=ALU.subtract), reads=[K_("cum")], writes=[K_("ex")])
        yield P.op("act", lambda q: q.activation(out=ex[:], in_=ex[:], func=AF.Exp), reads=[K_("ex")], writes=[K_("ex")])
        yield P.op("dve", lambda q: q.tensor_tensor(out=kh[:], in0=ks[:], in1=ex[:], op=ALU.mult), reads=[K_("ks"), K_("ex")], writes=[K_("kh")])
        yield P.op("act", lambda q: q.activation(out=dec[:], in_=cum3[:, :, 15], func=AF.Exp), reads=[K_("cum")], writes=[K_("dec")])
        bs = []
        for i in range(NTL):
            bs.append(cnt[0] % 2)
            cnt[0] += 1

        def emit_T(i):
            b = bs[i]
            yield P.op("pe", lambda q: q.transpose(ps_t[b][:, 0:64], kh[:, i * 128:(i + 1) * 128], ident[0:64, 0:64]),
                 reads=[K_("kh"), "ident"], writes=[f"ps_t{b}"])
            yield P.op("act", lambda q: q.activation(out=khtok[b][:], in_=ps_t[b][:, 0:64], func=AF.Copy),
                 reads=[f"ps_t{b}"], writes=[f"khtok{b}"])
            yield P.op("dve", lambda q: q.tensor_tensor(out=vblk[b][:], in0=vb[:, i:i + 1, :].broadcast_to([128, 8, 64]),
                                                   in1=ind[:].unsqueeze(2).broadcast_to([128, 8, 64]), op=ALU.mult),
                 reads=[K_("vb"), "ind"], writes=[f"vblk{b}"])

        def emit_KV(i):
            b = bs[i]
            yield P.op("pe", lambda q: q.matmul(ps_kv[b][0:64, :], lhsT=khtok[b][:], rhs=vblk[b][:].rearrange("p c v -> p (c v)"),
                                          start=True, stop=True), reads=[f"khtok{b}", f"vblk{b}"], writes=[f"ps_kv{b}"])
            yield P.op("act", lambda q: q.activation(out=kvs3[:, :, 8 * i:8 * i + 8],
                                               in_=ps_kv[b][0:64, :].rearrange("p (c v) -> p v c", v=64), func=AF.Copy),
                 reads=[f"ps_kv{b}"], writes=[K_("kvs")])

        yield from emit_T(0)
        for i in range(NTL):
            if i + 1 < NTL:
                yield from emit_T(i + 1)
            yield from emit_KV(i)

    def stageB(sgi, pb, K_, qt, kt, vb, dec, kvs, kvs3, sprev, obuf, tsl):
        yield P.op("act", lambda q: q.activation(out=decz[:, 1:NCH], in_=dec[:, 1:NCH], func=AF.Copy), reads=[K_("dec")], writes=["decz"])
        yield P.op("act", lambda q: q.activation(out=dec3, in_=decz[:].unsqueeze(1).broadcast_to([64, 64, NCH]), func=AF.Copy),
             reads=["decz"], writes=["decrep"])
        yield P.op("dve", lambda q: q.scalar_tensor_tensor(out=kvs3[:, :, 0], in0=s_in[:], scalar=dec[:, 0:1], in1=kvs3[:, :, 0],
                                                     op0=ALU.mult, op1=ALU.add), reads=["s_in", K_("dec"), K_("kvs")], writes=[K_("kvs")])
        yield P.op("dve", lambda q: q.tensor_tensor_scan(out=sall[:], data0=decrep[:], data1=kvs[:], initial=0.0,
                                                   op0=ALU.mult, op1=ALU.add), reads=["decrep", K_("kvs")], writes=["sall"])
        yield P.op("act", lambda q: q.activation(out=sprev[:, 0, :], in_=s_in[:], func=AF.Copy), reads=["s_in"], writes=[K_("sprev")])
        yield P.op("act", lambda q: q.activation(out=sprev[:, 1:NCH, :], in_=sall3[:, :, 0:NCH - 1].rearrange("p v n -> p n v"), func=AF.Copy),
             reads=["sall"], writes=[K_("sprev")])
        yield P.op("dve", lambda q: q.tensor_copy(out=s_in[:], in_=sall3[:, :, NCH - 1]), reads=["sall", K_("sprev")], writes=["s_in"])
        bs = []
        for i in range(NTL):
            bs.append(cnt[0] % 2)
            cnt[0] += 1

        def emit_A(i):
            b = bs[i]
            csl = slice(i * 128, (i + 1) * 128)
            yield P.op("pe", lambda q: q.matmul(ps_a[b][:, 0:128], lhsT=kt[:, csl], rhs=qt[:, csl], start=True, stop=True),
                 reads=[K_("kt"), K_("qt")], writes=[f"ps_a{b}"])
            yield P.op("dve", lambda q: q.tensor_tensor(out=am[b][:], in0=ps_a[b][:, 0:128], in1=tri[:], op=ALU.mult),
                 reads=[f"ps_a{b}", "tri"], writes=[f"am{b}"])

        def emit_O(i):
            b = bs[i]
            csl = slice(i * 128, (i + 1) * 128)
            yield P.op("pe", lambda q: q.matmul(ps_o[b][0:64, 0:128], lhsT=vb[:, i, :], rhs=am[b][:], start=True, stop=False),
                 reads=[K_("vb"), f"am{b}"], writes=[f"ps_o{b}"])
            for c in range(8):
                n = 8 * i + c
                yield P.op("pe", lambda q, c=c, n=n: q.matmul(ps_o[b][0:64, 16 * c:16 * c + 16], lhsT=sprev[:, n, :],
                                                       rhs=qt[:, i * 128 + 16 * c:i * 128 + 16 * c + 16],
                                                       start=False, stop=(c == 7), skip_group_check=True),
                     reads=[K_("sprev"), K_("qt")], writes=[f"ps_o{b}"])
            yield P.op("act", lambda q: q.activation(out=obuf[:, csl], in_=ps_o[b][0:64, 0:128], func=AF.Copy),
                 reads=[f"ps_o{b}"], writes=[K_("obuf")])

        yield from emit_A(0)
        for i in range(NTL):
            if i + 1 < NTL:
                yield from emit_A(i + 1)
            yield from emit_O(i)
        yield P.dma("sp", oT[:, tsl], obuf[:], reads=[K_("obuf")], is_output=True)

    def drain(*gens):
        gens = [g for g in gens if g is not None]
        while gens:
            for g in list(gens):
                try:
                    next(g)
                except StopIteration:
                    gens.remove(g)

    drain(stage(0, "A"))
    for sgi in range(NSEG):
        drain(stage(sgi, "B"), stage(sgi + 1, "A") if sgi + 1 < NSEG else None)
    return P.finish()


def run_k2a(o32_full):
    ident, tri, ind = hgrn_consts()
    in_maps = []
    for c in range(NCORES):
        h, d = c % 4, c // 4
        rows = slice(h * 64, (h + 1) * 64)
        q = o32_full["qa"][rows]
        k = o32_full["kf" if d == 0 else "kb"][rows]
        l = o32_full["lf" if d == 0 else "lb"][rows]
        v = o32_full["va"][rows]
        if d == 1:
            q, k, l, v = q[:, ::-1], k[:, ::-1], l[:, ::-1], v[:, ::-1]
        in_maps.append({"qT": np.ascontiguousarray(q), "kT": np.ascontiguousarray(k), "lT": np.ascontiguousarray(l),
                        "vtok": np.ascontiguousarray(v.T), "ident_d": ident, "tri_d": tri, "ind_d": ind})
    res = run(("k2a",), build_k2a, in_maps)
    of = np.concatenate([res[h]["oT"] for h in range(4)], 0)
    ob = np.concatenate([res[4 + h]["oT"][:, ::-1] for h in range(4)], 0)
    return of, np.ascontiguousarray(ob)


GU = 16
C_PAT = ((128, 1), (512, 4), (2048, 16))
NUC = 3 * 4 * 16
NUD = 4 * 16


def attn_masks():
    k = np.arange(256)[:, None]
    q = np.arange(128)[None, :]
    mc = np.where(np.abs(k - 64 - q) <= 64, 1.0, 0.0).astype(np.float32)
    k = np.arange(384)[:, None]
    md = np.where(np.abs(k - 128 - q) <= 128, 1.0, 0.0).astype(np.float32)
    mc = mc.reshape(2, 128, 128).transpose(1, 0, 2)
    md = md.reshape(3, 128, 128).transpose(1, 0, 2)
    return (np.ascontiguousarray(mc).astype(ml_dtypes.bfloat16), np.ascontiguousarray(md).astype(ml_dtypes.bfloat16))


def build_k2b():
    P = Prog()
    specs = []
    for nm, nu, nb in (("c", NUC, 2), ("d", NUD, 3)):
        ng = nu // GU
        specs.append(dict(nm=nm, nb=nb, ng=ng,
                          Q=P.dram_in(nm + "Q", [ng, 64, GU * 128], BF16),
                          K=P.dram_in(nm + "K", [ng, 64, GU * nb * 128], BF16),
                          V=P.dram_in(nm + "V", [ng, 128, GU * nb * 65], BF16),
                          M=P.dram_in(nm + "M", [128, nb, 128], BF16),
                          O=P.dram_out(nm + "O", [ng, 65, GU * 128], F32)))
    ident_d = P.dram_in("ident_d", [128, 128], BF16)
    ident = P.sb("ident", [128, 128], BF16)
    P.dma("sp", ident[:], ident_d, writes=["ident"])
    Qg = [P.sb(f"Qg{i}", [64, GU * 128], BF16) for i in range(2)]
    Kg = [P.sb(f"Kg{i}", [64, GU * 3 * 128], BF16) for i in range(2)]
    Vg = [P.sb(f"Vg{i}", [128, GU * 3 * 65], BF16) for i in range(2)]
    Og = [P.sb(f"Og{i}", [65, GU * 128], F32) for i in range(2)]
    Pt = [P.sb(f"Pt{i}", [128, 384], BF16) for i in range(3)]
    psS = [P.ps(f"psS{i}", [128, 512], F32) for i in range(4)]
    psO = [P.ps(f"psO{i}", [128, 512], F32) for i in range(3)]
    gi = 0
    ui = 0
    for sp_ in specs:
        nb = sp_["nb"]
        mk = P.sb("mask_" + sp_["nm"], [128, nb, 128], BF16)
        mkk = "mask_" + sp_["nm"]
        P.dma("sp", mk[:], sp_["M"], writes=[mkk])
        for g in range(sp_["ng"]):
            b2 = gi % 2
            gi += 1
            P.dma("sp", Qg[b2][:], sp_["Q"][g], writes=[f"Qg{b2}"])
            P.dma("sp", Kg[b2][:, 0:GU * nb * 128], sp_["K"][g], writes=[f"Kg{b2}"])
            P.dma("sp", Vg[b2][:, 0:GU * nb * 65], sp_["V"][g], writes=[f"Vg{b2}"])
            def emit_S(u, s3, p2):
                for b in range(nb):
                    ksl = slice((u * nb + b) * 128, (u * nb + b + 1) * 128)
                    P.op("pe", lambda q, b=b, ksl=ksl: q.matmul(
                        psS[s3][:, b * 128:(b + 1) * 128], lhsT=Kg[b2][:, ksl], rhs=Qg[b2][:, u * 128:(u + 1) * 128],
                        start=True, stop=True), reads=[f"Kg{b2}", f"Qg{b2}"], writes=[f"psS{s3}"])
                P.op("act", lambda q: q.activation(out=Pt[p2][:, 0:nb * 128], in_=psS[s3][:, 0:nb * 128], func=AF.Exp, scale=0.125),
                     reads=[f"psS{s3}"], writes=[f"Pt{p2}"])
                P.op("dve", lambda q: q.tensor_tensor(out=Pt[p2][:, 0:nb * 128], in0=Pt[p2][:, 0:nb * 128],
                                                      in1=mk[:].rearrange("p b q -> p (b q)"), op=ALU.mult),
                     reads=[f"Pt{p2}", mkk], writes=[f"Pt{p2}"])

            def emit_PV(u, s3, p2, so):
                for b in range(nb):
                    vsl = slice((u * nb + b) * 65, (u * nb + b + 1) * 65)
                    P.op("pe", lambda q, b=b, vsl=vsl: q.matmul(
                        psO[so][0:65, 0:128], lhsT=Vg[b2][:, vsl], rhs=Pt[p2][:, b * 128:(b + 1) * 128],
                        start=(b == 0), stop=(b == nb - 1)), reads=[f"Vg{b2}", f"Pt{p2}"], writes=[f"psO{so}"])
                P.op("act", lambda q: q.activation(out=Og[b2][:, u * 128:(u + 1) * 128], in_=psO[so][0:65, 0:128], func=AF.Copy),
                     reads=[f"psO{so}"], writes=[f"Og{b2}"])

            ids = []
            for u in range(GU):
                ids.append((u, ui % 4, ui % 3, ui % 3))
                ui += 1
            emit_S(*ids[0][:3])
            emit_S(*ids[1][:3])
            for j in range(GU):
                if j + 2 < GU:
                    emit_S(*ids[j + 2][:3])
                emit_PV(*ids[j])
            P.dma("sp", sp_["O"][g], Og[b2][:], reads=[f"Og{b2}"], is_output=True)
    return P.finish()


def _windows(Kseq, Vseq, nb, halo, ntile):
    L = Kseq.shape[1]
    W = nb * 128
    Kp = np.zeros((64, L + 2 * halo + 128), Kseq.dtype)
    Kp[:, halo:halo + L] = Kseq
    Vp = np.zeros((L + 2 * halo + 128, 65), Vseq.dtype)
    Vp[halo:halo + L, :64] = Vseq.T
    Vp[halo:halo + L, 64] = 1.0
    Kw = np.stack([Kp[:, 128 * j:128 * j + W] for j in range(ntile)], 0)
    Vw = np.stack([Vp[128 * j:128 * j + W] for j in range(ntile)], 0)
    return Kw, Vw


def _pack_units(Qu, Ku, Vu, nb):
    U = Qu.shape[0]
    ng = U // GU
    Q = Qu.reshape(ng, GU, 64, 128).transpose(0, 2, 1, 3).reshape(ng, 64, GU * 128)
    K = Ku.reshape(ng, GU, 64, nb * 128).transpose(0, 2, 1, 3).reshape(ng, 64, GU * nb * 128)
    V = Vu.reshape(ng, GU, nb, 128, 65).transpose(0, 3, 1, 2, 4).reshape(ng, 128, GU * nb * 65)
    return np.ascontiguousarray(Q), np.ascontiguousarray(K), np.ascontiguousarray(V)


def run_k2b(obf_full):
    mc, md = attn_masks()
    ident = np.eye(128, dtype=np.float32).astype(ml_dtypes.bfloat16)
    cQ = np.zeros((3, 4, 128, 64, 128), ml_dtypes.bfloat16)
    cK = np.zeros((3, 4, 128, 64, 256), ml_dtypes.bfloat16)
    cV = np.zeros((3, 4, 128, 256, 65), ml_dtypes.bfloat16)
    for p, (w, d) in enumerate(C_PAT):
        L = SEQ // d
        nt = L // 128
        for h in range(4):
            rows = slice(h * 64, (h + 1) * 64)
            Qs = obf_full["cq"][rows].reshape(64, L, d)
            Ks = obf_full["ck"][rows].reshape(64, L, d)
            Vs = obf_full["cv"][rows].reshape(64, L, d)
            for r in range(d):
                Kw, Vw = _windows(Ks[:, :, r], Vs[:, :, r], 2, 64, nt)
                cK[p, h, r * nt:(r + 1) * nt] = Kw
                cV[p, h, r * nt:(r + 1) * nt] = Vw
                cQ[p, h, r * nt:(r + 1) * nt] = Qs[:, :, r].reshape(64, nt, 128).transpose(1, 0, 2)
    dQ = np.zeros((4, 128, 64, 128), ml_dtypes.bfloat16)
    dK = np.zeros((4, 128, 64, 384), ml_dtypes.bfloat16)
    dV = np.zeros((4, 128, 384, 65), ml_dtypes.bfloat16)
    for h in range(4):
        kvh = h // 2
        Kw, Vw = _windows(obf_full["dk"][kvh * 64:(kvh + 1) * 64], obf_full["dv"][kvh * 64:(kvh + 1) * 64], 3, 128, 128)
        dK[h] = Kw
        dV[h] = Vw
        dQ[h] = obf_full["dq"][h * 64:(h + 1) * 64].reshape(64, 128, 128).transpose(1, 0, 2)
    in_maps = []
    for c in range(NCORES):
        ts = slice(16 * c, 16 * c + 16)
        q, k, v = _pack_units(cQ[:, :, ts].reshape(NUC, 64, 128), cK[:, :, ts].reshape(NUC, 64, 256), cV[:, :, ts].reshape(NUC, 256, 65), 2)
        q2, k2, v2 = _pack_units(dQ[:, ts].reshape(NUD, 64, 128), dK[:, ts].reshape(NUD, 64, 384), dV[:, ts].reshape(NUD, 384, 65), 3)
        in_maps.append({"cQ": q, "cK": k, "cV": v, "cM": mc, "dQ": q2, "dK": k2, "dV": v2, "dM": md, "ident_d": ident})
    res = run(("k2b",), build_k2b, in_maps)
    numC = np.zeros((3, 256, SEQ), np.float32)
    denC = np.zeros((3, 4, SEQ), np.float32)
    numD = np.zeros((256, SEQ), np.float32)
    denD = np.zeros((4, SEQ), np.float32)
    for c in range(NCORES):
        co = res[c]["cO"].reshape(NUC // GU, 65, GU, 128).transpose(0, 2, 1, 3).reshape(3, 4, 16, 65, 128)
        do = res[c]["dO"].reshape(NUD // GU, 65, GU, 128).transpose(0, 2, 1, 3).reshape(4, 16, 65, 128)
        for p, (w, d) in enumerate(C_PAT):
            L = SEQ // d
            nt = L // 128
            for h in range(4):
                nv = numC[p, h * 64:(h + 1) * 64].reshape(64, L, d)
                dv_ = denC[p, h].reshape(L, d)
                for tl in range(16):
                    tau = 16 * c + tl
                    r, j = tau // nt, tau % nt
                    nv[:, 128 * j:128 * j + 128, r] = co[p, h, tl, 0:64]
                    dv_[128 * j:128 * j + 128, r] = co[p, h, tl, 64]
        for h in range(4):
            for tl in range(16):
                tau = 16 * c + tl
                numD[h * 64:(h + 1) * 64, 128 * tau:128 * tau + 128] = do[h, tl, 0:64]
                denD[h, 128 * tau:128 * tau + 128] = do[h, tl, 64]
    return numC, denC, numD, denD


FB = 4


def build_k3(moe):
    P = Prog()
    T = TOK
    xT = P.dram_in("xT", [D_MODEL, T], F32)
    modc_d = P.dram_in("modc", [128, 48], F32)
    ofT = P.dram_in("ofT", [256, T], F32)
    obT = P.dram_in("obT", [256, T], F32)
    gaT = P.dram_in("gaT", [256, T], F32)
    uext = P.dram_in("uext", [256, T + 32], F32)
    numC = P.dram_in("numC", [3, 256, T], F32)
    denC = P.dram_in("denC", [3, 256, T], F32)
    numD = P.dram_in("numD", [256, T], F32)
    denD = P.dram_in("denD", [256, T], F32)
    smalls_d = P.dram_in("smalls", [128, 128], F32)
    blk64_d = P.dram_in("blk64", [128, 128], F32)
    identb_d = P.dram_in("identb", [128, 128], BF16)
    w_out = P.dram_in("w_out", [1024, 1024], F32)
    if moe:
        wr_d = P.dram_in("wr", [1024, 8], F32)
        w_up = P.dram_in("w_up", [N_EXPERTS, 1024, 2 * EXPERT_DIM], F32)
        w_down = P.dram_in("w_down", [N_EXPERTS, EXPERT_DIM, 1024], F32)
        sel_d = P.dram_in("sel", [8, 8 * 128], F32)
        identf_d = P.dram_in("identf", [128, 128], F32)
    else:
        w_up = P.dram_in("w_up", [1024, 2 * FFN_DIM], F32)
        w_down = P.dram_in("w_down", [FFN_DIM, 1024], F32)
    xo = P.dram_out("xo", [D_MODEL, T], F32)

    pp = PsumPool(P)
    sm = P.sb("sm", [128, 128], F32)
    P.dma("sp", sm[:], smalls_d, writes=["sm"])
    O_ANW, O_CW, O_CB, O_BNW, O_BNB, O_SINK, O_LNW, O_LNB = 0, 1, 63, 65, 67, 69, 71, 87
    modc = P.sb("modc", [128, 48], F32)
    P.dma("sp", modc[:], modc_d, writes=["modc"])
    blk64 = P.sb("blk64", [128, 128], F32)
    P.dma("sp", blk64[:], blk64_d, writes=["blk64"])
    ones_f = P.sb("ones_f", [128, 128], F32)
    P.op("pool", lambda q: q.memset(ones_f[:], 1.0), writes=["ones_f"])
    eps = P.sb("eps", [128, 2], F32)
    P.op("pool", lambda q: q.memset(eps[:, 0:1], LN_EPS), writes=["eps"])
    P.op("pool", lambda q: q.memset(eps[:, 1:2], RMS_EPS), reads=["eps"], writes=["eps"])
    dcol = P.sb("dcol", [128, 32], F32)
    P.op("dve", lambda q: q.tensor_scalar(out=dcol[:, 0:8], in0=modc[:, 16:24], scalar1=1.0, scalar2=None, op0=ALU.add),
         reads=["modc"], writes=["dcol"])
    P.op("dve", lambda q: q.tensor_scalar(out=dcol[:, 8:16], in0=modc[:, 32:40], scalar1=1.0, scalar2=None, op0=ALU.add),
         reads=["modc", "dcol"], writes=["dcol"])
    P.op("dve", lambda q: q.tensor_scalar(out=dcol[:, 16:24], in0=modc[:, 40:48], scalar1=1.0, scalar2=None, op0=ALU.add),
         reads=["modc", "dcol"], writes=["dcol"])
    P.op("act", lambda q: q.activation(out=dcol[:, 24:26], in_=sm[:, O_SINK:O_SINK + 2], func=AF.Exp),
         reads=["sm", "dcol"], writes=["dcol"])
    G1, SC2, G2, ESINK = 0, 8, 16, 24

    x = P.sb("x", [128, 8, T], F32)
    y = P.sb("y", [128, 8, T], BF16)
    wu = [P.sb(f"wu{i}", [128, 8192], BF16) for i in range(2)]
    wd = [P.sb(f"wd{i}", [128, FB, 1024], BF16) for i in range(2)]
    abuf = P.sb("abuf", [128, FB, T], BF16)
    S = [P.sb(f"S{i}", [128, T + 32], F32) for i in range(3)]
    NT = 5
    tmp = [P.sb(f"tmp{i}", [128, 512], F32) for i in range(NT)]
    lnm = P.sb("lnm", [128, 512], F32)
    lnv = P.sb("lnv", [128, 512], F32)
    tcnt = [0]

    def gett():
        i = tcnt[0] % NT
        tcnt[0] += 1
        return tmp[i], f"tmp{i}"

    def tsl(tg):
        return slice(tg * 512, (tg + 1) * 512)

    xk = lambda i, tg: f"x{i}_{tg}"
    yk = lambda i, tg: f"y{i}_{tg}"

    xT_v = xT.rearrange("(k p) t -> p k t", p=128)
    for k in range(8):
        P.dma("sp", x[:, k, :], xT_v[:, k, :], writes=[xk(k, tg) for tg in range(4)])
    wo_v = w_out.rearrange("(k p) e -> p k e", p=128)
    wo_s = wu[0][:].rearrange("p (k e) -> p k e", k=8)
    for k in range(8):
        P.dma("pool", wo_s[:, k, :], wo_v[:, k, :], writes=["wu0"])

    def ln_fm(srcs, nfeat, wcol, bcol, dst, silu=False):
        n = len(srcs)
        for tg in range(4):
            ps_s, ks_ = pp.get()
            ps_q, kq_ = pp.get()
            for i, (af, kf) in enumerate(srcs):
                P.op("pe", lambda q, af=af, i=i: q.matmul(ps_s[:, :], lhsT=ones_f[:], rhs=af(tg), start=(i == 0), stop=(i == n - 1)),
                     reads=["ones_f", kf(tg)], writes=[ks_])
            for i, (af, kf) in enumerate(srcs):
                sq, sqk = gett()
                P.op("act", lambda q, af=af, sq=sq: q.activation(out=sq[:], in_=af(tg), func=AF.Square), reads=[kf(tg)], writes=[sqk])
                P.op("pe", lambda q, sq=sq, i=i: q.matmul(ps_q[:, :], lhsT=ones_f[:], rhs=sq[:], start=(i == 0), stop=(i == n - 1)),
                     reads=["ones_f", sqk], writes=[kq_])
            mean, mk = lnm, "lnm"
            P.op("act", lambda q: q.activation(out=mean[:], in_=ps_s[:], func=AF.Copy, scale=1.0 / nfeat), reads=[ks_], writes=[mk])
            var, vk = lnv, "lnv"
            P.op("dve", lambda q: q.tensor_tensor(out=var[:], in0=mean[:], in1=mean[:], op=ALU.mult), reads=[mk], writes=[vk])
            P.op("dve", lambda q: q.scalar_tensor_tensor(out=var[:], in0=ps_q[:], scalar=1.0 / nfeat, in1=var[:], op0=ALU.mult,
                                                         op1=ALU.subtract), reads=[kq_, vk], writes=[vk])
            P.op("act", lambda q: q.activation(out=var[:], in_=var[:], func=AF.Sqrt, bias=eps[:, 0:1], scale=1.0), reads=[vk, "eps"], writes=[vk])
            P.op("dve", lambda q: q.reciprocal(out=var[:], in_=var[:]), reads=[vk], writes=[vk])
            for i, ((af, kf), (df, dkf)) in enumerate(zip(srcs, dst)):
                t, tk = gett()
                P.op("dve", lambda q, af=af, t=t: q.tensor_tensor(out=t[:], in0=af(tg), in1=mean[:], op=ALU.subtract),
                     reads=[kf(tg), mk], writes=[tk])
                P.op("dve", lambda q, t=t: q.tensor_tensor(out=t[:], in0=t[:], in1=var[:], op=ALU.mult), reads=[tk, vk], writes=[tk])
                if silu:
                    P.op("act", lambda q, t=t, df=df, i=i: q.activation(out=df(tg), in_=t[:], func=AF.Silu, scale=wcol(i), bias=bcol(i)),
                         reads=[tk, "sm"], writes=[dkf(tg)])
                else:
                    P.op("act", lambda q, t=t, df=df, i=i: q.activation(out=df(tg), in_=t[:], func=AF.Identity, scale=wcol(i), bias=bcol(i)),
                         reads=[tk, "sm"], writes=[dkf(tg)])

    wu1f = wu[1][:].bitcast(F32)
    cpool = [wu1f[:, i_ * 512:(i_ + 1) * 512] for i_ in range(8)]
    ccnt = [0]

    class _CT:
        def __init__(self, ap):
            self.ap = ap

        def __getitem__(self, idx):
            return self.ap

    def getc():
        i_ = ccnt[0] % 8
        ccnt[0] += 1
        return _CT(cpool[i_]), f"ctmp{i_}"

    def stream_A():
        for a in range(2):
            rows = slice(a * 128, (a + 1) * 128)
            for tg in range(4):
                t0, k0 = gett()
                t1, k1 = gett()
                t2, k2 = gett()
                yield P.dma("sp", t0[:], ofT[rows, tsl(tg)], writes=[k0])
                yield P.dma("sp", t1[:], obT[rows, tsl(tg)], writes=[k1])
                yield P.dma("sp", t2[:], gaT[rows, tsl(tg)], writes=[k2])
                yield P.op("dve", lambda q, t0=t0, t1=t1: q.tensor_tensor(out=t0[:], in0=t0[:], in1=t1[:], op=ALU.add), reads=[k0, k1], writes=[k0])
                yield P.op("act", lambda q, t0=t0, t1=t1: q.activation(out=t1[:], in_=t0[:], func=AF.Square), reads=[k0], writes=[k1])
                ps, pk = pp.t[0], pp.k[0]
                yield P.op("pe", lambda q, ps=ps, t1=t1: q.matmul(ps[:, :], lhsT=blk64[:], rhs=t1[:], start=True, stop=True), reads=["blk64", k1], writes=[pk])
                yield P.op("act", lambda q, ps=ps, t1=t1: q.activation(out=t1[:], in_=ps[:], func=AF.Sqrt, bias=eps[:, 1:2], scale=1.0 / 64),
                     reads=[pk, "eps"], writes=[k1])
                yield P.op("dve", lambda q, t1=t1: q.reciprocal(out=t1[:], in_=t1[:]), reads=[k1], writes=[k1])
                yield P.op("dve", lambda q, t0=t0, t1=t1: q.scalar_tensor_tensor(out=t0[:], in0=t0[:], scalar=sm[:, O_ANW:O_ANW + 1], in1=t1[:],
                                                                         op0=ALU.mult, op1=ALU.mult), reads=[k0, k1, "sm"], writes=[k0])
                yield P.op("dve", lambda q, t0=t0, t2=t2, a=a, tg=tg: q.tensor_tensor(out=y[:, a, tsl(tg)], in0=t0[:], in1=t2[:], op=ALU.mult),
                     reads=[k0, k2], writes=[yk(a, tg)])


        yield None

    def stream_conv():
        identb = wd[1][:, 1, 0:128]
        yield P.dma("sp", identb, identb_d, writes=["identb"])
        dg = [wd[1][:, 0, i * 128:(i + 1) * 128] for i in range(4)]
        ubf = abuf[:].rearrange("p f t -> p (f t)")[:, 0:T + 32]
        for b in range(2):
            ub, ubk = S[0], "S0"
            acc, acck = S[1 + b], f"S{1 + b}"
            yield P.dma("sp", ub[:], uext[b * 128:(b + 1) * 128, :], writes=[ubk])
            yield P.op("act", lambda q: q.activation(out=ubf, in_=ub[:], func=AF.Copy), reads=[ubk], writes=["ubf"])
            pss = [(pp.t[1 + i_], pp.k[1 + i_]) for i_ in range(4)]
            for j in range(B_KERNEL):
                d_, dk_ = dg[j % 4], f"dg{j % 4}"
                yield P.op("dve", lambda q, d_=d_, j=j, b=b: q.tensor_scalar(out=d_, in0=identb, scalar1=sm[:, O_CW + b * 31 + j:O_CW + b * 31 + j + 1],
                                                                     scalar2=None, op0=ALU.mult), reads=["identb", "sm"], writes=[dk_])
                for tg in range(4):
                    ps, pk = pss[tg]
                    yield P.op("pe", lambda q, ps=ps, d_=d_, j=j, tg=tg: q.matmul(ps[:, :], lhsT=d_, rhs=ubf[:, tg * 512 + j:tg * 512 + j + 512],
                                                                            start=(j == 0), stop=(j == B_KERNEL - 1)),
                         reads=[dk_, "ubf"], writes=[pk])
            for tg in range(4):
                ps, pk = pss[tg]
                yield P.op("act", lambda q, ps=ps, acc=acc, tg=tg, b=b: q.activation(out=acc[:, tsl(tg)], in_=ps[:], func=AF.Identity,
                                                                             bias=sm[:, O_CB + b:O_CB + b + 1], scale=1.0),
                     reads=[pk, "sm"], writes=[acck])

        yield None

    def stream_CD():
        for a in range(2):
            rows = slice(a * 128, (a + 1) * 128)
            for tg in range(4):
                n0, kn0 = getc()
                d0, kd0 = getc()
                yield P.dma("sp", n0[:], numC[0, rows, tsl(tg)], writes=[kn0])
                yield P.dma("sp", d0[:], denC[0, rows, tsl(tg)], writes=[kd0])
                n1, kn1 = getc()
                d1, kd1 = getc()
                for p in (1, 2):
                    yield P.dma("sp", n1[:], numC[p, rows, tsl(tg)], writes=[kn1])
                    yield P.dma("sp", d1[:], denC[p, rows, tsl(tg)], writes=[kd1])
                    yield P.op("dve", lambda q, n0=n0, n1=n1: q.tensor_tensor(out=n0[:], in0=n0[:], in1=n1[:], op=ALU.add), reads=[kn0, kn1], writes=[kn0])
                    yield P.op("dve", lambda q, d0=d0, d1=d1: q.tensor_tensor(out=d0[:], in0=d0[:], in1=d1[:], op=ALU.add), reads=[kd0, kd1], writes=[kd0])
                yield P.op("dve", lambda q, d0=d0: q.reciprocal(out=d0[:], in_=d0[:]), reads=[kd0], writes=[kd0])
                yield P.op("dve", lambda q, n0=n0, d0=d0, a=a, tg=tg: q.tensor_tensor(out=y[:, 4 + a, tsl(tg)], in0=n0[:], in1=d0[:], op=ALU.mult),
                     reads=[kn0, kd0], writes=[yk(4 + a, tg)])
                n0, kn0 = getc()
                d0, kd0 = getc()
                yield P.dma("sp", n0[:], numD[rows, tsl(tg)], writes=[kn0])
                yield P.dma("sp", d0[:], denD[rows, tsl(tg)], writes=[kd0])
                yield P.op("dve", lambda q, d0=d0, a=a: q.tensor_scalar(out=d0[:], in0=d0[:], scalar1=dcol[:, ESINK + a:ESINK + a + 1], scalar2=None,
                                                                op0=ALU.add), reads=[kd0, "dcol"], writes=[kd0])
                yield P.op("dve", lambda q, d0=d0: q.reciprocal(out=d0[:], in_=d0[:]), reads=[kd0], writes=[kd0])
                yield P.op("dve", lambda q, n0=n0, d0=d0, a=a, tg=tg: q.tensor_tensor(out=y[:, 6 + a, tsl(tg)], in0=n0[:], in1=d0[:], op=ALU.mult),
                     reads=[kn0, kd0], writes=[yk(6 + a, tg)])


        yield None

    def drain(*gens):
        gens = list(gens)
        while gens:
            for g in list(gens):
                try:
                    next(g)
                except StopIteration:
                    gens.remove(g)

    drain(stream_A(), stream_conv(), stream_CD())
    ln_fm([(lambda tg, b=b: S[1 + b][:, tsl(tg)], lambda tg, b=b: f"S{1 + b}") for b in range(2)], 256,
          lambda i: sm[:, O_BNW + i:O_BNW + i + 1], lambda i: sm[:, O_BNB + i:O_BNB + i + 1],
          [(lambda tg, b=b: y[:, 2 + b, tsl(tg)], lambda tg, b=b: yk(2 + b, tg)) for b in range(2)], silu=True)

    for k in range(8):
        for tg in range(4):
            P.op("act", lambda q, k=k, tg=tg: q.activation(out=x[:, k, tsl(tg)], in_=x[:, k, tsl(tg)], func=AF.Copy, scale=DEEPNORM_ALPHA),
                 reads=[xk(k, tg)], writes=[xk(k, tg)])
    for dc in range(8):
        for tg in range(4):
            ps, pk = pp.get()
            for k in range(8):
                P.op("pe", lambda q, ps=ps, k=k, dc=dc, tg=tg: q.matmul(ps[:, :], lhsT=wo_s[:, k, dc * 128:(dc + 1) * 128], rhs=y[:, k, tsl(tg)],
                                                                        start=(k == 0), stop=(k == 7)), reads=["wu0", yk(k, tg)], writes=[pk])
            P.op("dve", lambda q, ps=ps, dc=dc, tg=tg: q.scalar_tensor_tensor(out=x[:, dc, tsl(tg)], in0=ps[:], scalar=dcol[:, G1 + dc:G1 + dc + 1],
                                                                             in1=x[:, dc, tsl(tg)], op0=ALU.mult, op1=ALU.add),
                 reads=[pk, "dcol", xk(dc, tg)], writes=[xk(dc, tg)])
    xs = [(lambda tg, k=k: x[:, k, tsl(tg)], lambda tg, k=k: xk(k, tg)) for k in range(8)]
    ln_fm(xs, 1024, lambda i: sm[:, O_LNW + i:O_LNW + i + 1], lambda i: sm[:, O_LNB + i:O_LNB + i + 1], xs)

    for k in range(8):
        for tg in range(4):
            P.op("dve", lambda q, k=k, tg=tg: q.tensor_scalar(out=y[:, k, tsl(tg)], in0=x[:, k, tsl(tg)], scalar1=dcol[:, SC2 + k:SC2 + k + 1],
                                                            scalar2=modc[:, 24 + k:25 + k], op0=ALU.mult, op1=ALU.add),
                 reads=[xk(k, tg), "dcol", "modc"], writes=[yk(k, tg)])

    gT = None
    if moe:
        wr = P.sb("wr", [128, 8, 8], F32)
        P.dma("sp", wr[:], wr_d.rearrange("(k p) e -> p k e", p=128), writes=["wr"])
        sel = P.sb("sel", [8, 8 * 128], F32)
        P.dma("sp", sel[:], sel_d, writes=["sel"])
        identf = P.sb("identf", [128, 128], F32)
        P.dma("sp", identf[:], identf_d, writes=["identf"])
        psl, pslk = pp.get()
        for k in range(8):
            h2f, hk = S[0], "S0"
            P.op("dve", lambda q, k=k: q.tensor_scalar(out=h2f[:, 0:T], in0=x[:, k, :], scalar1=dcol[:, SC2 + k:SC2 + k + 1],
                                                       scalar2=modc[:, 24 + k:25 + k], op0=ALU.mult, op1=ALU.add),
                 reads=[xk(k, tg) for tg in range(4)] + ["dcol", "modc"], writes=[hk])
            for ti in range(16):
                P.op("pe", lambda q, k=k, ti=ti: q.matmul(psl[:, ti * 8:(ti + 1) * 8], lhsT=h2f[:, ti * 128:(ti + 1) * 128], rhs=wr[:, k, :],
                                                          start=(k == 0 and ti == 0), stop=(k == 7 and ti == 15), skip_group_check=True),
                     reads=[hk, "wr"], writes=[pslk])
        lg = P.sb("lg", [128, 16, 8], F32)
        lg2 = P.sb("lg2", [128, 16, 8], F32)
        eq1 = S[1][:, 0:128].rearrange("p (t e) -> p t e", e=8)
        eq2 = S[1][:, 128:256].rearrange("p (t e) -> p t e", e=8)
        m1 = P.sb("m1", [128, 16], F32)
        m2 = P.sb("m2", [128, 16], F32)
        g1 = P.sb("g1", [128, 16], F32)
        bc3 = lambda t: t[:].unsqueeze(2).broadcast_to([128, 16, 8])
        P.op("dve", lambda q: q.tensor_copy(out=lg[:], in_=psl[:, 0:128].rearrange("p (t e) -> p t e", e=8)), reads=[pslk], writes=["lg"])
        P.op("dve", lambda q: q.tensor_reduce(out=m1[:], in_=lg[:], axis=AX.X, op=ALU.max), reads=["lg"], writes=["m1"])
        P.op("dve", lambda q: q.tensor_tensor(out=eq1, in0=lg[:], in1=bc3(m1), op=ALU.is_equal), reads=["lg", "m1"], writes=["eq1"])
        P.op("dve", lambda q: q.scalar_tensor_tensor(out=lg2[:], in0=eq1, scalar=-1e30, in1=lg[:], op0=ALU.mult, op1=ALU.add),
             reads=["eq1", "lg"], writes=["lg2"])
        P.op("dve", lambda q: q.tensor_reduce(out=m2[:], in_=lg2[:], axis=AX.X, op=ALU.max), reads=["lg2"], writes=["m2"])
        P.op("dve", lambda q: q.tensor_tensor(out=eq2, in0=lg2[:], in1=bc3(m2), op=ALU.is_equal), reads=["lg2", "m2"], writes=["eq2"])
        P.op("dve", lambda q: q.tensor_tensor(out=m2[:], in0=m2[:], in1=m1[:], op=ALU.subtract), reads=["m2", "m1"], writes=["m2"])
        P.op("act", lambda q: q.activation(out=m2[:], in_=m2[:], func=AF.Exp), reads=["m2"], writes=["m2"])
        P.op("dve", lambda q: q.tensor_scalar(out=g1[:], in0=m2[:], scalar1=1.0, scalar2=None, op0=ALU.add), reads=["m2"], writes=["g1"])
        P.op("dve", lambda q: q.reciprocal(out=g1[:], in_=g1[:]), reads=["g1"], writes=["g1"])
        P.op("dve", lambda q: q.tensor_tensor(out=m2[:], in0=m2[:], in1=g1[:], op=ALU.mult), reads=["m2", "g1"], writes=["m2"])
        P.op("dve", lambda q: q.tensor_tensor(out=eq1, in0=eq1, in1=bc3(g1), op=ALU.mult), reads=["eq1", "g1"], writes=["eq1"])
        P.op("dve", lambda q: q.tensor_tensor(out=eq2, in0=eq2, in1=bc3(m2), op=ALU.mult), reads=["eq2", "m2"], writes=["eq2"])
        P.op("dve", lambda q: q.tensor_tensor(out=eq1, in0=eq1, in1=eq2, op=ALU.add), reads=["eq1", "eq2"], writes=["eq1"])
        gT = S[0][0:8, 0:T]
        for tg in range(4):
            ps, pk = pp.get()
            for j in range(4):
                ti = tg * 4 + j
                P.op("pe", lambda q, ps=ps, j=j, ti=ti: q.transpose(ps[0:8, j * 128:(j + 1) * 128], eq1[:, ti, :], identf[:]),
                     reads=["eq1", "identf"], writes=[pk])
            P.op("act", lambda q, ps=ps, tg=tg: q.activation(out=gT[:, tsl(tg)], in_=ps[0:8, :], func=AF.Copy), reads=[pk], writes=["S0"])

    for k in range(8):
        for tg in range(4):
            P.op("act", lambda q, k=k, tg=tg: q.activation(out=x[:, k, tsl(tg)], in_=x[:, k, tsl(tg)], func=AF.Copy, scale=DEEPNORM_ALPHA),
                 reads=[xk(k, tg)], writes=[xk(k, tg)])

    blocks = []
    if moe:
        for e in range(N_EXPERTS):
            for (f0, nf) in chunks(EXPERT_DIM // 128, FB):
                blocks.append((w_up[e], w_down[e], EXPERT_DIM, f0, nf, e))
    else:
        for (f0, nf) in chunks(FFN_DIM // 128, FB):
            blocks.append((w_up, w_down, FFN_DIM, f0, nf, None))

    stg = [S[2][:, i * 512:(i + 1) * 512] for i in range(4)]
    stg_n = [0]

    def block_pieces(bi):
        wup, wdn, F, f0, nf, e = blocks[bi]
        b2 = bi % 2
        wuv = wu[b2][:].rearrange("p (k s c) -> p k s c", k=8, s=2)
        upv = wup.rearrange("(k p) c -> p k c", p=128)
        pcs = []
        for k in range(8):
            for s_ in range(2):
                pcs.append((wuv[:, k, s_, 0:nf * 128], upv[:, k, s_ * F + f0 * 128:s_ * F + (f0 + nf) * 128], nf * 128, f"wu{b2}"))
        for fc in range(nf):
            for hf in range(2):
                pcs.append((wd[b2][:, fc, hf * 512:(hf + 1) * 512],
                            wdn[(f0 + fc) * 128:(f0 + fc + 1) * 128, hf * 512:(hf + 1) * 512], 512, f"wd{b2}"))
        return pcs

    def emit_piece(pc):
        dst, src, n, dkey = pc
        i = stg_n[0] % 4
        first = stg_n[0] < 4
        stg_n[0] += 1
        P.dma("sp", stg[i][:, 0:n], src, writes=(["S2", f"stg{i}"] if first else [f"stg{i}"]))
        P.op("act", lambda q: q.activation(out=dst, in_=stg[i][:, 0:n], func=AF.Copy), reads=[f"stg{i}"], writes=[dkey])

    for pc in block_pieces(0):
        emit_piece(pc)
    for bi, (wup, wdn, F, f0, nf, e) in enumerate(blocks):
        b2 = bi % 2
        nxt = block_pieces(bi + 1) if bi + 1 < len(blocks) else []
        per_it = -(-len(nxt) // (nf * 4)) if nxt else 0
        gmul = None
        gk = None
        if moe:
            gmul, gk = S[1], "S1"
            if f0 == 0:
                for tg in range(4):
                    ps, pk = pp.get()
                    P.op("pe", lambda q, ps=ps, e=e, tg=tg: q.matmul(ps[:, :], lhsT=sel[:, e * 128:(e + 1) * 128], rhs=gT[:, tsl(tg)],
                                                                    start=True, stop=True), reads=["sel", "S0"], writes=[pk])
                    P.op("act", lambda q, ps=ps, gmul=gmul, tg=tg: q.activation(out=gmul[:, tsl(tg)], in_=ps[:], func=AF.Copy),
                         reads=[pk], writes=[gk + f"_{tg}"])
        wuv = wu[b2][:].rearrange("p (k s c) -> p k s c", k=8, s=2)
        for fc in range(nf):
            for tg in range(4):
                psg, kg = pp.get()
                psu, ku = pp.get()
                for s_, ps_, pk_ in ((0, psg, kg), (1, psu, ku)):
                    for k in range(8):
                        P.op("pe", lambda q, ps_=ps_, k=k, s_=s_, fc=fc, tg=tg: q.matmul(ps_[:, :], lhsT=wuv[:, k, s_, fc * 128:(fc + 1) * 128],
                                                                                      rhs=y[:, k, tsl(tg)], start=(k == 0), stop=(k == 7)),
                             reads=[f"wu{b2}", yk(k, tg)], writes=[pk_])
                sg, sgk = gett()
                P.op("act", lambda q, sg=sg, psg=psg: q.activation(out=sg[:], in_=psg[:], func=AF.Silu), reads=[kg], writes=[sgk])
                if gmul is not None:
                    P.op("pool", lambda q, sg=sg, gmul=gmul, tg=tg: q.tensor_tensor(out=sg[:], in0=sg[:], in1=gmul[:, tsl(tg)], op=ALU.mult),
                         reads=[sgk, gk + f"_{tg}"], writes=[sgk])
                P.op("dve", lambda q, sg=sg, psu=psu, fc=fc, tg=tg: q.tensor_tensor(out=abuf[:, fc, tsl(tg)], in0=sg[:], in1=psu[:], op=ALU.mult),
                     reads=[sgk, ku], writes=[f"a{fc}_{tg}"])
                for _ in range(per_it):
                    if nxt:
                        emit_piece(nxt.pop(0))
        while nxt:
            emit_piece(nxt.pop(0))
        for dc in range(8):
            for tg in range(4):
                ps, pk = pp.get()
                for fc in range(nf):
                    P.op("pe", lambda q, ps=ps, fc=fc, dc=dc, tg=tg: q.matmul(ps[:, :], lhsT=wd[b2][:, fc, dc * 128:(dc + 1) * 128],
                                                                              rhs=abuf[:, fc, tsl(tg)], start=(fc == 0), stop=(fc == nf - 1)),
                         reads=[f"wd{b2}", f"a{fc}_{tg}"], writes=[pk])
                P.op("dve", lambda q, ps=ps, dc=dc, tg=tg: q.scalar_tensor_tensor(out=x[:, dc, tsl(tg)], in0=ps[:], scalar=dcol[:, G2 + dc:G2 + dc + 1],
                                                                                 in1=x[:, dc, tsl(tg)], op0=ALU.mult, op1=ALU.add),
                     reads=[pk, "dcol", xk(dc, tg)], writes=[xk(dc, tg)])

    ln_fm(xs, 1024, lambda i: sm[:, O_LNW + 8 + i:O_LNW + 9 + i], lambda i: sm[:, O_LNB + 8 + i:O_LNB + 9 + i], xs)
    xo_v = xo.rearrange("(k p) t -> p k t", p=128)
    for k in range(8):
        P.dma("sp", xo_v[:, k, :], x[:, k, :], reads=[xk(k, tg) for tg in range(4)], is_output=True)
    return P.finish()


def run_k3(layer, xT_shards, mods, of, ob, o32_full, numC, denC, numD, denD, inp):
    moe = (layer % 2 == 1)
    T = TOK
    sm = np.zeros((128, 128), np.float32)
    anw = np.asarray(inp["a_norm_w"])[layer]
    sm[:, 0] = np.concatenate([anw, anw])
    cw = np.asarray(inp["b_conv_w"])[layer]
    for b in range(2):
        sm[:, 1 + b * 31:1 + (b + 1) * 31] = cw[:, b * 128:(b + 1) * 128].T
    sm[:, 63:65] = col128(np.asarray(inp["b_conv_b"])[layer])
    sm[:, 65:67] = col128(np.asarray(inp["b_norm_w"])[layer])
    sm[:, 67:69] = col128(np.asarray(inp["b_norm_b"])[layer])
    sink = np.asarray(inp["d_sink"])[layer]
    sm[:, 69:71] = col128(np.repeat(sink, 64))
    sm[:, 71:79] = col128(np.asarray(inp["ln_w"])[layer, 0])
    sm[:, 79:87] = col128(np.asarray(inp["ln_w"])[layer, 1])
    sm[:, 87:95] = col128(np.asarray(inp["ln_b"])[layer, 0])
    sm[:, 95:103] = col128(np.asarray(inp["ln_b"])[layer, 1])
    blk64 = np.kron(np.eye(2, dtype=np.float32), np.ones((64, 64), np.float32))
    u = o32_full["u"]
    upad = np.zeros((256, SEQ + 32), np.float32)
    upad[:, 15:15 + SEQ] = u
    denC_rep = np.repeat(denC, 64, axis=1)
    denD_rep = np.repeat(denD, 64, axis=0)
    w_out = np.ascontiguousarray(inp["w_out"][layer])
    common = {"smalls": sm, "blk64": blk64, "w_out": w_out, "identb": np.eye(128, dtype=np.float32).astype(ml_dtypes.bfloat16)}
    if moe:
        li = layer // 2
        sel = np.zeros((8, 8 * 128), np.float32)
        for e in range(8):
            sel[e, e * 128:(e + 1) * 128] = 1.0
        common.update({"wr": np.ascontiguousarray(inp["moe_router"][li]), "w_up": np.ascontiguousarray(inp["moe_w_up"][li]),
                       "w_down": np.ascontiguousarray(inp["moe_w_down"][li]), "sel": sel, "identf": np.eye(128, dtype=np.float32)})
    else:
        li = layer // 2
        common.update({"w_up": np.ascontiguousarray(inp["ffn_w_up"][li]), "w_down": np.ascontiguousarray(inp["ffn_w_down"][li])})
    in_maps = []
    for c in range(NCORES):
        ts = slice(c * T, (c + 1) * T)
        m = dict(common)
        m.update({"xT": xT_shards[c], "modc": mods[c], "ofT": np.ascontiguousarray(of[:, ts]), "obT": np.ascontiguousarray(ob[:, ts]),
                  "gaT": np.ascontiguousarray(o32_full["ga"][:, ts]), "uext": np.ascontiguousarray(upad[:, c * T:c * T + T + 32]),
                  "numC": np.ascontiguousarray(numC[:, :, ts]), "denC": np.ascontiguousarray(denC_rep[:, :, ts]),
                  "numD": np.ascontiguousarray(numD[:, ts]), "denD": np.ascontiguousarray(denD_rep[:, ts])})
        in_maps.append(m)
    res = run(("k3", moe), lambda: build_k3(moe), in_maps)
    return [r["xo"] for r in res]


def run_layer(layer, xT_shards, inp, modc=None):
    if modc is None:
        modc = run_k0(inp)[layer]
    r1 = run_k1(layer, xT_shards, modc, inp)
    o32_full = {nm: np.concatenate([r["o32"][off:off + 256] for r in r1], axis=1) for nm, off in O32.items()}
    obf_full = {nm: np.concatenate([r["obf"][off:off + (128 if nm in ("dk", "dv") else 256)] for r in r1], axis=1)
                for nm, off in OBF.items()}
    mods = [modc for _ in range(NCORES)]
    of, ob = run_k2a(o32_full)
    numC, denC, numD, denD = run_k2b(obf_full)
    return run_k3(layer, xT_shards, mods, of, ob, o32_full, numC, denC, numD, denD, inp)


def kernel(**inp):
    inp = {k: np.asarray(v) for k, v in inp.items()}
    x = inp["x"][0]
    xT_shards = [np.ascontiguousarray(x[c * TOK:(c + 1) * TOK].T) for c in range(NCORES)]
    modcs = run_k0(inp)
    for layer in range(DEPTH):
        xT_shards = run_layer(layer, xT_shards, inp, modcs[layer])
    out = np.concatenate([s.T for s in xT_shards], axis=0)[None]
    return np.ascontiguousarray(out.astype(np.float32))
```

```python
import math
from contextlib import ExitStack

import numpy as np
import ml_dtypes

import concourse.bass as bass
import concourse.mybir as mybir
from concourse.bass_utils import run_bass_kernel_spmd

F32 = mybir.dt.float32
BF16 = mybir.dt.bfloat16
I32 = mybir.dt.int32
AF = mybir.ActivationFunctionType
ALU = mybir.AluOpType
AX = mybir.AxisListType

NCORES = 8
D_MODEL = 1024
SEQ = 16384
TOK = SEQ // NCORES
DEPTH = 2
HD = 64
FFN_DIM = 2816
N_EXPERTS = 8
EXPERT_DIM = 3584
B_KERNEL = 31
ROPE_THETA = 500000.0
ROPE_DIM = 16
DEEPNORM_ALPHA = (2 * DEPTH) ** 0.25
LN_EPS = 1e-5
RMS_EPS = 1e-6
NEG = -30000.0
TWO_PI = 2.0 * math.pi


class Prog:
    NDS = 24

    def __init__(self):
        self.nc = bass.Bass("TRN2", target_bir_lowering=False)
        nc = self.nc
        self.es = ExitStack()
        self.q = {"pe": nc.tensor, "dve": nc.vector, "act": nc.scalar, "pool": nc.gpsimd, "sp": nc.sync}
        self.esem = {e: self.es.enter_context(nc.semaphore("es_" + e)) for e in ("pe", "dve", "act", "pool")}
        self.ecnt = {e: 0 for e in self.esem}
        self.dsem = [self.es.enter_context(nc.semaphore(f"ds{i}")) for i in range(self.NDS)]
        self.dval = [0] * self.NDS
        self.dnext = 0
        self.seen = {e: {} for e in self.q}
        self.lastw = {}
        self.readers = {}
        self.out_tokens = []
        self.n_inst = 0
        self._ps_id = 0

    def dram_in(self, name, shape, dt):
        return self.nc.dram_tensor(name, list(shape), dt, kind="ExternalInput").ap()

    def dram_out(self, name, shape, dt):
        return self.nc.dram_tensor(name, list(shape), dt, kind="ExternalOutput").ap()

    def sb(self, name, shape, dt):
        return self.es.enter_context(self.nc.sbuf_tensor("sb_" + name, list(shape), dt))

    def ps(self, name, shape, dt=F32):
        return self.es.enter_context(self.nc.psum_tensor("pm_" + name, list(shape), dt))

    def _wait(self, e, tok):
        sem, v, owner = tok
        if owner == e and e == "pe":
            return
        k = id(sem)
        if self.seen[e].get(k, 0) >= v:
            return
        self.q[e].wait_ge(sem, v)
        self.seen[e][k] = v

    def _deps(self, e, reads, writes):
        for k in reads:
            t = self.lastw.get(k)
            if t is not None:
                self._wait(e, t)
        for k in writes:
            t = self.lastw.get(k)
            if t is not None:
                self._wait(e, t)
            for t in self.readers.get(k, {}).values():
                self._wait(e, t)

    def _record(self, tok, reads, writes):
        for k in writes:
            self.lastw[k] = tok
            self.readers[k] = {}
        for k in reads:
            self.readers.setdefault(k, {})[id(tok[0])] = tok

    def op(self, e, fn, reads=(), writes=()):
        self._deps(e, reads, writes)
        inst = fn(self.q[e])
        self.ecnt[e] += 1
        inst.then_inc(self.esem[e], 1)
        tok = (self.esem[e], self.ecnt[e], e)
        self._record(tok, reads, writes)
        self.n_inst += 1
        return tok

    def dma(self, e, out, in_, reads=(), writes=(), is_output=False, **kw):
        self._deps(e, reads, writes)
        j = self.dnext
        self.dnext = (self.dnext + 1) % self.NDS
        if self.dval[j] > 0:
            self._wait(e, (self.dsem[j], self.dval[j], "dma"))
        self.q[e].dma_start(out=out, in_=in_, **kw).then_inc(self.dsem[j], 16)
        self.dval[j] += 16
        tok = (self.dsem[j], self.dval[j], "dma")
        self._record(tok, reads, writes)
        if is_output:
            self.out_tokens.append(tok)
        self.n_inst += 1
        return tok

    def finish(self):
        for j in range(self.NDS):
            if self.dval[j] > 0:
                self._wait("sp", (self.dsem[j], self.dval[j], "dma"))
        return self.nc


def chunks(n, c):
    return [(i, min(c, n - i)) for i in range(0, n, c)]


class PsumPool:
    def __init__(self, P, n=8, prefix="pb"):
        self.t = [P.ps(f"{prefix}{i}", [128, 512], F32) for i in range(n)]
        self.k = [f"{prefix}{i}" for i in range(n)]
        self.i = 0
        self.n = n

    def get(self):
        i = self.i
        self.i = (self.i + 1) % self.n
        return self.t[i], self.k[i]


def build_consts(P):
    c = {}
    c["ones_f"] = P.sb("ones_f", [128, 512], F32)
    P.op("pool", lambda q: q.memset(c["ones_f"][:], 1.0), writes=["ones_f"])
    c["ones_b"] = P.sb("ones_b", [128, 512], BF16)
    P.op("pool", lambda q: q.memset(c["ones_b"][:], 1.0), writes=["ones_b"])
    return c


CH = {"aq": (0, 2), "aff": (2, 2), "afb": (4, 2), "ai": (6, 2), "ag": (8, 2), "bv": (10, 2), "bg": (12, 2),
      "cq": (14, 2), "ck": (16, 2), "cv": (18, 2), "dq": (20, 2), "dk": (22, 1), "dv": (23, 1)}
ROT_CHUNKS = [14, 15, 16, 17, 20, 21, 22]
O32 = {"qa": 0, "kf": 256, "lf": 512, "kb": 768, "lb": 1024, "va": 1280, "ga": 1536, "u": 1792}
OBF = {"cq": 0, "ck": 256, "cv": 512, "dq": 768, "dk": 1024, "dv": 1152}
NBF = 1280


def build_k1(layer):
    P = Prog()
    nc = P.nc
    T = TOK
    xT = P.dram_in("xT", [D_MODEL, T], F32)
    modc_in = P.dram_in("modc", [128, 48], F32)
    w_in = P.dram_in("w_in", [D_MODEL, 3072], F32)
    w_sw = P.dram_in("w_sw", [D_MODEL, 7 * 128], F32)
    pos = P.dram_in("pos", [T], I32)
    rcol = P.dram_in("rcol", [128, 2], F32)
    alb = P.dram_in("alb", [128, 4], F32)
    o32 = P.dram_out("o32", [2048, T], F32)
    obf = P.dram_out("obf", [NBF, T], BF16)

    C = build_consts(P)
    pp = PsumPool(P)

    mod = P.sb("mod", [128, 48], F32)
    P.dma("sp", mod[:], modc_in, writes=["mod"])
    sc1 = P.sb("sc1", [128, 8], F32)
    P.op("dve", lambda q: q.tensor_scalar(out=sc1[:], in0=mod[:, 8:16], scalar1=1.0, scalar2=None, op0=ALU.add),
         reads=["mod"], writes=["sc1"])

    hT = P.sb("hT", [128, 8, T], BF16)
    xst = [P.sb(f"xst{i}", [128, T], F32) for i in range(2)]
    xT_v = xT.rearrange("(k p) t -> p k t", p=128)
    for k in range(8):
        st = xst[k % 2]
        sk = f"xst{k % 2}"
        P.dma("sp", st[:], xT_v[:, k, :], writes=[sk])
        P.op("dve", lambda q, k=k, st=st: q.tensor_scalar(out=hT[:, k, :], in0=st[:], scalar1=sc1[:, k:k + 1],
                                                        scalar2=mod[:, k:k + 1], op0=ALU.mult, op1=ALU.add),
             reads=[sk, "sc1", "mod"], writes=[f"hT{k}"])
    hkeys = [f"hT{k}" for k in range(8)]

    wb = P.sb("wb", [128, 8, 3072], BF16)
    wsw = P.sb("wsw", [128, 8, 896], BF16)
    w_in_v = w_in.rearrange("(k p) e -> p k e", p=128)
    w_sw_v = w_sw.rearrange("(k p) e -> p k e", p=128)
    for (c0, nch) in ((10, 4), (0, 4), (4, 4), (8, 2)):
        P.dma("pool", wb[:, :, c0 * 128:(c0 + nch) * 128], w_in_v[:, :, c0 * 128:(c0 + nch) * 128], writes=[f"wbc{c}" for c in range(c0, c0 + nch)])
    P.dma("pool", wb[:, :, 14 * 128:18 * 128], w_in_v[:, :, 14 * 128:18 * 128], writes=[f"wbc{c}" for c in range(14, 18)])
    P.dma("pool", wsw[:, :, 0:4 * 128], w_sw_v[:, :, 0:4 * 128], writes=[f"wswc{c}" for c in range(0, 4)])
    P.dma("pool", wb[:, :, 18 * 128:24 * 128], w_in_v[:, :, 18 * 128:24 * 128], writes=[f"wbc{c}" for c in range(18, 24)])
    P.dma("pool", wsw[:, :, 4 * 128:7 * 128], w_sw_v[:, :, 4 * 128:7 * 128], writes=[f"wswc{c}" for c in range(4, 7)])

    rc = P.sb("rc", [128, 2], F32)
    P.dma("sp", rc[:], rcol, writes=["rc"])
    tmpi = P.sb("tmpi", [128, T], I32)
    P.dma("sp", tmpi[:], pos.partition_broadcast(128), writes=["tmpi"])
    ang = P.sb("ang", [128, T], F32)
    cosT = P.sb("cosT", [128, T], F32)
    sinT = P.sb("sinT", [128, T], F32)
    tmpf = P.sb("tmpf", [128, T], F32)
    P.op("dve", lambda q: q.tensor_copy(out=ang[:], in_=tmpi[:]), reads=["tmpi"], writes=["ang"])
    P.op("dve", lambda q: q.tensor_scalar(out=ang[:], in0=ang[:], scalar1=rc[:, 0:1], scalar2=None, op0=ALU.mult),
         reads=["ang", "rc"], writes=["ang"])
    C1 = 6.28125
    C2 = TWO_PI - C1

    def sin_table(dst, dkey, phase):
        P.op("dve", lambda q: q.tensor_scalar(out=tmpf[:], in0=ang[:], scalar1=phase, scalar2=1.0 / TWO_PI,
                                              op0=ALU.add, op1=ALU.mult), reads=["ang"], writes=["tmpf"])
        P.op("dve", lambda q: q.tensor_copy(out=tmpi[:], in_=tmpf[:]), reads=["tmpf"], writes=["tmpi"])
        P.op("dve", lambda q: q.tensor_copy(out=tmpf[:], in_=tmpi[:]), reads=["tmpi"], writes=["tmpf"])
        P.op("dve", lambda q: q.scalar_tensor_tensor(out=dst[:], in0=tmpf[:], scalar=-C1, in1=ang[:],
                                                     op0=ALU.mult, op1=ALU.add), reads=["tmpf", "ang"], writes=[dkey])
        P.op("dve", lambda q: q.scalar_tensor_tensor(out=dst[:], in0=tmpf[:], scalar=-C2, in1=dst[:],
                                                     op0=ALU.mult, op1=ALU.add), reads=["tmpf", dkey], writes=[dkey])
        P.op("dve", lambda q: q.tensor_scalar(out=dst[:], in0=dst[:], scalar1=phase, scalar2=-math.pi,
                                              op0=ALU.add, op1=ALU.max), reads=[dkey], writes=[dkey])
        P.op("dve", lambda q: q.tensor_scalar(out=dst[:], in0=dst[:], scalar1=math.pi, scalar2=None,
                                              op0=ALU.min), reads=[dkey], writes=[dkey])
        P.op("act", lambda q: q.activation(out=dst[:], in_=dst[:], func=AF.Sin), reads=[dkey], writes=[dkey])

    sin_table(cosT, "cosT", math.pi / 2)
    sin_table(sinT, "sinT", 0.0)
    P.op("dve", lambda q: q.tensor_scalar(out=sinT[:], in0=sinT[:], scalar1=rc[:, 1:2], scalar2=None, op0=ALU.mult),
         reads=["sinT", "rc"], writes=["sinT"])

    albs = P.sb("albs", [128, 4], F32)
    P.dma("sp", albs[:], alb, writes=["albs"])
    lbc = P.sb("lbc", [128, 2], F32)
    oml = P.sb("oml", [128, 2], F32)
    if layer == 0:
        P.op("pool", lambda q: q.memset(lbc[:], 0.0), writes=["lbc"])
        P.op("pool", lambda q: q.memset(oml[:], 1.0), writes=["oml"])
    else:
        ex = P.sb("alb_ex", [128, 4], F32)
        P.op("act", lambda q: q.activation(out=ex[:], in_=albs[:], func=AF.Exp), reads=["albs"], writes=["alb_ex"])
        exv = ex[:].rearrange("p (t l) -> p t l", l=2)
        sm = P.sb("alb_sm", [128, 2], F32)
        P.op("dve", lambda q: q.tensor_tensor(out=sm[:], in0=exv[:, :, 0], in1=exv[:, :, 1], op=ALU.add),
             reads=["alb_ex"], writes=["alb_sm"])
        P.op("dve", lambda q: q.reciprocal(out=sm[:], in_=sm[:]), reads=["alb_sm"], writes=["alb_sm"])
        P.op("dve", lambda q: q.tensor_tensor(out=lbc[:], in0=exv[:, :, 1], in1=sm[:], op=ALU.mult),
             reads=["alb_ex", "alb_sm"], writes=["lbc"])
        P.op("dve", lambda q: q.tensor_scalar(out=oml[:], in0=lbc[:], scalar1=-1.0, scalar2=1.0, op0=ALU.mult,
                                              op1=ALU.add), reads=["lbc"], writes=["oml"])

    ob_f = [P.sb(f"obf{i}", [128, 512], F32) for i in range(4)]
    ob_b = [P.sb(f"obb{i}", [128, 512], BF16) for i in range(4)]
    sg = [P.sb(f"sg{i}", [128, T], F32) for i in range(2)]
    cnt = {"f": 0, "b": 0}

    def getf():
        i = cnt["f"] % 4
        cnt["f"] += 1
        return ob_f[i], f"obf{i}"

    def getb():
        i = cnt["b"] % 4
        cnt["b"] += 1
        return ob_b[i], f"obb{i}"

    def proj(ps, psk, wt, wkey, col0, tg):
        for k in range(8):
            P.op("pe", lambda q, k=k: q.matmul(ps[:, :], lhsT=wt[:, k, col0:col0 + 128],
                                               rhs=hT[:, k, tg * 512:(tg + 1) * 512], start=(k == 0), stop=(k == 7)),
                 reads=[wkey, hkeys[k]], writes=[psk])

    def store32(name, tile_i, tg, buf, bkey):
        r0 = O32[name] + tile_i * 128
        P.dma("sp", o32[r0:r0 + 128, tg * 512:(tg + 1) * 512], buf[:], reads=[bkey], is_output=True)

    def storebf(name, tile_i, tg, buf, bkey):
        r0 = OBF[name] + tile_i * 128
        P.dma("sp", obf[r0:r0 + 128, tg * 512:(tg + 1) * 512], buf[:], reads=[bkey], is_output=True)

    order = ["bg", "bv", "aq", "aff", "afb", "ai", "ag", "cq", "ck", "cv", "dq", "dk", "dv"]
    for name in order:
        c0, ncn = CH[name]
        for ti in range(ncn):
            ch = c0 + ti
            for tg in range(4):
                tsl = slice(tg * 512, (tg + 1) * 512)
                ps, psk = pp.get()
                proj(ps, psk, wb, f"wbc{ch}", ch * 128, tg)
                if name == "bg":
                    P.op("act", lambda q, ps=ps, ti=ti, tsl=tsl: q.activation(out=sg[ti][:, tsl], in_=ps[:], func=AF.Sigmoid),
                         reads=[psk], writes=[f"sg{ti}_{tg}"])
                elif name == "bv":
                    b, bk = getf()
                    P.op("dve", lambda q, ps=ps, b=b, ti=ti, tsl=tsl: q.tensor_tensor(out=b[:], in0=ps[:], in1=sg[ti][:, tsl], op=ALU.mult),
                         reads=[psk, f"sg{ti}_{tg}"], writes=[bk])
                    store32("u", ti, tg, b, bk)
                elif name in ("aq", "ag"):
                    b, bk = getf()
                    P.op("act", lambda q, ps=ps, b=b: q.activation(out=b[:], in_=ps[:], func=AF.Silu), reads=[psk], writes=[bk])
                    store32("qa" if name == "aq" else "ga", ti, tg, b, bk)
                elif name == "ai":
                    b, bk = getf()
                    P.op("act", lambda q, ps=ps, b=b: q.activation(out=b[:], in_=ps[:], func=AF.Copy), reads=[psk], writes=[bk])
                    store32("va", ti, tg, b, bk)
                elif name in ("aff", "afb"):
                    fb_, fk = getf()
                    P.op("act", lambda q, ps=ps, fb_=fb_: q.activation(out=fb_[:], in_=ps[:], func=AF.Exp, scale=-1.0), reads=[psk], writes=[fk])
                    P.op("dve", lambda q, fb_=fb_: q.tensor_scalar(out=fb_[:], in0=fb_[:], scalar1=1.0, scalar2=None, op0=ALU.add), reads=[fk], writes=[fk])
                    P.op("dve", lambda q, fb_=fb_: q.reciprocal(out=fb_[:], in_=fb_[:]), reads=[fk], writes=[fk])
                    P.op("dve", lambda q, fb_=fb_, ti=ti: q.tensor_scalar(out=fb_[:], in0=fb_[:], scalar1=oml[:, ti:ti + 1],
                                                                        scalar2=lbc[:, ti:ti + 1], op0=ALU.mult, op1=ALU.add),
                         reads=[fk, "oml", "lbc"], writes=[fk])
                    kb_, kk = getf()
                    P.op("dve", lambda q, fb_=fb_, kb_=kb_: q.tensor_scalar(out=kb_[:], in0=fb_[:], scalar1=-1.0, scalar2=1.0,
                                                                          op0=ALU.mult, op1=ALU.add), reads=[fk], writes=[kk])
                    store32("kf" if name == "aff" else "kb", ti, tg, kb_, kk)
                    P.op("act", lambda q, fb_=fb_: q.activation(out=fb_[:], in_=fb_[:], func=AF.Ln), reads=[fk], writes=[fk])
                    store32("lf" if name == "aff" else "lb", ti, tg, fb_, fk)
                elif name in ("cv", "dv"):
                    b, bk = getb()
                    P.op("act", lambda q, ps=ps, b=b: q.activation(out=b[:], in_=ps[:], func=AF.Copy), reads=[psk], writes=[bk])
                    storebf(name, ti, tg, b, bk)
                else:
                    ri = ROT_CHUNKS.index(ch)
                    ps2, psk2 = pp.get()
                    proj(ps2, psk2, wsw, f"wswc{ri}", ri * 128, tg)
                    t1, t1k = getf()
                    t2, t2k = getf()
                    P.op("dve", lambda q, ps=ps, t1=t1, tsl=tsl: q.tensor_tensor(out=t1[:], in0=ps[:], in1=cosT[:, tsl], op=ALU.mult),
                         reads=[psk, "cosT"], writes=[t1k])
                    P.op("dve", lambda q, ps2=ps2, t2=t2, tsl=tsl: q.tensor_tensor(out=t2[:], in0=ps2[:], in1=sinT[:, tsl], op=ALU.mult),
                         reads=[psk2, "sinT"], writes=[t2k])
                    b, bk = getb()
                    P.op("pool", lambda q, t1=t1, t2=t2, b=b: q.tensor_tensor(out=b[:], in0=t1[:], in1=t2[:], op=ALU.add),
                         reads=[t1k, t2k], writes=[bk])
                    storebf(name, ti, tg, b, bk)
    return P.finish()


def col128(v):
    v = np.asarray(v)
    return np.ascontiguousarray(v.reshape(-1, 128).T)


def rot_cols():
    idx = []
    for ch in ROT_CHUNKS:
        for h in range(2):
            base = ch * 128 + h * 64
            loc = np.arange(64)
            loc[:8] = np.arange(8, 16)
            loc[8:16] = np.arange(0, 8)
            idx.append(base + loc)
    return np.concatenate(idx)


def rot_consts():
    half = ROPE_DIM // 2
    inv = (np.float32(ROPE_THETA) ** (-np.arange(half, dtype=np.float32) * np.float32(2.0 / ROPE_DIM))).astype(np.float32)
    rc = np.zeros((128, 2), np.float32)
    for p in range(128):
        d = p % 64
        if d < 16:
            rc[p, 0] = inv[d % 8]
            rc[p, 1] = -1.0 if d < 8 else 1.0
    return rc


_cache = {}


def run(nc_key, builder, in_maps):
    if nc_key not in _cache:
        _cache[nc_key] = builder()
    nc = _cache[nc_key]
    res = run_bass_kernel_spmd(nc, in_maps, core_ids=list(range(NCORES)))
    return res.results


def build_k0():
    P = Prog()
    ccol = P.dram_in("ccol", [128, 8], F32)
    w = P.dram_in("w", [D_MODEL, 1536], F32)
    bias = P.dram_in("bias", [1, 1536], F32)
    o = P.dram_out("o", [1, 1536], F32)
    sc = P.sb("sc", [128, 8], F32)
    P.dma("sp", sc[:], ccol, writes=["sc"])
    P.op("act", lambda q: q.activation(out=sc[:], in_=sc[:], func=AF.Silu), reads=["sc"], writes=["sc"])
    bs = P.sb("bs", [1, 1536], F32)
    P.dma("sp", bs[:], bias, writes=["bs"])
    ws = P.sb("ws", [128, 8, 1536], F32)
    wv = w.rearrange("(k p) e -> p k e", p=128)
    for g in range(3):
        P.dma("sp", ws[:, :, g * 512:(g + 1) * 512], wv[:, :, g * 512:(g + 1) * 512], writes=[f"ws{g}"])
    ob = P.sb("ob", [1, 1536], F32)
    pss = [P.ps(f"p{i}", [128, 512], F32) for i in range(3)]
    for g in range(3):
        for k in range(8):
            P.op("pe", lambda q, g=g, k=k: q.matmul(pss[g][0:1, :], lhsT=sc[:, k:k + 1], rhs=ws[:, k, g * 512:(g + 1) * 512],
                                                   start=(k == 0), stop=(k == 7)), reads=["sc", f"ws{g}"], writes=[f"p{g}"])
        P.op("dve", lambda q, g=g: q.tensor_tensor(out=ob[:, g * 512:(g + 1) * 512], in0=pss[g][0:1, :], in1=bs[:, g * 512:(g + 1) * 512], op=ALU.add),
             reads=[f"p{g}", "bs"], writes=["ob"])
    P.dma("sp", o, ob[:], reads=["ob"], is_output=True)
    return P.finish()


def run_k0(inp):
    wa = np.asarray(inp["w_ada"])
    wcat = np.concatenate([wa[l] for l in range(DEPTH)], axis=1)
    bcat = np.concatenate([np.asarray(inp["b_ada"])[l] for l in range(DEPTH)])[None, :]
    ccol = col128(np.asarray(inp["c"])[0])
    in_maps = [{"ccol": ccol, "w": np.ascontiguousarray(wcat[:, c * 1536:(c + 1) * 1536]),
                "bias": np.ascontiguousarray(bcat[:, c * 1536:(c + 1) * 1536])} for c in range(NCORES)]
    res = run(("k0",), build_k0, in_maps)
    mod = np.concatenate([r["o"][0] for r in res])
    return [col128(mod[l * 6144:(l + 1) * 6144]) for l in range(DEPTH)]


def run_k1(layer, xT_shards, modc, inp):
    rc = rot_consts()
    sw = rot_cols()
    w_in_l = np.ascontiguousarray(inp["w_in"][layer])
    w_sw = np.ascontiguousarray(w_in_l[:, sw])
    alb = np.zeros((128, 4), np.float32)
    a = np.asarray(inp["a_lower_bound"])
    for t in range(2):
        for l in range(2):
            alb[:, t * 2 + l] = a[l, t * 128:(t + 1) * 128]
    pos = np.asarray(inp["positions"])[0].astype(np.int32)
    in_maps = []
    for c in range(NCORES):
        in_maps.append({"xT": xT_shards[c], "modc": modc, "w_in": w_in_l, "w_sw": w_sw,
                        "pos": np.ascontiguousarray(pos[c * TOK:(c + 1) * TOK]), "rcol": rc, "alb": alb})
    return run(("k1", layer), lambda: build_k1(layer), in_maps)


SEG = 1024
NSEG = SEQ // SEG
NCH = SEG // 16
NTL = SEG // 128


def hgrn_consts():
    ident = np.eye(128, dtype=np.float32).astype(ml_dtypes.bfloat16)
    s = np.arange(128)
    tri = ((s[:, None] // 16 == s[None, :] // 16) & (s[:, None] <= s[None, :])).astype(np.float32).astype(ml_dtypes.bfloat16)
    ind = (s[:, None] // 16 == np.arange(8)[None, :]).astype(np.float32).astype(ml_dtypes.bfloat16)
    return ident, tri, ind


def build_k2a():
    P = Prog()
    qT = P.dram_in("qT", [64, SEQ], F32)
    kT = P.dram_in("kT", [64, SEQ], F32)
    lT = P.dram_in("lT", [64, SEQ], F32)
    vtok = P.dram_in("vtok", [SEQ, 64], F32)
    ident_d = P.dram_in("ident_d", [128, 128], BF16)
    tri_d = P.dram_in("tri_d", [128, 128], BF16)
    ind_d = P.dram_in("ind_d", [128, 8], BF16)
    oT = P.dram_out("oT", [64, SEQ], F32)

    ident = P.sb("ident", [128, 128], BF16)
    tri = P.sb("tri", [128, 128], BF16)
    ind = P.sb("ind", [128, 8], BF16)
    P.dma("sp", ident[:], ident_d, writes=["ident"])
    P.dma("sp", tri[:], tri_d, writes=["tri"])
    P.dma("sp", ind[:], ind_d, writes=["ind"])

    reset = P.sb("reset", [64, SEG], F32)
    P.op("pool", lambda q: q.memset(reset[:], 1.0), writes=["reset"])
    P.op("pool", lambda q: q.memset(reset[:].rearrange("p (n c) -> p n c", c=16)[:, :, 0:1], 0.0), reads=["reset"], writes=["reset"])

    def two(name, shape, dt):
        return [P.sb(f"{name}{i}", shape, dt) for i in range(2)]

    def three(name, shape, dt):
        return [P.sb(f"{name}{i}", shape, dt) for i in range(3)]

    qs_, ks_, ls_ = three("qs", [64, SEG], F32), three("ks", [64, SEG], F32), three("ls", [64, SEG], F32)
    vb_ = three("vb", [128, NTL, 64], BF16)
    cum_, ex_ = three("cum", [64, SEG], F32), three("ex", [64, SEG], F32)
    qt_, kt_, kh_ = three("qt", [64, SEG], BF16), three("kt", [64, SEG], BF16), three("kh", [64, SEG], BF16)
    dec_ = three("dec", [64, NCH], F32)
    kvs_ = two("kvs", [64, 64 * NCH], F32)
    sprev_ = two("sprev", [64, NCH, 64], BF16)
    obuf_ = two("obuf", [64, SEG], F32)
    decrep = P.sb("decrep", [64, 64 * NCH], F32)
    decz = P.sb("decz", [64, NCH], F32)
    P.op("pool", lambda q: q.memset(decz[:], 0.0), writes=["decz"])
    sall = P.sb("sall", [64, 64 * NCH], F32)
    s_in = P.sb("s_in", [64, 64], F32)
    khtok = [P.sb(f"khtok{i}", [128, 64], BF16) for i in range(2)]
    vblk = [P.sb(f"vblk{i}", [128, 8, 64], BF16) for i in range(2)]
    am = [P.sb(f"am{i}", [128, 128], BF16) for i in range(2)]
    P.op("pool", lambda q: q.memset(s_in[:], 0.0), writes=["s_in"])

    ps_kv = [P.ps(f"ps_kv{i}", [128, 512], F32) for i in range(2)]
    ps_a = [P.ps(f"ps_a{i}", [128, 512], F32) for i in range(2)]
    ps_o = [P.ps(f"ps_o{i}", [128, 512], F32) for i in range(2)]
    ps_t = [P.ps(f"ps_t{i}", [128, 1024], BF16) for i in range(2)]

    dec3 = decrep[:].rearrange("p (v n) -> p v n", n=NCH)
    sall3 = sall[:].rearrange("p (v n) -> p v n", n=NCH)
    vt_v = vtok.rearrange("(s i p) v -> s p i v", p=128, i=NTL)
    cnt = [0]

    def stage(sgi, part):
        pb = sgi % 2
        p3 = sgi % 3
        K_ = lambda n: f"{n}{pb if n in ('kvs', 'sprev', 'obuf') else p3}"
        qs, ks, ls, vb, cum, ex = qs_[p3], ks_[p3], ls_[p3], vb_[p3], cum_[p3], ex_[p3]
        qt, kt, kh, dec, kvs, sprev, obuf = qt_[p3], kt_[p3], kh_[p3], dec_[p3], kvs_[pb], sprev_[pb], obuf_[pb]
        kvs3 = kvs[:].rearrange("p (v n) -> p v n", n=NCH)
        cum3 = cum[:].rearrange("p (n c) -> p n c", c=16)
        ex3 = ex[:].rearrange("p (n c) -> p n c", c=16)
        tsl = slice(sgi * SEG, (sgi + 1) * SEG)
        if part == "B":
            yield from stageB(sgi, pb, K_, qt, kt, vb, dec, kvs, kvs3, sprev, obuf, tsl)
            return
        if part == "A1":
            yield P.dma("sp", qs[:], qT[:, tsl], writes=[K_("qs")])
            yield P.dma("sp", ks[:], kT[:, tsl], writes=[K_("ks")])
            yield P.dma("sp", ls[:], lT[:, tsl], writes=[K_("ls")])
            yield P.dma("pool", vb[:], vt_v[sgi], writes=[K_("vb")])
            yield P.op("dve", lambda q: q.tensor_tensor_scan(out=cum[:], data0=reset[:], data1=ls[:], initial=0.0,
                                                       op0=ALU.mult, op1=ALU.add), reads=["reset", K_("ls")], writes=[K_("cum")])
            yield P.op("act", lambda q: q.activation(out=ex[:], in_=cum[:], func=AF.Exp), reads=[K_("cum")], writes=[K_("ex")])
            yield P.op("dve", lambda q: q.tensor_tensor(out=qt[:], in0=qs[:], in1=ex[:], op=ALU.mult), reads=[K_("qs"), K_("ex")], writes=[K_("qt")])
            yield P.op("act", lambda q: q.activation(out=ex[:], in_=cum[:], func=AF.Exp, scale=-1.0), reads=[K_("cum")], writes=[K_("ex")])
            yield P.op("dve", lambda q: q.tensor_tensor(out=kt[:], in0=ks[:], in1=ex[:], op=ALU.mult), reads=[K_("ks"), K_("ex")], writes=[K_("kt")])
            yield P.op("dve", lambda q: q.tensor_tensor(out=ex3, in0=cum3[:, :, 15:16].broadcast_to([64, NCH, 16]), in1=cum3,
                                                  op=ALU.subtract), reads=[K_("cum")], writes=[K_("ex")])
            yield P.op("act", lambda q: q.activation(out=ex[:], in_=ex[:], func=AF.Exp), reads=[K_("ex")], writes=[K_("ex")])
            yield P.op("dve", lambda q: q.tensor_tensor(out=kh[:], in0=ks[:], in1=ex[:], op=ALU.mult), reads=[K_("ks"), K_("ex")], writes=[K_("kh")])
            yield P.op("act", lambda q: q.activation(out=dec[:], in_=cum3[:, :, 15], func=AF.Exp), reads=[K_("cum")], writes=[K_("dec")])
            return
        bs = []
        for i in range(NTL):
            bs.append(cnt[0] % 2)
            cnt[0] += 1

        def emit_T(i):
            b = bs[i]
            yield P.op("pe", lambda q: q.transpose(ps_t[b][:, 0:64], kh[:, i * 128:(i + 1) * 128], ident[0:64, 0:64]),
                 reads=[K_("kh"), "ident"], writes=[f"ps_t{b}"])
            yield P.op("act", lambda q: q.activation(out=khtok[b][:], in_=ps_t[b][:, 0:64], func=AF.Copy),
                 reads=[f"ps_t{b}"], writes=[f"khtok{b}"])
            yield P.op("dve", lambda q: q.tensor_tensor(out=vblk[b][:], in0=vb[:, i:i + 1, :].broadcast_to([128, 8, 64]),
                                                   in1=ind[:].unsqueeze(2).broadcast_to([128, 8, 64]), op=ALU.mult),
                 reads=[K_("vb"), "ind"], writes=[f"vblk{b}"])

        def emit_KV(i):
            b = bs[i]
            yield P.op("pe", lambda q: q.matmul(ps_kv[b][0:64, :], lhsT=khtok[b][:], rhs=vblk[b][:].rearrange("p c v -> p (c v)"),
                                          start=True, stop=True), reads=[f"khtok{b}", f"vblk{b}"], writes=[f"ps_kv{b}"])
            yield P.op("act", lambda q: q.activation(out=kvs3[:, :, 8 * i:8 * i + 8],
                                               in_=ps_kv[b][0:64, :].rearrange("p (c v) -> p v c", v=64), func=AF.Copy),
                 reads=[f"ps_kv{b}"], writes=[K_("kvs")])

        yield from emit_T(0)
        for i in range(NTL):
            if i + 1 < NTL:
                yield from emit_T(i + 1)
            yield from emit_KV(i)

    def stageB(sgi, pb, K_, qt, kt, vb, dec, kvs, kvs3, sprev, obuf, tsl):
        yield P.op("act", lambda q: q.activation(out=decz[:, 1:NCH], in_=dec[:, 1:NCH], func=AF.Copy), reads=[K_("dec")], writes=["decz"])
        yield P.op("act", lambda q: q.activation(out=dec3, in_=decz[:].unsqueeze(1).broadcast_to([64, 64, NCH]), func=AF.Copy),
             reads=["decz"], writes=["decrep"])
        yield P.op("dve", lambda q: q.scalar_tensor_tensor(out=kvs3[:, :, 0], in0=s_in[:], scalar=dec[:, 0:1], in1=kvs3[:, :, 0],
                                                     op0=ALU.mult, op1=ALU.add), reads=["s_in", K_("dec"), K_("kvs")], writes=[K_("kvs")])
        yield P.op("dve", lambda q: q.tensor_tensor_scan(out=sall[:], data0=decrep[:], data1=kvs[:], initial=0.0,
                                                   op0=ALU.mult, op1=ALU.add), reads=["decrep", K_("kvs")], writes=["sall"])
        yield P.op("act", lambda q: q.activation(out=sprev[:, 0, :], in_=s_in[:], func=AF.Copy), reads=["s_in"], writes=[K_("sprev")])
        yield P.op("act", lambda q: q.activation(out=sprev[:, 1:NCH, :], in_=sall3[:, :, 0:NCH - 1].rearrange("p v n -> p n v"), func=AF.Copy),
             reads=["sall"], writes=[K_("sprev")])
        yield P.op("dve", lambda q: q.tensor_copy(out=s_in[:], in_=sall3[:, :, NCH - 1]), reads=["sall", K_("sprev")], writes=["s_in"])
        bs = []
        for i in range(NTL):
            bs.append(cnt[0] % 2)
            cnt[0] += 1

        def emit_A(i):
            b = bs[i]
            csl = slice(i * 128, (i + 1) * 128)
            yield P.op("pe", lambda q: q.matmul(ps_a[b][:, 0:128], lhsT=kt[:, csl], rhs=qt[:, csl], start=True, stop=True),
                 reads=[K_("kt"), K_("qt")], writes=[f"ps_a{b}"])
            yield P.op("dve", lambda q: q.tensor_tensor(out=am[b][:], in0=ps_a[b][:, 0:128], in1=tri[:], op=ALU.mult),
                 reads=[f"ps_a{b}", "tri"], writes=[f"am{b}"])

        def emit_O(i):
            b = bs[i]
            csl = slice(i * 128, (i + 1) * 128)
            yield P.op("pe", lambda q: q.matmul(ps_o[b][0:64, 0:128], lhsT=vb[:, i, :], rhs=am[b][:], start=True, stop=False),
                 reads=[K_("vb"), f"am{b}"], writes=[f"ps_o{b}"])
            for c in range(8):
                n = 8 * i + c
                yield P.op("pe", lambda q, c=c, n=n: q.matmul(ps_o[b][0:64, 16 * c:16 * c + 16], lhsT=sprev[:, n, :],
                                                       rhs=qt[:, i * 128 + 16 * c:i * 128 + 16 * c + 16],
                                                       start=False, stop=(c == 7), skip_group_check=True),
                     reads=[K_("sprev"), K_("qt")], writes=[f"ps_o{b}"])
            yield P.op("act", lambda q: q.activation(out=obuf[:, csl], in_=ps_o[b][0:64, 0:128], func=AF.Copy),
                 reads=[f"ps_o{b}"], writes=[K_("obuf")])

        yield from emit_A(0)
        for i in range(NTL):
            if i + 1 < NTL:
                yield from emit_A(i + 1)
            yield from emit_O(i)
        yield P.dma("sp", oT[:, tsl], obuf[:], reads=[K_("obuf")], is_output=True)

    def drain(*gens):
        gens = [g for g in gens if g is not None]
        while gens:
            for g in list(gens):
                try:
                    next(g)
                except StopIteration:
                    gens.remove(g)

    drain(stage(0, "A1"))
    drain(stage(0, "A2"), stage(1, "A1"))
    for sgi in range(NSEG):
        drain(stage(sgi, "B"), stage(sgi + 1, "A2") if sgi + 1 < NSEG else None,
              stage(sgi + 2, "A1") if sgi + 2 < NSEG else None)
    return P.finish()


def run_k2a(o32_full):
    ident, tri, ind = hgrn_consts()
    in_maps = []
    for c in range(NCORES):
        h, d = c % 4, c // 4
        rows = slice(h * 64, (h + 1) * 64)
        q = o32_full["qa"][rows]
        k = o32_full["kf" if d == 0 else "kb"][rows]
        l = o32_full["lf" if d == 0 else "lb"][rows]
        v = o32_full["va"][rows]
        if d == 1:
            q, k, l, v = q[:, ::-1], k[:, ::-1], l[:, ::-1], v[:, ::-1]
        in_maps.append({"qT": np.ascontiguousarray(q), "kT": np.ascontiguousarray(k), "lT": np.ascontiguousarray(l),
                        "vtok": np.ascontiguousarray(v.T), "ident_d": ident, "tri_d": tri, "ind_d": ind})
    res = run(("k2a",), build_k2a, in_maps)
    of = np.concatenate([res[h]["oT"] for h in range(4)], 0)
    ob = np.concatenate([res[4 + h]["oT"][:, ::-1] for h in range(4)], 0)
    return of, np.ascontiguousarray(ob)


GU = 16
C_PAT = ((128, 1), (512, 4), (2048, 16))
NUC = 3 * 4 * 16
NUD = 4 * 16


def attn_masks():
    k = np.arange(256)[:, None]
    q = np.arange(128)[None, :]
    mc = np.where(np.abs(k - 64 - q) <= 64, 1.0, 0.0).astype(np.float32)
    k = np.arange(384)[:, None]
    md = np.where(np.abs(k - 128 - q) <= 128, 1.0, 0.0).astype(np.float32)
    mc = mc.reshape(2, 128, 128).transpose(1, 0, 2)
    md = md.reshape(3, 128, 128).transpose(1, 0, 2)
    return (np.ascontiguousarray(mc).astype(ml_dtypes.bfloat16), np.ascontiguousarray(md).astype(ml_dtypes.bfloat16))


def build_k2b():
    P = Prog()
    specs = []
    for nm, nu, nb in (("c", NUC, 2), ("d", NUD, 3)):
        ng = nu // GU
        specs.append(dict(nm=nm, nb=nb, ng=ng,
                          Q=P.dram_in(nm + "Q", [ng, 64, GU * 128], BF16),
                          K=P.dram_in(nm + "K", [ng, 64, GU * nb * 128], BF16),
                          V=P.dram_in(nm + "V", [ng, 128, GU * nb * 65], BF16),
                          M=P.dram_in(nm + "M", [128, nb, 128], BF16),
                          O=P.dram_out(nm + "O", [ng, 65, GU * 128], F32)))
    ident_d = P.dram_in("ident_d", [128, 128], BF16)
    ident = P.sb("ident", [128, 128], BF16)
    P.dma("sp", ident[:], ident_d, writes=["ident"])
    Qg = [P.sb(f"Qg{i}", [64, GU * 128], BF16) for i in range(2)]
    Kg = [P.sb(f"Kg{i}", [64, GU * 3 * 128], BF16) for i in range(2)]
    Vg = [P.sb(f"Vg{i}", [128, GU * 3 * 65], BF16) for i in range(2)]
    Og = [P.sb(f"Og{i}", [65, GU * 128], F32) for i in range(2)]
    Pt = [P.sb(f"Pt{i}", [128, 384], BF16) for i in range(3)]
    psS = [P.ps(f"psS{i}", [128, 512], F32) for i in range(4)]
    psO = [P.ps(f"psO{i}", [128, 512], F32) for i in range(3)]
    gi = 0
    ui = 0
    for sp_ in specs:
        nb = sp_["nb"]
        mk = P.sb("mask_" + sp_["nm"], [128, nb, 128], BF16)
        mkk = "mask_" + sp_["nm"]
        P.dma("sp", mk[:], sp_["M"], writes=[mkk])
        for g in range(sp_["ng"]):
            b2 = gi % 2
            gi += 1
            P.dma("sp", Qg[b2][:], sp_["Q"][g], writes=[f"Qg{b2}"])
            P.dma("sp", Kg[b2][:, 0:GU * nb * 128], sp_["K"][g], writes=[f"Kg{b2}"])
            P.dma("sp", Vg[b2][:, 0:GU * nb * 65], sp_["V"][g], writes=[f"Vg{b2}"])
            def emit_S(u, s3, p2):
                for b in range(nb):
                    ksl = slice((u * nb + b) * 128, (u * nb + b + 1) * 128)
                    P.op("pe", lambda q, b=b, ksl=ksl: q.matmul(
                        psS[s3][:, b * 128:(b + 1) * 128], lhsT=Kg[b2][:, ksl], rhs=Qg[b2][:, u * 128:(u + 1) * 128],
                        start=True, stop=True), reads=[f"Kg{b2}", f"Qg{b2}"], writes=[f"psS{s3}"])
                P.op("act", lambda q: q.activation(out=Pt[p2][:, 0:nb * 128], in_=psS[s3][:, 0:nb * 128], func=AF.Exp, scale=0.125),
                     reads=[f"psS{s3}"], writes=[f"Pt{p2}"])
                P.op("dve", lambda q: q.tensor_tensor(out=Pt[p2][:, 0:nb * 128], in0=Pt[p2][:, 0:nb * 128],
                                                      in1=mk[:].rearrange("p b q -> p (b q)"), op=ALU.mult),
                     reads=[f"Pt{p2}", mkk], writes=[f"Pt{p2}"])

            def emit_PV(u, s3, p2, so):
                for b in range(nb):
                    vsl = slice((u * nb + b) * 65, (u * nb + b + 1) * 65)
                    P.op("pe", lambda q, b=b, vsl=vsl: q.matmul(
                        psO[so][0:65, 0:128], lhsT=Vg[b2][:, vsl], rhs=Pt[p2][:, b * 128:(b + 1) * 128],
                        start=(b == 0), stop=(b == nb - 1)), reads=[f"Vg{b2}", f"Pt{p2}"], writes=[f"psO{so}"])
                P.op("act", lambda q: q.activation(out=Og[b2][:, u * 128:(u + 1) * 128], in_=psO[so][0:65, 0:128], func=AF.Copy),
                     reads=[f"psO{so}"], writes=[f"Og{b2}"])

            ids = []
            for u in range(GU):
                ids.append((u, ui % 4, ui % 3, ui % 3))
                ui += 1
            emit_S(*ids[0][:3])
            emit_S(*ids[1][:3])
            for j in range(GU):
                if j + 2 < GU:
                    emit_S(*ids[j + 2][:3])
                emit_PV(*ids[j])
            P.dma("sp", sp_["O"][g], Og[b2][:], reads=[f"Og{b2}"], is_output=True)
    return P.finish()


def _windows(Kseq, Vseq, nb, halo, ntile):
    L = Kseq.shape[1]
    W = nb * 128
    Kp = np.zeros((64, L + 2 * halo + 128), Kseq.dtype)
    Kp[:, halo:halo + L] = Kseq
    Vp = np.zeros((L + 2 * halo + 128, 65), Vseq.dtype)
    Vp[halo:halo + L, :64] = Vseq.T
    Vp[halo:halo + L, 64] = 1.0
    Kw = np.stack([Kp[:, 128 * j:128 * j + W] for j in range(ntile)], 0)
    Vw = np.stack([Vp[128 * j:128 * j + W] for j in range(ntile)], 0)
    return Kw, Vw


def _pack_units(Qu, Ku, Vu, nb):
    U = Qu.shape[0]
    ng = U // GU
    Q = Qu.reshape(ng, GU, 64, 128).transpose(0, 2, 1, 3).reshape(ng, 64, GU * 128)
    K = Ku.reshape(ng, GU, 64, nb * 128).transpose(0, 2, 1, 3).reshape(ng, 64, GU * nb * 128)
    V = Vu.reshape(ng, GU, nb, 128, 65).transpose(0, 3, 1, 2, 4).reshape(ng, 128, GU * nb * 65)
    return np.ascontiguousarray(Q), np.ascontiguousarray(K), np.ascontiguousarray(V)


def run_k2b(obf_full):
    mc, md = attn_masks()
    ident = np.eye(128, dtype=np.float32).astype(ml_dtypes.bfloat16)
    cQ = np.zeros((3, 4, 128, 64, 128), ml_dtypes.bfloat16)
    cK = np.zeros((3, 4, 128, 64, 256), ml_dtypes.bfloat16)
    cV = np.zeros((3, 4, 128, 256, 65), ml_dtypes.bfloat16)
    for p, (w, d) in enumerate(C_PAT):
        L = SEQ // d
        nt = L // 128
        for h in range(4):
            rows = slice(h * 64, (h + 1) * 64)
            Qs = obf_full["cq"][rows].reshape(64, L, d)
            Ks = obf_full["ck"][rows].reshape(64, L, d)
            Vs = obf_full["cv"][rows].reshape(64, L, d)
            for r in range(d):
                Kw, Vw = _windows(Ks[:, :, r], Vs[:, :, r], 2, 64, nt)
                cK[p, h, r * nt:(r + 1) * nt] = Kw
                cV[p, h, r * nt:(r + 1) * nt] = Vw
                cQ[p, h, r * nt:(r + 1) * nt] = Qs[:, :, r].reshape(64, nt, 128).transpose(1, 0, 2)
    dQ = np.zeros((4, 128, 64, 128), ml_dtypes.bfloat16)
    dK = np.zeros((4, 128, 64, 384), ml_dtypes.bfloat16)
    dV = np.zeros((4, 128, 384, 65), ml_dtypes.bfloat16)
    for h in range(4):
        kvh = h // 2
        Kw, Vw = _windows(obf_full["dk"][kvh * 64:(kvh + 1) * 64], obf_full["dv"][kvh * 64:(kvh + 1) * 64], 3, 128, 128)
        dK[h] = Kw
        dV[h] = Vw
        dQ[h] = obf_full["dq"][h * 64:(h + 1) * 64].reshape(64, 128, 128).transpose(1, 0, 2)
    in_maps = []
    for c in range(NCORES):
        ts = slice(16 * c, 16 * c + 16)
        q, k, v = _pack_units(cQ[:, :, ts].reshape(NUC, 64, 128), cK[:, :, ts].reshape(NUC, 64, 256), cV[:, :, ts].reshape(NUC, 256, 65), 2)
        q2, k2, v2 = _pack_units(dQ[:, ts].reshape(NUD, 64, 128), dK[:, ts].reshape(NUD, 64, 384), dV[:, ts].reshape(NUD, 384, 65), 3)
        in_maps.append({"cQ": q, "cK": k, "cV": v, "cM": mc, "dQ": q2, "dK": k2, "dV": v2, "dM": md, "ident_d": ident})
    res = run(("k2b",), build_k2b, in_maps)
    numC = np.zeros((3, 256, SEQ), np.float32)
    denC = np.zeros((3, 4, SEQ), np.float32)
    numD = np.zeros((256, SEQ), np.float32)
    denD = np.zeros((4, SEQ), np.float32)
    for c in range(NCORES):
        co = res[c]["cO"].reshape(NUC // GU, 65, GU, 128).transpose(0, 2, 1, 3).reshape(3, 4, 16, 65, 128)
        do = res[c]["dO"].reshape(NUD // GU, 65, GU, 128).transpose(0, 2, 1, 3).reshape(4, 16, 65, 128)
        for p, (w, d) in enumerate(C_PAT):
            L = SEQ // d
            nt = L // 128
            for h in range(4):
                nv = numC[p, h * 64:(h + 1) * 64].reshape(64, L, d)
                dv_ = denC[p, h].reshape(L, d)
                for tl in range(16):
                    tau = 16 * c + tl
                    r, j = tau // nt, tau % nt
                    nv[:, 128 * j:128 * j + 128, r] = co[p, h, tl, 0:64]
                    dv_[128 * j:128 * j + 128, r] = co[p, h, tl, 64]
        for h in range(4):
            for tl in range(16):
                tau = 16 * c + tl
                numD[h * 64:(h + 1) * 64, 128 * tau:128 * tau + 128] = do[h, tl, 0:64]
                denD[h, 128 * tau:128 * tau + 128] = do[h, tl, 64]
    return numC, denC, numD, denD


FB = 4


def build_k3(moe):
    P = Prog()
    T = TOK
    xT = P.dram_in("xT", [D_MODEL, T], F32)
    modc_d = P.dram_in("modc", [128, 48], F32)
    ofT = P.dram_in("ofT", [256, T], F32)
    obT = P.dram_in("obT", [256, T], F32)
    gaT = P.dram_in("gaT", [256, T], F32)
    uext = P.dram_in("uext", [256, T + 32], F32)
    numC = P.dram_in("numC", [3, 256, T], F32)
    denC = P.dram_in("denC", [3, 256, T], F32)
    numD = P.dram_in("numD", [256, T], F32)
    denD = P.dram_in("denD", [256, T], F32)
    smalls_d = P.dram_in("smalls", [128, 128], F32)
    blk64_d = P.dram_in("blk64", [128, 128], F32)
    identb_d = P.dram_in("identb", [128, 128], BF16)
    w_out = P.dram_in("w_out", [1024, 1024], F32)
    if moe:
        wr_d = P.dram_in("wr", [1024, 8], F32)
        w_up = P.dram_in("w_up", [N_EXPERTS, 1024, 2 * EXPERT_DIM], F32)
        w_down = P.dram_in("w_down", [N_EXPERTS, EXPERT_DIM, 1024], F32)
        sel_d = P.dram_in("sel", [8, 8 * 128], F32)
        identf_d = P.dram_in("identf", [128, 128], F32)
    else:
        w_up = P.dram_in("w_up", [1024, 2 * FFN_DIM], F32)
        w_down = P.dram_in("w_down", [FFN_DIM, 1024], F32)
    xo = P.dram_out("xo", [D_MODEL, T], F32)

    pp = PsumPool(P)
    sm = P.sb("sm", [128, 128], F32)
    P.dma("sp", sm[:], smalls_d, writes=["sm"])
    O_ANW, O_CW, O_CB, O_BNW, O_BNB, O_SINK, O_LNW, O_LNB = 0, 1, 63, 65, 67, 69, 71, 87
    modc = P.sb("modc", [128, 48], F32)
    P.dma("sp", modc[:], modc_d, writes=["modc"])
    blk64 = P.sb("blk64", [128, 128], F32)
    P.dma("sp", blk64[:], blk64_d, writes=["blk64"])
    ones_f = P.sb("ones_f", [128, 128], F32)
    P.op("pool", lambda q: q.memset(ones_f[:], 1.0), writes=["ones_f"])
    eps = P.sb("eps", [128, 2], F32)
    P.op("pool", lambda q: q.memset(eps[:, 0:1], LN_EPS), writes=["eps"])
    P.op("pool", lambda q: q.memset(eps[:, 1:2], RMS_EPS), reads=["eps"], writes=["eps"])
    dcol = P.sb("dcol", [128, 32], F32)
    P.op("dve", lambda q: q.tensor_scalar(out=dcol[:, 0:8], in0=modc[:, 16:24], scalar1=1.0, scalar2=None, op0=ALU.add),
         reads=["modc"], writes=["dcol"])
    P.op("dve", lambda q: q.tensor_scalar(out=dcol[:, 8:16], in0=modc[:, 32:40], scalar1=1.0, scalar2=None, op0=ALU.add),
         reads=["modc", "dcol"], writes=["dcol"])
    P.op("dve", lambda q: q.tensor_scalar(out=dcol[:, 16:24], in0=modc[:, 40:48], scalar1=1.0, scalar2=None, op0=ALU.add),
         reads=["modc", "dcol"], writes=["dcol"])
    P.op("act", lambda q: q.activation(out=dcol[:, 24:26], in_=sm[:, O_SINK:O_SINK + 2], func=AF.Exp),
         reads=["sm", "dcol"], writes=["dcol"])
    G1, SC2, G2, ESINK = 0, 8, 16, 24

    x = P.sb("x", [128, 8, T], F32)
    y = P.sb("y", [128, 8, T], BF16)
    wu = [P.sb(f"wu{i}", [128, 8192], BF16) for i in range(2)]
    wd = [P.sb(f"wd{i}", [128, FB, 1024], BF16) for i in range(2)]
    abuf = P.sb("abuf", [128, FB, T], BF16)
    S = [P.sb(f"S{i}", [128, T + 32], F32) for i in range(3)]
    NT = 5
    tmp = [P.sb(f"tmp{i}", [128, 512], F32) for i in range(NT)]
    lnm = P.sb("lnm", [128, 512], F32)
    lnv = P.sb("lnv", [128, 512], F32)
    tcnt = [0]

    def gett():
        i = tcnt[0] % NT
        tcnt[0] += 1
        return tmp[i], f"tmp{i}"

    def tsl(tg):
        return slice(tg * 512, (tg + 1) * 512)

    xk = lambda i, tg: f"x{i}_{tg}"
    yk = lambda i, tg: f"y{i}_{tg}"

    xT_v = xT.rearrange("(k p) t -> p k t", p=128)
    for k in range(8):
        P.dma("sp", x[:, k, :], xT_v[:, k, :], writes=[xk(k, tg) for tg in range(4)])
    wo_v = w_out.rearrange("(k p) e -> p k e", p=128)
    wo_s = wu[0][:].rearrange("p (k e) -> p k e", k=8)
    for k in range(8):
        P.dma("pool", wo_s[:, k, :], wo_v[:, k, :], writes=["wu0"])

    def ln_fm(srcs, nfeat, wcol, bcol, dst, silu=False):
        n = len(srcs)
        for tg in range(4):
            ps_s, ks_ = pp.get()
            ps_q, kq_ = pp.get()
            for i, (af, kf) in enumerate(srcs):
                P.op("pe", lambda q, af=af, i=i: q.matmul(ps_s[:, :], lhsT=ones_f[:], rhs=af(tg), start=(i == 0), stop=(i == n - 1)),
                     reads=["ones_f", kf(tg)], writes=[ks_])
            for i, (af, kf) in enumerate(srcs):
                sq, sqk = gett()
                P.op("act", lambda q, af=af, sq=sq: q.activation(out=sq[:], in_=af(tg), func=AF.Square), reads=[kf(tg)], writes=[sqk])
                P.op("pe", lambda q, sq=sq, i=i: q.matmul(ps_q[:, :], lhsT=ones_f[:], rhs=sq[:], start=(i == 0), stop=(i == n - 1)),
                     reads=["ones_f", sqk], writes=[kq_])
            mean, mk = lnm, "lnm"
            P.op("act", lambda q: q.activation(out=mean[:], in_=ps_s[:], func=AF.Copy, scale=1.0 / nfeat), reads=[ks_], writes=[mk])
            var, vk = lnv, "lnv"
            P.op("dve", lambda q: q.tensor_tensor(out=var[:], in0=mean[:], in1=mean[:], op=ALU.mult), reads=[mk], writes=[vk])
            P.op("dve", lambda q: q.scalar_tensor_tensor(out=var[:], in0=ps_q[:], scalar=1.0 / nfeat, in1=var[:], op0=ALU.mult,
                                                         op1=ALU.subtract), reads=[kq_, vk], writes=[vk])
            P.op("act", lambda q: q.activation(out=var[:], in_=var[:], func=AF.Sqrt, bias=eps[:, 0:1], scale=1.0), reads=[vk, "eps"], writes=[vk])
            P.op("dve", lambda q: q.reciprocal(out=var[:], in_=var[:]), reads=[vk], writes=[vk])
            for i, ((af, kf), (df, dkf)) in enumerate(zip(srcs, dst)):
                t, tk = gett()
                P.op("dve", lambda q, af=af, t=t: q.tensor_tensor(out=t[:], in0=af(tg), in1=mean[:], op=ALU.subtract),
                     reads=[kf(tg), mk], writes=[tk])
                P.op("dve", lambda q, t=t: q.tensor_tensor(out=t[:], in0=t[:], in1=var[:], op=ALU.mult), reads=[tk, vk], writes=[tk])
                if silu:
                    P.op("act", lambda q, t=t, df=df, i=i: q.activation(out=df(tg), in_=t[:], func=AF.Silu, scale=wcol(i), bias=bcol(i)),
                         reads=[tk, "sm"], writes=[dkf(tg)])
                else:
                    P.op("act", lambda q, t=t, df=df, i=i: q.activation(out=df(tg), in_=t[:], func=AF.Identity, scale=wcol(i), bias=bcol(i)),
                         reads=[tk, "sm"], writes=[dkf(tg)])

    wu1f = wu[1][:].bitcast(F32)
    cpool = [wu1f[:, i_ * 512:(i_ + 1) * 512] for i_ in range(8)]
    ccnt = [0]

    class _CT:
        def __init__(self, ap):
            self.ap = ap

        def __getitem__(self, idx):
            return self.ap

    def getc():
        i_ = ccnt[0] % 8
        ccnt[0] += 1
        return _CT(cpool[i_]), f"ctmp{i_}"

    def stream_A():
        for a in range(2):
            rows = slice(a * 128, (a + 1) * 128)
            for tg in range(4):
                t0, k0 = gett()
                t1, k1 = gett()
                t2, k2 = gett()
                yield P.dma("sp", t0[:], ofT[rows, tsl(tg)], writes=[k0])
                yield P.dma("sp", t1[:], obT[rows, tsl(tg)], writes=[k1])
                yield P.dma("sp", t2[:], gaT[rows, tsl(tg)], writes=[k2])
                yield P.op("dve", lambda q, t0=t0, t1=t1: q.tensor_tensor(out=t0[:], in0=t0[:], in1=t1[:], op=ALU.add), reads=[k0, k1], writes=[k0])
                yield P.op("act", lambda q, t0=t0, t1=t1: q.activation(out=t1[:], in_=t0[:], func=AF.Square), reads=[k0], writes=[k1])
                ps, pk = pp.t[0], pp.k[0]
                yield P.op("pe", lambda q, ps=ps, t1=t1: q.matmul(ps[:, :], lhsT=blk64[:], rhs=t1[:], start=True, stop=True), reads=["blk64", k1], writes=[pk])
                yield P.op("act", lambda q, ps=ps, t1=t1: q.activation(out=t1[:], in_=ps[:], func=AF.Sqrt, bias=eps[:, 1:2], scale=1.0 / 64),
                     reads=[pk, "eps"], writes=[k1])
                yield P.op("dve", lambda q, t1=t1: q.reciprocal(out=t1[:], in_=t1[:]), reads=[k1], writes=[k1])
                yield P.op("dve", lambda q, t0=t0, t1=t1: q.scalar_tensor_tensor(out=t0[:], in0=t0[:], scalar=sm[:, O_ANW:O_ANW + 1], in1=t1[:],
                                                                         op0=ALU.mult, op1=ALU.mult), reads=[k0, k1, "sm"], writes=[k0])
                yield P.op("dve", lambda q, t0=t0, t2=t2, a=a, tg=tg: q.tensor_tensor(out=y[:, a, tsl(tg)], in0=t0[:], in1=t2[:], op=ALU.mult),
                     reads=[k0, k2], writes=[yk(a, tg)])


        yield None

    def stream_conv():
        identb = wd[1][:, 1, 0:128]
        yield P.dma("sp", identb, identb_d, writes=["identb"])
        dg = [wd[1][:, 0, i * 128:(i + 1) * 128] for i in range(4)]
        ubf = abuf[:].rearrange("p f t -> p (f t)")[:, 0:T + 32]
        for b in range(2):
            ub, ubk = S[0], "S0"
            acc, acck = S[1 + b], f"S{1 + b}"
            yield P.dma("sp", ub[:], uext[b * 128:(b + 1) * 128, :], writes=[ubk])
            yield P.op("act", lambda q: q.activation(out=ubf, in_=ub[:], func=AF.Copy), reads=[ubk], writes=["ubf"])
            pss = [(pp.t[1 + i_], pp.k[1 + i_]) for i_ in range(4)]
            for j in range(B_KERNEL):
                d_, dk_ = dg[j % 4], f"dg{j % 4}"
                yield P.op("dve", lambda q, d_=d_, j=j, b=b: q.tensor_scalar(out=d_, in0=identb, scalar1=sm[:, O_CW + b * 31 + j:O_CW + b * 31 + j + 1],
                                                                     scalar2=None, op0=ALU.mult), reads=["identb", "sm"], writes=[dk_])
                for tg in range(4):
                    ps, pk = pss[tg]
                    yield P.op("pe", lambda q, ps=ps, d_=d_, j=j, tg=tg: q.matmul(ps[:, :], lhsT=d_, rhs=ubf[:, tg * 512 + j:tg * 512 + j + 512],
                                                                            start=(j == 0), stop=(j == B_KERNEL - 1)),
                         reads=[dk_, "ubf"], writes=[pk])
            for tg in range(4):
                ps, pk = pss[tg]
                yield P.op("act", lambda q, ps=ps, acc=acc, tg=tg, b=b: q.activation(out=acc[:, tsl(tg)], in_=ps[:], func=AF.Identity,
                                                                             bias=sm[:, O_CB + b:O_CB + b + 1], scale=1.0),
                     reads=[pk, "sm"], writes=[acck])

        yield None

    def stream_CD():
        for a in range(2):
            rows = slice(a * 128, (a + 1) * 128)
            for tg in range(4):
                n0, kn0 = getc()
                d0, kd0 = getc()
                yield P.dma("sp", n0[:], numC[0, rows, tsl(tg)], writes=[kn0])
                yield P.dma("sp", d0[:], denC[0, rows, tsl(tg)], writes=[kd0])
                n1, kn1 = getc()
                d1, kd1 = getc()
                for p in (1, 2):
                    yield P.dma("sp", n1[:], numC[p, rows, tsl(tg)], writes=[kn1])
                    yield P.dma("sp", d1[:], denC[p, rows, tsl(tg)], writes=[kd1])
                    yield P.op("dve", lambda q, n0=n0, n1=n1: q.tensor_tensor(out=n0[:], in0=n0[:], in1=n1[:], op=ALU.add), reads=[kn0, kn1], writes=[kn0])
                    yield P.op("dve", lambda q, d0=d0, d1=d1: q.tensor_tensor(out=d0[:], in0=d0[:], in1=d1[:], op=ALU.add), reads=[kd0, kd1], writes=[kd0])
                yield P.op("dve", lambda q, d0=d0: q.reciprocal(out=d0[:], in_=d0[:]), reads=[kd0], writes=[kd0])
                yield P.op("dve", lambda q, n0=n0, d0=d0, a=a, tg=tg: q.tensor_tensor(out=y[:, 4 + a, tsl(tg)], in0=n0[:], in1=d0[:], op=ALU.mult),
                     reads=[kn0, kd0], writes=[yk(4 + a, tg)])
                n0, kn0 = getc()
                d0, kd0 = getc()
                yield P.dma("sp", n0[:], numD[rows, tsl(tg)], writes=[kn0])
                yield P.dma("sp", d0[:], denD[rows, tsl(tg)], writes=[kd0])
                yield P.op("dve", lambda q, d0=d0, a=a: q.tensor_scalar(out=d0[:], in0=d0[:], scalar1=dcol[:, ESINK + a:ESINK + a + 1], scalar2=None,
                                                                op0=ALU.add), reads=[kd0, "dcol"], writes=[kd0])
                yield P.op("dve", lambda q, d0=d0: q.reciprocal(out=d0[:], in_=d0[:]), reads=[kd0], writes=[kd0])
                yield P.op("dve", lambda q, n0=n0, d0=d0, a=a, tg=tg: q.tensor_tensor(out=y[:, 6 + a, tsl(tg)], in0=n0[:], in1=d0[:], op=ALU.mult),
                     reads=[kn0, kd0], writes=[yk(6 + a, tg)])


        yield None

    def drain(*gens):
        gens = list(gens)
        while gens:
            for g in list(gens):
                try:
                    next(g)
                except StopIteration:
                    gens.remove(g)

    drain(stream_A(), stream_conv(), stream_CD())
    ln_fm([(lambda tg, b=b: S[1 + b][:, tsl(tg)], lambda tg, b=b: f"S{1 + b}") for b in range(2)], 256,
          lambda i: sm[:, O_BNW + i:O_BNW + i + 1], lambda i: sm[:, O_BNB + i:O_BNB + i + 1],
          [(lambda tg, b=b: y[:, 2 + b, tsl(tg)], lambda tg, b=b: yk(2 + b, tg)) for b in range(2)], silu=True)

    for k in range(8):
        for tg in range(4):
            P.op("act", lambda q, k=k, tg=tg: q.activation(out=x[:, k, tsl(tg)], in_=x[:, k, tsl(tg)], func=AF.Copy, scale=DEEPNORM_ALPHA),
                 reads=[xk(k, tg)], writes=[xk(k, tg)])
    for dc in range(8):
        for tg in range(4):
            ps, pk = pp.get()
            for k in range(8):
                P.op("pe", lambda q, ps=ps, k=k, dc=dc, tg=tg: q.matmul(ps[:, :], lhsT=wo_s[:, k, dc * 128:(dc + 1) * 128], rhs=y[:, k, tsl(tg)],
                                                                        start=(k == 0), stop=(k == 7)), reads=["wu0", yk(k, tg)], writes=[pk])
            P.op("dve", lambda q, ps=ps, dc=dc, tg=tg: q.scalar_tensor_tensor(out=x[:, dc, tsl(tg)], in0=ps[:], scalar=dcol[:, G1 + dc:G1 + dc + 1],
                                                                             in1=x[:, dc, tsl(tg)], op0=ALU.mult, op1=ALU.add),
                 reads=[pk, "dcol", xk(dc, tg)], writes=[xk(dc, tg)])
    xs = [(lambda tg, k=k: x[:, k, tsl(tg)], lambda tg, k=k: xk(k, tg)) for k in range(8)]
    ln_fm(xs, 1024, lambda i: sm[:, O_LNW + i:O_LNW + i + 1], lambda i: sm[:, O_LNB + i:O_LNB + i + 1], xs)

    for k in range(8):
        for tg in range(4):
            P.op("dve", lambda q, k=k, tg=tg: q.tensor_scalar(out=y[:, k, tsl(tg)], in0=x[:, k, tsl(tg)], scalar1=dcol[:, SC2 + k:SC2 + k + 1],
                                                            scalar2=modc[:, 24 + k:25 + k], op0=ALU.mult, op1=ALU.add),
                 reads=[xk(k, tg), "dcol", "modc"], writes=[yk(k, tg)])

    gT = None
    if moe:
        wr = P.sb("wr", [128, 8, 8], F32)
        P.dma("sp", wr[:], wr_d.rearrange("(k p) e -> p k e", p=128), writes=["wr"])
        sel = P.sb("sel", [8, 8 * 128], F32)
        P.dma("sp", sel[:], sel_d, writes=["sel"])
        identf = P.sb("identf", [128, 128], F32)
        P.dma("sp", identf[:], identf_d, writes=["identf"])
        psl, pslk = pp.get()
        for k in range(8):
            h2f, hk = S[0], "S0"
            P.op("dve", lambda q, k=k: q.tensor_scalar(out=h2f[:, 0:T], in0=x[:, k, :], scalar1=dcol[:, SC2 + k:SC2 + k + 1],
                                                       scalar2=modc[:, 24 + k:25 + k], op0=ALU.mult, op1=ALU.add),
                 reads=[xk(k, tg) for tg in range(4)] + ["dcol", "modc"], writes=[hk])
            for ti in range(16):
                P.op("pe", lambda q, k=k, ti=ti: q.matmul(psl[:, ti * 8:(ti + 1) * 8], lhsT=h2f[:, ti * 128:(ti + 1) * 128], rhs=wr[:, k, :],
                                                          start=(k == 0 and ti == 0), stop=(k == 7 and ti == 15), skip_group_check=True),
                     reads=[hk, "wr"], writes=[pslk])
        lg = P.sb("lg", [128, 16, 8], F32)
        lg2 = P.sb("lg2", [128, 16, 8], F32)
        eq1 = S[1][:, 0:128].rearrange("p (t e) -> p t e", e=8)
        eq2 = S[1][:, 128:256].rearrange("p (t e) -> p t e", e=8)
        m1 = P.sb("m1", [128, 16], F32)
        m2 = P.sb("m2", [128, 16], F32)
        g1 = P.sb("g1", [128, 16], F32)
        bc3 = lambda t: t[:].unsqueeze(2).broadcast_to([128, 16, 8])
        P.op("dve", lambda q: q.tensor_copy(out=lg[:], in_=psl[:, 0:128].rearrange("p (t e) -> p t e", e=8)), reads=[pslk], writes=["lg"])
        P.op("dve", lambda q: q.tensor_reduce(out=m1[:], in_=lg[:], axis=AX.X, op=ALU.max), reads=["lg"], writes=["m1"])
        P.op("dve", lambda q: q.tensor_tensor(out=eq1, in0=lg[:], in1=bc3(m1), op=ALU.is_equal), reads=["lg", "m1"], writes=["eq1"])
        P.op("dve", lambda q: q.scalar_tensor_tensor(out=lg2[:], in0=eq1, scalar=-1e30, in1=lg[:], op0=ALU.mult, op1=ALU.add),
             reads=["eq1", "lg"], writes=["lg2"])
        P.op("dve", lambda q: q.tensor_reduce(out=m2[:], in_=lg2[:], axis=AX.X, op=ALU.max), reads=["lg2"], writes=["m2"])
        P.op("dve", lambda q: q.tensor_tensor(out=eq2, in0=lg2[:], in1=bc3(m2), op=ALU.is_equal), reads=["lg2", "m2"], writes=["eq2"])
        P.op("dve", lambda q: q.tensor_tensor(out=m2[:], in0=m2[:], in1=m1[:], op=ALU.subtract), reads=["m2", "m1"], writes=["m2"])
        P.op("act", lambda q: q.activation(out=m2[:], in_=m2[:], func=AF.Exp), reads=["m2"], writes=["m2"])
        P.op("dve", lambda q: q.tensor_scalar(out=g1[:], in0=m2[:], scalar1=1.0, scalar2=None, op0=ALU.add), reads=["m2"], writes=["g1"])
        P.op("dve", lambda q: q.reciprocal(out=g1[:], in_=g1[:]), reads=["g1"], writes=["g1"])
        P.op("dve", lambda q: q.tensor_tensor(out=m2[:], in0=m2[:], in1=g1[:], op=ALU.mult), reads=["m2", "g1"], writes=["m2"])
        P.op("dve", lambda q: q.tensor_tensor(out=eq1, in0=eq1, in1=bc3(g1), op=ALU.mult), reads=["eq1", "g1"], writes=["eq1"])
        P.op("dve", lambda q: q.tensor_tensor(out=eq2, in0=eq2, in1=bc3(m2), op=ALU.mult), reads=["eq2", "m2"], writes=["eq2"])
        P.op("dve", lambda q: q.tensor_tensor(out=eq1, in0=eq1, in1=eq2, op=ALU.add), reads=["eq1", "eq2"], writes=["eq1"])
        gT = S[0][0:8, 0:T]
        for tg in range(4):
            ps, pk = pp.get()
            for j in range(4):
                ti = tg * 4 + j
                P.op("pe", lambda q, ps=ps, j=j, ti=ti: q.transpose(ps[0:8, j * 128:(j + 1) * 128], eq1[:, ti, :], identf[:]),
                     reads=["eq1", "identf"], writes=[pk])
            P.op("act", lambda q, ps=ps, tg=tg: q.activation(out=gT[:, tsl(tg)], in_=ps[0:8, :], func=AF.Copy), reads=[pk], writes=["S0"])

    for k in range(8):
        for tg in range(4):
            P.op("act", lambda q, k=k, tg=tg: q.activation(out=x[:, k, tsl(tg)], in_=x[:, k, tsl(tg)], func=AF.Copy, scale=DEEPNORM_ALPHA),
                 reads=[xk(k, tg)], writes=[xk(k, tg)])

    blocks = []
    if moe:
        for e in range(N_EXPERTS):
            for (f0, nf) in chunks(EXPERT_DIM // 128, FB):
                blocks.append((w_up[e], w_down[e], EXPERT_DIM, f0, nf, e))
    else:
        for (f0, nf) in chunks(FFN_DIM // 128, FB):
            blocks.append((w_up, w_down, FFN_DIM, f0, nf, None))

    stg = [S[2][:, i * 512:(i + 1) * 512] for i in range(4)]
    stg_n = [0]

    def block_pieces(bi):
        wup, wdn, F, f0, nf, e = blocks[bi]
        b2 = bi % 2
        wuv = wu[b2][:].rearrange("p (k s c) -> p k s c", k=8, s=2)
        upv = wup.rearrange("(k p) c -> p k c", p=128)
        pcs = []
        for k in range(8):
            for s_ in range(2):
                pcs.append((wuv[:, k, s_, 0:nf * 128], upv[:, k, s_ * F + f0 * 128:s_ * F + (f0 + nf) * 128], nf * 128, f"wu{b2}"))
        for fc in range(nf):
            for hf in range(2):
                pcs.append((wd[b2][:, fc, hf * 512:(hf + 1) * 512],
                            wdn[(f0 + fc) * 128:(f0 + fc + 1) * 128, hf * 512:(hf + 1) * 512], 512, f"wd{b2}"))
        return pcs

    def emit_piece(pc):
        dst, src, n, dkey = pc
        i = stg_n[0] % 4
        first = stg_n[0] < 4
        stg_n[0] += 1
        P.dma("sp", stg[i][:, 0:n], src, writes=(["S2", f"stg{i}"] if first else [f"stg{i}"]))
        P.op("act", lambda q: q.activation(out=dst, in_=stg[i][:, 0:n], func=AF.Copy), reads=[f"stg{i}"], writes=[dkey])

    for pc in block_pieces(0):
        emit_piece(pc)
    for bi, (wup, wdn, F, f0, nf, e) in enumerate(blocks):
        b2 = bi % 2
        nxt = block_pieces(bi + 1) if bi + 1 < len(blocks) else []
        per_it = -(-len(nxt) // (nf * 4)) if nxt else 0
        gmul = None
        gk = None
        if moe:
            gmul, gk = S[1], "S1"
            if f0 == 0:
                for tg in range(4):
                    ps, pk = pp.get()
                    P.op("pe", lambda q, ps=ps, e=e, tg=tg: q.matmul(ps[:, :], lhsT=sel[:, e * 128:(e + 1) * 128], rhs=gT[:, tsl(tg)],
                                                                    start=True, stop=True), reads=["sel", "S0"], writes=[pk])
                    P.op("act", lambda q, ps=ps, gmul=gmul, tg=tg: q.activation(out=gmul[:, tsl(tg)], in_=ps[:], func=AF.Copy),
                         reads=[pk], writes=[gk + f"_{tg}"])
        wuv = wu[b2][:].rearrange("p (k s c) -> p k s c", k=8, s=2)
        for fc in range(nf):
            for tg in range(4):
                psg, kg = pp.get()
                psu, ku = pp.get()
                for s_, ps_, pk_ in ((0, psg, kg), (1, psu, ku)):
                    for k in range(8):
                        P.op("pe", lambda q, ps_=ps_, k=k, s_=s_, fc=fc, tg=tg: q.matmul(ps_[:, :], lhsT=wuv[:, k, s_, fc * 128:(fc + 1) * 128],
                                                                                      rhs=y[:, k, tsl(tg)], start=(k == 0), stop=(k == 7)),
                             reads=[f"wu{b2}", yk(k, tg)], writes=[pk_])
                sg, sgk = gett()
                P.op("act", lambda q, sg=sg, psg=psg: q.activation(out=sg[:], in_=psg[:], func=AF.Silu), reads=[kg], writes=[sgk])
                if gmul is not None:
                    P.op("pool", lambda q, sg=sg, gmul=gmul, tg=tg: q.tensor_tensor(out=sg[:], in0=sg[:], in1=gmul[:, tsl(tg)], op=ALU.mult),
                         reads=[sgk, gk + f"_{tg}"], writes=[sgk])
                P.op("dve", lambda q, sg=sg, psu=psu, fc=fc, tg=tg: q.tensor_tensor(out=abuf[:, fc, tsl(tg)], in0=sg[:], in1=psu[:], op=ALU.mult),
                     reads=[sgk, ku], writes=[f"a{fc}_{tg}"])
                for _ in range(per_it):
                    if nxt:
                        emit_piece(nxt.pop(0))
        while nxt:
            emit_piece(nxt.pop(0))
        for dc in range(8):
            for tg in range(4):
                ps, pk = pp.get()
                for fc in range(nf):
                    P.op("pe", lambda q, ps=ps, fc=fc, dc=dc, tg=tg: q.matmul(ps[:, :], lhsT=wd[b2][:, fc, dc * 128:(dc + 1) * 128],
                                                                              rhs=abuf[:, fc, tsl(tg)], start=(fc == 0), stop=(fc == nf - 1)),
                         reads=[f"wd{b2}", f"a{fc}_{tg}"], writes=[pk])
                P.op("dve", lambda q, ps=ps, dc=dc, tg=tg: q.scalar_tensor_tensor(out=x[:, dc, tsl(tg)], in0=ps[:], scalar=dcol[:, G2 + dc:G2 + dc + 1],
                                                                                 in1=x[:, dc, tsl(tg)], op0=ALU.mult, op1=ALU.add),
                     reads=[pk, "dcol", xk(dc, tg)], writes=[xk(dc, tg)])

    ln_fm(xs, 1024, lambda i: sm[:, O_LNW + 8 + i:O_LNW + 9 + i], lambda i: sm[:, O_LNB + 8 + i:O_LNB + 9 + i], xs)
    xo_v = xo.rearrange("(k p) t -> p k t", p=128)
    for k in range(8):
        P.dma("sp", xo_v[:, k, :], x[:, k, :], reads=[xk(k, tg) for tg in range(4)], is_output=True)
    return P.finish()


def run_k3(layer, xT_shards, mods, of, ob, o32_full, numC, denC, numD, denD, inp):
    moe = (layer % 2 == 1)
    T = TOK
    sm = np.zeros((128, 128), np.float32)
    anw = np.asarray(inp["a_norm_w"])[layer]
    sm[:, 0] = np.concatenate([anw, anw])
    cw = np.asarray(inp["b_conv_w"])[layer]
    for b in range(2):
        sm[:, 1 + b * 31:1 + (b + 1) * 31] = cw[:, b * 128:(b + 1) * 128].T
    sm[:, 63:65] = col128(np.asarray(inp["b_conv_b"])[layer])
    sm[:, 65:67] = col128(np.asarray(inp["b_norm_w"])[layer])
    sm[:, 67:69] = col128(np.asarray(inp["b_norm_b"])[layer])
    sink = np.asarray(inp["d_sink"])[layer]
    sm[:, 69:71] = col128(np.repeat(sink, 64))
    sm[:, 71:79] = col128(np.asarray(inp["ln_w"])[layer, 0])
    sm[:, 79:87] = col128(np.asarray(inp["ln_w"])[layer, 1])
    sm[:, 87:95] = col128(np.asarray(inp["ln_b"])[layer, 0])
    sm[:, 95:103] = col128(np.asarray(inp["ln_b"])[layer, 1])
    blk64 = np.kron(np.eye(2, dtype=np.float32), np.ones((64, 64), np.float32))
    u = o32_full["u"]
    upad = np.zeros((256, SEQ + 32), np.float32)
    upad[:, 15:15 + SEQ] = u
    denC_rep = np.repeat(denC, 64, axis=1)
    denD_rep = np.repeat(denD, 64, axis=0)
    w_out = np.ascontiguousarray(inp["w_out"][layer])
    common = {"smalls": sm, "blk64": blk64, "w_out": w_out, "identb": np.eye(128, dtype=np.float32).astype(ml_dtypes.bfloat16)}
    if moe:
        li = layer // 2
        sel = np.zeros((8, 8 * 128), np.float32)
        for e in range(8):
            sel[e, e * 128:(e + 1) * 128] = 1.0
        common.update({"wr": np.ascontiguousarray(inp["moe_router"][li]), "w_up": np.ascontiguousarray(inp["moe_w_up"][li]),
                       "w_down": np.ascontiguousarray(inp["moe_w_down"][li]), "sel": sel, "identf": np.eye(128, dtype=np.float32)})
    else:
        li = layer // 2
        common.update({"w_up": np.ascontiguousarray(inp["ffn_w_up"][li]), "w_down": np.ascontiguousarray(inp["ffn_w_down"][li])})
    in_maps = []
    for c in range(NCORES):
        ts = slice(c * T, (c + 1) * T)
        m = dict(common)
        m.update({"xT": xT_shards[c], "modc": mods[c], "ofT": np.ascontiguousarray(of[:, ts]), "obT": np.ascontiguousarray(ob[:, ts]),
                  "gaT": np.ascontiguousarray(o32_full["ga"][:, ts]), "uext": np.ascontiguousarray(upad[:, c * T:c * T + T + 32]),
                  "numC": np.ascontiguousarray(numC[:, :, ts]), "denC": np.ascontiguousarray(denC_rep[:, :, ts]),
                  "numD": np.ascontiguousarray(numD[:, ts]), "denD": np.ascontiguousarray(denD_rep[:, ts])})
        in_maps.append(m)
    res = run(("k3", moe), lambda: build_k3(moe), in_maps)
    return [r["xo"] for r in res]


def run_layer(layer, xT_shards, inp, modc=None):
    if modc is None:
        modc = run_k0(inp)[layer]
    r1 = run_k1(layer, xT_shards, modc, inp)
    o32_full = {nm: np.concatenate([r["o32"][off:off + 256] for r in r1], axis=1) for nm, off in O32.items()}
    obf_full = {nm: np.concatenate([r["obf"][off:off + (128 if nm in ("dk", "dv") else 256)] for r in r1], axis=1)
                for nm, off in OBF.items()}
    mods = [modc for _ in range(NCORES)]
    of, ob = run_k2a(o32_full)
    numC, denC, numD, denD = run_k2b(obf_full)
    return run_k3(layer, xT_shards, mods, of, ob, o32_full, numC, denC, numD, denD, inp)


def kernel(**inp):
    inp = {k: np.asarray(v) for k, v in inp.items()}
    x = inp["x"][0]
    xT_shards = [np.ascontiguousarray(x[c * TOK:(c + 1) * TOK].T) for c in range(NCORES)]
    modcs = run_k0(inp)
    for layer in range(DEPTH):
        xT_shards = run_layer(layer, xT_shards, inp, modcs[layer])
    out = np.concatenate([s.T for s in xT_shards], axis=0)[None]
    return np.ascontiguousarray(out.astype(np.float32))
```

```python
import math
from contextlib import ExitStack

import numpy as np
import ml_dtypes

import concourse.bass as bass
import concourse.mybir as mybir
from concourse.bass_utils import run_bass_kernel_spmd

F32 = mybir.dt.float32
BF16 = mybir.dt.bfloat16
I32 = mybir.dt.int32
AF = mybir.ActivationFunctionType
ALU = mybir.AluOpType
AX = mybir.AxisListType

NCORES = 8
D_MODEL = 1024
SEQ = 16384
TOK = SEQ // NCORES
DEPTH = 2
HD = 64
FFN_DIM = 2816
N_EXPERTS = 8
EXPERT_DIM = 3584
B_KERNEL = 31
ROPE_THETA = 500000.0
ROPE_DIM = 16
DEEPNORM_ALPHA = (2 * DEPTH) ** 0.25
LN_EPS = 1e-5
RMS_EPS = 1e-6
NEG = -30000.0
TWO_PI = 2.0 * math.pi


class Prog:
    NDS = 20

    def __init__(self):
        self.nc = bass.Bass("TRN2", target_bir_lowering=False)
        nc = self.nc
        self.es = ExitStack()
        self.q = {"pe": nc.tensor, "dve": nc.vector, "act": nc.scalar, "pool": nc.gpsimd, "sp": nc.sync}
        self.esem = {e: self.es.enter_context(nc.semaphore("es_" + e)) for e in ("pe", "dve", "act", "pool")}
        self.ecnt = {e: 0 for e in self.esem}
        self.dsem = [self.es.enter_context(nc.semaphore(f"ds{i}")) for i in range(2 * self.NDS)]
        self.dval = [0] * (2 * self.NDS)
        self.dnext = {False: 0, True: 0}
        self.seen = {e: {} for e in self.q}
        self.lastw = {}
        self.readers = {}
        self.out_tokens = []
        self.n_inst = 0
        self._ps_id = 0

    def dram_in(self, name, shape, dt):
        return self.nc.dram_tensor(name, list(shape), dt, kind="ExternalInput").ap()

    def dram_out(self, name, shape, dt):
        return self.nc.dram_tensor(name, list(shape), dt, kind="ExternalOutput").ap()

    def sb(self, name, shape, dt):
        return self.es.enter_context(self.nc.sbuf_tensor("sb_" + name, list(shape), dt))

    def ps(self, name, shape, dt=F32):
        return self.es.enter_context(self.nc.psum_tensor("pm_" + name, list(shape), dt))

    def _wait(self, e, tok):
        sem, v, owner = tok
        if owner == e and e == "pe":
            return
        k = id(sem)
        if self.seen[e].get(k, 0) >= v:
            return
        self.q[e].wait_ge(sem, v)
        self.seen[e][k] = v

    def _deps(self, e, reads, writes):
        for k in reads:
            t = self.lastw.get(k)
            if t is not None:
                self._wait(e, t)
        for k in writes:
            t = self.lastw.get(k)
            if t is not None:
                self._wait(e, t)
            for t in self.readers.get(k, {}).values():
                self._wait(e, t)

    def _record(self, tok, reads, writes):
        for k in writes:
            self.lastw[k] = tok
            self.readers[k] = {}
        for k in reads:
            self.readers.setdefault(k, {})[id(tok[0])] = tok

    def op(self, e, fn, reads=(), writes=()):
        self._deps(e, reads, writes)
        inst = fn(self.q[e])
        self.ecnt[e] += 1
        inst.then_inc(self.esem[e], 1)
        tok = (self.esem[e], self.ecnt[e], e)
        self._record(tok, reads, writes)
        self.n_inst += 1
        return tok

    def dma(self, e, out, in_, reads=(), writes=(), is_output=False, **kw):
        self._deps(e, reads, writes)
        sw = (e == "pool")
        j = self.dnext[sw] + (self.NDS if sw else 0)
        self.dnext[sw] = (self.dnext[sw] + 1) % self.NDS
        if self.dval[j] > 0:
            self._wait(e, (self.dsem[j], self.dval[j], "dma"))
        self.q[e].dma_start(out=out, in_=in_, **kw).then_inc(self.dsem[j], 16)
        self.dval[j] += 16
        tok = (self.dsem[j], self.dval[j], "dma")
        self._record(tok, reads, writes)
        if is_output:
            self.out_tokens.append(tok)
        self.n_inst += 1
        return tok

    def finish(self):
        for j in range(2 * self.NDS):
            if self.dval[j] > 0:
                self._wait("sp", (self.dsem[j], self.dval[j], "dma"))
        return self.nc


def chunks(n, c):
    return [(i, min(c, n - i)) for i in range(0, n, c)]


class PsumPool:
    def __init__(self, P, n=8, prefix="pb"):
        self.t = [P.ps(f"{prefix}{i}", [128, 512], F32) for i in range(n)]
        self.k = [f"{prefix}{i}" for i in range(n)]
        self.i = 0
        self.n = n

    def get(self):
        i = self.i
        self.i = (self.i + 1) % self.n
        return self.t[i], self.k[i]


def build_consts(P):
    c = {}
    c["ones_f"] = P.sb("ones_f", [128, 512], F32)
    P.op("pool", lambda q: q.memset(c["ones_f"][:], 1.0), writes=["ones_f"])
    c["ones_b"] = P.sb("ones_b", [128, 512], BF16)
    P.op("pool", lambda q: q.memset(c["ones_b"][:], 1.0), writes=["ones_b"])
    return c


CH = {"aq": (0, 2), "aff": (2, 2), "afb": (4, 2), "ai": (6, 2), "ag": (8, 2), "bv": (10, 2), "bg": (12, 2),
      "cq": (14, 2), "ck": (16, 2), "cv": (18, 2), "dq": (20, 2), "dk": (22, 1), "dv": (23, 1)}
ROT_CHUNKS = [14, 15, 16, 17, 20, 21, 22]
O32 = {"qa": 0, "kf": 256, "lf": 512, "kb": 768, "lb": 1024, "va": 1280, "ga": 1536, "u": 1792}
OBF = {"cq": 0, "ck": 256, "cv": 512, "dq": 768, "dk": 1024, "dv": 1152}
NBF = 1280


def build_k1(layer):
    P = Prog()
    nc = P.nc
    T = TOK
    xT = P.dram_in("xT", [D_MODEL, T], F32)
    modc_in = P.dram_in("modc", [128, 48], F32)
    w_in = P.dram_in("w_in", [D_MODEL, 3072], F32)
    w_sw = P.dram_in("w_sw", [D_MODEL, 7 * 128], F32)
    pos = P.dram_in("pos", [T], I32)
    rcol = P.dram_in("rcol", [128, 2], F32)
    alb = P.dram_in("alb", [128, 4], F32)
    o32 = P.dram_out("o32", [2048, T], F32)
    obf = P.dram_out("obf", [NBF, T], BF16)

    C = build_consts(P)
    pp = PsumPool(P)

    mod = P.sb("mod", [128, 48], F32)
    P.dma("sp", mod[:], modc_in, writes=["mod"])
    sc1 = P.sb("sc1", [128, 8], F32)
    P.op("dve", lambda q: q.tensor_scalar(out=sc1[:], in0=mod[:, 8:16], scalar1=1.0, scalar2=None, op0=ALU.add),
         reads=["mod"], writes=["sc1"])

    hT = P.sb("hT", [128, 8, T], BF16)
    xst = [P.sb(f"xst{i}", [128, T], F32) for i in range(2)]
    xT_v = xT.rearrange("(k p) t -> p k t", p=128)
    for k in range(8):
        st = xst[k % 2]
        sk = f"xst{k % 2}"
        P.dma("sp", st[:], xT_v[:, k, :], writes=[sk])
        P.op("dve", lambda q, k=k, st=st: q.tensor_scalar(out=hT[:, k, :], in0=st[:], scalar1=sc1[:, k:k + 1],
                                                        scalar2=mod[:, k:k + 1], op0=ALU.mult, op1=ALU.add),
             reads=[sk, "sc1", "mod"], writes=[f"hT{k}"])
    hkeys = [f"hT{k}" for k in range(8)]

    wb = P.sb("wb", [128, 8, 3072], BF16)
    wsw = P.sb("wsw", [128, 8, 896], BF16)
    w_in_v = w_in.rearrange("(k p) e -> p k e", p=128)
    w_sw_v = w_sw.rearrange("(k p) e -> p k e", p=128)
    for (c0, nch) in ((10, 4), (0, 4), (4, 4), (8, 2)):
        P.dma("pool", wb[:, :, c0 * 128:(c0 + nch) * 128], w_in_v[:, :, c0 * 128:(c0 + nch) * 128], writes=[f"wbc{c}" for c in range(c0, c0 + nch)])
    P.dma("pool", wb[:, :, 14 * 128:18 * 128], w_in_v[:, :, 14 * 128:18 * 128], writes=[f"wbc{c}" for c in range(14, 18)])
    P.dma("pool", wsw[:, :, 0:4 * 128], w_sw_v[:, :, 0:4 * 128], writes=[f"wswc{c}" for c in range(0, 4)])
    P.dma("pool", wb[:, :, 18 * 128:24 * 128], w_in_v[:, :, 18 * 128:24 * 128], writes=[f"wbc{c}" for c in range(18, 24)])
    P.dma("pool", wsw[:, :, 4 * 128:7 * 128], w_sw_v[:, :, 4 * 128:7 * 128], writes=[f"wswc{c}" for c in range(4, 7)])

    rc = P.sb("rc", [128, 2], F32)
    P.dma("sp", rc[:], rcol, writes=["rc"])
    tmpi = P.sb("tmpi", [128, T], I32)
    P.dma("sp", tmpi[:], pos.partition_broadcast(128), writes=["tmpi"])
    ang = P.sb("ang", [128, T], F32)
    cosT = P.sb("cosT", [128, T], F32)
    sinT = P.sb("sinT", [128, T], F32)
    tmpf = P.sb("tmpf", [128, T], F32)
    P.op("dve", lambda q: q.tensor_copy(out=ang[:], in_=tmpi[:]), reads=["tmpi"], writes=["ang"])
    P.op("dve", lambda q: q.tensor_scalar(out=ang[:], in0=ang[:], scalar1=rc[:, 0:1], scalar2=None, op0=ALU.mult),
         reads=["ang", "rc"], writes=["ang"])
    C1 = 6.28125
    C2 = TWO_PI - C1

    def sin_table(dst, dkey, phase):
        P.op("dve", lambda q: q.tensor_scalar(out=tmpf[:], in0=ang[:], scalar1=phase, scalar2=1.0 / TWO_PI,
                                              op0=ALU.add, op1=ALU.mult), reads=["ang"], writes=["tmpf"])
        P.op("dve", lambda q: q.tensor_copy(out=tmpi[:], in_=tmpf[:]), reads=["tmpf"], writes=["tmpi"])
        P.op("dve", lambda q: q.tensor_copy(out=tmpf[:], in_=tmpi[:]), reads=["tmpi"], writes=["tmpf"])
        P.op("dve", lambda q: q.scalar_tensor_tensor(out=dst[:], in0=tmpf[:], scalar=-C1, in1=ang[:],
                                                     op0=ALU.mult, op1=ALU.add), reads=["tmpf", "ang"], writes=[dkey])
        P.op("dve", lambda q: q.scalar_tensor_tensor(out=dst[:], in0=tmpf[:], scalar=-C2, in1=dst[:],
                                                     op0=ALU.mult, op1=ALU.add), reads=["tmpf", dkey], writes=[dkey])
        P.op("dve", lambda q: q.tensor_scalar(out=dst[:], in0=dst[:], scalar1=phase, scalar2=-math.pi,
                                              op0=ALU.add, op1=ALU.max), reads=[dkey], writes=[dkey])
        P.op("dve", lambda q: q.tensor_scalar(out=dst[:], in0=dst[:], scalar1=math.pi, scalar2=None,
                                              op0=ALU.min), reads=[dkey], writes=[dkey])
        P.op("act", lambda q: q.activation(out=dst[:], in_=dst[:], func=AF.Sin), reads=[dkey], writes=[dkey])

    sin_table(cosT, "cosT", math.pi / 2)
    sin_table(sinT, "sinT", 0.0)
    P.op("dve", lambda q: q.tensor_scalar(out=sinT[:], in0=sinT[:], scalar1=rc[:, 1:2], scalar2=None, op0=ALU.mult),
         reads=["sinT", "rc"], writes=["sinT"])

    albs = P.sb("albs", [128, 4], F32)
    P.dma("sp", albs[:], alb, writes=["albs"])
    lbc = P.sb("lbc", [128, 2], F32)
    oml = P.sb("oml", [128, 2], F32)
    if layer == 0:
        P.op("pool", lambda q: q.memset(lbc[:], 0.0), writes=["lbc"])
        P.op("pool", lambda q: q.memset(oml[:], 1.0), writes=["oml"])
    else:
        ex = P.sb("alb_ex", [128, 4], F32)
        P.op("act", lambda q: q.activation(out=ex[:], in_=albs[:], func=AF.Exp), reads=["albs"], writes=["alb_ex"])
        exv = ex[:].rearrange("p (t l) -> p t l", l=2)
        sm = P.sb("alb_sm", [128, 2], F32)
        P.op("dve", lambda q: q.tensor_tensor(out=sm[:], in0=exv[:, :, 0], in1=exv[:, :, 1], op=ALU.add),
             reads=["alb_ex"], writes=["alb_sm"])
        P.op("dve", lambda q: q.reciprocal(out=sm[:], in_=sm[:]), reads=["alb_sm"], writes=["alb_sm"])
        P.op("dve", lambda q: q.tensor_tensor(out=lbc[:], in0=exv[:, :, 1], in1=sm[:], op=ALU.mult),
             reads=["alb_ex", "alb_sm"], writes=["lbc"])
        P.op("dve", lambda q: q.tensor_scalar(out=oml[:], in0=lbc[:], scalar1=-1.0, scalar2=1.0, op0=ALU.mult,
                                              op1=ALU.add), reads=["lbc"], writes=["oml"])

    ob_f = [P.sb(f"obf{i}", [128, 512], F32) for i in range(4)]
    ob_b = [P.sb(f"obb{i}", [128, 512], BF16) for i in range(4)]
    sg = [P.sb(f"sg{i}", [128, T], F32) for i in range(2)]
    cnt = {"f": 0, "b": 0}

    def getf():
        i = cnt["f"] % 4
        cnt["f"] += 1
        return ob_f[i], f"obf{i}"

    def getb():
        i = cnt["b"] % 4
        cnt["b"] += 1
        return ob_b[i], f"obb{i}"

    def proj(ps, psk, wt, wkey, col0, tg):
        for k in range(8):
            P.op("pe", lambda q, k=k: q.matmul(ps[:, :], lhsT=wt[:, k, col0:col0 + 128],
                                               rhs=hT[:, k, tg * 512:(tg + 1) * 512], start=(k == 0), stop=(k == 7)),
                 reads=[wkey, hkeys[k]], writes=[psk])

    def store32(name, tile_i, tg, buf, bkey):
        r0 = O32[name] + tile_i * 128
        P.dma("sp", o32[r0:r0 + 128, tg * 512:(tg + 1) * 512], buf[:], reads=[bkey], is_output=True)

    def storebf(name, tile_i, tg, buf, bkey):
        r0 = OBF[name] + tile_i * 128
        P.dma("sp", obf[r0:r0 + 128, tg * 512:(tg + 1) * 512], buf[:], reads=[bkey], is_output=True)

    order = ["bg", "bv", "aq", "aff", "afb", "ai", "ag", "cq", "ck", "cv", "dq", "dk", "dv"]
    for name in order:
        c0, ncn = CH[name]
        for ti in range(ncn):
            ch = c0 + ti
            for tg in range(4):
                tsl = slice(tg * 512, (tg + 1) * 512)
                ps, psk = pp.get()
                proj(ps, psk, wb, f"wbc{ch}", ch * 128, tg)
                if name == "bg":
                    P.op("act", lambda q, ps=ps, ti=ti, tsl=tsl: q.activation(out=sg[ti][:, tsl], in_=ps[:], func=AF.Sigmoid),
                         reads=[psk], writes=[f"sg{ti}_{tg}"])
                elif name == "bv":
                    b, bk = getf()
                    P.op("dve", lambda q, ps=ps, b=b, ti=ti, tsl=tsl: q.tensor_tensor(out=b[:], in0=ps[:], in1=sg[ti][:, tsl], op=ALU.mult),
                         reads=[psk, f"sg{ti}_{tg}"], writes=[bk])
                    store32("u", ti, tg, b, bk)
                elif name in ("aq", "ag"):
                    b, bk = getf()
                    P.op("act", lambda q, ps=ps, b=b: q.activation(out=b[:], in_=ps[:], func=AF.Silu), reads=[psk], writes=[bk])
                    store32("qa" if name == "aq" else "ga", ti, tg, b, bk)
                elif name == "ai":
                    b, bk = getf()
                    P.op("act", lambda q, ps=ps, b=b: q.activation(out=b[:], in_=ps[:], func=AF.Copy), reads=[psk], writes=[bk])
                    store32("va", ti, tg, b, bk)
                elif name in ("aff", "afb"):
                    fb_, fk = getf()
                    P.op("act", lambda q, ps=ps, fb_=fb_: q.activation(out=fb_[:], in_=ps[:], func=AF.Exp, scale=-1.0), reads=[psk], writes=[fk])
                    P.op("dve", lambda q, fb_=fb_: q.tensor_scalar(out=fb_[:], in0=fb_[:], scalar1=1.0, scalar2=None, op0=ALU.add), reads=[fk], writes=[fk])
                    P.op("dve", lambda q, fb_=fb_: q.reciprocal(out=fb_[:], in_=fb_[:]), reads=[fk], writes=[fk])
                    P.op("dve", lambda q, fb_=fb_, ti=ti: q.tensor_scalar(out=fb_[:], in0=fb_[:], scalar1=oml[:, ti:ti + 1],
                                                                        scalar2=lbc[:, ti:ti + 1], op0=ALU.mult, op1=ALU.add),
                         reads=[fk, "oml", "lbc"], writes=[fk])
                    kb_, kk = getf()
                    P.op("dve", lambda q, fb_=fb_, kb_=kb_: q.tensor_scalar(out=kb_[:], in0=fb_[:], scalar1=-1.0, scalar2=1.0,
                                                                          op0=ALU.mult, op1=ALU.add), reads=[fk], writes=[kk])
                    store32("kf" if name == "aff" else "kb", ti, tg, kb_, kk)
                    P.op("act", lambda q, fb_=fb_: q.activation(out=fb_[:], in_=fb_[:], func=AF.Ln), reads=[fk], writes=[fk])
                    store32("lf" if name == "aff" else "lb", ti, tg, fb_, fk)
                elif name in ("cv", "dv"):
                    b, bk = getb()
                    P.op("act", lambda q, ps=ps, b=b: q.activation(out=b[:], in_=ps[:], func=AF.Copy), reads=[psk], writes=[bk])
                    storebf(name, ti, tg, b, bk)
                else:
                    ri = ROT_CHUNKS.index(ch)
                    ps2, psk2 = pp.get()
                    proj(ps2, psk2, wsw, f"wswc{ri}", ri * 128, tg)
                    t1, t1k = getf()
                    t2, t2k = getf()
                    P.op("dve", lambda q, ps=ps, t1=t1, tsl=tsl: q.tensor_tensor(out=t1[:], in0=ps[:], in1=cosT[:, tsl], op=ALU.mult),
                         reads=[psk, "cosT"], writes=[t1k])
                    P.op("dve", lambda q, ps2=ps2, t2=t2, tsl=tsl: q.tensor_tensor(out=t2[:], in0=ps2[:], in1=sinT[:, tsl], op=ALU.mult),
                         reads=[psk2, "sinT"], writes=[t2k])
                    b, bk = getb()
                    P.op("pool", lambda q, t1=t1, t2=t2, b=b: q.tensor_tensor(out=b[:], in0=t1[:], in1=t2[:], op=ALU.add),
                         reads=[t1k, t2k], writes=[bk])
                    storebf(name, ti, tg, b, bk)
    return P.finish()


def col128(v):
    v = np.asarray(v)
    return np.ascontiguousarray(v.reshape(-1, 128).T)


def rot_cols():
    idx = []
    for ch in ROT_CHUNKS:
        for h in range(2):
            base = ch * 128 + h * 64
            loc = np.arange(64)
            loc[:8] = np.arange(8, 16)
            loc[8:16] = np.arange(0, 8)
            idx.append(base + loc)
    return np.concatenate(idx)


def rot_consts():
    half = ROPE_DIM // 2
    inv = (np.float32(ROPE_THETA) ** (-np.arange(half, dtype=np.float32) * np.float32(2.0 / ROPE_DIM))).astype(np.float32)
    rc = np.zeros((128, 2), np.float32)
    for p in range(128):
        d = p % 64
        if d < 16:
            rc[p, 0] = inv[d % 8]
            rc[p, 1] = -1.0 if d < 8 else 1.0
    return rc


_cache = {}


def run(nc_key, builder, in_maps):
    if nc_key not in _cache:
        _cache[nc_key] = builder()
    nc = _cache[nc_key]
    res = run_bass_kernel_spmd(nc, in_maps, core_ids=list(range(NCORES)))
    return res.results


def build_k0():
    P = Prog()
    ccol = P.dram_in("ccol", [128, 8], F32)
    w = P.dram_in("w", [D_MODEL, 1536], F32)
    bias = P.dram_in("bias", [1, 1536], F32)
    o = P.dram_out("o", [1, 1536], F32)
    sc = P.sb("sc", [128, 8], F32)
    P.dma("sp", sc[:], ccol, writes=["sc"])
    P.op("act", lambda q: q.activation(out=sc[:], in_=sc[:], func=AF.Silu), reads=["sc"], writes=["sc"])
    bs = P.sb("bs", [1, 1536], F32)
    P.dma("sp", bs[:], bias, writes=["bs"])
    ws = P.sb("ws", [128, 8, 1536], F32)
    wv = w.rearrange("(k p) e -> p k e", p=128)
    for g in range(3):
        P.dma("sp", ws[:, :, g * 512:(g + 1) * 512], wv[:, :, g * 512:(g + 1) * 512], writes=[f"ws{g}"])
    ob = P.sb("ob", [1, 1536], F32)
    pss = [P.ps(f"p{i}", [128, 512], F32) for i in range(3)]
    for g in range(3):
        for k in range(8):
            P.op("pe", lambda q, g=g, k=k: q.matmul(pss[g][0:1, :], lhsT=sc[:, k:k + 1], rhs=ws[:, k, g * 512:(g + 1) * 512],
                                                   start=(k == 0), stop=(k == 7)), reads=["sc", f"ws{g}"], writes=[f"p{g}"])
        P.op("dve", lambda q, g=g: q.tensor_tensor(out=ob[:, g * 512:(g + 1) * 512], in0=pss[g][0:1, :], in1=bs[:, g * 512:(g + 1) * 512], op=ALU.add),
             reads=[f"p{g}", "bs"], writes=["ob"])
    P.dma("sp", o, ob[:], reads=["ob"], is_output=True)
    return P.finish()


def run_k0(inp):
    wa = np.asarray(inp["w_ada"])
    wcat = np.concatenate([wa[l] for l in range(DEPTH)], axis=1)
    bcat = np.concatenate([np.asarray(inp["b_ada"])[l] for l in range(DEPTH)])[None, :]
    ccol = col128(np.asarray(inp["c"])[0])
    in_maps = [{"ccol": ccol, "w": np.ascontiguousarray(wcat[:, c * 1536:(c + 1) * 1536]),
                "bias": np.ascontiguousarray(bcat[:, c * 1536:(c + 1) * 1536])} for c in range(NCORES)]
    res = run(("k0",), build_k0, in_maps)
    mod = np.concatenate([r["o"][0] for r in res])
    return [col128(mod[l * 6144:(l + 1) * 6144]) for l in range(DEPTH)]


def run_k1(layer, xT_shards, modc, inp):
    rc = rot_consts()
    sw = rot_cols()
    w_in_l = np.ascontiguousarray(inp["w_in"][layer])
    w_sw = np.ascontiguousarray(w_in_l[:, sw])
    alb = np.zeros((128, 4), np.float32)
    a = np.asarray(inp["a_lower_bound"])
    for t in range(2):
        for l in range(2):
            alb[:, t * 2 + l] = a[l, t * 128:(t + 1) * 128]
    pos = np.asarray(inp["positions"])[0].astype(np.int32)
    in_maps = []
    for c in range(NCORES):
        in_maps.append({"xT": xT_shards[c], "modc": modc, "w_in": w_in_l, "w_sw": w_sw,
                        "pos": np.ascontiguousarray(pos[c * TOK:(c + 1) * TOK]), "rcol": rc, "alb": alb})
    return run(("k1", layer), lambda: build_k1(layer), in_maps)


SEG = 1024
NSEG = SEQ // SEG
NCH = SEG // 16
NTL = SEG // 128


def hgrn_consts():
    ident = np.eye(128, dtype=np.float32).astype(ml_dtypes.bfloat16)
    s = np.arange(128)
    tri = ((s[:, None] // 16 == s[None, :] // 16) & (s[:, None] <= s[None, :])).astype(np.float32).astype(ml_dtypes.bfloat16)
    ind = (s[:, None] // 16 == np.arange(8)[None, :]).astype(np.float32).astype(ml_dtypes.bfloat16)
    return ident, tri, ind


def build_k2a():
    P = Prog()
    qT = P.dram_in("qT", [64, SEQ], F32)
    kT = P.dram_in("kT", [64, SEQ], F32)
    lT = P.dram_in("lT", [64, SEQ], F32)
    vtok = P.dram_in("vtok", [SEQ, 64], F32)
    ident_d = P.dram_in("ident_d", [128, 128], BF16)
    tri_d = P.dram_in("tri_d", [128, 128], BF16)
    ind_d = P.dram_in("ind_d", [128, 8], BF16)
    oT = P.dram_out("oT", [64, SEQ], F32)

    ident = P.sb("ident", [128, 128], BF16)
    tri = P.sb("tri", [128, 128], BF16)
    ind = P.sb("ind", [128, 8], BF16)
    P.dma("sp", ident[:], ident_d, writes=["ident"])
    P.dma("sp", tri[:], tri_d, writes=["tri"])
    P.dma("sp", ind[:], ind_d, writes=["ind"])

    reset = P.sb("reset", [64, SEG], F32)
    P.op("pool", lambda q: q.memset(reset[:], 1.0), writes=["reset"])
    P.op("pool", lambda q: q.memset(reset[:].rearrange("p (n c) -> p n c", c=16)[:, :, 0:1], 0.0), reads=["reset"], writes=["reset"])

    def two(name, shape, dt):
        return [P.sb(f"{name}{i}", shape, dt) for i in range(2)]

    def three(name, shape, dt):
        return [P.sb(f"{name}{i}", shape, dt) for i in range(3)]

    qs_, ks_, ls_ = three("qs", [64, SEG], F32), three("ks", [64, SEG], F32), three("ls", [64, SEG], F32)
    vb_ = three("vb", [128, NTL, 64], BF16)
    cum_, ex_ = three("cum", [64, SEG], F32), three("ex", [64, SEG], F32)
    qt_, kt_, kh_ = three("qt", [64, SEG], BF16), three("kt", [64, SEG], BF16), three("kh", [64, SEG], BF16)
    dec_ = three("dec", [64, NCH], F32)
    kvs_ = two("kvs", [64, 64 * NCH], F32)
    sprev_ = two("sprev", [64, NCH, 64], BF16)
    obuf_ = two("obuf", [64, SEG], F32)
    decrep = P.sb("decrep", [64, 64 * NCH], F32)
    decz = P.sb("decz", [64, NCH], F32)
    P.op("pool", lambda q: q.memset(decz[:], 0.0), writes=["decz"])
    sall = P.sb("sall", [64, 64 * NCH], F32)
    s_in = P.sb("s_in", [64, 64], F32)
    khtok = [P.sb(f"khtok{i}", [128, 64], BF16) for i in range(2)]
    vblk = [P.sb(f"vblk{i}", [128, 8, 64], BF16) for i in range(2)]
    am = [P.sb(f"am{i}", [128, 128], BF16) for i in range(2)]
    P.op("pool", lambda q: q.memset(s_in[:], 0.0), writes=["s_in"])

    ps_kv = [P.ps(f"ps_kv{i}", [128, 512], F32) for i in range(2)]
    ps_a = [P.ps(f"ps_a{i}", [128, 512], F32) for i in range(2)]
    ps_o = [P.ps(f"ps_o{i}", [128, 512], F32) for i in range(2)]
    ps_t = [P.ps(f"ps_t{i}", [128, 1024], BF16) for i in range(2)]

    dec3 = decrep[:].rearrange("p (v n) -> p v n", n=NCH)
    sall3 = sall[:].rearrange("p (v n) -> p v n", n=NCH)
    vt_v = vtok.rearrange("(s i p) v -> s p i v", p=128, i=NTL)
    cnt = [0]

    def stage(sgi, part):
        pb = sgi % 2
        p3 = sgi % 3
        K_ = lambda n: f"{n}{pb if n in ('kvs', 'sprev', 'obuf') else p3}"
        qs, ks, ls, vb, cum, ex = qs_[p3], ks_[p3], ls_[p3], vb_[p3], cum_[p3], ex_[p3]
        qt, kt, kh, dec, kvs, sprev, obuf = qt_[p3], kt_[p3], kh_[p3], dec_[p3], kvs_[pb], sprev_[pb], obuf_[pb]
        kvs3 = kvs[:].rearrange("p (v n) -> p v n", n=NCH)
        cum3 = cum[:].rearrange("p (n c) -> p n c", c=16)
        ex3 = ex[:].rearrange("p (n c) -> p n c", c=16)
        tsl = slice(sgi * SEG, (sgi + 1) * SEG)
        if part == "B":
            yield from stageB(sgi, pb, K_, qt, kt, vb, dec, kvs, kvs3, sprev, obuf, tsl)
            return
        if part == "A1":
            yield P.dma("sp", qs[:], qT[:, tsl], writes=[K_("qs")])
            yield P.dma("sp", ks[:], kT[:, tsl], writes=[K_("ks")])
            yield P.dma("sp", ls[:], lT[:, tsl], writes=[K_("ls")])
            yield P.dma("pool", vb[:], vt_v[sgi], writes=[K_("vb")])
            yield P.op("dve", lambda q: q.tensor_tensor_scan(out=cum[:], data0=reset[:], data1=ls[:], initial=0.0,
                                                       op0=ALU.mult, op1=ALU.add), reads=["reset", K_("ls")], writes=[K_("cum")])
            yield P.op("act", lambda q: q.activation(out=ex[:], in_=cum[:], func=AF.Exp), reads=[K_("cum")], writes=[K_("ex")])
            yield P.op("dve", lambda q: q.tensor_tensor(out=qt[:], in0=qs[:], in1=ex[:], op=ALU.mult), reads=[K_("qs"), K_("ex")], writes=[K_("qt")])
            yield P.op("act", lambda q: q.activation(out=ex[:], in_=cum[:], func=AF.Exp, scale=-1.0), reads=[K_("cum")], writes=[K_("ex")])
            yield P.op("dve", lambda q: q.tensor_tensor(out=kt[:], in0=ks[:], in1=ex[:], op=ALU.mult), reads=[K_("ks"), K_("ex")], writes=[K_("kt")])
            yield P.op("dve", lambda q: q.tensor_tensor(out=ex3, in0=cum3[:, :, 15:16].broadcast_to([64, NCH, 16]), in1=cum3,
                                                  op=ALU.subtract), reads=[K_("cum")], writes=[K_("ex")])
            yield P.op("act", lambda q: q.activation(out=ex[:], in_=ex[:], func=AF.Exp), reads=[K_("ex")], writes=[K_("ex")])
            yield P.op("dve", lambda q: q.tensor_tensor(out=kh[:], in0=ks[:], in1=ex[:], op=ALU.mult), reads=[K_("ks"), K_("ex")], writes=[K_("kh")])
            yield P.op("act", lambda q: q.activation(out=dec[:], in_=cum3[:, :, 15], func=AF.Exp), reads=[K_("cum")], writes=[K_("dec")])
            return
        bs = []
        for i in range(NTL):
            bs.append(cnt[0] % 2)
            cnt[0] += 1

        def emit_T(i):
            b = bs[i]
            yield P.op("pe", lambda q: q.transpose(ps_t[b][:, 0:64], kh[:, i * 128:(i + 1) * 128], ident[0:64, 0:64]),
                 reads=[K_("kh"), "ident"], writes=[f"ps_t{b}"])
            yield P.op("act", lambda q: q.activation(out=khtok[b][:], in_=ps_t[b][:, 0:64], func=AF.Copy),
                 reads=[f"ps_t{b}"], writes=[f"khtok{b}"])
            yield P.op("dve", lambda q: q.tensor_tensor(out=vblk[b][:], in0=vb[:, i:i + 1, :].broadcast_to([128, 8, 64]),
                                                   in1=ind[:].unsqueeze(2).broadcast_to([128, 8, 64]), op=ALU.mult),
                 reads=[K_("vb"), "ind"], writes=[f"vblk{b}"])

        def emit_KV(i):
            b = bs[i]
            yield P.op("pe", lambda q: q.matmul(ps_kv[b][0:64, :], lhsT=khtok[b][:], rhs=vblk[b][:].rearrange("p c v -> p (c v)"),
                                          start=True, stop=True), reads=[f"khtok{b}", f"vblk{b}"], writes=[f"ps_kv{b}"])
            yield P.op("act", lambda q: q.activation(out=kvs3[:, :, 8 * i:8 * i + 8],
                                               in_=ps_kv[b][0:64, :].rearrange("p (c v) -> p v c", v=64), func=AF.Copy),
                 reads=[f"ps_kv{b}"], writes=[K_("kvs")])

        yield from emit_T(0)
        for i in range(NTL):
            if i + 1 < NTL:
                yield from emit_T(i + 1)
            yield from emit_KV(i)

    def stageB(sgi, pb, K_, qt, kt, vb, dec, kvs, kvs3, sprev, obuf, tsl):
        yield P.op("act", lambda q: q.activation(out=decz[:, 1:NCH], in_=dec[:, 1:NCH], func=AF.Copy), reads=[K_("dec")], writes=["decz"])
        yield P.op("act", lambda q: q.activation(out=dec3, in_=decz[:].unsqueeze(1).broadcast_to([64, 64, NCH]), func=AF.Copy),
             reads=["decz"], writes=["decrep"])
        yield P.op("dve", lambda q: q.scalar_tensor_tensor(out=kvs3[:, :, 0], in0=s_in[:], scalar=dec[:, 0:1], in1=kvs3[:, :, 0],
                                                     op0=ALU.mult, op1=ALU.add), reads=["s_in", K_("dec"), K_("kvs")], writes=[K_("kvs")])
        yield P.op("dve", lambda q: q.tensor_tensor_scan(out=sall[:], data0=decrep[:], data1=kvs[:], initial=0.0,
                                                   op0=ALU.mult, op1=ALU.add), reads=["decrep", K_("kvs")], writes=["sall"])
        yield P.op("act", lambda q: q.activation(out=sprev[:, 0, :], in_=s_in[:], func=AF.Copy), reads=["s_in"], writes=[K_("sprev")])
        yield P.op("act", lambda q: q.activation(out=sprev[:, 1:NCH, :], in_=sall3[:, :, 0:NCH - 1].rearrange("p v n -> p n v"), func=AF.Copy),
             reads=["sall"], writes=[K_("sprev")])
        yield P.op("dve", lambda q: q.tensor_copy(out=s_in[:], in_=sall3[:, :, NCH - 1]), reads=["sall", K_("sprev")], writes=["s_in"])
        bs = []
        for i in range(NTL):
            bs.append(cnt[0] % 2)
            cnt[0] += 1

        def emit_A(i):
            b = bs[i]
            csl = slice(i * 128, (i + 1) * 128)
            yield P.op("pe", lambda q: q.matmul(ps_a[b][:, 0:128], lhsT=kt[:, csl], rhs=qt[:, csl], start=True, stop=True),
                 reads=[K_("kt"), K_("qt")], writes=[f"ps_a{b}"])
            yield P.op("dve", lambda q: q.tensor_tensor(out=am[b][:], in0=ps_a[b][:, 0:128], in1=tri[:], op=ALU.mult),
                 reads=[f"ps_a{b}", "tri"], writes=[f"am{b}"])

        def emit_O(i):
            b = bs[i]
            csl = slice(i * 128, (i + 1) * 128)
            yield P.op("pe", lambda q: q.matmul(ps_o[b][0:64, 0:128], lhsT=vb[:, i, :], rhs=am[b][:], start=True, stop=False),
                 reads=[K_("vb"), f"am{b}"], writes=[f"ps_o{b}"])
            for c in range(8):
                n = 8 * i + c
                yield P.op("pe", lambda q, c=c, n=n: q.matmul(ps_o[b][0:64, 16 * c:16 * c + 16], lhsT=sprev[:, n, :],
                                                       rhs=qt[:, i * 128 + 16 * c:i * 128 + 16 * c + 16],
                                                       start=False, stop=(c == 7), skip_group_check=True),
                     reads=[K_("sprev"), K_("qt")], writes=[f"ps_o{b}"])
            yield P.op("act", lambda q: q.activation(out=obuf[:, csl], in_=ps_o[b][0:64, 0:128], func=AF.Copy),
                 reads=[f"ps_o{b}"], writes=[K_("obuf")])

        yield from emit_A(0)
        for i in range(NTL):
            if i + 1 < NTL:
                yield from emit_A(i + 1)
            yield from emit_O(i)
        yield P.dma("sp", oT[:, tsl], obuf[:], reads=[K_("obuf")], is_output=True)

    def drain(*gens):
        gens = [g for g in gens if g is not None]
        while gens:
            for g in list(gens):
                try:
                    next(g)
                except StopIteration:
                    gens.remove(g)

    drain(stage(0, "A1"))
    drain(stage(0, "A2"), stage(1, "A1"))
    for sgi in range(NSEG):
        drain(stage(sgi, "B"), stage(sgi + 1, "A2") if sgi + 1 < NSEG else None,
              stage(sgi + 2, "A1") if sgi + 2 < NSEG else None)
    return P.finish()


def run_k2a(o32_full):
    ident, tri, ind = hgrn_consts()
    in_maps = []
    for c in range(NCORES):
        h, d = c % 4, c // 4
        rows = slice(h * 64, (h + 1) * 64)
        q = o32_full["qa"][rows]
        k = o32_full["kf" if d == 0 else "kb"][rows]
        l = o32_full["lf" if d == 0 else "lb"][rows]
        v = o32_full["va"][rows]
        if d == 1:
            q, k, l, v = q[:, ::-1], k[:, ::-1], l[:, ::-1], v[:, ::-1]
        in_maps.append({"qT": np.ascontiguousarray(q), "kT": np.ascontiguousarray(k), "lT": np.ascontiguousarray(l),
                        "vtok": np.ascontiguousarray(v.T), "ident_d": ident, "tri_d": tri, "ind_d": ind})
    res = run(("k2a",), build_k2a, in_maps)
    of = np.concatenate([res[h]["oT"] for h in range(4)], 0)
    ob = np.concatenate([res[4 + h]["oT"][:, ::-1] for h in range(4)], 0)
    return of, np.ascontiguousarray(ob)


GU = 16
C_PAT = ((128, 1), (512, 4), (2048, 16))
NUC = 3 * 4 * 16
NUD = 4 * 16


def attn_masks():
    k = np.arange(256)[:, None]
    q = np.arange(128)[None, :]
    mc = np.where(np.abs(k - 64 - q) <= 64, 1.0, 0.0).astype(np.float32)
    k = np.arange(384)[:, None]
    md = np.where(np.abs(k - 128 - q) <= 128, 1.0, 0.0).astype(np.float32)
    mc = mc.reshape(2, 128, 128).transpose(1, 0, 2)
    md = md.reshape(3, 128, 128).transpose(1, 0, 2)
    return (np.ascontiguousarray(mc).astype(ml_dtypes.bfloat16), np.ascontiguousarray(md).astype(ml_dtypes.bfloat16))


def build_k2b():
    P = Prog()
    specs = []
    for nm, nu, nb in (("c", NUC, 2), ("d", NUD, 3)):
        ng = nu // GU
        specs.append(dict(nm=nm, nb=nb, ng=ng,
                          Q=P.dram_in(nm + "Q", [ng, 64, GU * 128], BF16),
                          K=P.dram_in(nm + "K", [ng, 64, GU * nb * 128], BF16),
                          V=P.dram_in(nm + "V", [ng, 128, GU * nb * 65], BF16),
                          M=P.dram_in(nm + "M", [128, nb, 128], BF16),
                          O=P.dram_out(nm + "O", [ng, 65, GU * 128], F32)))
    ident_d = P.dram_in("ident_d", [128, 128], BF16)
    ident = P.sb("ident", [128, 128], BF16)
    P.dma("sp", ident[:], ident_d, writes=["ident"])
    Qg = [P.sb(f"Qg{i}", [64, GU * 128], BF16) for i in range(2)]
    Kg = [P.sb(f"Kg{i}", [64, GU * 3 * 128], BF16) for i in range(2)]
    Vg = [P.sb(f"Vg{i}", [128, GU * 3 * 65], BF16) for i in range(2)]
    Og = [P.sb(f"Og{i}", [65, GU * 128], F32) for i in range(2)]
    Pt = [P.sb(f"Pt{i}", [128, 384], BF16) for i in range(3)]
    psS = [P.ps(f"psS{i}", [128, 512], F32) for i in range(4)]
    psO = [P.ps(f"psO{i}", [128, 512], F32) for i in range(3)]
    gi = 0
    ui = 0
    for sp_ in specs:
        nb = sp_["nb"]
        mk = P.sb("mask_" + sp_["nm"], [128, nb, 128], BF16)
        mkk = "mask_" + sp_["nm"]
        P.dma("sp", mk[:], sp_["M"], writes=[mkk])
        for g in range(sp_["ng"]):
            b2 = gi % 2
            gi += 1
            P.dma("sp", Qg[b2][:], sp_["Q"][g], writes=[f"Qg{b2}"])
            P.dma("sp", Kg[b2][:, 0:GU * nb * 128], sp_["K"][g], writes=[f"Kg{b2}"])
            P.dma("sp", Vg[b2][:, 0:GU * nb * 65], sp_["V"][g], writes=[f"Vg{b2}"])
            def emit_S(u, s3, p2):
                for b in range(nb):
                    ksl = slice((u * nb + b) * 128, (u * nb + b + 1) * 128)
                    P.op("pe", lambda q, b=b, ksl=ksl: q.matmul(
                        psS[s3][:, b * 128:(b + 1) * 128], lhsT=Kg[b2][:, ksl], rhs=Qg[b2][:, u * 128:(u + 1) * 128],
                        start=True, stop=True), reads=[f"Kg{b2}", f"Qg{b2}"], writes=[f"psS{s3}"])
                P.op("act", lambda q: q.activation(out=Pt[p2][:, 0:nb * 128], in_=psS[s3][:, 0:nb * 128], func=AF.Exp, scale=0.125),
                     reads=[f"psS{s3}"], writes=[f"Pt{p2}"])
                P.op("dve", lambda q: q.tensor_tensor(out=Pt[p2][:, 0:nb * 128], in0=Pt[p2][:, 0:nb * 128],
                                                      in1=mk[:].rearrange("p b q -> p (b q)"), op=ALU.mult),
                     reads=[f"Pt{p2}", mkk], writes=[f"Pt{p2}"])

            def emit_PV(u, s3, p2, so):
                for b in range(nb):
                    vsl = slice((u * nb + b) * 65, (u * nb + b + 1) * 65)
                    P.op("pe", lambda q, b=b, vsl=vsl: q.matmul(
                        psO[so][0:65, 0:128], lhsT=Vg[b2][:, vsl], rhs=Pt[p2][:, b * 128:(b + 1) * 128],
                        start=(b == 0), stop=(b == nb - 1)), reads=[f"Vg{b2}", f"Pt{p2}"], writes=[f"psO{so}"])
                P.op("act", lambda q: q.activation(out=Og[b2][:, u * 128:(u + 1) * 128], in_=psO[so][0:65, 0:128], func=AF.Copy),
                     reads=[f"psO{so}"], writes=[f"Og{b2}"])

            ids = []
            for u in range(GU):
                ids.append((u, ui % 4, ui % 3, ui % 3))
                ui += 1
            emit_S(*ids[0][:3])
            emit_S(*ids[1][:3])
            for j in range(GU):
                if j + 2 < GU:
                    emit_S(*ids[j + 2][:3])
                emit_PV(*ids[j])
            P.dma("sp", sp_["O"][g], Og[b2][:], reads=[f"Og{b2}"], is_output=True)
    return P.finish()


def _windows(Kseq, Vseq, nb, halo, ntile):
    L = Kseq.shape[1]
    W = nb * 128
    Kp = np.zeros((64, L + 2 * halo + 128), Kseq.dtype)
    Kp[:, halo:halo + L] = Kseq
    Vp = np.zeros((L + 2 * halo + 128, 65), Vseq.dtype)
    Vp[halo:halo + L, :64] = Vseq.T
    Vp[halo:halo + L, 64] = 1.0
    Kw = np.stack([Kp[:, 128 * j:128 * j + W] for j in range(ntile)], 0)
    Vw = np.stack([Vp[128 * j:128 * j + W] for j in range(ntile)], 0)
    return Kw, Vw


def _pack_units(Qu, Ku, Vu, nb):
    U = Qu.shape[0]
    ng = U // GU
    Q = Qu.reshape(ng, GU, 64, 128).transpose(0, 2, 1, 3).reshape(ng, 64, GU * 128)
    K = Ku.reshape(ng, GU, 64, nb * 128).transpose(0, 2, 1, 3).reshape(ng, 64, GU * nb * 128)
    V = Vu.reshape(ng, GU, nb, 128, 65).transpose(0, 3, 1, 2, 4).reshape(ng, 128, GU * nb * 65)
    return np.ascontiguousarray(Q), np.ascontiguousarray(K), np.ascontiguousarray(V)


def run_k2b(obf_full):
    mc, md = attn_masks()
    ident = np.eye(128, dtype=np.float32).astype(ml_dtypes.bfloat16)
    cQ = np.zeros((3, 4, 128, 64, 128), ml_dtypes.bfloat16)
    cK = np.zeros((3, 4, 128, 64, 256), ml_dtypes.bfloat16)
    cV = np.zeros((3, 4, 128, 256, 65), ml_dtypes.bfloat16)
    for p, (w, d) in enumerate(C_PAT):
        L = SEQ // d
        nt = L // 128
        for h in range(4):
            rows = slice(h * 64, (h + 1) * 64)
            Qs = obf_full["cq"][rows].reshape(64, L, d)
            Ks = obf_full["ck"][rows].reshape(64, L, d)
            Vs = obf_full["cv"][rows].reshape(64, L, d)
            for r in range(d):
                Kw, Vw = _windows(Ks[:, :, r], Vs[:, :, r], 2, 64, nt)
                cK[p, h, r * nt:(r + 1) * nt] = Kw
                cV[p, h, r * nt:(r + 1) * nt] = Vw
                cQ[p, h, r * nt:(r + 1) * nt] = Qs[:, :, r].reshape(64, nt, 128).transpose(1, 0, 2)
    dQ = np.zeros((4, 128, 64, 128), ml_dtypes.bfloat16)
    dK = np.zeros((4, 128, 64, 384), ml_dtypes.bfloat16)
    dV = np.zeros((4, 128, 384, 65), ml_dtypes.bfloat16)
    for h in range(4):
        kvh = h // 2
        Kw, Vw = _windows(obf_full["dk"][kvh * 64:(kvh + 1) * 64], obf_full["dv"][kvh * 64:(kvh + 1) * 64], 3, 128, 128)
        dK[h] = Kw
        dV[h] = Vw
        dQ[h] = obf_full["dq"][h * 64:(h + 1) * 64].reshape(64, 128, 128).transpose(1, 0, 2)
    in_maps = []
    for c in range(NCORES):
        ts = slice(16 * c, 16 * c + 16)
        q, k, v = _pack_units(cQ[:, :, ts].reshape(NUC, 64, 128), cK[:, :, ts].reshape(NUC, 64, 256), cV[:, :, ts].reshape(NUC, 256, 65), 2)
        q2, k2, v2 = _pack_units(dQ[:, ts].reshape(NUD, 64, 128), dK[:, ts].reshape(NUD, 64, 384), dV[:, ts].reshape(NUD, 384, 65), 3)
        in_maps.append({"cQ": q, "cK": k, "cV": v, "cM": mc, "dQ": q2, "dK": k2, "dV": v2, "dM": md, "ident_d": ident})
    res = run(("k2b",), build_k2b, in_maps)
    numC = np.zeros((3, 256, SEQ), np.float32)
    denC = np.zeros((3, 4, SEQ), np.float32)
    numD = np.zeros((256, SEQ), np.float32)
    denD = np.zeros((4, SEQ), np.float32)
    for c in range(NCORES):
        co = res[c]["cO"].reshape(NUC // GU, 65, GU, 128).transpose(0, 2, 1, 3).reshape(3, 4, 16, 65, 128)
        do = res[c]["dO"].reshape(NUD // GU, 65, GU, 128).transpose(0, 2, 1, 3).reshape(4, 16, 65, 128)
        for p, (w, d) in enumerate(C_PAT):
            L = SEQ // d
            nt = L // 128
            for h in range(4):
                nv = numC[p, h * 64:(h + 1) * 64].reshape(64, L, d)
                dv_ = denC[p, h].reshape(L, d)
                for tl in range(16):
                    tau = 16 * c + tl
                    r, j = tau // nt, tau % nt
                    nv[:, 128 * j:128 * j + 128, r] = co[p, h, tl, 0:64]
                    dv_[128 * j:128 * j + 128, r] = co[p, h, tl, 64]
        for h in range(4):
            for tl in range(16):
                tau = 16 * c + tl
                numD[h * 64:(h + 1) * 64, 128 * tau:128 * tau + 128] = do[h, tl, 0:64]
                denD[h, 128 * tau:128 * tau + 128] = do[h, tl, 64]
    return numC, denC, numD, denD


FB = 4


def build_k3(moe):
    P = Prog()
    T = TOK
    xT = P.dram_in("xT", [D_MODEL, T], F32)
    modc_d = P.dram_in("modc", [128, 48], F32)
    ofT = P.dram_in("ofT", [256, T], F32)
    obT = P.dram_in("obT", [256, T], F32)
    gaT = P.dram_in("gaT", [256, T], F32)
    uext = P.dram_in("uext", [256, T + 32], F32)
    numC = P.dram_in("numC", [3, 256, T], F32)
    denC = P.dram_in("denC", [3, 256, T], F32)
    numD = P.dram_in("numD", [256, T], F32)
    denD = P.dram_in("denD", [256, T], F32)
    smalls_d = P.dram_in("smalls", [128, 128], F32)
    blk64_d = P.dram_in("blk64", [128, 128], F32)
    identb_d = P.dram_in("identb", [128, 128], BF16)
    w_out = P.dram_in("w_out", [1024, 1024], F32)
    if moe:
        wr_d = P.dram_in("wr", [1024, 8], F32)
        w_up = P.dram_in("w_up", [N_EXPERTS, 1024, 2 * EXPERT_DIM], F32)
        w_down = P.dram_in("w_down", [N_EXPERTS, EXPERT_DIM, 1024], F32)
        sel_d = P.dram_in("sel", [8, 8 * 128], F32)
        identf_d = P.dram_in("identf", [128, 128], F32)
    else:
        w_up = P.dram_in("w_up", [1024, 2 * FFN_DIM], F32)
        w_down = P.dram_in("w_down", [FFN_DIM, 1024], F32)
    xo = P.dram_out("xo", [D_MODEL, T], F32)

    pp = PsumPool(P)
    sm = P.sb("sm", [128, 128], F32)
    P.dma("sp", sm[:], smalls_d, writes=["sm"])
    O_ANW, O_CW, O_CB, O_BNW, O_BNB, O_SINK, O_LNW, O_LNB = 0, 1, 63, 65, 67, 69, 71, 87
    modc = P.sb("modc", [128, 48], F32)
    P.dma("sp", modc[:], modc_d, writes=["modc"])
    blk64 = P.sb("blk64", [128, 128], F32)
    P.dma("sp", blk64[:], blk64_d, writes=["blk64"])
    ones_f = P.sb("ones_f", [128, 128], F32)
    P.op("pool", lambda q: q.memset(ones_f[:], 1.0), writes=["ones_f"])
    eps = P.sb("eps", [128, 2], F32)
    P.op("pool", lambda q: q.memset(eps[:, 0:1], LN_EPS), writes=["eps"])
    P.op("pool", lambda q: q.memset(eps[:, 1:2], RMS_EPS), reads=["eps"], writes=["eps"])
    dcol = P.sb("dcol", [128, 32], F32)
    P.op("dve", lambda q: q.tensor_scalar(out=dcol[:, 0:8], in0=modc[:, 16:24], scalar1=1.0, scalar2=None, op0=ALU.add),
         reads=["modc"], writes=["dcol"])
    P.op("dve", lambda q: q.tensor_scalar(out=dcol[:, 8:16], in0=modc[:, 32:40], scalar1=1.0, scalar2=None, op0=ALU.add),
         reads=["modc", "dcol"], writes=["dcol"])
    P.op("dve", lambda q: q.tensor_scalar(out=dcol[:, 16:24], in0=modc[:, 40:48], scalar1=1.0, scalar2=None, op0=ALU.add),
         reads=["modc", "dcol"], writes=["dcol"])
    P.op("act", lambda q: q.activation(out=dcol[:, 24:26], in_=sm[:, O_SINK:O_SINK + 2], func=AF.Exp),
         reads=["sm", "dcol"], writes=["dcol"])
    G1, SC2, G2, ESINK = 0, 8, 16, 24

    x = P.sb("x", [128, 8, T], F32)
    y = P.sb("y", [128, 8, T], BF16)
    wu = [P.sb(f"wu{i}", [128, 8192], BF16) for i in range(2)]
    wd = [P.sb(f"wd{i}", [128, FB, 1024], BF16) for i in range(2)]
    abuf = P.sb("abuf", [128, FB, T], BF16)
    S = [P.sb(f"S{i}", [128, T + 32], F32) for i in range(3)]
    NT = 5
    tmp = [P.sb(f"tmp{i}", [128, 512], F32) for i in range(NT)]
    lnm = P.sb("lnm", [128, 512], F32)
    lnv = P.sb("lnv", [128, 512], F32)
    tcnt = [0]

    def gett():
        i = tcnt[0] % NT
        tcnt[0] += 1
        return tmp[i], f"tmp{i}"

    def tsl(tg):
        return slice(tg * 512, (tg + 1) * 512)

    xk = lambda i, tg: f"x{i}_{tg}"
    yk = lambda i, tg: f"y{i}_{tg}"

    xT_v = xT.rearrange("(k p) t -> p k t", p=128)
    for k in range(8):
        P.dma("sp", x[:, k, :], xT_v[:, k, :], writes=[xk(k, tg) for tg in range(4)])
    wo_v = w_out.rearrange("(k p) e -> p k e", p=128)
    wo_s = wu[0][:].rearrange("p (k e) -> p k e", k=8)
    for k in range(8):
        P.dma("pool", wo_s[:, k, :], wo_v[:, k, :], writes=["wu0"])

    def ln_fm(srcs, nfeat, wcol, bcol, dst, silu=False):
        n = len(srcs)
        for tg in range(4):
            ps_s, ks_ = pp.get()
            ps_q, kq_ = pp.get()
            for i, (af, kf) in enumerate(srcs):
                P.op("pe", lambda q, af=af, i=i: q.matmul(ps_s[:, :], lhsT=ones_f[:], rhs=af(tg), start=(i == 0), stop=(i == n - 1)),
                     reads=["ones_f", kf(tg)], writes=[ks_])
            for i, (af, kf) in enumerate(srcs):
                sq, sqk = gett()
                P.op("act", lambda q, af=af, sq=sq: q.activation(out=sq[:], in_=af(tg), func=AF.Square), reads=[kf(tg)], writes=[sqk])
                P.op("pe", lambda q, sq=sq, i=i: q.matmul(ps_q[:, :], lhsT=ones_f[:], rhs=sq[:], start=(i == 0), stop=(i == n - 1)),
                     reads=["ones_f", sqk], writes=[kq_])
            mean, mk = lnm, "lnm"
            P.op("act", lambda q: q.activation(out=mean[:], in_=ps_s[:], func=AF.Copy, scale=1.0 / nfeat), reads=[ks_], writes=[mk])
            var, vk = lnv, "lnv"
            P.op("dve", lambda q: q.tensor_tensor(out=var[:], in0=mean[:], in1=mean[:], op=ALU.mult), reads=[mk], writes=[vk])
            P.op("dve", lambda q: q.scalar_tensor_tensor(out=var[:], in0=ps_q[:], scalar=1.0 / nfeat, in1=var[:], op0=ALU.mult,
                                                         op1=ALU.subtract), reads=[kq_, vk], writes=[vk])
            P.op("act", lambda q: q.activation(out=var[:], in_=var[:], func=AF.Sqrt, bias=eps[:, 0:1], scale=1.0), reads=[vk, "eps"], writes=[vk])
            P.op("dve", lambda q: q.reciprocal(out=var[:], in_=var[:]), reads=[vk], writes=[vk])
            for i, ((af, kf), (df, dkf)) in enumerate(zip(srcs, dst)):
                t, tk = gett()
                P.op("dve", lambda q, af=af, t=t: q.tensor_tensor(out=t[:], in0=af(tg), in1=mean[:], op=ALU.subtract),
                     reads=[kf(tg), mk], writes=[tk])
                P.op("dve", lambda q, t=t: q.tensor_tensor(out=t[:], in0=t[:], in1=var[:], op=ALU.mult), reads=[tk, vk], writes=[tk])
                if silu:
                    P.op("act", lambda q, t=t, df=df, i=i: q.activation(out=df(tg), in_=t[:], func=AF.Silu, scale=wcol(i), bias=bcol(i)),
                         reads=[tk, "sm"], writes=[dkf(tg)])
                else:
                    P.op("act", lambda q, t=t, df=df, i=i: q.activation(out=df(tg), in_=t[:], func=AF.Identity, scale=wcol(i), bias=bcol(i)),
                         reads=[tk, "sm"], writes=[dkf(tg)])

    wu1f = wu[1][:].bitcast(F32)
    cpool = [wu1f[:, i_ * 512:(i_ + 1) * 512] for i_ in range(8)]
    ccnt = [0]

    class _CT:
        def __init__(self, ap):
            self.ap = ap

        def __getitem__(self, idx):
            return self.ap

    def getc():
        i_ = ccnt[0] % 8
        ccnt[0] += 1
        return _CT(cpool[i_]), f"ctmp{i_}"

    def stream_A():
        for a in range(2):
            rows = slice(a * 128, (a + 1) * 128)
            for tg in range(4):
                t0, k0 = gett()
                t1, k1 = gett()
                t2, k2 = gett()
                yield P.dma("sp", t0[:], ofT[rows, tsl(tg)], writes=[k0])
                yield P.dma("sp", t1[:], obT[rows, tsl(tg)], writes=[k1])
                yield P.dma("sp", t2[:], gaT[rows, tsl(tg)], writes=[k2])
                yield P.op("dve", lambda q, t0=t0, t1=t1: q.tensor_tensor(out=t0[:], in0=t0[:], in1=t1[:], op=ALU.add), reads=[k0, k1], writes=[k0])
                yield P.op("act", lambda q, t0=t0, t1=t1: q.activation(out=t1[:], in_=t0[:], func=AF.Square), reads=[k0], writes=[k1])
                ps, pk = pp.t[0], pp.k[0]
                yield P.op("pe", lambda q, ps=ps, t1=t1: q.matmul(ps[:, :], lhsT=blk64[:], rhs=t1[:], start=True, stop=True), reads=["blk64", k1], writes=[pk])
                yield P.op("act", lambda q, ps=ps, t1=t1: q.activation(out=t1[:], in_=ps[:], func=AF.Sqrt, bias=eps[:, 1:2], scale=1.0 / 64),
                     reads=[pk, "eps"], writes=[k1])
                yield P.op("dve", lambda q, t1=t1: q.reciprocal(out=t1[:], in_=t1[:]), reads=[k1], writes=[k1])
                yield P.op("dve", lambda q, t0=t0, t1=t1: q.scalar_tensor_tensor(out=t0[:], in0=t0[:], scalar=sm[:, O_ANW:O_ANW + 1], in1=t1[:],
                                                                         op0=ALU.mult, op1=ALU.mult), reads=[k0, k1, "sm"], writes=[k0])
                yield P.op("dve", lambda q, t0=t0, t2=t2, a=a, tg=tg: q.tensor_tensor(out=y[:, a, tsl(tg)], in0=t0[:], in1=t2[:], op=ALU.mult),
                     reads=[k0, k2], writes=[yk(a, tg)])


        yield None

    def stream_conv():
        identb = wd[1][:, 1, 0:128]
        yield P.dma("sp", identb, identb_d, writes=["identb"])
        dg = [wd[1][:, 0, i * 128:(i + 1) * 128] for i in range(4)]
        ubf = abuf[:].rearrange("p f t -> p (f t)")[:, 0:T + 32]
        for b in range(2):
            ub, ubk = S[0], "S0"
            acc, acck = S[1 + b], f"S{1 + b}"
            yield P.dma("sp", ub[:], uext[b * 128:(b + 1) * 128, :], writes=[ubk])
            yield P.op("act", lambda q: q.activation(out=ubf, in_=ub[:], func=AF.Copy), reads=[ubk], writes=["ubf"])
            pss = [(pp.t[1 + i_], pp.k[1 + i_]) for i_ in range(4)]
            for j in range(B_KERNEL):
                d_, dk_ = dg[j % 4], f"dg{j % 4}"
                yield P.op("dve", lambda q, d_=d_, j=j, b=b: q.tensor_scalar(out=d_, in0=identb, scalar1=sm[:, O_CW + b * 31 + j:O_CW + b * 31 + j + 1],
                                                                     scalar2=None, op0=ALU.mult), reads=["identb", "sm"], writes=[dk_])
                for tg in range(4):
                    ps, pk = pss[tg]
                    yield P.op("pe", lambda q, ps=ps, d_=d_, j=j, tg=tg: q.matmul(ps[:, :], lhsT=d_, rhs=ubf[:, tg * 512 + j:tg * 512 + j + 512],
                                                                            start=(j == 0), stop=(j == B_KERNEL - 1)),
                         reads=[dk_, "ubf"], writes=[pk])
            for tg in range(4):
                ps, pk = pss[tg]
                yield P.op("act", lambda q, ps=ps, acc=acc, tg=tg, b=b: q.activation(out=acc[:, tsl(tg)], in_=ps[:], func=AF.Identity,
                                                                             bias=sm[:, O_CB + b:O_CB + b + 1], scale=1.0),
                     reads=[pk, "sm"], writes=[acck])

        yield None

    def stream_CD():
        for a in range(2):
            rows = slice(a * 128, (a + 1) * 128)
            for tg in range(4):
                n0, kn0 = getc()
                d0, kd0 = getc()
                yield P.dma("sp", n0[:], numC[0, rows, tsl(tg)], writes=[kn0])
                yield P.dma("sp", d0[:], denC[0, rows, tsl(tg)], writes=[kd0])
                n1, kn1 = getc()
                d1, kd1 = getc()
                for p in (1, 2):
                    yield P.dma("sp", n1[:], numC[p, rows, tsl(tg)], writes=[kn1])
                    yield P.dma("sp", d1[:], denC[p, rows, tsl(tg)], writes=[kd1])
                    yield P.op("dve", lambda q, n0=n0, n1=n1: q.tensor_tensor(out=n0[:], in0=n0[:], in1=n1[:], op=ALU.add), reads=[kn0, kn1], writes=[kn0])
                    yield P.op("dve", lambda q, d0=d0, d1=d1: q.tensor_tensor(out=d0[:], in0=d0[:], in1=d1[:], op=ALU.add), reads=[kd0, kd1], writes=[kd0])
                yield P.op("dve", lambda q, d0=d0: q.reciprocal(out=d0[:], in_=d0[:]), reads=[kd0], writes=[kd0])
                yield P.op("dve", lambda q, n0=n0, d0=d0, a=a, tg=tg: q.tensor_tensor(out=y[:, 4 + a, tsl(tg)], in0=n0[:], in1=d0[:], op=ALU.mult),
                     reads=[kn0, kd0], writes=[yk(4 + a, tg)])
                n0, kn0 = getc()
                d0, kd0 = getc()
                yield P.dma("sp", n0[:], numD[rows, tsl(tg)], writes=[kn0])
                yield P.dma("sp", d0[:], denD[rows, tsl(tg)], writes=[kd0])
                yield P.op("dve", lambda q, d0=d0, a=a: q.tensor_scalar(out=d0[:], in0=d0[:], scalar1=dcol[:, ESINK + a:ESINK + a + 1], scalar2=None,
                                                                op0=ALU.add), reads=[kd0, "dcol"], writes=[kd0])
                yield P.op("dve", lambda q, d0=d0: q.reciprocal(out=d0[:], in_=d0[:]), reads=[kd0], writes=[kd0])
                yield P.op("dve", lambda q, n0=n0, d0=d0, a=a, tg=tg: q.tensor_tensor(out=y[:, 6 + a, tsl(tg)], in0=n0[:], in1=d0[:], op=ALU.mult),
                     reads=[kn0, kd0], writes=[yk(6 + a, tg)])


        yield None

    def drain(*gens):
        gens = list(gens)
        while gens:
            for g in list(gens):
                try:
                    next(g)
                except StopIteration:
                    gens.remove(g)

    drain(stream_A(), stream_conv(), stream_CD())
    ln_fm([(lambda tg, b=b: S[1 + b][:, tsl(tg)], lambda tg, b=b: f"S{1 + b}") for b in range(2)], 256,
          lambda i: sm[:, O_BNW + i:O_BNW + i + 1], lambda i: sm[:, O_BNB + i:O_BNB + i + 1],
          [(lambda tg, b=b: y[:, 2 + b, tsl(tg)], lambda tg, b=b: yk(2 + b, tg)) for b in range(2)], silu=True)

    for k in range(8):
        for tg in range(4):
            P.op("act", lambda q, k=k, tg=tg: q.activation(out=x[:, k, tsl(tg)], in_=x[:, k, tsl(tg)], func=AF.Copy, scale=DEEPNORM_ALPHA),
                 reads=[xk(k, tg)], writes=[xk(k, tg)])
    for dc in range(8):
        for tg in range(4):
            ps, pk = pp.get()
            for k in range(8):
                P.op("pe", lambda q, ps=ps, k=k, dc=dc, tg=tg: q.matmul(ps[:, :], lhsT=wo_s[:, k, dc * 128:(dc + 1) * 128], rhs=y[:, k, tsl(tg)],
                                                                        start=(k == 0), stop=(k == 7)), reads=["wu0", yk(k, tg)], writes=[pk])
            P.op("dve", lambda q, ps=ps, dc=dc, tg=tg: q.scalar_tensor_tensor(out=x[:, dc, tsl(tg)], in0=ps[:], scalar=dcol[:, G1 + dc:G1 + dc + 1],
                                                                             in1=x[:, dc, tsl(tg)], op0=ALU.mult, op1=ALU.add),
                 reads=[pk, "dcol", xk(dc, tg)], writes=[xk(dc, tg)])
    xs = [(lambda tg, k=k: x[:, k, tsl(tg)], lambda tg, k=k: xk(k, tg)) for k in range(8)]
    ln_fm(xs, 1024, lambda i: sm[:, O_LNW + i:O_LNW + i + 1], lambda i: sm[:, O_LNB + i:O_LNB + i + 1], xs)

    for k in range(8):
        for tg in range(4):
            P.op("dve", lambda q, k=k, tg=tg: q.tensor_scalar(out=y[:, k, tsl(tg)], in0=x[:, k, tsl(tg)], scalar1=dcol[:, SC2 + k:SC2 + k + 1],
                                                            scalar2=modc[:, 24 + k:25 + k], op0=ALU.mult, op1=ALU.add),
                 reads=[xk(k, tg), "dcol", "modc"], writes=[yk(k, tg)])

    gT = None
    if moe:
        wr = P.sb("wr", [128, 8, 8], F32)
        P.dma("sp", wr[:], wr_d.rearrange("(k p) e -> p k e", p=128), writes=["wr"])
        sel = P.sb("sel", [8, 8 * 128], F32)
        P.dma("sp", sel[:], sel_d, writes=["sel"])
        identf = P.sb("identf", [128, 128], F32)
        P.dma("sp", identf[:], identf_d, writes=["identf"])
        psl, pslk = pp.get()
        for k in range(8):
            h2f, hk = S[0], "S0"
            P.op("dve", lambda q, k=k: q.tensor_scalar(out=h2f[:, 0:T], in0=x[:, k, :], scalar1=dcol[:, SC2 + k:SC2 + k + 1],
                                                       scalar2=modc[:, 24 + k:25 + k], op0=ALU.mult, op1=ALU.add),
                 reads=[xk(k, tg) for tg in range(4)] + ["dcol", "modc"], writes=[hk])
            for ti in range(16):
                P.op("pe", lambda q, k=k, ti=ti: q.matmul(psl[:, ti * 8:(ti + 1) * 8], lhsT=h2f[:, ti * 128:(ti + 1) * 128], rhs=wr[:, k, :],
                                                          start=(k == 0 and ti == 0), stop=(k == 7 and ti == 15), skip_group_check=True),
                     reads=[hk, "wr"], writes=[pslk])
        lg = P.sb("lg", [128, 16, 8], F32)
        lg2 = P.sb("lg2", [128, 16, 8], F32)
        eq1 = S[1][:, 0:128].rearrange("p (t e) -> p t e", e=8)
        eq2 = S[1][:, 128:256].rearrange("p (t e) -> p t e", e=8)
        m1 = P.sb("m1", [128, 16], F32)
        m2 = P.sb("m2", [128, 16], F32)
        g1 = P.sb("g1", [128, 16], F32)
        bc3 = lambda t: t[:].unsqueeze(2).broadcast_to([128, 16, 8])
        P.op("dve", lambda q: q.tensor_copy(out=lg[:], in_=psl[:, 0:128].rearrange("p (t e) -> p t e", e=8)), reads=[pslk], writes=["lg"])
        P.op("dve", lambda q: q.tensor_reduce(out=m1[:], in_=lg[:], axis=AX.X, op=ALU.max), reads=["lg"], writes=["m1"])
        P.op("dve", lambda q: q.tensor_tensor(out=eq1, in0=lg[:], in1=bc3(m1), op=ALU.is_equal), reads=["lg", "m1"], writes=["eq1"])
        P.op("dve", lambda q: q.scalar_tensor_tensor(out=lg2[:], in0=eq1, scalar=-1e30, in1=lg[:], op0=ALU.mult, op1=ALU.add),
             reads=["eq1", "lg"], writes=["lg2"])
        P.op("dve", lambda q: q.tensor_reduce(out=m2[:], in_=lg2[:], axis=AX.X, op=ALU.max), reads=["lg2"], writes=["m2"])
        P.op("dve", lambda q: q.tensor_tensor(out=eq2, in0=lg2[:], in1=bc3(m2), op=ALU.is_equal), reads=["lg2", "m2"], writes=["eq2"])
        P.op("dve", lambda q: q.tensor_tensor(out=m2[:], in0=m2[:], in1=m1[:], op=ALU.subtract), reads=["m2", "m1"], writes=["m2"])
        P.op("act", lambda q: q.activation(out=m2[:], in_=m2[:], func=AF.Exp), reads=["m2"], writes=["m2"])
        P.op("dve", lambda q: q.tensor_scalar(out=g1[:], in0=m2[:], scalar1=1.0, scalar2=None, op0=ALU.add), reads=["m2"], writes=["g1"])
        P.op("dve", lambda q: q.reciprocal(out=g1[:], in_=g1[:]), reads=["g1"], writes=["g1"])
        P.op("dve", lambda q: q.tensor_tensor(out=m2[:], in0=m2[:], in1=g1[:], op=ALU.mult), reads=["m2", "g1"], writes=["m2"])
        P.op("dve", lambda q: q.tensor_tensor(out=eq1, in0=eq1, in1=bc3(g1), op=ALU.mult), reads=["eq1", "g1"], writes=["eq1"])
        P.op("dve", lambda q: q.tensor_tensor(out=eq2, in0=eq2, in1=bc3(m2), op=ALU.mult), reads=["eq2", "m2"], writes=["eq2"])
        P.op("dve", lambda q: q.tensor_tensor(out=eq1, in0=eq1, in1=eq2, op=ALU.add), reads=["eq1", "eq2"], writes=["eq1"])
        gT = S[0][0:8, 0:T]
        for tg in range(4):
            ps, pk = pp.get()
            for j in range(4):
                ti = tg * 4 + j
                P.op("pe", lambda q, ps=ps, j=j, ti=ti: q.transpose(ps[0:8, j * 128:(j + 1) * 128], eq1[:, ti, :], identf[:]),
                     reads=["eq1", "identf"], writes=[pk])
            P.op("act", lambda q, ps=ps, tg=tg: q.activation(out=gT[:, tsl(tg)], in_=ps[0:8, :], func=AF.Copy), reads=[pk], writes=["S0"])

    for k in range(8):
        for tg in range(4):
            P.op("act", lambda q, k=k, tg=tg: q.activation(out=x[:, k, tsl(tg)], in_=x[:, k, tsl(tg)], func=AF.Copy, scale=DEEPNORM_ALPHA),
                 reads=[xk(k, tg)], writes=[xk(k, tg)])

    blocks = []
    if moe:
        for e in range(N_EXPERTS):
            for (f0, nf) in chunks(EXPERT_DIM // 128, FB):
                blocks.append((w_up[e], w_down[e], EXPERT_DIM, f0, nf, e))
    else:
        for (f0, nf) in chunks(FFN_DIM // 128, FB):
            blocks.append((w_up, w_down, FFN_DIM, f0, nf, None))

    stg = [S[2][:, i * 512:(i + 1) * 512] for i in range(4)]
    stg_n = [0]

    def block_pieces(bi):
        wup, wdn, F, f0, nf, e = blocks[bi]
        b2 = bi % 2
        wuv = wu[b2][:].rearrange("p (k s c) -> p k s c", k=8, s=2)
        upv = wup.rearrange("(k p) c -> p k c", p=128)
        pcs = []
        for k in range(8):
            for s_ in range(2):
                pcs.append((wuv[:, k, s_, 0:nf * 128], upv[:, k, s_ * F + f0 * 128:s_ * F + (f0 + nf) * 128], nf * 128, f"wu{b2}"))
        for fc in range(nf):
            for hf in range(2):
                pcs.append((wd[b2][:, fc, hf * 512:(hf + 1) * 512],
                            wdn[(f0 + fc) * 128:(f0 + fc + 1) * 128, hf * 512:(hf + 1) * 512], 512, f"wd{b2}"))
        return pcs

    def emit_piece(pc):
        dst, src, n, dkey = pc
        i = stg_n[0] % 4
        first = stg_n[0] < 4
        stg_n[0] += 1
        P.dma("sp", stg[i][:, 0:n], src, writes=(["S2", f"stg{i}"] if first else [f"stg{i}"]))
        P.op("act", lambda q: q.activation(out=dst, in_=stg[i][:, 0:n], func=AF.Copy), reads=[f"stg{i}"], writes=[dkey])

    for pc in block_pieces(0):
        emit_piece(pc)
    for bi, (wup, wdn, F, f0, nf, e) in enumerate(blocks):
        b2 = bi % 2
        nxt = block_pieces(bi + 1) if bi + 1 < len(blocks) else []
        per_it = -(-len(nxt) // (nf * 4)) if nxt else 0
        gmul = None
        gk = None
        if moe:
            gmul, gk = S[1], "S1"
            if f0 == 0:
                for tg in range(4):
                    ps, pk = pp.get()
                    P.op("pe", lambda q, ps=ps, e=e, tg=tg: q.matmul(ps[:, :], lhsT=sel[:, e * 128:(e + 1) * 128], rhs=gT[:, tsl(tg)],
                                                                    start=True, stop=True), reads=["sel", "S0"], writes=[pk])
                    P.op("act", lambda q, ps=ps, gmul=gmul, tg=tg: q.activation(out=gmul[:, tsl(tg)], in_=ps[:], func=AF.Copy),
                         reads=[pk], writes=[gk + f"_{tg}"])
        wuv = wu[b2][:].rearrange("p (k s c) -> p k s c", k=8, s=2)
        for fc in range(nf):
            for tg in range(4):
                psg, kg = pp.get()
                psu, ku = pp.get()
                for s_, ps_, pk_ in ((0, psg, kg), (1, psu, ku)):
                    for k in range(8):
                        P.op("pe", lambda q, ps_=ps_, k=k, s_=s_, fc=fc, tg=tg: q.matmul(ps_[:, :], lhsT=wuv[:, k, s_, fc * 128:(fc + 1) * 128],
                                                                                      rhs=y[:, k, tsl(tg)], start=(k == 0), stop=(k == 7)),
                             reads=[f"wu{b2}", yk(k, tg)], writes=[pk_])
                sg, sgk = gett()
                P.op("act", lambda q, sg=sg, psg=psg: q.activation(out=sg[:], in_=psg[:], func=AF.Silu), reads=[kg], writes=[sgk])
                if gmul is not None:
                    P.op("pool", lambda q, sg=sg, gmul=gmul, tg=tg: q.tensor_tensor(out=sg[:], in0=sg[:], in1=gmul[:, tsl(tg)], op=ALU.mult),
                         reads=[sgk, gk + f"_{tg}"], writes=[sgk])
                P.op("dve", lambda q, sg=sg, psu=psu, fc=fc, tg=tg: q.tensor_tensor(out=abuf[:, fc, tsl(tg)], in0=sg[:], in1=psu[:], op=ALU.mult),
                     reads=[sgk, ku], writes=[f"a{fc}_{tg}"])
                for _ in range(per_it):
                    if nxt:
                        emit_piece(nxt.pop(0))
        while nxt:
            emit_piece(nxt.pop(0))
        for dc in range(8):
            for tg in range(4):
                ps, pk = pp.get()
                for fc in range(nf):
                    P.op("pe", lambda q, ps=ps, fc=fc, dc=dc, tg=tg: q.matmul(ps[:, :], lhsT=wd[b2][:, fc, dc * 128:(dc + 1) * 128],
                                                                              rhs=abuf[:, fc, tsl(tg)], start=(fc == 0), stop=(fc == nf - 1)),
                         reads=[f"wd{b2}", f"a{fc}_{tg}"], writes=[pk])
                P.op("dve", lambda q, ps=ps, dc=dc, tg=tg: q.scalar_tensor_tensor(out=x[:, dc, tsl(tg)], in0=ps[:], scalar=dcol[:, G2 + dc:G2 + dc + 1],
                                                                                 in1=x[:, dc, tsl(tg)], op0=ALU.mult, op1=ALU.add),
                     reads=[pk, "dcol", xk(dc, tg)], writes=[xk(dc, tg)])

    ln_fm(xs, 1024, lambda i: sm[:, O_LNW + 8 + i:O_LNW + 9 + i], lambda i: sm[:, O_LNB + 8 + i:O_LNB + 9 + i], xs)
    xo_v = xo.rearrange("(k p) t -> p k t", p=128)
    for k in range(8):
        P.dma("sp", xo_v[:, k, :], x[:, k, :], reads=[xk(k, tg) for tg in range(4)], is_output=True)
    return P.finish()


def run_k3(layer, xT_shards, mods, of, ob, o32_full, numC, denC, numD, denD, inp):
    moe = (layer % 2 == 1)
    T = TOK
    sm = np.zeros((128, 128), np.float32)
    anw = np.asarray(inp["a_norm_w"])[layer]
    sm[:, 0] = np.concatenate([anw, anw])
    cw = np.asarray(inp["b_conv_w"])[layer]
    for b in range(2):
        sm[:, 1 + b * 31:1 + (b + 1) * 31] = cw[:, b * 128:(b + 1) * 128].T
    sm[:, 63:65] = col128(np.asarray(inp["b_conv_b"])[layer])
    sm[:, 65:67] = col128(np.asarray(inp["b_norm_w"])[layer])
    sm[:, 67:69] = col128(np.asarray(inp["b_norm_b"])[layer])
    sink = np.asarray(inp["d_sink"])[layer]
    sm[:, 69:71] = col128(np.repeat(sink, 64))
    sm[:, 71:79] = col128(np.asarray(inp["ln_w"])[layer, 0])
    sm[:, 79:87] = col128(np.asarray(inp["ln_w"])[layer, 1])
    sm[:, 87:95] = col128(np.asarray(inp["ln_b"])[layer, 0])
    sm[:, 95:103] = col128(np.asarray(inp["ln_b"])[layer, 1])
    blk64 = np.kron(np.eye(2, dtype=np.float32), np.ones((64, 64), np.float32))
    u = o32_full["u"]
    upad = np.zeros((256, SEQ + 32), np.float32)
    upad[:, 15:15 + SEQ] = u
    denC_rep = np.repeat(denC, 64, axis=1)
    denD_rep = np.repeat(denD, 64, axis=0)
    w_out = np.ascontiguousarray(inp["w_out"][layer])
    common = {"smalls": sm, "blk64": blk64, "w_out": w_out, "identb": np.eye(128, dtype=np.float32).astype(ml_dtypes.bfloat16)}
    if moe:
        li = layer // 2
        sel = np.zeros((8, 8 * 128), np.float32)
        for e in range(8):
            sel[e, e * 128:(e + 1) * 128] = 1.0
        common.update({"wr": np.ascontiguousarray(inp["moe_router"][li]), "w_up": np.ascontiguousarray(inp["moe_w_up"][li]),
                       "w_down": np.ascontiguousarray(inp["moe_w_down"][li]), "sel": sel, "identf": np.eye(128, dtype=np.float32)})
    else:
        li = layer // 2
        common.update({"w_up": np.ascontiguousarray(inp["ffn_w_up"][li]), "w_down": np.ascontiguousarray(inp["ffn_w_down"][li])})
    in_maps = []
    for c in range(NCORES):
        ts = slice(c * T, (c + 1) * T)
        m = dict(common)
        m.update({"xT": xT_shards[c], "modc": mods[c], "ofT": np.ascontiguousarray(of[:, ts]), "obT": np.ascontiguousarray(ob[:, ts]),
                  "gaT": np.ascontiguousarray(o32_full["ga"][:, ts]), "uext": np.ascontiguousarray(upad[:, c * T:c * T + T + 32]),
                  "numC": np.ascontiguousarray(numC[:, :, ts]), "denC": np.ascontiguousarray(denC_rep[:, :, ts]),
                  "numD": np.ascontiguousarray(numD[:, ts]), "denD": np.ascontiguousarray(denD_rep[:, ts])})
        in_maps.append(m)
    res = run(("k3", moe), lambda: build_k3(moe), in_maps)
    return [r["xo"] for r in res]


def run_layer(layer, xT_shards, inp, modc=None):
    if modc is None:
        modc = run_k0(inp)[layer]
    r1 = run_k1(layer, xT_shards, modc, inp)
    o32_full = {nm: np.concatenate([r["o32"][off:off + 256] for r in r1], axis=1) for nm, off in O32.items()}
    obf_full = {nm: np.concatenate([r["obf"][off:off + (128 if nm in ("dk", "dv") else 256)] for r in r1], axis=1)
                for nm, off in OBF.items()}
    mods = [modc for _ in range(NCORES)]
    of, ob = run_k2a(o32_full)
    numC, denC, numD, denD = run_k2b(obf_full)
    return run_k3(layer, xT_shards, mods, of, ob, o32_full, numC, denC, numD, denD, inp)


def kernel(**inp):
    inp = {k: np.asarray(v) for k, v in inp.items()}
    x = inp["x"][0]
    xT_shards = [np.ascontiguousarray(x[c * TOK:(c + 1) * TOK].T) for c in range(NCORES)]
    modcs = run_k0(inp)
    for layer in range(DEPTH):
        xT_shards = run_layer(layer, xT_shards, inp, modcs[layer])
    out = np.concatenate([s.T for s in xT_shards], axis=0)[None]
    return np.ascontiguousarray(out.astype(np.float32))
```

```python
import math
from contextlib import ExitStack

import numpy as np
import ml_dtypes

import concourse.bass as bass
import concourse.mybir as mybir
from concourse.bass_utils import run_bass_kernel_spmd

F32 = mybir.dt.float32
BF16 = mybir.dt.bfloat16
I32 = mybir.dt.int32
AF = mybir.ActivationFunctionType
ALU = mybir.AluOpType
AX = mybir.AxisListType

NCORES = 8
D_MODEL = 1024
SEQ = 16384
TOK = SEQ // NCORES
DEPTH = 2
HD = 64
FFN_DIM = 2816
N_EXPERTS = 8
EXPERT_DIM = 3584
B_KERNEL = 31
ROPE_THETA = 500000.0
ROPE_DIM = 16
DEEPNORM_ALPHA = (2 * DEPTH) ** 0.25
LN_EPS = 1e-5
RMS_EPS = 1e-6
NEG = -30000.0
TWO_PI = 2.0 * math.pi


class Prog:
    NDS = 24

    def __init__(self):
        self.nc = bass.Bass("TRN2", target_bir_lowering=False)
        nc = self.nc
        self.es = ExitStack()
        self.q = {"pe": nc.tensor, "dve": nc.vector, "act": nc.scalar, "pool": nc.gpsimd, "sp": nc.sync}
        self.esem = {e: self.es.enter_context(nc.semaphore("es_" + e)) for e in ("pe", "dve", "act", "pool")}
        self.ecnt = {e: 0 for e in self.esem}
        self.dsem = [self.es.enter_context(nc.semaphore(f"ds{i}")) for i in range(self.NDS)]
        self.dval = [0] * self.NDS
        self.dnext = 0
        self.seen = {e: {} for e in self.q}
        self.lastw = {}
        self.readers = {}
        self.out_tokens = []
        self.n_inst = 0
        self._ps_id = 0

    def dram_in(self, name, shape, dt):
        return self.nc.dram_tensor(name, list(shape), dt, kind="ExternalInput").ap()

    def dram_out(self, name, shape, dt):
        return self.nc.dram_tensor(name, list(shape), dt, kind="ExternalOutput").ap()

    def sb(self, name, shape, dt):
        return self.es.enter_context(self.nc.sbuf_tensor("sb_" + name, list(shape), dt))

    def ps(self, name, shape, dt=F32):
        return self.es.enter_context(self.nc.psum_tensor("pm_" + name, list(shape), dt))

    def _wait(self, e, tok):
        sem, v, owner = tok
        if owner == e and e == "pe":
            return
        k = id(sem)
        if self.seen[e].get(k, 0) >= v:
            return
        self.q[e].wait_ge(sem, v)
        self.seen[e][k] = v

    def _deps(self, e, reads, writes):
        for k in reads:
            t = self.lastw.get(k)
            if t is not None:
                self._wait(e, t)
        for k in writes:
            t = self.lastw.get(k)
            if t is not None:
                self._wait(e, t)
            for t in self.readers.get(k, {}).values():
                self._wait(e, t)

    def _record(self, tok, reads, writes):
        for k in writes:
            self.lastw[k] = tok
            self.readers[k] = {}
        for k in reads:
            self.readers.setdefault(k, {})[id(tok[0])] = tok

    def op(self, e, fn, reads=(), writes=()):
        self._deps(e, reads, writes)
        inst = fn(self.q[e])
        self.ecnt[e] += 1
        inst.then_inc(self.esem[e], 1)
        tok = (self.esem[e], self.ecnt[e], e)
        self._record(tok, reads, writes)
        self.n_inst += 1
        return tok

    def dma(self, e, out, in_, reads=(), writes=(), is_output=False, **kw):
        self._deps(e, reads, writes)
        j = self.dnext
        self.dnext = (self.dnext + 1) % self.NDS
        if self.dval[j] > 0:
            self._wait(e, (self.dsem[j], self.dval[j], "dma"))
        self.q[e].dma_start(out=out, in_=in_, **kw).then_inc(self.dsem[j], 16)
        self.dval[j] += 16
        tok = (self.dsem[j], self.dval[j], "dma")
        self._record(tok, reads, writes)
        if is_output:
            self.out_tokens.append(tok)
        self.n_inst += 1
        return tok

    def finish(self):
        for j in range(self.NDS):
            if self.dval[j] > 0:
                self._wait("sp", (self.dsem[j], self.dval[j], "dma"))
        return self.nc


def chunks(n, c):
    return [(i, min(c, n - i)) for i in range(0, n, c)]


class PsumPool:
    def __init__(self, P, n=8, prefix="pb"):
        self.t = [P.ps(f"{prefix}{i}", [128, 512], F32) for i in range(n)]
        self.k = [f"{prefix}{i}" for i in range(n)]
        self.i = 0
        self.n = n

    def get(self):
        i = self.i
        self.i = (self.i + 1) % self.n
        return self.t[i], self.k[i]


def build_consts(P):
    c = {}
    c["ones_f"] = P.sb("ones_f", [128, 512], F32)
    P.op("pool", lambda q: q.memset(c["ones_f"][:], 1.0), writes=["ones_f"])
    c["ones_b"] = P.sb("ones_b", [128, 512], BF16)
    P.op("pool", lambda q: q.memset(c["ones_b"][:], 1.0), writes=["ones_b"])
    return c


CH = {"aq": (0, 2), "aff": (2, 2), "afb": (4, 2), "ai": (6, 2), "ag": (8, 2), "bv": (10, 2), "bg": (12, 2),
      "cq": (14, 2), "ck": (16, 2), "cv": (18, 2), "dq": (20, 2), "dk": (22, 1), "dv": (23, 1)}
ROT_CHUNKS = [14, 15, 16, 17, 20, 21, 22]
O32 = {"qa": 0, "kf": 256, "lf": 512, "kb": 768, "lb": 1024, "va": 1280, "ga": 1536, "u": 1792}
OBF = {"cq": 0, "ck": 256, "cv": 512, "dq": 768, "dk": 1024, "dv": 1152}
NBF = 1280


def build_k1(layer):
    P = Prog()
    nc = P.nc
    T = TOK
    xT = P.dram_in("xT", [D_MODEL, T], F32)
    modc_in = P.dram_in("modc", [128, 48], F32)
    w_in = P.dram_in("w_in", [D_MODEL, 3072], F32)
    w_sw = P.dram_in("w_sw", [D_MODEL, 7 * 128], F32)
    pos = P.dram_in("pos", [T], I32)
    rcol = P.dram_in("rcol", [128, 2], F32)
    alb = P.dram_in("alb", [128, 4], F32)
    o32 = P.dram_out("o32", [2048, T], F32)
    obf = P.dram_out("obf", [NBF, T], BF16)

    C = build_consts(P)
    pp = PsumPool(P)

    mod = P.sb("mod", [128, 48], F32)
    P.dma("sp", mod[:], modc_in, writes=["mod"])
    sc1 = P.sb("sc1", [128, 8], F32)
    P.op("dve", lambda q: q.tensor_scalar(out=sc1[:], in0=mod[:, 8:16], scalar1=1.0, scalar2=None, op0=ALU.add),
         reads=["mod"], writes=["sc1"])

    hT = P.sb("hT", [128, 8, T], BF16)
    xst = [P.sb(f"xst{i}", [128, T], F32) for i in range(2)]
    xT_v = xT.rearrange("(k p) t -> p k t", p=128)
    for k in range(8):
        st = xst[k % 2]
        sk = f"xst{k % 2}"
        P.dma("sp", st[:], xT_v[:, k, :], writes=[sk])
        P.op("dve", lambda q, k=k, st=st: q.tensor_scalar(out=hT[:, k, :], in0=st[:], scalar1=sc1[:, k:k + 1],
                                                        scalar2=mod[:, k:k + 1], op0=ALU.mult, op1=ALU.add),
             reads=[sk, "sc1", "mod"], writes=[f"hT{k}"])
    hkeys = [f"hT{k}" for k in range(8)]

    wb = P.sb("wb", [128, 8, 3072], BF16)
    wsw = P.sb("wsw", [128, 8, 896], BF16)
    w_in_v = w_in.rearrange("(k p) e -> p k e", p=128)
    w_sw_v = w_sw.rearrange("(k p) e -> p k e", p=128)
    for (c0, nch) in ((10, 4), (0, 4), (4, 4), (8, 2)):
        P.dma("pool", wb[:, :, c0 * 128:(c0 + nch) * 128], w_in_v[:, :, c0 * 128:(c0 + nch) * 128], writes=[f"wbc{c}" for c in range(c0, c0 + nch)])
    P.dma("pool", wb[:, :, 14 * 128:18 * 128], w_in_v[:, :, 14 * 128:18 * 128], writes=[f"wbc{c}" for c in range(14, 18)])
    P.dma("pool", wsw[:, :, 0:4 * 128], w_sw_v[:, :, 0:4 * 128], writes=[f"wswc{c}" for c in range(0, 4)])
    P.dma("pool", wb[:, :, 18 * 128:24 * 128], w_in_v[:, :, 18 * 128:24 * 128], writes=[f"wbc{c}" for c in range(18, 24)])
    P.dma("pool", wsw[:, :, 4 * 128:7 * 128], w_sw_v[:, :, 4 * 128:7 * 128], writes=[f"wswc{c}" for c in range(4, 7)])

    rc = P.sb("rc", [128, 2], F32)
    P.dma("sp", rc[:], rcol, writes=["rc"])
    tmpi = P.sb("tmpi", [128, T], I32)
    P.dma("sp", tmpi[:], pos.partition_broadcast(128), writes=["tmpi"])
    ang = P.sb("ang", [128, T], F32)
    cosT = P.sb("cosT", [128, T], F32)
    sinT = P.sb("sinT", [128, T], F32)
    tmpf = P.sb("tmpf", [128, T], F32)
    P.op("dve", lambda q: q.tensor_copy(out=ang[:], in_=tmpi[:]), reads=["tmpi"], writes=["ang"])
    P.op("dve", lambda q: q.tensor_scalar(out=ang[:], in0=ang[:], scalar1=rc[:, 0:1], scalar2=None, op0=ALU.mult),
         reads=["ang", "rc"], writes=["ang"])
    C1 = 6.28125
    C2 = TWO_PI - C1

    def sin_table(dst, dkey, phase):
        P.op("dve", lambda q: q.tensor_scalar(out=tmpf[:], in0=ang[:], scalar1=phase, scalar2=1.0 / TWO_PI,
                                              op0=ALU.add, op1=ALU.mult), reads=["ang"], writes=["tmpf"])
        P.op("dve", lambda q: q.tensor_copy(out=tmpi[:], in_=tmpf[:]), reads=["tmpf"], writes=["tmpi"])
        P.op("dve", lambda q: q.tensor_copy(out=tmpf[:], in_=tmpi[:]), reads=["tmpi"], writes=["tmpf"])
        P.op("dve", lambda q: q.scalar_tensor_tensor(out=dst[:], in0=tmpf[:], scalar=-C1, in1=ang[:],
                                                     op0=ALU.mult, op1=ALU.add), reads=["tmpf", "ang"], writes=[dkey])
        P.op("dve", lambda q: q.scalar_tensor_tensor(out=dst[:], in0=tmpf[:], scalar=-C2, in1=dst[:],
                                                     op0=ALU.mult, op1=ALU.add), reads=["tmpf", dkey], writes=[dkey])
        P.op("dve", lambda q: q.tensor_scalar(out=dst[:], in0=dst[:], scalar1=phase, scalar2=-math.pi,
                                              op0=ALU.add, op1=ALU.max), reads=[dkey], writes=[dkey])
        P.op("dve", lambda q: q.tensor_scalar(out=dst[:], in0=dst[:], scalar1=math.pi, scalar2=None,
                                              op0=ALU.min), reads=[dkey], writes=[dkey])
        P.op("act", lambda q: q.activation(out=dst[:], in_=dst[:], func=AF.Sin), reads=[dkey], writes=[dkey])

    sin_table(cosT, "cosT", math.pi / 2)
    sin_table(sinT, "sinT", 0.0)
    P.op("dve", lambda q: q.tensor_scalar(out=sinT[:], in0=sinT[:], scalar1=rc[:, 1:2], scalar2=None, op0=ALU.mult),
         reads=["sinT", "rc"], writes=["sinT"])

    albs = P.sb("albs", [128, 4], F32)
    P.dma("sp", albs[:], alb, writes=["albs"])
    lbc = P.sb("lbc", [128, 2], F32)
    oml = P.sb("oml", [128, 2], F32)
    if layer == 0:
        P.op("pool", lambda q: q.memset(lbc[:], 0.0), writes=["lbc"])
        P.op("pool", lambda q: q.memset(oml[:], 1.0), writes=["oml"])
    else:
        ex = P.sb("alb_ex", [128, 4], F32)
        P.op("act", lambda q: q.activation(out=ex[:], in_=albs[:], func=AF.Exp), reads=["albs"], writes=["alb_ex"])
        exv = ex[:].rearrange("p (t l) -> p t l", l=2)
        sm = P.sb("alb_sm", [128, 2], F32)
        P.op("dve", lambda q: q.tensor_tensor(out=sm[:], in0=exv[:, :, 0], in1=exv[:, :, 1], op=ALU.add),
             reads=["alb_ex"], writes=["alb_sm"])
        P.op("dve", lambda q: q.reciprocal(out=sm[:], in_=sm[:]), reads=["alb_sm"], writes=["alb_sm"])
        P.op("dve", lambda q: q.tensor_tensor(out=lbc[:], in0=exv[:, :, 1], in1=sm[:], op=ALU.mult),
             reads=["alb_ex", "alb_sm"], writes=["lbc"])
        P.op("dve", lambda q: q.tensor_scalar(out=oml[:], in0=lbc[:], scalar1=-1.0, scalar2=1.0, op0=ALU.mult,
                                              op1=ALU.add), reads=["lbc"], writes=["oml"])

    ob_f = [P.sb(f"obf{i}", [128, 512], F32) for i in range(4)]
    ob_b = [P.sb(f"obb{i}", [128, 512], BF16) for i in range(4)]
    sg = [P.sb(f"sg{i}", [128, T], F32) for i in range(2)]
    cnt = {"f": 0, "b": 0}

    def getf():
        i = cnt["f"] % 4
        cnt["f"] += 1
        return ob_f[i], f"obf{i}"

    def getb():
        i = cnt["b"] % 4
        cnt["b"] += 1
        return ob_b[i], f"obb{i}"

    def proj(ps, psk, wt, wkey, col0, tg):
        for k in range(8):
            P.op("pe", lambda q, k=k: q.matmul(ps[:, :], lhsT=wt[:, k, col0:col0 + 128],
                                               rhs=hT[:, k, tg * 512:(tg + 1) * 512], start=(k == 0), stop=(k == 7)),
                 reads=[wkey, hkeys[k]], writes=[psk])

    def store32(name, tile_i, tg, buf, bkey):
        r0 = O32[name] + tile_i * 128
        P.dma("sp", o32[r0:r0 + 128, tg * 512:(tg + 1) * 512], buf[:], reads=[bkey], is_output=True)

    def storebf(name, tile_i, tg, buf, bkey):
        r0 = OBF[name] + tile_i * 128
        P.dma("sp", obf[r0:r0 + 128, tg * 512:(tg + 1) * 512], buf[:], reads=[bkey], is_output=True)

    order = ["bg", "bv", "aq", "aff", "afb", "ai", "ag", "cq", "ck", "cv", "dq", "dk", "dv"]
    for name in order:
        c0, ncn = CH[name]
        for ti in range(ncn):
            ch = c0 + ti
            for tg in range(4):
                tsl = slice(tg * 512, (tg + 1) * 512)
                ps, psk = pp.get()
                proj(ps, psk, wb, f"wbc{ch}", ch * 128, tg)
                if name == "bg":
                    P.op("act", lambda q, ps=ps, ti=ti, tsl=tsl: q.activation(out=sg[ti][:, tsl], in_=ps[:], func=AF.Sigmoid),
                         reads=[psk], writes=[f"sg{ti}_{tg}"])
                elif name == "bv":
                    b, bk = getf()
                    P.op("dve", lambda q, ps=ps, b=b, ti=ti, tsl=tsl: q.tensor_tensor(out=b[:], in0=ps[:], in1=sg[ti][:, tsl], op=ALU.mult),
                         reads=[psk, f"sg{ti}_{tg}"], writes=[bk])
                    store32("u", ti, tg, b, bk)
                elif name in ("aq", "ag"):
                    b, bk = getf()
                    P.op("act", lambda q, ps=ps, b=b: q.activation(out=b[:], in_=ps[:], func=AF.Silu), reads=[psk], writes=[bk])
                    store32("qa" if name == "aq" else "ga", ti, tg, b, bk)
                elif name == "ai":
                    b, bk = getf()
                    P.op("act", lambda q, ps=ps, b=b: q.activation(out=b[:], in_=ps[:], func=AF.Copy), reads=[psk], writes=[bk])
                    store32("va", ti, tg, b, bk)
                elif name in ("aff", "afb"):
                    fb_, fk = getf()
                    P.op("act", lambda q, ps=ps, fb_=fb_: q.activation(out=fb_[:], in_=ps[:], func=AF.Exp, scale=-1.0), reads=[psk], writes=[fk])
                    P.op("dve", lambda q, fb_=fb_: q.tensor_scalar(out=fb_[:], in0=fb_[:], scalar1=1.0, scalar2=None, op0=ALU.add), reads=[fk], writes=[fk])
                    P.op("dve", lambda q, fb_=fb_: q.reciprocal(out=fb_[:], in_=fb_[:]), reads=[fk], writes=[fk])
                    P.op("dve", lambda q, fb_=fb_, ti=ti: q.tensor_scalar(out=fb_[:], in0=fb_[:], scalar1=oml[:, ti:ti + 1],
                                                                        scalar2=lbc[:, ti:ti + 1], op0=ALU.mult, op1=ALU.add),
                         reads=[fk, "oml", "lbc"], writes=[fk])
                    kb_, kk = getf()
                    P.op("dve", lambda q, fb_=fb_, kb_=kb_: q.tensor_scalar(out=kb_[:], in0=fb_[:], scalar1=-1.0, scalar2=1.0,
                                                                          op0=ALU.mult, op1=ALU.add), reads=[fk], writes=[kk])
                    store32("kf" if name == "aff" else "kb", ti, tg, kb_, kk)
                    P.op("act", lambda q, fb_=fb_: q.activation(out=fb_[:], in_=fb_[:], func=AF.Ln), reads=[fk], writes=[fk])
                    store32("lf" if name == "aff" else "lb", ti, tg, fb_, fk)
                elif name in ("cv", "dv"):
                    b, bk = getb()
                    P.op("act", lambda q, ps=ps, b=b: q.activation(out=b[:], in_=ps[:], func=AF.Copy), reads=[psk], writes=[bk])
                    storebf(name, ti, tg, b, bk)
                else:
                    ri = ROT_CHUNKS.index(ch)
                    ps2, psk2 = pp.get()
                    proj(ps2, psk2, wsw, f"wswc{ri}", ri * 128, tg)
                    t1, t1k = getf()
                    t2, t2k = getf()
                    P.op("dve", lambda q, ps=ps, t1=t1, tsl=tsl: q.tensor_tensor(out=t1[:], in0=ps[:], in1=cosT[:, tsl], op=ALU.mult),
                         reads=[psk, "cosT"], writes=[t1k])
                    P.op("dve", lambda q, ps2=ps2, t2=t2, tsl=tsl: q.tensor_tensor(out=t2[:], in0=ps2[:], in1=sinT[:, tsl], op=ALU.mult),
                         reads=[psk2, "sinT"], writes=[t2k])
                    b, bk = getb()
                    P.op("pool", lambda q, t1=t1, t2=t2, b=b: q.tensor_tensor(out=b[:], in0=t1[:], in1=t2[:], op=ALU.add),
                         reads=[t1k, t2k], writes=[bk])
                    storebf(name, ti, tg, b, bk)
    return P.finish()


def col128(v):
    v = np.asarray(v)
    return np.ascontiguousarray(v.reshape(-1, 128).T)


def rot_cols():
    idx = []
    for ch in ROT_CHUNKS:
        for h in range(2):
            base = ch * 128 + h * 64
            loc = np.arange(64)
            loc[:8] = np.arange(8, 16)
            loc[8:16] = np.arange(0, 8)
            idx.append(base + loc)
    return np.concatenate(idx)


def rot_consts():
    half = ROPE_DIM // 2
    inv = (np.float32(ROPE_THETA) ** (-np.arange(half, dtype=np.float32) * np.float32(2.0 / ROPE_DIM))).astype(np.float32)
    rc = np.zeros((128, 2), np.float32)
    for p in range(128):
        d = p % 64
        if d < 16:
            rc[p, 0] = inv[d % 8]
            rc[p, 1] = -1.0 if d < 8 else 1.0
    return rc


_cache = {}


def run(nc_key, builder, in_maps):
    if nc_key not in _cache:
        _cache[nc_key] = builder()
    nc = _cache[nc_key]
    res = run_bass_kernel_spmd(nc, in_maps, core_ids=list(range(NCORES)))
    return res.results


def build_k0():
    P = Prog()
    ccol = P.dram_in("ccol", [128, 8], F32)
    w = P.dram_in("w", [D_MODEL, 1536], F32)
    bias = P.dram_in("bias", [1, 1536], F32)
    o = P.dram_out("o", [1, 1536], F32)
    sc = P.sb("sc", [128, 8], F32)
    P.dma("sp", sc[:], ccol, writes=["sc"])
    P.op("act", lambda q: q.activation(out=sc[:], in_=sc[:], func=AF.Silu), reads=["sc"], writes=["sc"])
    bs = P.sb("bs", [1, 1536], F32)
    P.dma("sp", bs[:], bias, writes=["bs"])
    ws = P.sb("ws", [128, 8, 1536], F32)
    wv = w.rearrange("(k p) e -> p k e", p=128)
    for g in range(3):
        P.dma("sp", ws[:, :, g * 512:(g + 1) * 512], wv[:, :, g * 512:(g + 1) * 512], writes=[f"ws{g}"])
    ob = P.sb("ob", [1, 1536], F32)
    pss = [P.ps(f"p{i}", [128, 512], F32) for i in range(3)]
    for g in range(3):
        for k in range(8):
            P.op("pe", lambda q, g=g, k=k: q.matmul(pss[g][0:1, :], lhsT=sc[:, k:k + 1], rhs=ws[:, k, g * 512:(g + 1) * 512],
                                                   start=(k == 0), stop=(k == 7)), reads=["sc", f"ws{g}"], writes=[f"p{g}"])
        P.op("dve", lambda q, g=g: q.tensor_tensor(out=ob[:, g * 512:(g + 1) * 512], in0=pss[g][0:1, :], in1=bs[:, g * 512:(g + 1) * 512], op=ALU.add),
             reads=[f"p{g}", "bs"], writes=["ob"])
    P.dma("sp", o, ob[:], reads=["ob"], is_output=True)
    return P.finish()


def run_k0(inp):
    wa = np.asarray(inp["w_ada"])
    wcat = np.concatenate([wa[l] for l in range(DEPTH)], axis=1)
    bcat = np.concatenate([np.asarray(inp["b_ada"])[l] for l in range(DEPTH)])[None, :]
    ccol = col128(np.asarray(inp["c"])[0])
    in_maps = [{"ccol": ccol, "w": np.ascontiguousarray(wcat[:, c * 1536:(c + 1) * 1536]),
                "bias": np.ascontiguousarray(bcat[:, c * 1536:(c + 1) * 1536])} for c in range(NCORES)]
    res = run(("k0",), build_k0, in_maps)
    mod = np.concatenate([r["o"][0] for r in res])
    return [col128(mod[l * 6144:(l + 1) * 6144]) for l in range(DEPTH)]


def run_k1(layer, xT_shards, modc, inp):
    rc = rot_consts()
    sw = rot_cols()
    w_in_l = np.ascontiguousarray(inp["w_in"][layer])
    w_sw = np.ascontiguousarray(w_in_l[:, sw])
    alb = np.zeros((128, 4), np.float32)
    a = np.asarray(inp["a_lower_bound"])
    for t in range(2):
        for l in range(2):
            alb[:, t * 2 + l] = a[l, t * 128:(t + 1) * 128]
    pos = np.asarray(inp["positions"])[0].astype(np.int32)
    in_maps = []
    for c in range(NCORES):
        in_maps.append({"xT": xT_shards[c], "modc": modc, "w_in": w_in_l, "w_sw": w_sw,
                        "pos": np.ascontiguousarray(pos[c * TOK:(c + 1) * TOK]), "rcol": rc, "alb": alb})
    return run(("k1", layer), lambda: build_k1(layer), in_maps)


SEG = 1024
NSEG = SEQ // SEG
NCH = SEG // 16
NTL = SEG // 128


def hgrn_consts():
    ident = np.eye(128, dtype=np.float32).astype(ml_dtypes.bfloat16)
    s = np.arange(128)
    tri = ((s[:, None] // 16 == s[None, :] // 16) & (s[:, None] <= s[None, :])).astype(np.float32).astype(ml_dtypes.bfloat16)
    ind = (s[:, None] // 16 == np.arange(8)[None, :]).astype(np.float32).astype(ml_dtypes.bfloat16)
    return ident, tri, ind


def build_k2a():
    P = Prog()
    qT = P.dram_in("qT", [64, SEQ], F32)
    kT = P.dram_in("kT", [64, SEQ], F32)
    lT = P.dram_in("lT", [64, SEQ], F32)
    vtok = P.dram_in("vtok", [SEQ, 64], F32)
    ident_d = P.dram_in("ident_d", [128, 128], BF16)
    tri_d = P.dram_in("tri_d", [128, 128], BF16)
    ind_d = P.dram_in("ind_d", [128, 8], BF16)
    oT = P.dram_out("oT", [64, SEQ], F32)

    ident = P.sb("ident", [128, 128], BF16)
    tri = P.sb("tri", [128, 128], BF16)
    ind = P.sb("ind", [128, 8], BF16)
    P.dma("sp", ident[:], ident_d, writes=["ident"])
    P.dma("sp", tri[:], tri_d, writes=["tri"])
    P.dma("sp", ind[:], ind_d, writes=["ind"])

    reset = P.sb("reset", [64, SEG], F32)
    P.op("pool", lambda q: q.memset(reset[:], 1.0), writes=["reset"])
    P.op("pool", lambda q: q.memset(reset[:].rearrange("p (n c) -> p n c", c=16)[:, :, 0:1], 0.0), reads=["reset"], writes=["reset"])

    def two(name, shape, dt):
        return [P.sb(f"{name}{i}", shape, dt) for i in range(2)]

    def three(name, shape, dt):
        return [P.sb(f"{name}{i}", shape, dt) for i in range(3)]

    qs_, ks_, ls_ = three("qs", [64, SEG], F32), three("ks", [64, SEG], F32), three("ls", [64, SEG], F32)
    vb_ = three("vb", [128, NTL, 64], BF16)
    cum_, ex_ = three("cum", [64, SEG], F32), three("ex", [64, SEG], F32)
    qt_, kt_, kh_ = three("qt", [64, SEG], BF16), three("kt", [64, SEG], BF16), three("kh", [64, SEG], BF16)
    dec_ = three("dec", [64, NCH], F32)
    kvs_ = two("kvs", [64, 64 * NCH], F32)
    sprev_ = two("sprev", [64, NCH, 64], BF16)
    obuf_ = two("obuf", [64, SEG], F32)
    decrep = P.sb("decrep", [64, 64 * NCH], F32)
    decz = P.sb("decz", [64, NCH], F32)
    P.op("pool", lambda q: q.memset(decz[:], 0.0), writes=["decz"])
    sall = P.sb("sall", [64, 64 * NCH], F32)
    s_in = P.sb("s_in", [64, 64], F32)
    khtok = [P.sb(f"khtok{i}", [128, 64], BF16) for i in range(2)]
    vblk = [P.sb(f"vblk{i}", [128, 8, 64], BF16) for i in range(2)]
    am = [P.sb(f"am{i}", [128, 128], BF16) for i in range(2)]
    P.op("pool", lambda q: q.memset(s_in[:], 0.0), writes=["s_in"])

    ps_kv = [P.ps(f"ps_kv{i}", [128, 512], F32) for i in range(2)]
    ps_a = [P.ps(f"ps_a{i}", [128, 512], F32) for i in range(2)]
    ps_o = [P.ps(f"ps_o{i}", [128, 512], F32) for i in range(2)]
    ps_t = [P.ps(f"ps_t{i}", [128, 1024], BF16) for i in range(2)]

    dec3 = decrep[:].rearrange("p (v n) -> p v n", n=NCH)
    sall3 = sall[:].rearrange("p (v n) -> p v n", n=NCH)
    vt_v = vtok.rearrange("(s i p) v -> s p i v", p=128, i=NTL)
    cnt = [0]

    def stage(sgi, part):
        pb = sgi % 2
        p3 = sgi % 3
        K_ = lambda n: f"{n}{pb if n in ('kvs', 'sprev', 'obuf') else p3}"
        qs, ks, ls, vb, cum, ex = qs_[p3], ks_[p3], ls_[p3], vb_[p3], cum_[p3], ex_[p3]
        qt, kt, kh, dec, kvs, sprev, obuf = qt_[p3], kt_[p3], kh_[p3], dec_[p3], kvs_[pb], sprev_[pb], obuf_[pb]
        kvs3 = kvs[:].rearrange("p (v n) -> p v n", n=NCH)
        cum3 = cum[:].rearrange("p (n c) -> p n c", c=16)
        ex3 = ex[:].rearrange("p (n c) -> p n c", c=16)
        tsl = slice(sgi * SEG, (sgi + 1) * SEG)
        if part == "B":
            yield from stageB(sgi, pb, K_, qt, kt, vb, dec, kvs, kvs3, sprev, obuf, tsl)
            return
        if part == "A1":
            yield P.dma("sp", qs[:], qT[:, tsl], writes=[K_("qs")])
            yield P.dma("sp", ks[:], kT[:, tsl], writes=[K_("ks")])
            yield P.dma("sp", ls[:], lT[:, tsl], writes=[K_("ls")])
            yield P.dma("pool", vb[:], vt_v[sgi], writes=[K_("vb")])
            yield P.op("dve", lambda q: q.tensor_tensor_scan(out=cum[:], data0=reset[:], data1=ls[:], initial=0.0,
                                                       op0=ALU.mult, op1=ALU.add), reads=["reset", K_("ls")], writes=[K_("cum")])
            yield P.op("act", lambda q: q.activation(out=ex[:], in_=cum[:], func=AF.Exp), reads=[K_("cum")], writes=[K_("ex")])
            yield P.op("dve", lambda q: q.tensor_tensor(out=qt[:], in0=qs[:], in1=ex[:], op=ALU.mult), reads=[K_("qs"), K_("ex")], writes=[K_("qt")])
            yield P.op("act", lambda q: q.activation(out=ex[:], in_=cum[:], func=AF.Exp, scale=-1.0), reads=[K_("cum")], writes=[K_("ex")])
            yield P.op("dve", lambda q: q.tensor_tensor(out=kt[:], in0=ks[:], in1=ex[:], op=ALU.mult), reads=[K_("ks"), K_("ex")], writes=[K_("kt")])
            yield P.op("dve", lambda q: q.tensor_tensor(out=ex3, in0=cum3[:, :, 15:16].broadcast_to([64, NCH, 16]), in1=cum3,
                                                  op=ALU.subtract), reads=[K_("cum")], writes=[K_("ex")])
            yield P.op("act", lambda q: q.activation(out=ex[:], in_=ex[:], func=AF.Exp), reads=[K_("ex")], writes=[K_("ex")])
            yield P.op("dve", lambda q: q.tensor_tensor(out=kh[:], in0=ks[:], in1=ex[:], op=ALU.mult), reads=[K_("ks"), K_("ex")], writes=[K_("kh")])
            yield P.op("act", lambda q: q.activation(out=dec[:], in_=cum3[:, :, 15], func=AF.Exp), reads=[K_("cum")], writes=[K_("dec")])
            return
        bs = []
        for i in range(NTL):
            bs.append(cnt[0] % 2)
            cnt[0] += 1

        def emit_T(i):
            b = bs[i]
            yield P.op("pe", lambda q: q.transpose(ps_t[b][:, 0:64], kh[:, i * 128:(i + 1) * 128], ident[0:64, 0:64]),
                 reads=[K_("kh"), "ident"], writes=[f"ps_t{b}"])
            yield P.op("act", lambda q: q.activation(out=khtok[b][:], in_=ps_t[b][:, 0:64], func=AF.Copy),
                 reads=[f"ps_t{b}"], writes=[f"khtok{b}"])
            yield P.op("dve", lambda q: q.tensor_tensor(out=vblk[b][:], in0=vb[:, i:i + 1, :].broadcast_to([128, 8, 64]),
                                                   in1=ind[:].unsqueeze(2).broadcast_to([128, 8, 64]), op=ALU.mult),
                 reads=[K_("vb"), "ind"], writes=[f"vblk{b}"])

        def emit_KV(i):
            b = bs[i]
            yield P.op("pe", lambda q: q.matmul(ps_kv[b][0:64, :], lhsT=khtok[b][:], rhs=vblk[b][:].rearrange("p c v -> p (c v)"),
                                          start=True, stop=True), reads=[f"khtok{b}", f"vblk{b}"], writes=[f"ps_kv{b}"])
            yield P.op("act", lambda q: q.activation(out=kvs3[:, :, 8 * i:8 * i + 8],
                                               in_=ps_kv[b][0:64, :].rearrange("p (c v) -> p v c", v=64), func=AF.Copy),
                 reads=[f"ps_kv{b}"], writes=[K_("kvs")])

        yield from emit_T(0)
        for i in range(NTL):
            if i + 1 < NTL:
                yield from emit_T(i + 1)
            yield from emit_KV(i)

    def stageB(sgi, pb, K_, qt, kt, vb, dec, kvs, kvs3, sprev, obuf, tsl):
        yield P.op("act", lambda q: q.activation(out=decz[:, 1:NCH], in_=dec[:, 1:NCH], func=AF.Copy), reads=[K_("dec")], writes=["decz"])
        yield P.op("act", lambda q: q.activation(out=dec3, in_=decz[:].unsqueeze(1).broadcast_to([64, 64, NCH]), func=AF.Copy),
             reads=["decz"], writes=["decrep"])
        yield P.op("dve", lambda q: q.scalar_tensor_tensor(out=kvs3[:, :, 0], in0=s_in[:], scalar=dec[:, 0:1], in1=kvs3[:, :, 0],
                                                     op0=ALU.mult, op1=ALU.add), reads=["s_in", K_("dec"), K_("kvs")], writes=[K_("kvs")])
        yield P.op("dve", lambda q: q.tensor_tensor_scan(out=sall[:], data0=decrep[:], data1=kvs[:], initial=0.0,
                                                   op0=ALU.mult, op1=ALU.add), reads=["decrep", K_("kvs")], writes=["sall"])
        yield P.op("act", lambda q: q.activation(out=sprev[:, 0, :], in_=s_in[:], func=AF.Copy), reads=["s_in"], writes=[K_("sprev")])
        yield P.op("act", lambda q: q.activation(out=sprev[:, 1:NCH, :], in_=sall3[:, :, 0:NCH - 1].rearrange("p v n -> p n v"), func=AF.Copy),
             reads=["sall"], writes=[K_("sprev")])
        yield P.op("dve", lambda q: q.tensor_copy(out=s_in[:], in_=sall3[:, :, NCH - 1]), reads=["sall", K_("sprev")], writes=["s_in"])
        bs = []
        for i in range(NTL):
            bs.append(cnt[0] % 2)
            cnt[0] += 1

        def emit_A(i):
            b = bs[i]
            csl = slice(i * 128, (i + 1) * 128)
            yield P.op("pe", lambda q: q.matmul(ps_a[b][:, 0:128], lhsT=kt[:, csl], rhs=qt[:, csl], start=True, stop=True),
                 reads=[K_("kt"), K_("qt")], writes=[f"ps_a{b}"])
            yield P.op("dve", lambda q: q.tensor_tensor(out=am[b][:], in0=ps_a[b][:, 0:128], in1=tri[:], op=ALU.mult),
                 reads=[f"ps_a{b}", "tri"], writes=[f"am{b}"])

        def emit_O(i):
            b = bs[i]
            csl = slice(i * 128, (i + 1) * 128)
            yield P.op("pe", lambda q: q.matmul(ps_o[b][0:64, 0:128], lhsT=vb[:, i, :], rhs=am[b][:], start=True, stop=False),
                 reads=[K_("vb"), f"am{b}"], writes=[f"ps_o{b}"])
            for c in range(8):
                n = 8 * i + c
                yield P.op("pe", lambda q, c=c, n=n: q.matmul(ps_o[b][0:64, 16 * c:16 * c + 16], lhsT=sprev[:, n, :],
                                                       rhs=qt[:, i * 128 + 16 * c:i * 128 + 16 * c + 16],
                                                       start=False, stop=(c == 7), skip_group_check=True),
                     reads=[K_("sprev"), K_("qt")], writes=[f"ps_o{b}"])
            yield P.op("act", lambda q: q.activation(out=obuf[:, csl], in_=ps_o[b][0:64, 0:128], func=AF.Copy),
                 reads=[f"ps_o{b}"], writes=[K_("obuf")])

        yield from emit_A(0)
        for i in range(NTL):
            if i + 1 < NTL:
                yield from emit_A(i + 1)
            yield from emit_O(i)
        yield P.dma("sp", oT[:, tsl], obuf[:], reads=[K_("obuf")], is_output=True)

    def drain(*gens):
        gens = [g for g in gens if g is not None]
        while gens:
            for g in list(gens):
                try:
                    next(g)
                except StopIteration:
                    gens.remove(g)

    drain(stage(0, "A1"))
    drain(stage(0, "A2"), stage(1, "A1"))
    for sgi in range(NSEG):
        drain(stage(sgi, "B"), stage(sgi + 1, "A2") if sgi + 1 < NSEG else None,
              stage(sgi + 2, "A1") if sgi + 2 < NSEG else None)
    return P.finish()


def run_k2a(o32_full):
    ident, tri, ind = hgrn_consts()
    in_maps = []
    for c in range(NCORES):
        h, d = c % 4, c // 4
        rows = slice(h * 64, (h + 1) * 64)
        q = o32_full["qa"][rows]
        k = o32_full["kf" if d == 0 else "kb"][rows]
        l = o32_full["lf" if d == 0 else "lb"][rows]
        v = o32_full["va"][rows]
        if d == 1:
            q, k, l, v = q[:, ::-1], k[:, ::-1], l[:, ::-1], v[:, ::-1]
        in_maps.append({"qT": np.ascontiguousarray(q), "kT": np.ascontiguousarray(k), "lT": np.ascontiguousarray(l),
                        "vtok": np.ascontiguousarray(v.T), "ident_d": ident, "tri_d": tri, "ind_d": ind})
    res = run(("k2a",), build_k2a, in_maps)
    of = np.concatenate([res[h]["oT"] for h in range(4)], 0)
    ob = np.concatenate([res[4 + h]["oT"][:, ::-1] for h in range(4)], 0)
    return of, np.ascontiguousarray(ob)


GU = 32
C_PAT = ((128, 1), (512, 4), (2048, 16))
NUC = 3 * 4 * 16
NUD = 4 * 16


def attn_masks():
    k = np.arange(256)[:, None]
    q = np.arange(128)[None, :]
    mc = np.where(np.abs(k - 64 - q) <= 64, 1.0, 0.0).astype(np.float32)
    k = np.arange(384)[:, None]
    md = np.where(np.abs(k - 128 - q) <= 128, 1.0, 0.0).astype(np.float32)
    mc = mc.reshape(2, 128, 128).transpose(1, 0, 2)
    md = md.reshape(3, 128, 128).transpose(1, 0, 2)
    return (np.ascontiguousarray(mc).astype(ml_dtypes.bfloat16), np.ascontiguousarray(md).astype(ml_dtypes.bfloat16))


def build_k2b():
    P = Prog()
    specs = []
    for nm, nu, nb in (("c", NUC, 2), ("d", NUD, 3)):
        ng = nu // GU
        specs.append(dict(nm=nm, nb=nb, ng=ng,
                          Q=P.dram_in(nm + "Q", [ng, 64, GU * 128], BF16),
                          K=P.dram_in(nm + "K", [ng, 64, GU * nb * 128], BF16),
                          V=P.dram_in(nm + "V", [ng, 128, GU * nb * 65], BF16),
                          M=P.dram_in(nm + "M", [128, nb, 128], BF16),
                          O=P.dram_out(nm + "O", [ng, 65, GU * 128], F32)))
    ident_d = P.dram_in("ident_d", [128, 128], BF16)
    ident = P.sb("ident", [128, 128], BF16)
    P.dma("sp", ident[:], ident_d, writes=["ident"])
    Qg = [P.sb(f"Qg{i}", [64, GU * 128], BF16) for i in range(2)]
    Kg = [P.sb(f"Kg{i}", [64, GU * 3 * 128], BF16) for i in range(2)]
    Vg = [P.sb(f"Vg{i}", [128, GU * 3 * 65], BF16) for i in range(2)]
    Og = [P.sb(f"Og{i}", [65, GU * 128], F32) for i in range(2)]
    Pt = [P.sb(f"Pt{i}", [128, 384], BF16) for i in range(3)]
    psS = [P.ps(f"psS{i}", [128, 512], F32) for i in range(4)]
    psO = [P.ps(f"psO{i}", [128, 512], F32) for i in range(3)]
    gi = 0
    ui = 0
    for sp_ in specs:
        nb = sp_["nb"]
        mk = P.sb("mask_" + sp_["nm"], [128, nb, 128], BF16)
        mkk = "mask_" + sp_["nm"]
        P.dma("sp", mk[:], sp_["M"], writes=[mkk])
        for g in range(sp_["ng"]):
            b2 = gi % 2
            gi += 1
            P.dma("sp", Qg[b2][:], sp_["Q"][g], writes=[f"Qg{b2}"])
            P.dma("sp", Kg[b2][:, 0:GU * nb * 128], sp_["K"][g], writes=[f"Kg{b2}"])
            P.dma("sp", Vg[b2][:, 0:GU * nb * 65], sp_["V"][g], writes=[f"Vg{b2}"])
            def emit_S(u, s3, p2):
                for b in range(nb):
                    ksl = slice((u * nb + b) * 128, (u * nb + b + 1) * 128)
                    P.op("pe", lambda q, b=b, ksl=ksl: q.matmul(
                        psS[s3][:, b * 128:(b + 1) * 128], lhsT=Kg[b2][:, ksl], rhs=Qg[b2][:, u * 128:(u + 1) * 128],
                        start=True, stop=True), reads=[f"Kg{b2}", f"Qg{b2}"], writes=[f"psS{s3}"])
                P.op("act", lambda q: q.activation(out=Pt[p2][:, 0:nb * 128], in_=psS[s3][:, 0:nb * 128], func=AF.Exp, scale=0.125),
                     reads=[f"psS{s3}"], writes=[f"Pt{p2}"])
                P.op("dve", lambda q: q.tensor_tensor(out=Pt[p2][:, 0:nb * 128], in0=Pt[p2][:, 0:nb * 128],
                                                      in1=mk[:].rearrange("p b q -> p (b q)"), op=ALU.mult),
                     reads=[f"Pt{p2}", mkk], writes=[f"Pt{p2}"])

            def emit_PV(u, s3, p2, so):
                for b in range(nb):
                    vsl = slice((u * nb + b) * 65, (u * nb + b + 1) * 65)
                    P.op("pe", lambda q, b=b, vsl=vsl: q.matmul(
                        psO[so][0:65, 0:128], lhsT=Vg[b2][:, vsl], rhs=Pt[p2][:, b * 128:(b + 1) * 128],
                        start=(b == 0), stop=(b == nb - 1)), reads=[f"Vg{b2}", f"Pt{p2}"], writes=[f"psO{so}"])
                P.op("act", lambda q: q.activation(out=Og[b2][:, u * 128:(u + 1) * 128], in_=psO[so][0:65, 0:128], func=AF.Copy),
                     reads=[f"psO{so}"], writes=[f"Og{b2}"])

            ids = []
            for u in range(GU):
                ids.append((u, ui % 4, ui % 3, ui % 3))
                ui += 1
            emit_S(*ids[0][:3])
            emit_S(*ids[1][:3])
            for j in range(GU):
                if j + 2 < GU:
                    emit_S(*ids[j + 2][:3])
                emit_PV(*ids[j])
            P.dma("sp", sp_["O"][g], Og[b2][:], reads=[f"Og{b2}"], is_output=True)
    return P.finish()


def _windows(Kseq, Vseq, nb, halo, ntile):
    L = Kseq.shape[1]
    W = nb * 128
    Kp = np.zeros((64, L + 2 * halo + 128), Kseq.dtype)
    Kp[:, halo:halo + L] = Kseq
    Vp = np.zeros((L + 2 * halo + 128, 65), Vseq.dtype)
    Vp[halo:halo + L, :64] = Vseq.T
    Vp[halo:halo + L, 64] = 1.0
    Kw = np.stack([Kp[:, 128 * j:128 * j + W] for j in range(ntile)], 0)
    Vw = np.stack([Vp[128 * j:128 * j + W] for j in range(ntile)], 0)
    return Kw, Vw


def _pack_units(Qu, Ku, Vu, nb):
    U = Qu.shape[0]
    ng = U // GU
    Q = Qu.reshape(ng, GU, 64, 128).transpose(0, 2, 1, 3).reshape(ng, 64, GU * 128)
    K = Ku.reshape(ng, GU, 64, nb * 128).transpose(0, 2, 1, 3).reshape(ng, 64, GU * nb * 128)
    V = Vu.reshape(ng, GU, nb, 128, 65).transpose(0, 3, 1, 2, 4).reshape(ng, 128, GU * nb * 65)
    return np.ascontiguousarray(Q), np.ascontiguousarray(K), np.ascontiguousarray(V)


def run_k2b(obf_full):
    mc, md = attn_masks()
    ident = np.eye(128, dtype=np.float32).astype(ml_dtypes.bfloat16)
    cQ = np.zeros((3, 4, 128, 64, 128), ml_dtypes.bfloat16)
    cK = np.zeros((3, 4, 128, 64, 256), ml_dtypes.bfloat16)
    cV = np.zeros((3, 4, 128, 256, 65), ml_dtypes.bfloat16)
    for p, (w, d) in enumerate(C_PAT):
        L = SEQ // d
        nt = L // 128
        for h in range(4):
            rows = slice(h * 64, (h + 1) * 64)
            Qs = obf_full["cq"][rows].reshape(64, L, d)
            Ks = obf_full["ck"][rows].reshape(64, L, d)
            Vs = obf_full["cv"][rows].reshape(64, L, d)
            for r in range(d):
                Kw, Vw = _windows(Ks[:, :, r], Vs[:, :, r], 2, 64, nt)
                cK[p, h, r * nt:(r + 1) * nt] = Kw
                cV[p, h, r * nt:(r + 1) * nt] = Vw
                cQ[p, h, r * nt:(r + 1) * nt] = Qs[:, :, r].reshape(64, nt, 128).transpose(1, 0, 2)
    dQ = np.zeros((4, 128, 64, 128), ml_dtypes.bfloat16)
    dK = np.zeros((4, 128, 64, 384), ml_dtypes.bfloat16)
    dV = np.zeros((4, 128, 384, 65), ml_dtypes.bfloat16)
    for h in range(4):
        kvh = h // 2
        Kw, Vw = _windows(obf_full["dk"][kvh * 64:(kvh + 1) * 64], obf_full["dv"][kvh * 64:(kvh + 1) * 64], 3, 128, 128)
        dK[h] = Kw
        dV[h] = Vw
        dQ[h] = obf_full["dq"][h * 64:(h + 1) * 64].reshape(64, 128, 128).transpose(1, 0, 2)
    in_maps = []
    for c in range(NCORES):
        ts = slice(16 * c, 16 * c + 16)
        q, k, v = _pack_units(cQ[:, :, ts].reshape(NUC, 64, 128), cK[:, :, ts].reshape(NUC, 64, 256), cV[:, :, ts].reshape(NUC, 256, 65), 2)
        q2, k2, v2 = _pack_units(dQ[:, ts].reshape(NUD, 64, 128), dK[:, ts].reshape(NUD, 64, 384), dV[:, ts].reshape(NUD, 384, 65), 3)
        in_maps.append({"cQ": q, "cK": k, "cV": v, "cM": mc, "dQ": q2, "dK": k2, "dV": v2, "dM": md, "ident_d": ident})
    res = run(("k2b",), build_k2b, in_maps)
    numC = np.zeros((3, 256, SEQ), np.float32)
    denC = np.zeros((3, 4, SEQ), np.float32)
    numD = np.zeros((256, SEQ), np.float32)
    denD = np.zeros((4, SEQ), np.float32)
    for c in range(NCORES):
        co = res[c]["cO"].reshape(NUC // GU, 65, GU, 128).transpose(0, 2, 1, 3).reshape(3, 4, 16, 65, 128)
        do = res[c]["dO"].reshape(NUD // GU, 65, GU, 128).transpose(0, 2, 1, 3).reshape(4, 16, 65, 128)
        for p, (w, d) in enumerate(C_PAT):
            L = SEQ // d
            nt = L // 128
            for h in range(4):
                nv = numC[p, h * 64:(h + 1) * 64].reshape(64, L, d)
                dv_ = denC[p, h].reshape(L, d)
                for tl in range(16):
                    tau = 16 * c + tl
                    r, j = tau // nt, tau % nt
                    nv[:, 128 * j:128 * j + 128, r] = co[p, h, tl, 0:64]
                    dv_[128 * j:128 * j + 128, r] = co[p, h, tl, 64]
        for h in range(4):
            for tl in range(16):
                tau = 16 * c + tl
                numD[h * 64:(h + 1) * 64, 128 * tau:128 * tau + 128] = do[h, tl, 0:64]
                denD[h, 128 * tau:128 * tau + 128] = do[h, tl, 64]
    return numC, denC, numD, denD


FB = 4


def build_k3(moe):
    P = Prog()
    T = TOK
    xT = P.dram_in("xT", [D_MODEL, T], F32)
    modc_d = P.dram_in("modc", [128, 48], F32)
    ofT = P.dram_in("ofT", [256, T], F32)
    obT = P.dram_in("obT", [256, T], F32)
    gaT = P.dram_in("gaT", [256, T], F32)
    uext = P.dram_in("uext", [256, T + 32], F32)
    numC = P.dram_in("numC", [3, 256, T], F32)
    denC = P.dram_in("denC", [3, 256, T], F32)
    numD = P.dram_in("numD", [256, T], F32)
    denD = P.dram_in("denD", [256, T], F32)
    smalls_d = P.dram_in("smalls", [128, 128], F32)
    blk64_d = P.dram_in("blk64", [128, 128], F32)
    identb_d = P.dram_in("identb", [128, 128], BF16)
    w_out = P.dram_in("w_out", [1024, 1024], F32)
    if moe:
        wr_d = P.dram_in("wr", [1024, 8], F32)
        w_up = P.dram_in("w_up", [N_EXPERTS, 1024, 2 * EXPERT_DIM], F32)
        w_down = P.dram_in("w_down", [N_EXPERTS, EXPERT_DIM, 1024], F32)
        sel_d = P.dram_in("sel", [8, 8 * 128], F32)
        identf_d = P.dram_in("identf", [128, 128], F32)
    else:
        w_up = P.dram_in("w_up", [1024, 2 * FFN_DIM], F32)
        w_down = P.dram_in("w_down", [FFN_DIM, 1024], F32)
    xo = P.dram_out("xo", [D_MODEL, T], F32)

    pp = PsumPool(P)
    sm = P.sb("sm", [128, 128], F32)
    P.dma("sp", sm[:], smalls_d, writes=["sm"])
    O_ANW, O_CW, O_CB, O_BNW, O_BNB, O_SINK, O_LNW, O_LNB = 0, 1, 63, 65, 67, 69, 71, 87
    modc = P.sb("modc", [128, 48], F32)
    P.dma("sp", modc[:], modc_d, writes=["modc"])
    blk64 = P.sb("blk64", [128, 128], F32)
    P.dma("sp", blk64[:], blk64_d, writes=["blk64"])
    ones_f = P.sb("ones_f", [128, 128], F32)
    P.op("pool", lambda q: q.memset(ones_f[:], 1.0), writes=["ones_f"])
    eps = P.sb("eps", [128, 2], F32)
    P.op("pool", lambda q: q.memset(eps[:, 0:1], LN_EPS), writes=["eps"])
    P.op("pool", lambda q: q.memset(eps[:, 1:2], RMS_EPS), reads=["eps"], writes=["eps"])
    dcol = P.sb("dcol", [128, 32], F32)
    P.op("dve", lambda q: q.tensor_scalar(out=dcol[:, 0:8], in0=modc[:, 16:24], scalar1=1.0, scalar2=None, op0=ALU.add),
         reads=["modc"], writes=["dcol"])
    P.op("dve", lambda q: q.tensor_scalar(out=dcol[:, 8:16], in0=modc[:, 32:40], scalar1=1.0, scalar2=None, op0=ALU.add),
         reads=["modc", "dcol"], writes=["dcol"])
    P.op("dve", lambda q: q.tensor_scalar(out=dcol[:, 16:24], in0=modc[:, 40:48], scalar1=1.0, scalar2=None, op0=ALU.add),
         reads=["modc", "dcol"], writes=["dcol"])
    P.op("act", lambda q: q.activation(out=dcol[:, 24:26], in_=sm[:, O_SINK:O_SINK + 2], func=AF.Exp),
         reads=["sm", "dcol"], writes=["dcol"])
    G1, SC2, G2, ESINK = 0, 8, 16, 24

    x = P.sb("x", [128, 8, T], F32)
    y = P.sb("y", [128, 8, T], BF16)
    wu = [P.sb(f"wu{i}", [128, 8192], BF16) for i in range(2)]
    wd = [P.sb(f"wd{i}", [128, FB, 1024], BF16) for i in range(2)]
    abuf = P.sb("abuf", [128, FB, T], BF16)
    S = [P.sb(f"S{i}", [128, T + 32], F32) for i in range(3)]
    NT = 5
    tmp = [P.sb(f"tmp{i}", [128, 512], F32) for i in range(NT)]
    lnm = P.sb("lnm", [128, 512], F32)
    lnv = P.sb("lnv", [128, 512], F32)
    tcnt = [0]

    def gett():
        i = tcnt[0] % NT
        tcnt[0] += 1
        return tmp[i], f"tmp{i}"

    def tsl(tg):
        return slice(tg * 512, (tg + 1) * 512)

    xk = lambda i, tg: f"x{i}_{tg}"
    yk = lambda i, tg: f"y{i}_{tg}"

    xT_v = xT.rearrange("(k p) t -> p k t", p=128)
    for k in range(8):
        P.dma("sp", x[:, k, :], xT_v[:, k, :], writes=[xk(k, tg) for tg in range(4)])
    wo_v = w_out.rearrange("(k p) e -> p k e", p=128)
    wo_s = wu[0][:].rearrange("p (k e) -> p k e", k=8)
    for k in range(8):
        P.dma("pool", wo_s[:, k, :], wo_v[:, k, :], writes=["wu0"])

    def ln_fm(srcs, nfeat, wcol, bcol, dst, silu=False):
        n = len(srcs)
        for tg in range(4):
            ps_s, ks_ = pp.get()
            ps_q, kq_ = pp.get()
            for i, (af, kf) in enumerate(srcs):
                P.op("pe", lambda q, af=af, i=i: q.matmul(ps_s[:, :], lhsT=ones_f[:], rhs=af(tg), start=(i == 0), stop=(i == n - 1)),
                     reads=["ones_f", kf(tg)], writes=[ks_])
            for i, (af, kf) in enumerate(srcs):
                sq, sqk = gett()
                P.op("act", lambda q, af=af, sq=sq: q.activation(out=sq[:], in_=af(tg), func=AF.Square), reads=[kf(tg)], writes=[sqk])
                P.op("pe", lambda q, sq=sq, i=i: q.matmul(ps_q[:, :], lhsT=ones_f[:], rhs=sq[:], start=(i == 0), stop=(i == n - 1)),
                     reads=["ones_f", sqk], writes=[kq_])
            mean, mk = lnm, "lnm"
            P.op("act", lambda q: q.activation(out=mean[:], in_=ps_s[:], func=AF.Copy, scale=1.0 / nfeat), reads=[ks_], writes=[mk])
            var, vk = lnv, "lnv"
            P.op("dve", lambda q: q.tensor_tensor(out=var[:], in0=mean[:], in1=mean[:], op=ALU.mult), reads=[mk], writes=[vk])
            P.op("dve", lambda q: q.scalar_tensor_tensor(out=var[:], in0=ps_q[:], scalar=1.0 / nfeat, in1=var[:], op0=ALU.mult,
                                                         op1=ALU.subtract), reads=[kq_, vk], writes=[vk])
            P.op("act", lambda q: q.activation(out=var[:], in_=var[:], func=AF.Sqrt, bias=eps[:, 0:1], scale=1.0), reads=[vk, "eps"], writes=[vk])
            P.op("dve", lambda q: q.reciprocal(out=var[:], in_=var[:]), reads=[vk], writes=[vk])
            for i, ((af, kf), (df, dkf)) in enumerate(zip(srcs, dst)):
                t, tk = gett()
                P.op("dve", lambda q, af=af, t=t: q.tensor_tensor(out=t[:], in0=af(tg), in1=mean[:], op=ALU.subtract),
                     reads=[kf(tg), mk], writes=[tk])
                P.op("dve", lambda q, t=t: q.tensor_tensor(out=t[:], in0=t[:], in1=var[:], op=ALU.mult), reads=[tk, vk], writes=[tk])
                if silu:
                    P.op("act", lambda q, t=t, df=df, i=i: q.activation(out=df(tg), in_=t[:], func=AF.Silu, scale=wcol(i), bias=bcol(i)),
                         reads=[tk, "sm"], writes=[dkf(tg)])
                else:
                    P.op("act", lambda q, t=t, df=df, i=i: q.activation(out=df(tg), in_=t[:], func=AF.Identity, scale=wcol(i), bias=bcol(i)),
                         reads=[tk, "sm"], writes=[dkf(tg)])

    wu1f = wu[1][:].bitcast(F32)
    cpool = [wu1f[:, i_ * 512:(i_ + 1) * 512] for i_ in range(8)]
    ccnt = [0]

    class _CT:
        def __init__(self, ap):
            self.ap = ap

        def __getitem__(self, idx):
            return self.ap

    def getc():
        i_ = ccnt[0] % 8
        ccnt[0] += 1
        return _CT(cpool[i_]), f"ctmp{i_}"

    def stream_A():
        for a in range(2):
            rows = slice(a * 128, (a + 1) * 128)
            for tg in range(4):
                t0, k0 = gett()
                t1, k1 = gett()
                t2, k2 = gett()
                yield P.dma("sp", t0[:], ofT[rows, tsl(tg)], writes=[k0])
                yield P.dma("sp", t1[:], obT[rows, tsl(tg)], writes=[k1])
                yield P.dma("sp", t2[:], gaT[rows, tsl(tg)], writes=[k2])
                yield P.op("dve", lambda q, t0=t0, t1=t1: q.tensor_tensor(out=t0[:], in0=t0[:], in1=t1[:], op=ALU.add), reads=[k0, k1], writes=[k0])
                yield P.op("act", lambda q, t0=t0, t1=t1: q.activation(out=t1[:], in_=t0[:], func=AF.Square), reads=[k0], writes=[k1])
                ps, pk = pp.t[0], pp.k[0]
                yield P.op("pe", lambda q, ps=ps, t1=t1: q.matmul(ps[:, :], lhsT=blk64[:], rhs=t1[:], start=True, stop=True), reads=["blk64", k1], writes=[pk])
                yield P.op("act", lambda q, ps=ps, t1=t1: q.activation(out=t1[:], in_=ps[:], func=AF.Sqrt, bias=eps[:, 1:2], scale=1.0 / 64),
                     reads=[pk, "eps"], writes=[k1])
                yield P.op("dve", lambda q, t1=t1: q.reciprocal(out=t1[:], in_=t1[:]), reads=[k1], writes=[k1])
                yield P.op("dve", lambda q, t0=t0, t1=t1: q.scalar_tensor_tensor(out=t0[:], in0=t0[:], scalar=sm[:, O_ANW:O_ANW + 1], in1=t1[:],
                                                                         op0=ALU.mult, op1=ALU.mult), reads=[k0, k1, "sm"], writes=[k0])
                yield P.op("dve", lambda q, t0=t0, t2=t2, a=a, tg=tg: q.tensor_tensor(out=y[:, a, tsl(tg)], in0=t0[:], in1=t2[:], op=ALU.mult),
                     reads=[k0, k2], writes=[yk(a, tg)])


        yield None

    def stream_conv():
        identb = wd[1][:, 1, 0:128]
        yield P.dma("sp", identb, identb_d, writes=["identb"])
        dg = [wd[1][:, 0, i * 128:(i + 1) * 128] for i in range(4)]
        ubf = abuf[:].rearrange("p f t -> p (f t)")[:, 0:T + 32]
        for b in range(2):
            ub, ubk = S[0], "S0"
            acc, acck = S[1 + b], f"S{1 + b}"
            yield P.dma("sp", ub[:], uext[b * 128:(b + 1) * 128, :], writes=[ubk])
            yield P.op("act", lambda q: q.activation(out=ubf, in_=ub[:], func=AF.Copy), reads=[ubk], writes=["ubf"])
            pss = [(pp.t[1 + i_], pp.k[1 + i_]) for i_ in range(4)]
            for j in range(B_KERNEL):
                d_, dk_ = dg[j % 4], f"dg{j % 4}"
                yield P.op("dve", lambda q, d_=d_, j=j, b=b: q.tensor_scalar(out=d_, in0=identb, scalar1=sm[:, O_CW + b * 31 + j:O_CW + b * 31 + j + 1],
                                                                     scalar2=None, op0=ALU.mult), reads=["identb", "sm"], writes=[dk_])
                for tg in range(4):
                    ps, pk = pss[tg]
                    yield P.op("pe", lambda q, ps=ps, d_=d_, j=j, tg=tg: q.matmul(ps[:, :], lhsT=d_, rhs=ubf[:, tg * 512 + j:tg * 512 + j + 512],
                                                                            start=(j == 0), stop=(j == B_KERNEL - 1)),
                         reads=[dk_, "ubf"], writes=[pk])
            for tg in range(4):
                ps, pk = pss[tg]
                yield P.op("act", lambda q, ps=ps, acc=acc, tg=tg, b=b: q.activation(out=acc[:, tsl(tg)], in_=ps[:], func=AF.Identity,
                                                                             bias=sm[:, O_CB + b:O_CB + b + 1], scale=1.0),
                     reads=[pk, "sm"], writes=[acck])

        yield None

    def stream_CD():
        for a in range(2):
            rows = slice(a * 128, (a + 1) * 128)
            for tg in range(4):
                n0, kn0 = getc()
                d0, kd0 = getc()
                yield P.dma("sp", n0[:], numC[0, rows, tsl(tg)], writes=[kn0])
                yield P.dma("sp", d0[:], denC[0, rows, tsl(tg)], writes=[kd0])
                n1, kn1 = getc()
                d1, kd1 = getc()
                for p in (1, 2):
                    yield P.dma("sp", n1[:], numC[p, rows, tsl(tg)], writes=[kn1])
                    yield P.dma("sp", d1[:], denC[p, rows, tsl(tg)], writes=[kd1])
                    yield P.op("dve", lambda q, n0=n0, n1=n1: q.tensor_tensor(out=n0[:], in0=n0[:], in1=n1[:], op=ALU.add), reads=[kn0, kn1], writes=[kn0])
                    yield P.op("dve", lambda q, d0=d0, d1=d1: q.tensor_tensor(out=d0[:], in0=d0[:], in1=d1[:], op=ALU.add), reads=[kd0, kd1], writes=[kd0])
                yield P.op("dve", lambda q, d0=d0: q.reciprocal(out=d0[:], in_=d0[:]), reads=[kd0], writes=[kd0])
                yield P.op("dve", lambda q, n0=n0, d0=d0, a=a, tg=tg: q.tensor_tensor(out=y[:, 4 + a, tsl(tg)], in0=n0[:], in1=d0[:], op=ALU.mult),
                     reads=[kn0, kd0], writes=[yk(4 + a, tg)])
                n0, kn0 = getc()
                d0, kd0 = getc()
                yield P.dma("sp", n0[:], numD[rows, tsl(tg)], writes=[kn0])
                yield P.dma("sp", d0[:], denD[rows, tsl(tg)], writes=[kd0])
                yield P.op("dve", lambda q, d0=d0, a=a: q.tensor_scalar(out=d0[:], in0=d0[:], scalar1=dcol[:, ESINK + a:ESINK + a + 1], scalar2=None,
                                                                op0=ALU.add), reads=[kd0, "dcol"], writes=[kd0])
                yield P.op("dve", lambda q, d0=d0: q.reciprocal(out=d0[:], in_=d0[:]), reads=[kd0], writes=[kd0])
                yield P.op("dve", lambda q, n0=n0, d0=d0, a=a, tg=tg: q.tensor_tensor(out=y[:, 6 + a, tsl(tg)], in0=n0[:], in1=d0[:], op=ALU.mult),
                     reads=[kn0, kd0], writes=[yk(6 + a, tg)])


        yield None

    def drain(*gens):
        gens = list(gens)
        while gens:
            for g in list(gens):
                try:
                    next(g)
                except StopIteration:
                    gens.remove(g)

    drain(stream_A(), stream_conv(), stream_CD())
    ln_fm([(lambda tg, b=b: S[1 + b][:, tsl(tg)], lambda tg, b=b: f"S{1 + b}") for b in range(2)], 256,
          lambda i: sm[:, O_BNW + i:O_BNW + i + 1], lambda i: sm[:, O_BNB + i:O_BNB + i + 1],
          [(lambda tg, b=b: y[:, 2 + b, tsl(tg)], lambda tg, b=b: yk(2 + b, tg)) for b in range(2)], silu=True)

    for k in range(8):
        for tg in range(4):
            P.op("act", lambda q, k=k, tg=tg: q.activation(out=x[:, k, tsl(tg)], in_=x[:, k, tsl(tg)], func=AF.Copy, scale=DEEPNORM_ALPHA),
                 reads=[xk(k, tg)], writes=[xk(k, tg)])
    for dc in range(8):
        for tg in range(4):
            ps, pk = pp.get()
            for k in range(8):
                P.op("pe", lambda q, ps=ps, k=k, dc=dc, tg=tg: q.matmul(ps[:, :], lhsT=wo_s[:, k, dc * 128:(dc + 1) * 128], rhs=y[:, k, tsl(tg)],
                                                                        start=(k == 0), stop=(k == 7)), reads=["wu0", yk(k, tg)], writes=[pk])
            P.op("dve", lambda q, ps=ps, dc=dc, tg=tg: q.scalar_tensor_tensor(out=x[:, dc, tsl(tg)], in0=ps[:], scalar=dcol[:, G1 + dc:G1 + dc + 1],
                                                                             in1=x[:, dc, tsl(tg)], op0=ALU.mult, op1=ALU.add),
                 reads=[pk, "dcol", xk(dc, tg)], writes=[xk(dc, tg)])
    xs = [(lambda tg, k=k: x[:, k, tsl(tg)], lambda tg, k=k: xk(k, tg)) for k in range(8)]
    ln_fm(xs, 1024, lambda i: sm[:, O_LNW + i:O_LNW + i + 1], lambda i: sm[:, O_LNB + i:O_LNB + i + 1], xs)

    for k in range(8):
        for tg in range(4):
            P.op("dve", lambda q, k=k, tg=tg: q.tensor_scalar(out=y[:, k, tsl(tg)], in0=x[:, k, tsl(tg)], scalar1=dcol[:, SC2 + k:SC2 + k + 1],
                                                            scalar2=modc[:, 24 + k:25 + k], op0=ALU.mult, op1=ALU.add),
                 reads=[xk(k, tg), "dcol", "modc"], writes=[yk(k, tg)])

    gT = None
    if moe:
        wr = P.sb("wr", [128, 8, 8], F32)
        P.dma("sp", wr[:], wr_d.rearrange("(k p) e -> p k e", p=128), writes=["wr"])
        sel = P.sb("sel", [8, 8 * 128], F32)
        P.dma("sp", sel[:], sel_d, writes=["sel"])
        identf = P.sb("identf", [128, 128], F32)
        P.dma("sp", identf[:], identf_d, writes=["identf"])
        psl, pslk = pp.get()
        for k in range(8):
            h2f, hk = S[0], "S0"
            P.op("dve", lambda q, k=k: q.tensor_scalar(out=h2f[:, 0:T], in0=x[:, k, :], scalar1=dcol[:, SC2 + k:SC2 + k + 1],
                                                       scalar2=modc[:, 24 + k:25 + k], op0=ALU.mult, op1=ALU.add),
                 reads=[xk(k, tg) for tg in range(4)] + ["dcol", "modc"], writes=[hk])
            for ti in range(16):
                P.op("pe", lambda q, k=k, ti=ti: q.matmul(psl[:, ti * 8:(ti + 1) * 8], lhsT=h2f[:, ti * 128:(ti + 1) * 128], rhs=wr[:, k, :],
                                                          start=(k == 0 and ti == 0), stop=(k == 7 and ti == 15), skip_group_check=True),
                     reads=[hk, "wr"], writes=[pslk])
        lg = P.sb("lg", [128, 16, 8], F32)
        lg2 = P.sb("lg2", [128, 16, 8], F32)
        eq1 = S[1][:, 0:128].rearrange("p (t e) -> p t e", e=8)
        eq2 = S[1][:, 128:256].rearrange("p (t e) -> p t e", e=8)
        m1 = P.sb("m1", [128, 16], F32)
        m2 = P.sb("m2", [128, 16], F32)
        g1 = P.sb("g1", [128, 16], F32)
        bc3 = lambda t: t[:].unsqueeze(2).broadcast_to([128, 16, 8])
        P.op("dve", lambda q: q.tensor_copy(out=lg[:], in_=psl[:, 0:128].rearrange("p (t e) -> p t e", e=8)), reads=[pslk], writes=["lg"])
        P.op("dve", lambda q: q.tensor_reduce(out=m1[:], in_=lg[:], axis=AX.X, op=ALU.max), reads=["lg"], writes=["m1"])
        P.op("dve", lambda q: q.tensor_tensor(out=eq1, in0=lg[:], in1=bc3(m1), op=ALU.is_equal), reads=["lg", "m1"], writes=["eq1"])
        P.op("dve", lambda q: q.scalar_tensor_tensor(out=lg2[:], in0=eq1, scalar=-1e30, in1=lg[:], op0=ALU.mult, op1=ALU.add),
             reads=["eq1", "lg"], writes=["lg2"])
        P.op("dve", lambda q: q.tensor_reduce(out=m2[:], in_=lg2[:], axis=AX.X, op=ALU.max), reads=["lg2"], writes=["m2"])
        P.op("dve", lambda q: q.tensor_tensor(out=eq2, in0=lg2[:], in1=bc3(m2), op=ALU.is_equal), reads=["lg2", "m2"], writes=["eq2"])
        P.op("dve", lambda q: q.tensor_tensor(out=m2[:], in0=m2[:], in1=m1[:], op=ALU.subtract), reads=["m2", "m1"], writes=["m2"])
        P.op("act", lambda q: q.activation(out=m2[:], in_=m2[:], func=AF.Exp), reads=["m2"], writes=["m2"])
        P.op("dve", lambda q: q.tensor_scalar(out=g1[:], in0=m2[:], scalar1=1.0, scalar2=None, op0=ALU.add), reads=["m2"], writes=["g1"])
        P.op("dve", lambda q: q.reciprocal(out=g1[:], in_=g1[:]), reads=["g1"], writes=["g1"])
        P.op("dve", lambda q: q.tensor_tensor(out=m2[:], in0=m2[:], in1=g1[:], op=ALU.mult), reads=["m2", "g1"], writes=["m2"])
        P.op("dve", lambda q: q.tensor_tensor(out=eq1, in0=eq1, in1=bc3(g1), op=ALU.mult), reads=["eq1", "g1"], writes=["eq1"])
        P.op("dve", lambda q: q.tensor_tensor(out=eq2, in0=eq2, in1=bc3(m2), op=ALU.mult), reads=["eq2", "m2"], writes=["eq2"])
        P.op("dve", lambda q: q.tensor_tensor(out=eq1, in0=eq1, in1=eq2, op=ALU.add), reads=["eq1", "eq2"], writes=["eq1"])
        gT = S[0][0:8, 0:T]
        for tg in range(4):
            ps, pk = pp.get()
            for j in range(4):
                ti = tg * 4 + j
                P.op("pe", lambda q, ps=ps, j=j, ti=ti: q.transpose(ps[0:8, j * 128:(j + 1) * 128], eq1[:, ti, :], identf[:]),
                     reads=["eq1", "identf"], writes=[pk])
            P.op("act", lambda q, ps=ps, tg=tg: q.activation(out=gT[:, tsl(tg)], in_=ps[0:8, :], func=AF.Copy), reads=[pk], writes=["S0"])

    for k in range(8):
        for tg in range(4):
            P.op("act", lambda q, k=k, tg=tg: q.activation(out=x[:, k, tsl(tg)], in_=x[:, k, tsl(tg)], func=AF.Copy, scale=DEEPNORM_ALPHA),
                 reads=[xk(k, tg)], writes=[xk(k, tg)])

    blocks = []
    if moe:
        for e in range(N_EXPERTS):
            for (f0, nf) in chunks(EXPERT_DIM // 128, FB):
                blocks.append((w_up[e], w_down[e], EXPERT_DIM, f0, nf, e))
    else:
        for (f0, nf) in chunks(FFN_DIM // 128, FB):
            blocks.append((w_up, w_down, FFN_DIM, f0, nf, None))

    stg = [S[2][:, i * 512:(i + 1) * 512] for i in range(4)]
    stg_n = [0]

    def block_pieces(bi):
        wup, wdn, F, f0, nf, e = blocks[bi]
        b2 = bi % 2
        wuv = wu[b2][:].rearrange("p (k s c) -> p k s c", k=8, s=2)
        upv = wup.rearrange("(k p) c -> p k c", p=128)
        pcs = []
        for k in range(8):
            for s_ in range(2):
                pcs.append((wuv[:, k, s_, 0:nf * 128], upv[:, k, s_ * F + f0 * 128:s_ * F + (f0 + nf) * 128], nf * 128, f"wu{b2}"))
        for fc in range(nf):
            for hf in range(2):
                pcs.append((wd[b2][:, fc, hf * 512:(hf + 1) * 512],
                            wdn[(f0 + fc) * 128:(f0 + fc + 1) * 128, hf * 512:(hf + 1) * 512], 512, f"wd{b2}"))
        return pcs

    def emit_piece(pc):
        dst, src, n, dkey = pc
        i = stg_n[0] % 4
        first = stg_n[0] < 4
        stg_n[0] += 1
        P.dma("sp", stg[i][:, 0:n], src, writes=(["S2", f"stg{i}"] if first else [f"stg{i}"]))
        P.op("act", lambda q: q.activation(out=dst, in_=stg[i][:, 0:n], func=AF.Copy), reads=[f"stg{i}"], writes=[dkey])

    for pc in block_pieces(0):
        emit_piece(pc)
    for bi, (wup, wdn, F, f0, nf, e) in enumerate(blocks):
        b2 = bi % 2
        nxt = block_pieces(bi + 1) if bi + 1 < len(blocks) else []
        per_it = -(-len(nxt) // (nf * 4)) if nxt else 0
        gmul = None
        gk = None
        if moe:
            gmul, gk = S[1], "S1"
            if f0 == 0:
                for tg in range(4):
                    ps, pk = pp.get()
                    P.op("pe", lambda q, ps=ps, e=e, tg=tg: q.matmul(ps[:, :], lhsT=sel[:, e * 128:(e + 1) * 128], rhs=gT[:, tsl(tg)],
                                                                    start=True, stop=True), reads=["sel", "S0"], writes=[pk])
                    P.op("act", lambda q, ps=ps, gmul=gmul, tg=tg: q.activation(out=gmul[:, tsl(tg)], in_=ps[:], func=AF.Copy),
                         reads=[pk], writes=[gk + f"_{tg}"])
        wuv = wu[b2][:].rearrange("p (k s c) -> p k s c", k=8, s=2)
        for fc in range(nf):
            for tg in range(4):
                psg, kg = pp.get()
                psu, ku = pp.get()
                for s_, ps_, pk_ in ((0, psg, kg), (1, psu, ku)):
                    for k in range(8):
                        P.op("pe", lambda q, ps_=ps_, k=k, s_=s_, fc=fc, tg=tg: q.matmul(ps_[:, :], lhsT=wuv[:, k, s_, fc * 128:(fc + 1) * 128],
                                                                                      rhs=y[:, k, tsl(tg)], start=(k == 0), stop=(k == 7)),
                             reads=[f"wu{b2}", yk(k, tg)], writes=[pk_])
                sg, sgk = gett()
                P.op("act", lambda q, sg=sg, psg=psg: q.activation(out=sg[:], in_=psg[:], func=AF.Silu), reads=[kg], writes=[sgk])
                if gmul is not None:
                    P.op("pool", lambda q, sg=sg, gmul=gmul, tg=tg: q.tensor_tensor(out=sg[:], in0=sg[:], in1=gmul[:, tsl(tg)], op=ALU.mult),
                         reads=[sgk, gk + f"_{tg}"], writes=[sgk])
                P.op("dve", lambda q, sg=sg, psu=psu, fc=fc, tg=tg: q.tensor_tensor(out=abuf[:, fc, tsl(tg)], in0=sg[:], in1=psu[:], op=ALU.mult),
                     reads=[sgk, ku], writes=[f"a{fc}_{tg}"])
                for _ in range(per_it):
                    if nxt:
                        emit_piece(nxt.pop(0))
        while nxt:
            emit_piece(nxt.pop(0))
        for dc in range(8):
            for tg in range(4):
                ps, pk = pp.get()
                for fc in range(nf):
                    P.op("pe", lambda q, ps=ps, fc=fc, dc=dc, tg=tg: q.matmul(ps[:, :], lhsT=wd[b2][:, fc, dc * 128:(dc + 1) * 128],
                                                                              rhs=abuf[:, fc, tsl(tg)], start=(fc == 0), stop=(fc == nf - 1)),
                         reads=[f"wd{b2}", f"a{fc}_{tg}"], writes=[pk])
                P.op("dve", lambda q, ps=ps, dc=dc, tg=tg: q.scalar_tensor_tensor(out=x[:, dc, tsl(tg)], in0=ps[:], scalar=dcol[:, G2 + dc:G2 + dc + 1],
                                                                                 in1=x[:, dc, tsl(tg)], op0=ALU.mult, op1=ALU.add),
                     reads=[pk, "dcol", xk(dc, tg)], writes=[xk(dc, tg)])

    ln_fm(xs, 1024, lambda i: sm[:, O_LNW + 8 + i:O_LNW + 9 + i], lambda i: sm[:, O_LNB + 8 + i:O_LNB + 9 + i], xs)
    xo_v = xo.rearrange("(k p) t -> p k t", p=128)
    for k in range(8):
        P.dma("sp", xo_v[:, k, :], x[:, k, :], reads=[xk(k, tg) for tg in range(4)], is_output=True)
    return P.finish()


def run_k3(layer, xT_shards, mods, of, ob, o32_full, numC, denC, numD, denD, inp):
    moe = (layer % 2 == 1)
    T = TOK
    sm = np.zeros((128, 128), np.float32)
    anw = np.asarray(inp["a_norm_w"])[layer]
    sm[:, 0] = np.concatenate([anw, anw])
    cw = np.asarray(inp["b_conv_w"])[layer]
    for b in range(2):
        sm[:, 1 + b * 31:1 + (b + 1) * 31] = cw[:, b * 128:(b + 1) * 128].T
    sm[:, 63:65] = col128(np.asarray(inp["b_conv_b"])[layer])
    sm[:, 65:67] = col128(np.asarray(inp["b_norm_w"])[layer])
    sm[:, 67:69] = col128(np.asarray(inp["b_norm_b"])[layer])
    sink = np.asarray(inp["d_sink"])[layer]
    sm[:, 69:71] = col128(np.repeat(sink, 64))
    sm[:, 71:79] = col128(np.asarray(inp["ln_w"])[layer, 0])
    sm[:, 79:87] = col128(np.asarray(inp["ln_w"])[layer, 1])
    sm[:, 87:95] = col128(np.asarray(inp["ln_b"])[layer, 0])
    sm[:, 95:103] = col128(np.asarray(inp["ln_b"])[layer, 1])
    blk64 = np.kron(np.eye(2, dtype=np.float32), np.ones((64, 64), np.float32))
    u = o32_full["u"]
    upad = np.zeros((256, SEQ + 32), np.float32)
    upad[:, 15:15 + SEQ] = u
    denC_rep = np.repeat(denC, 64, axis=1)
    denD_rep = np.repeat(denD, 64, axis=0)
    w_out = np.ascontiguousarray(inp["w_out"][layer])
    common = {"smalls": sm, "blk64": blk64, "w_out": w_out, "identb": np.eye(128, dtype=np.float32).astype(ml_dtypes.bfloat16)}
    if moe:
        li = layer // 2
        sel = np.zeros((8, 8 * 128), np.float32)
        for e in range(8):
            sel[e, e * 128:(e + 1) * 128] = 1.0
        common.update({"wr": np.ascontiguousarray(inp["moe_router"][li]), "w_up": np.ascontiguousarray(inp["moe_w_up"][li]),
                       "w_down": np.ascontiguousarray(inp["moe_w_down"][li]), "sel": sel, "identf": np.eye(128, dtype=np.float32)})
    else:
        li = layer // 2
        common.update({"w_up": np.ascontiguousarray(inp["ffn_w_up"][li]), "w_down": np.ascontiguousarray(inp["ffn_w_down"][li])})
    in_maps = []
    for c in range(NCORES):
        ts = slice(c * T, (c + 1) * T)
        m = dict(common)
        m.update({"xT": xT_shards[c], "modc": mods[c], "ofT": np.ascontiguousarray(of[:, ts]), "obT": np.ascontiguousarray(ob[:, ts]),
                  "gaT": np.ascontiguousarray(o32_full["ga"][:, ts]), "uext": np.ascontiguousarray(upad[:, c * T:c * T + T + 32]),
                  "numC": np.ascontiguousarray(numC[:, :, ts]), "denC": np.ascontiguousarray(denC_rep[:, :, ts]),
                  "numD": np.ascontiguousarray(numD[:, ts]), "denD": np.ascontiguousarray(denD_rep[:, ts])})
        in_maps.append(m)
    res = run(("k3", moe), lambda: build_k3(moe), in_maps)
    return [r["xo"] for r in res]


def run_layer(layer, xT_shards, inp, modc=None):
    if modc is None:
        modc = run_k0(inp)[layer]
    r1 = run_k1(layer, xT_shards, modc, inp)
    o32_full = {nm: np.concatenate([r["o32"][off:off + 256] for r in r1], axis=1) for nm, off in O32.items()}
    obf_full = {nm: np.concatenate([r["obf"][off:off + (128 if nm in ("dk", "dv") else 256)] for r in r1], axis=1)
                for nm, off in OBF.items()}
    mods = [modc for _ in range(NCORES)]
    of, ob = run_k2a(o32_full)
    numC, denC, numD, denD = run_k2b(obf_full)
    return run_k3(layer, xT_shards, mods, of, ob, o32_full, numC, denC, numD, denD, inp)


def kernel(**inp):
    inp = {k: np.asarray(v) for k, v in inp.items()}
    x = inp["x"][0]
    xT_shards = [np.ascontiguousarray(x[c * TOK:(c + 1) * TOK].T) for c in range(NCORES)]
    modcs = run_k0(inp)
    for layer in range(DEPTH):
        xT_shards = run_layer(layer, xT_shards, inp, modcs[layer])
    out = np.concatenate([s.T for s in xT_shards], axis=0)[None]
    return np.ascontiguousarray(out.astype(np.float32))
```
